# Optimizing a Trainium2 kernel written in Bass

```python
import math
import jax
import jax.numpy as jnp
from jax import lax
import numpy as np

D_MODEL = 1024
BATCH = 8
SEQ = 4096
DEPTH = 4

N_BRANCH = 4
BR_WIDTH = 512
CONV_K = 4
NORM_EPS = 1e-6

GDN_HEADS = 4
GDN_DK = 128
GDN_DV = 128
GDN_CHUNK = 64

GLA_HEADS = 4
GLA_DK = 64
GLA_DV = 128
GLA_LOWRANK = 16
GLA_GATE_NORMALIZER = 16.0
GLA_CHUNK = 64

SSD_HEADS = 8
SSD_HEADDIM = 64
SSD_STATE = 128
SSD_GROUPS = 2
SSD_CHUNK = 64

NSA_HEADS = 8
NSA_KV_HEADS = 2
NSA_HEADDIM = 64
CMP_BLOCK = 32
CMP_STRIDE = 16
SEL_BLOCK = 64
SEL_TOPK = 16
WINDOW = 512
NSA_QBLOCK = 64
FORCED_SCORE = 1e4

GDN_QK = GDN_HEADS * GDN_DK
GDN_V = GDN_HEADS * GDN_DV
GLA_K = GLA_HEADS * GLA_DK
GLA_V = GLA_HEADS * GLA_DV
SSD_INNER = SSD_HEADS * SSD_HEADDIM
SSD_BC = SSD_GROUPS * SSD_STATE
NSA_Q = NSA_HEADS * NSA_HEADDIM
NSA_KV = NSA_KV_HEADS * NSA_HEADDIM

IN_SPLITS = (
    ('gdn_q', GDN_QK), ('gdn_k', GDN_QK), ('gdn_v', GDN_V), ('gdn_beta', GDN_HEADS), ('gdn_a', GDN_HEADS), ('gdn_z', BR_WIDTH),
    ('gla_q', GLA_K), ('gla_k', GLA_K), ('gla_v', GLA_V), ('gla_gk', GLA_LOWRANK), ('gla_z', BR_WIDTH),
    ('ssd_x', SSD_INNER), ('ssd_b', SSD_BC), ('ssd_c', SSD_BC), ('ssd_dt', SSD_HEADS), ('ssd_z', BR_WIDTH),
    ('nsa_q', NSA_Q), ('nsa_kc', NSA_KV), ('nsa_vc', NSA_KV), ('nsa_ks', NSA_KV), ('nsa_vs', NSA_KV),
    ('nsa_kw', NSA_KV), ('nsa_vw', NSA_KV), ('nsa_gate', 3 * NSA_HEADS), ('nsa_z', BR_WIDTH),
    ('merge_gate', N_BRANCH * D_MODEL),
)
D_IN = sum(w for _, w in IN_SPLITS)
F32 = jnp.float32

kernel_name = 'hybrid_gdn_gla_ssd_nsa_block'


def _in_offsets():
    offs, start = {}, 0
    for name, width in IN_SPLITS:
        offs[name] = (start, start + width)
        start += width
    return offs


def _cols(u, offs, first, last):
    return u[..., offs[first][0]:offs[last][1]]


def rmsnorm(x, gain):
    xf = x.astype(F32)
    y = xf * lax.rsqrt(jnp.mean(xf * xf, axis=-1, keepdims=True) + NORM_EPS)
    return (y * gain.astype(F32)).astype(x.dtype)


def l2norm(x):
    return x * lax.rsqrt(jnp.sum(x * x, axis=-1, keepdims=True) + NORM_EPS)


def causal_conv(x, w):
    k_width, ch = w.shape
    return lax.conv_general_dilated(x, w[:, None, :], window_strides=(1,), padding=((k_width - 1, 0),),
                                    dimension_numbers=('NWC', 'WIO', 'NWC'), feature_group_count=ch)


def masked_softmax(s, mask):
    s = jnp.where(mask, s.astype(F32), -jnp.inf)
    m = jnp.max(s, axis=-1, keepdims=True)
    m = jnp.where(jnp.isfinite(m), m, 0.0)
    e = jnp.exp(s - m)
    return e / jnp.maximum(jnp.sum(e, axis=-1, keepdims=True), 1e-30)


def alibi_slopes(n):
    return 2.0 ** (-8.0 * jnp.arange(1, n + 1, dtype=F32) / n)


def gated_delta_rule(q, k, v, beta, g):
    bsz, nh, s_len, dk = q.shape
    dv = v.shape[-1]
    c = GDN_CHUNK
    n = s_len // c
    ch = lambda t: t.reshape(bsz, nh, n, c, *t.shape[3:])
    q = ch(q * dk ** -0.5)
    k, v, beta = ch(k), ch(v), ch(beta)
    gc = jnp.cumsum(ch(g), axis=-1)
    tri = jnp.tril(jnp.ones((c, c), bool))
    stri = jnp.tril(jnp.ones((c, c), bool), -1)
    decay = jnp.exp(jnp.where(tri, gc[..., :, None] - gc[..., None, :], -jnp.inf))
    kb = k * beta[..., None]
    lower = jnp.where(stri, jnp.einsum('bhnid,bhnjd->bhnij', kb, k) * decay, 0.0)
    rhs = jnp.concatenate([v * beta[..., None], kb * jnp.exp(gc)[..., None]], axis=-1)
    sol = lax.linalg.triangular_solve(lower + jnp.eye(c, dtype=F32), rhs, left_side=True, lower=True,
                                      unit_diagonal=True)
    u_c, w_c = sol[..., :dv], sol[..., dv:]
    attn = jnp.einsum('bhnid,bhnjd->bhnij', q, k) * decay
    qg = q * jnp.exp(gc)[..., None]
    kd = k * jnp.exp(gc[..., -1:] - gc)[..., None]
    dec = jnp.exp(gc[..., -1])

    def step(state, inp):
        u_i, w_i, qg_i, kd_i, a_i, d_i = inp
        v_new = u_i - jnp.einsum('bhcd,bhde->bhce', w_i, state)
        o = jnp.einsum('bhcd,bhde->bhce', qg_i, state) + jnp.einsum('bhij,bhje->bhie', a_i, v_new)
        state = state * d_i[..., None, None] + jnp.einsum('bhcd,bhce->bhde', kd_i, v_new)
        return state, o

    xs = tuple(jnp.moveaxis(t, 2, 0) for t in (u_c, w_c, qg, kd, attn, dec))
    _, o = lax.scan(step, jnp.zeros((bsz, nh, dk, dv), F32), xs)
    return jnp.moveaxis(o, 0, 2).reshape(bsz, nh, s_len, dv)


def gdn_branch(u, offs, conv_w, a_log, dt_bias, onorm):
    bsz, s_len, _ = u.shape
    c = lambda name: _cols(u, offs, name, name).astype(F32)
    qkv = jax.nn.silu(causal_conv(_cols(u, offs, 'gdn_q', 'gdn_v').astype(F32), conv_w.astype(F32)))
    q, k, v = jnp.split(qkv, [GDN_QK, 2 * GDN_QK], axis=-1)
    heads = lambda t, d: t.reshape(bsz, s_len, GDN_HEADS, d).transpose(0, 2, 1, 3)
    q, k, v = l2norm(heads(q, GDN_DK)), l2norm(heads(k, GDN_DK)), heads(v, GDN_DV)
    beta = jax.nn.sigmoid(c('gdn_beta')).transpose(0, 2, 1)
    g = (-jnp.exp(a_log.astype(F32)) * jax.nn.softplus(c('gdn_a') + dt_bias.astype(F32))).transpose(0, 2, 1)
    o = gated_delta_rule(q, k, v, beta, g)
    o = rmsnorm(o, onorm).transpose(0, 2, 1, 3).reshape(bsz, s_len, GDN_V)
    return o * jax.nn.silu(c('gdn_z'))


def gla_chunked(q, k, v, gk):
    bsz, nh, s_len, dk = q.shape
    dv = v.shape[-1]
    c = GLA_CHUNK
    n = s_len // c
    ch = lambda t: t.reshape(bsz, nh, n, c, t.shape[-1])
    q = ch(q * dk ** -0.5)
    k, v = ch(k), ch(v)
    b = jnp.cumsum(ch(gk), axis=3)
    bref = b[:, :, :, c // 2:c // 2 + 1]
    tri = jnp.tril(jnp.ones((c, c), bool))
    a_intra = jnp.where(tri, jnp.einsum('bhnid,bhnjd->bhnij', q * jnp.exp(b - bref), k * jnp.exp(bref - b)), 0.0)
    o_intra = jnp.einsum('bhnij,bhnje->bhnie', a_intra, v)
    qg = q * jnp.exp(b)
    kd = k * jnp.exp(b[:, :, :, -1:] - b)
    dec = jnp.exp(b[:, :, :, -1])

    def step(state, inp):
        qg_i, kd_i, v_i, d_i = inp
        o = jnp.einsum('bhcd,bhde->bhce', qg_i, state)
        state = state * d_i[..., None] + jnp.einsum('bhcd,bhce->bhde', kd_i, v_i)
        return state, o

    xs = tuple(jnp.moveaxis(t, 2, 0) for t in (qg, kd, v, dec))
    _, o_inter = lax.scan(step, jnp.zeros((bsz, nh, dk, dv), F32), xs)
    return (o_intra + jnp.moveaxis(o_inter, 0, 2)).reshape(bsz, nh, s_len, dv)


def gla_branch(u, offs, w_gk, b_gk, onorm):
    bsz, s_len, _ = u.shape
    c = lambda name: _cols(u, offs, name, name).astype(F32)
    gk = jax.nn.log_sigmoid(c('gla_gk') @ w_gk.astype(F32) + b_gk.astype(F32)) / GLA_GATE_NORMALIZER
    heads = lambda t, d: t.reshape(bsz, s_len, GLA_HEADS, d).transpose(0, 2, 1, 3)
    o = gla_chunked(heads(c('gla_q'), GLA_DK), heads(c('gla_k'), GLA_DK), heads(c('gla_v'), GLA_DV), heads(gk, GLA_DK))
    o = rmsnorm(o, onorm).transpose(0, 2, 1, 3).reshape(bsz, s_len, GLA_V)
    return o * jax.nn.silu(c('gla_z'))


def ssd_chunked(x, dt, a_head, bm, cm, d_skip):
    bsz, s_len, ng, hg, p = x.shape
    ns = bm.shape[-1]
    c = SSD_CHUNK
    n = s_len // c
    xc = x.reshape(bsz, n, c, ng, hg, p)
    dtc = dt.reshape(bsz, n, c, ng, hg)
    bc = bm.reshape(bsz, n, c, ng, ns)
    cc = cm.reshape(bsz, n, c, ng, ns)
    acs = jnp.cumsum(dtc * a_head, axis=2)
    xdt = xc * dtc[..., None]
    acs_h = jnp.moveaxis(acs, 2, -1)
    tri = jnp.tril(jnp.ones((c, c), bool))
    lmat = jnp.exp(jnp.where(tri, acs_h[..., :, None] - acs_h[..., None, :], -jnp.inf))
    cb = jnp.einsum('bnigs,bnjgs->bngij', cc, bc)
    y_diag = jnp.einsum('bngij,bnghij,bnjghp->bnighp', cb, lmat, xdt)
    decay_states = jnp.exp(acs[:, :, -1:] - acs)
    states = jnp.einsum('bnjgs,bnjgh,bnjghp->bnghps', bc, decay_states, xdt)
    chunk_decay = jnp.exp(acs[:, :, -1])

    def step(h, inp):
        st, dc = inp
        return h * dc[..., None, None] + st, h

    _, h_prev = lax.scan(step, jnp.zeros((bsz, ng, hg, p, ns), F32),
                         (jnp.moveaxis(states, 1, 0), jnp.moveaxis(chunk_decay, 1, 0)))
    h_prev = jnp.moveaxis(h_prev, 0, 1)
    y_off = jnp.einsum('bnigs,bnghps,bnigh->bnighp', cc, h_prev, jnp.exp(acs))
    y = y_diag + y_off + xc * d_skip[..., None]
    return y.reshape(bsz, s_len, ng, hg, p)


def ssd_branch(u, offs, conv_w, conv_b, a_log, dt_bias, d_skip, onorm):
    bsz, s_len, _ = u.shape
    hg = SSD_HEADS // SSD_GROUPS
    c = lambda name: _cols(u, offs, name, name).astype(F32)
    xbc = jax.nn.silu(causal_conv(_cols(u, offs, 'ssd_x', 'ssd_c').astype(F32), conv_w.astype(F32)) + conv_b.astype(F32))
    xs, bm, cm = jnp.split(xbc, [SSD_INNER, SSD_INNER + SSD_BC], axis=-1)
    xs = xs.reshape(bsz, s_len, SSD_GROUPS, hg, SSD_HEADDIM)
    bm = bm.reshape(bsz, s_len, SSD_GROUPS, SSD_STATE)
    cm = cm.reshape(bsz, s_len, SSD_GROUPS, SSD_STATE)
    dt = jax.nn.softplus(c('ssd_dt') + dt_bias.astype(F32)).reshape(bsz, s_len, SSD_GROUPS, hg)
    a_head = -jnp.exp(a_log.astype(F32)).reshape(SSD_GROUPS, hg)
    y = ssd_chunked(xs, dt, a_head, bm, cm, d_skip.astype(F32).reshape(SSD_GROUPS, hg)).reshape(bsz, s_len, SSD_INNER)
    return rmsnorm(y * jax.nn.silu(c('ssd_z')), onorm)


def compress_tokens(kv, pos_emb, w1, w2):
    bsz, ng, s_len, hd = kv.shape
    r = CMP_BLOCK // CMP_STRIDE
    n_sub = s_len // CMP_STRIDE
    nc = n_sub - r + 1
    sub = kv.reshape(bsz, ng, n_sub, CMP_STRIDE, hd)
    blocks = jnp.concatenate([sub[:, :, i:i + nc] for i in range(r)], axis=3) + pos_emb.astype(F32)
    flat = blocks.reshape(bsz, ng, nc, CMP_BLOCK * hd)
    return jax.nn.silu(flat @ w1.astype(F32)) @ w2.astype(F32)


def nsa_attention(q, kc, vc, ks, vs, kw, vw, gates):
    bsz, ng, hg, s_len, hd = q.shape
    nc = kc.shape[2]
    nsel = s_len // SEL_BLOCK
    topk = min(SEL_TOPK, nsel)
    scale = hd ** -0.5
    slopes = alibi_slopes(ng * hg).reshape(ng, hg, 1, 1)
    cmp_start = jnp.arange(nc) * CMP_STRIDE
    cmp_pos = cmp_start + CMP_BLOCK - 1
    sel_start = jnp.arange(nsel) * SEL_BLOCK
    overlap = ((cmp_start[:, None] <= sel_start[None, :] + SEL_BLOCK - 1)
               & (cmp_pos[:, None] >= sel_start[None, :])).astype(F32)
    ks_blk = ks.reshape(bsz, ng, nsel, SEL_BLOCK, hd)
    vs_blk = vs.reshape(bsz, ng, nsel, SEL_BLOCK, hd)
    kw_pad = jnp.pad(kw, ((0, 0), (0, 0), (WINDOW, 0), (0, 0)))
    vw_pad = jnp.pad(vw, ((0, 0), (0, 0), (WINDOW, 0), (0, 0)))
    bi = jnp.arange(bsz)[:, None, None, None]
    gi = jnp.arange(ng)[None, :, None, None]
    jj = jnp.arange(nsel)
    in_blk = jnp.arange(SEL_BLOCK)

    def block(q0):
        qb = lax.dynamic_slice_in_dim(q, q0, NSA_QBLOCK, axis=3)
        t = q0 + jnp.arange(NSA_QBLOCK)
        s = jnp.einsum('bghqd,bgkd->bghqk', qb, kc) * scale - slopes * (t[:, None] - cmp_pos[None, :]).astype(F32)
        p_c = masked_softmax(s, cmp_pos[None, :] <= t[:, None])
        o_c = jnp.einsum('bghqk,bgkd->bghqd', p_c, vc)
        imp = jnp.einsum('bgqk,kj->bgqj', jnp.sum(p_c, axis=2), overlap)
        cur = t // SEL_BLOCK
        blk_valid = jj[None, :] <= cur[:, None]
        forced = (jj[None, :] == 0) | (jj[None, :] == cur[:, None]) | (jj[None, :] == cur[:, None] - 1)
        imp = jnp.where(blk_valid, jnp.where(forced, FORCED_SCORE, imp), -1.0)
        vals, idx = lax.top_k(imp, topk)
        pos_b = idx[..., None] * SEL_BLOCK + in_blk
        ok = ((vals >= 0.0)[..., None] & (pos_b <= t[:, None, None])).reshape(bsz, ng, NSA_QBLOCK, topk * SEL_BLOCK)
        pos_s = pos_b.reshape(bsz, ng, NSA_QBLOCK, topk * SEL_BLOCK)
        k_g = ks_blk[bi, gi, idx].reshape(bsz, ng, NSA_QBLOCK, topk * SEL_BLOCK, hd)
        v_g = vs_blk[bi, gi, idx].reshape(bsz, ng, NSA_QBLOCK, topk * SEL_BLOCK, hd)
        s = jnp.einsum('bghqd,bgqkd->bghqk', qb, k_g) * scale - slopes * (t[:, None] - pos_s[:, :, None]).astype(F32)
        p_s = masked_softmax(s, ok[:, :, None])
        o_s = jnp.einsum('bghqk,bgqkd->bghqd', p_s, v_g)
        kwb = lax.dynamic_slice_in_dim(kw_pad, q0, WINDOW + NSA_QBLOCK, axis=2)
        vwb = lax.dynamic_slice_in_dim(vw_pad, q0, WINDOW + NSA_QBLOCK, axis=2)
        pos_w = q0 - WINDOW + jnp.arange(WINDOW + NSA_QBLOCK)
        dist = t[:, None] - pos_w[None, :]
        okw = (dist >= 0) & (dist < WINDOW) & (pos_w[None, :] >= 0)
        s = jnp.einsum('bghqd,bgkd->bghqk', qb, kwb) * scale - slopes * dist.astype(F32)
        o_w = jnp.einsum('bghqk,bgkd->bghqd', masked_softmax(s, okw), vwb)
        gb = lax.dynamic_slice_in_dim(gates, q0, NSA_QBLOCK, axis=3)
        return gb[..., 0:1] * o_c + gb[..., 1:2] * o_s + gb[..., 2:3] * o_w

    starts = jnp.arange(s_len // NSA_QBLOCK, dtype=jnp.int32) * NSA_QBLOCK
    out = lax.map(block, starts)
    return jnp.moveaxis(out, 0, 3).reshape(bsz, ng, hg, s_len, hd)


def nsa_branch(u, offs, cmp_pos_k, cmp_pos_v, w_ck1, w_ck2, w_cv1, w_cv2):
    bsz, s_len, _ = u.shape
    ng, hg, hd = NSA_KV_HEADS, NSA_HEADS // NSA_KV_HEADS, NSA_HEADDIM
    c = lambda name: _cols(u, offs, name, name).astype(F32)
    q = c('nsa_q').reshape(bsz, s_len, ng, hg, hd).transpose(0, 2, 3, 1, 4)
    kvh = lambda t: t.reshape(bsz, s_len, ng, hd).transpose(0, 2, 1, 3)
    kc = compress_tokens(kvh(c('nsa_kc')), cmp_pos_k, w_ck1, w_ck2)
    vc = compress_tokens(kvh(c('nsa_vc')), cmp_pos_v, w_cv1, w_cv2)
    gates = jax.nn.sigmoid(c('nsa_gate')).reshape(bsz, s_len, ng, hg, 3).transpose(0, 2, 3, 1, 4)
    o = nsa_attention(q, kc, vc, kvh(c('nsa_ks')), kvh(c('nsa_vs')), kvh(c('nsa_kw')), kvh(c('nsa_vw')), gates)
    o = o.transpose(0, 3, 1, 2, 4).reshape(bsz, s_len, NSA_Q)
    return o * jax.nn.silu(c('nsa_z'))


def _dt_bias(key, n):
    dt = jnp.exp(jax.random.uniform(key, (DEPTH, n), F32, minval=math.log(1e-3), maxval=math.log(1e-1)))
    return dt + jnp.log(-jnp.expm1(-dt))


def setup_inputs(seed: int = 0) -> dict:
    key = jax.random.key(seed)
    ks = jax.random.split(key, 32)
    nrm = lambda k, shape, sc: jax.random.normal(k, shape, F32) * sc
    gain = lambda k, n: 1.0 + 0.02 * jax.random.normal(k, (DEPTH, n), F32)
    a_log = lambda k, n: jnp.log(jax.random.uniform(k, (DEPTH, n), F32, minval=1.0, maxval=16.0))
    hd = NSA_HEADDIM
    return {
        'x': jax.random.normal(ks[0], (BATCH, SEQ, D_MODEL), F32),
        'norm_pre': gain(ks[1], D_MODEL),
        'norm_post': gain(ks[2], D_MODEL),
        'w_in': nrm(ks[3], (DEPTH, D_MODEL, D_IN), D_MODEL ** -0.5),
        'conv_a': nrm(ks[4], (DEPTH, CONV_K, 2 * GDN_QK + GDN_V), CONV_K ** -0.5),
        'a_log_a': a_log(ks[5], GDN_HEADS),
        'dt_bias_a': _dt_bias(ks[6], GDN_HEADS),
        'onorm_a': gain(ks[7], GDN_DV),
        'w_gk': nrm(ks[8], (DEPTH, GLA_LOWRANK, GLA_K), GLA_LOWRANK ** -0.5),
        'b_gk': nrm(ks[9], (DEPTH, GLA_K), 0.02),
        'onorm_b': gain(ks[10], GLA_DV),
        'conv_c': nrm(ks[11], (DEPTH, CONV_K, SSD_INNER + 2 * SSD_BC), CONV_K ** -0.5),
        'conv_bias_c': nrm(ks[12], (DEPTH, SSD_INNER + 2 * SSD_BC), 0.02),
        'a_log_c': a_log(ks[13], SSD_HEADS),
        'dt_bias_c': _dt_bias(ks[14], SSD_HEADS),
        'd_skip_c': 1.0 + 0.1 * jax.random.normal(ks[15], (DEPTH, SSD_HEADS), F32),
        'onorm_c': gain(ks[16], SSD_INNER),
        'cmp_pos_k': nrm(ks[17], (DEPTH, CMP_BLOCK, hd), 0.02),
        'cmp_pos_v': nrm(ks[18], (DEPTH, CMP_BLOCK, hd), 0.02),
        'w_ck1': nrm(ks[19], (DEPTH, CMP_BLOCK * hd, hd), (CMP_BLOCK * hd) ** -0.5),
        'w_ck2': nrm(ks[20], (DEPTH, hd, hd), hd ** -0.5),
        'w_cv1': nrm(ks[21], (DEPTH, CMP_BLOCK * hd, hd), (CMP_BLOCK * hd) ** -0.5),
        'w_cv2': nrm(ks[22], (DEPTH, hd, hd), hd ** -0.5),
        'w_br': nrm(ks[23], (DEPTH, N_BRANCH, BR_WIDTH, D_MODEL), BR_WIDTH ** -0.5),
        'w_out': nrm(ks[24], (DEPTH, D_MODEL, D_MODEL), D_MODEL ** -0.5),
    }


def reference(x, norm_pre, norm_post, w_in, conv_a, a_log_a, dt_bias_a, onorm_a, w_gk, b_gk, onorm_b,
              conv_c, conv_bias_c, a_log_c, dt_bias_c, d_skip_c, onorm_c, cmp_pos_k, cmp_pos_v,
              w_ck1, w_ck2, w_cv1, w_cv2, w_br, w_out):
    offs = _in_offsets()
    bsz, s_len, _ = x.shape
    for l in range(DEPTH):
        h = rmsnorm(x, norm_pre[l])
        u = h @ w_in[l]
        branches = (
            gdn_branch(u, offs, conv_a[l], a_log_a[l], dt_bias_a[l], onorm_a[l]),
            gla_branch(u, offs, w_gk[l], b_gk[l], onorm_b[l]),
            ssd_branch(u, offs, conv_c[l], conv_bias_c[l], a_log_c[l], dt_bias_c[l], d_skip_c[l], onorm_c[l]),
            nsa_branch(u, offs, cmp_pos_k[l], cmp_pos_v[l], w_ck1[l], w_ck2[l], w_cv1[l], w_cv2[l]),
        )
        gates = jax.nn.sigmoid(_cols(u, offs, 'merge_gate', 'merge_gate').astype(F32)).reshape(bsz, s_len, N_BRANCH, D_MODEL)
        merged = jnp.zeros((bsz, s_len, D_MODEL), F32)
        for n, y in enumerate(branches):
            merged = merged + gates[:, :, n] * (y @ w_br[l, n].astype(F32))
        out = merged.astype(x.dtype) @ w_out[l]
        x = x + rmsnorm(out, norm_post[l])
    return x
```

```python
from contextlib import ExitStack
import os
NSTAGE = float(os.environ.get('NSTAGE', '99'))
import numpy as np
import concourse.bass as bass
import concourse.mybir as mybir
from concourse.bass_utils import run_bass_kernel_spmd

F32 = mybir.dt.float32
BF16 = mybir.dt.bfloat16
I32 = mybir.dt.int32
ALU = mybir.AluOpType
AF = mybir.ActivationFunctionType
AX = mybir.AxisListType

ENGS = ('pe', 'act', 'dve', 'pool', 'sp')
NDSEM = 12


class Tile:
    def __init__(self, name, ap, rid):
        self.name, self.ap, self.rid = name, ap, rid

    def __getitem__(self, k):
        return Tile(self.name, self.ap[k], self.rid)

    def re(self, s, **kw):
        return Tile(self.name, self.ap.rearrange(s, **kw), self.rid)

    def bc(self, axis, shape):
        return Tile(self.name, self.ap.unsqueeze(axis).to_broadcast(list(shape)), self.rid)

    def v(self, ap):
        return Tile(self.name, ap, self.rid)


class Op:
    __slots__ = ('eng', 'fn', 'deps', 'signal', 'count', 'dma', 'dsem', 'dcount', 'idx')


class Sched:
    def __init__(self, nc):
        self.nc = nc
        self.es = ExitStack()
        self.ops = {e: [] for e in ENGS}
        self.lastw = {}
        self.readers = {}
        self.ndma = {e: 0 for e in ENGS}
        self.dma_ops = {e: [] for e in ENGS}
        self.nres = 0

    def sb(self, name, shape, dtype):
        t = self.es.enter_context(self.nc.sbuf_tensor("sb_" + name, list(shape), dtype))
        self.nres += 1
        return Tile(name, t[:] if hasattr(t, '__getitem__') else t, self.nres)

    def ps(self, name, shape, dtype):
        t = self.es.enter_context(self.nc.psum_tensor("pm_" + name, list(shape), dtype))
        self.nres += 1
        return Tile(name, t[:], self.nres)

    def res(self, name):
        self.nres += 1
        return Tile(name, None, self.nres)

    def view(self, tile, ap, own=False):
        if own:
            self.nres += 1
            return Tile(tile.name, ap, self.nres)
        return Tile(tile.name, ap, tile.rid)

    def _deps(self, reads, writes):
        deps = []
        for r in reads:
            w = self.lastw.get(r.rid)
            if w is not None:
                deps.append(w)
        for r in writes:
            w = self.lastw.get(r.rid)
            if w is not None:
                deps.append(w)
            deps.extend(self.readers.get(r.rid, ()))
        return deps

    def _record(self, op, reads, writes):
        for r in writes:
            self.lastw[r.rid] = op
            self.readers[r.rid] = []
        for r in reads:
            if self.lastw.get(r.rid) is op:
                continue
            self.readers.setdefault(r.rid, []).append(op)

    def op(self, eng, fn, reads=(), writes=()):
        o = Op()
        o.eng, o.fn, o.dma, o.signal, o.count = eng, fn, False, False, 0
        o.deps = self._deps(reads, writes)
        o.idx = len(self.ops[eng])
        self.ops[eng].append(o)
        self._record(o, reads, writes)
        return o

    def dma(self, eng, out_ap, in_ap, reads=(), writes=(), out=False, **kw):
        o = Op()
        o.eng, o.dma, o.signal, o.count = eng, True, True, 0
        oa = out_ap.ap if isinstance(out_ap, Tile) else out_ap
        ia = in_ap.ap if isinstance(in_ap, Tile) else in_ap
        o.fn = lambda e: e.dma_start(out=oa, in_=ia, **kw)
        o.deps = self._deps(reads, writes)
        i = self.ndma[eng]
        self.ndma[eng] += 1
        o.dsem = (eng, i % NDSEM)
        o.dcount = 16 * (i // NDSEM + 1)
        if i >= NDSEM:
            o.deps.append(self.dma_ops[eng][i - NDSEM])
        self.dma_ops[eng].append(o)
        o.idx = len(self.ops[eng])
        self.ops[eng].append(o)
        self._record(o, reads, writes)
        if out:
            self.out_dmas = getattr(self, 'out_dmas', []) + [o]
        return o

    def emit(self):
        nc = self.nc
        fin = Op()
        fin.eng, fin.dma, fin.signal, fin.count, fin.fn = 'sp', False, False, 0, None
        fin.deps = list(getattr(self, 'out_dmas', []))
        self.ops['sp'].append(fin)
        for e in ENGS:
            for o in self.ops[e]:
                for d in o.deps:
                    if not d.dma:
                        if d.eng == 'pe' and o.eng == 'pe':
                            continue
                        d.signal = True
        for e in ENGS:
            c = 0
            for o in self.ops[e]:
                if o.signal and not o.dma:
                    c += 1
                    o.count = c
        sems = {e: self.es.enter_context(nc.semaphore("s_" + e)) for e in ENGS}
        dsems = {}
        for e in ENGS:
            if self.ndma[e]:
                for k in range(min(NDSEM, self.ndma[e])):
                    dsems[(e, k)] = self.es.enter_context(nc.semaphore("d_%s%d" % (e, k)))
        self.stats = {}

        def run(ename, eng):
            known = {}
            nw = 0
            for o in self.ops[ename]:
                need = {}
                for d in o.deps:
                    if d.dma:
                        key, val = ('d',) + d.dsem, d.dcount
                    else:
                        if d.eng == 'pe' and ename == 'pe':
                            continue
                        key, val = ('c', d.eng), d.count
                    if known.get(key, 0) >= val:
                        continue
                    if need.get(key, 0) < val:
                        need[key] = val
                for key, val in need.items():
                    s = sems[key[1]] if key[0] == 'c' else dsems[(key[1], key[2])]
                    eng.wait_ge(s, val)
                    known[key] = val
                    nw += 1
                if o.fn is None:
                    continue
                ins = o.fn(eng)
                if o.dma:
                    ins.then_inc(dsems[o.dsem], 16)
                elif o.signal:
                    ins.then_inc(sems[ename], 1)
            self.stats[ename] = (len(self.ops[ename]), nw)

        with nc.Block() as block:
            @block.sync
            def _(eng):
                run('sp', eng)

            @block.scalar
            def _(eng):
                run('act', eng)

            @block.vector
            def _(eng):
                run('dve', eng)

            @block.gpsimd
            def _(eng):
                run('pool', eng)

            @block.tensor
            def _(eng):
                run('pe', eng)
        self.es.close()


class B:
    def __init__(self, S):
        self.S = S

    @staticmethod
    def _t(xs):
        return [x for x in xs if isinstance(x, Tile)]

    @staticmethod
    def _a(x):
        return x.ap if isinstance(x, Tile) else x

    def mm(self, out, lhsT, rhs, start=True, stop=True):
        o, l, r = out.ap, lhsT.ap, rhs.ap
        self.S.op('pe', lambda e: e.matmul(o, l, r, start=start, stop=stop, skip_group_check=True),
                  reads=[lhsT, rhs], writes=[out])

    def tr(self, out, in_, ident):
        o, i, d = out.ap, in_.ap, ident.ap
        self.S.op('pe', lambda e: e.transpose(o, i, d), reads=[in_, ident], writes=[out])

    def act(self, out, in_, func, bias=None, scale=None, accum=None, eng='act'):
        o, i = out.ap, in_.ap
        kw = {}
        if bias is not None:
            kw['bias'] = self._a(bias)
        if scale is not None:
            kw['scale'] = self._a(scale)
        if accum is not None:
            kw['accum_out'] = accum.ap
        w = [out] + ([accum] if accum is not None else [])
        self.S.op('act', lambda e: e.activation(o, i, func, **kw),
                  reads=self._t([in_, bias, scale]), writes=w)

    def tt(self, eng, out, a, b, op):
        o, x, y = out.ap, a.ap, b.ap
        self.S.op(eng, lambda e: e.tensor_tensor(o, x, y, op), reads=[a, b], writes=[out])

    def ts(self, eng, out, a, s1, op0, s2=None, op1=None):
        o, x = out.ap, a.ap
        a1, a2 = self._a(s1), self._a(s2)
        if op1 is None:
            self.S.op(eng, lambda e: e.tensor_scalar(o, x, a1, None, op0), reads=self._t([a, s1]), writes=[out])
        else:
            self.S.op(eng, lambda e: e.tensor_scalar(o, x, a1, a2, op0, op1), reads=self._t([a, s1, s2]), writes=[out])

    def stt(self, out, a, s, b, op0, op1):
        o, x, y, sc = out.ap, a.ap, b.ap, self._a(s)
        self.S.op('dve', lambda e: e.scalar_tensor_tensor(o, x, sc, y, op0, op1), reads=self._t([a, s, b]), writes=[out])

    def cp(self, eng, out, in_):
        o, i = out.ap, in_.ap
        if eng == 'act':
            self.S.op('act', lambda e: e.copy(o, i), reads=[in_], writes=[out])
        else:
            self.S.op(eng, lambda e: e.tensor_copy(o, i), reads=[in_], writes=[out])

    def red(self, out, in_, op=None, axis=None):
        o, i = out.ap, in_.ap
        op = op or ALU.add
        axis = axis or AX.X
        self.S.op('dve', lambda e: e.tensor_reduce(o, i, axis, op), reads=[in_], writes=[out])

    def memset(self, eng, out, val):
        o = out.ap
        self.S.op(eng, lambda e: e.memset(o, val), reads=[], writes=[out])

    def asel(self, out, in_, pattern, cmp, fill, base, cm):
        o, i = out.ap, in_.ap
        def fn(e):
            try:
                return e.affine_select(o, i, pattern, cmp, fill, base=base, channel_multiplier=cm)
            except Exception:
                print("ASEL FAIL", pattern, base, cm, fill, o)
                raise
        self.S.op('pool', fn, reads=[in_], writes=[out])

    def scan(self, out, d0, d1, init, op0, op1):
        o, x, y, ii = out.ap, d0.ap, d1.ap, self._a(init)
        self.S.op('dve', lambda e: e.tensor_tensor_scan(o, x, y, ii, op0, op1), reads=self._t([d0, d1, init]), writes=[out])

    def recip(self, out, in_):
        o, i = out.ap, in_.ap
        self.S.op('dve', lambda e: e.reciprocal(o, i), reads=[in_], writes=[out])

    def max8(self, out, in_):
        o, i = out.ap, in_.ap
        self.S.op('dve', lambda e: e.max(o, i), reads=[in_], writes=[out])

    def mrep(self, out, rep, vals, imm):
        o, r, v = out.ap, rep.ap, vals.ap
        self.S.op('dve', lambda e: e.match_replace(o, r, v, imm), reads=[rep, vals], writes=[out])

    def dma(self, eng, out, in_, out_final=False, **kw):
        self.S.dma(eng, out, in_, reads=self._t([in_]), writes=self._t([out]), out=out_final, **kw)


D_MODEL = 1024
NORM_EPS = 1e-6
IN_SPLITS = (
    ('gdn_q', 512), ('gdn_k', 512), ('gdn_v', 512), ('gdn_beta', 4), ('gdn_a', 4), ('gdn_z', 512),
    ('gla_q', 256), ('gla_k', 256), ('gla_v', 512), ('gla_gk', 16), ('gla_z', 512),
    ('ssd_x', 512), ('ssd_b', 256), ('ssd_c', 256), ('ssd_dt', 8), ('ssd_z', 512),
    ('nsa_q', 512), ('nsa_kc', 128), ('nsa_vc', 128), ('nsa_ks', 128), ('nsa_vs', 128),
    ('nsa_kw', 128), ('nsa_vw', 128), ('nsa_gate', 24), ('nsa_z', 512),
    ('merge_gate', 4096),
)
OFF = {}
_s = 0
for _n, _w in IN_SPLITS:
    OFF[_n] = _s
    _s += _w
D_IN = _s
MT = 512
NEG = -30000.0


def host_constants(S_len):
    c = {}
    ident = np.eye(128, dtype=np.float32)
    U = np.triu(np.ones((128, 128), np.float32))
    ones = np.ones((128, 128), np.float32)
    jidx = np.tile(np.arange(64, dtype=np.float32)[None, :], (128, 1))
    e0 = np.zeros((128, 64), np.float32)
    e0[:, 0] = 1.0
    half = (np.arange(128) >= 64).astype(np.float32)[:, None]
    c['cst'] = np.concatenate([ident, U, ones, jidx, e0, half], axis=1)
    slopes = 2.0 ** (-np.arange(1, 9, dtype=np.float64))
    t = np.arange(S_len)
    qaug = np.zeros((4, 8, S_len), np.float32)
    for h in range(8):
        qaug[0, h] = slopes[h] * 64
        qaug[1, h] = slopes[h]
        qaug[2, h] = -slopes[h] * 64 * (t // 64)
        qaug[3, h] = -slopes[h] * (t % 64)
    c['qaug'] = qaug
    kaug = np.stack([t // 64, t % 64, np.ones_like(t), np.ones_like(t)]).astype(np.float32)
    c['kaug'] = kaug
    ncp = 256
    cp = np.arange(ncp) * 16 + 31
    kc_ = np.stack([cp // 64, cp % 64, np.ones_like(cp), np.ones_like(cp)]).astype(np.float32)
    c['kaugc'] = np.concatenate([np.zeros((4, 1), np.float32), kc_[:, :-1]], axis=1)
    nsel = S_len // 64
    cs = np.arange(ncp) * 16
    ss = np.arange(64) * 64
    ov = ((cs[:, None] <= ss[None, :] + 63) & (cp[:, None] >= ss[None, :])).astype(np.float32)
    ov[:, nsel:] = 0.0
    ov = np.concatenate([np.zeros((1, 64), np.float32), ov[:-1]], axis=0)
    c['overlap'] = ov
    E = np.zeros((64, S_len), np.float32)
    E[t // 64, t] = 1.0
    c['esel'] = E
    return c


PARAM_SHAPES = {
    'norm_pre': [1024], 'norm_post': [1024],
    'conv_aT': [128, 12, 4], 'a_log_a': [4], 'dt_bias_a': [4], 'onorm_a': [128],
    'w_gk': [16, 256], 'b_gkT': [128, 2], 'onorm_b': [128],
    'conv_cT': [128, 8, 4], 'conv_bias_cT': [128, 8], 'a_log_c': [8], 'dt_bias_c': [8], 'd_skip_c': [8],
    'onorm_c': [512], 'cmp_pos_kT': [64, 32], 'cmp_pos_vT': [64, 32],
    'w_ck1': [2048, 64], 'w_ck2': [64, 64], 'w_cv1': [2048, 64], 'w_cv2': [64, 64],
    'w_br': [4, 512, 1024], 'w_out': [1024, 1024], 'w_in': [1024, D_IN],
}


def prep_inputs(inp, depth):
    o = {}
    f = lambda a: np.ascontiguousarray(np.asarray(a, dtype=np.float32))
    for k in ('norm_pre', 'norm_post', 'a_log_a', 'dt_bias_a', 'onorm_a', 'w_gk', 'onorm_b', 'a_log_c',
              'dt_bias_c', 'd_skip_c', 'onorm_c', 'w_ck1', 'w_ck2', 'w_cv1', 'w_cv2', 'w_br', 'w_out', 'w_in'):
        o[k] = f(inp[k][:depth])
    o['conv_aT'] = f(np.asarray(inp['conv_a'])[:depth].reshape(depth, 4, 12, 128).transpose(0, 3, 2, 1))
    o['conv_cT'] = f(np.asarray(inp['conv_c'])[:depth].reshape(depth, 4, 8, 128).transpose(0, 3, 2, 1))
    o['conv_bias_cT'] = f(np.asarray(inp['conv_bias_c'])[:depth].reshape(depth, 8, 128).transpose(0, 2, 1))
    o['b_gkT'] = f(np.asarray(inp['b_gk'])[:depth].reshape(depth, 2, 128).transpose(0, 2, 1))
    o['cmp_pos_kT'] = f(np.asarray(inp['cmp_pos_k'])[:depth].transpose(0, 2, 1))
    o['cmp_pos_vT'] = f(np.asarray(inp['cmp_pos_v'])[:depth].transpose(0, 2, 1))
    return o


def build(S_len=4096, depth=4, branches=(0, 1, 2, 3)):
    nc = bass.Bass("TRN2", target_bir_lowering=False)
    NT = S_len // 128
    NM = S_len // MT
    S = Sched(nc)
    b = B(S)
    dr = {}
    dr['x'] = nc.dram_tensor("x", [S_len, 1024], F32, kind="ExternalInput").ap()
    for k, shp in PARAM_SHAPES.items():
        dr[k] = nc.dram_tensor(k, [depth] + shp, F32, kind="ExternalInput").ap()
    hc = host_constants(S_len)
    for k, v in hc.items():
        dr[k] = nc.dram_tensor(k, list(v.shape), F32, kind="ExternalInput").ap()
    y = nc.dram_tensor("y", [S_len, 1024], F32, kind="ExternalOutput").ap()
    yres = [S.res("y%d" % g) for g in range(NT)]

    def dt(ap, res=None):
        return Tile('dram', ap, res.rid if res is not None else 0)

    cst = S.sb("cst", [128, 513], F32)
    b.dma('sp', cst, dt(dr['cst']))
    ident_f, U_f, ones_f = cst[:, 0:128], cst[:, 128:256], cst[:, 256:384]
    cstb = S.sb("cstb", [128, 384], BF16)
    b.cp('dve', cstb, cst[:, 0:384])
    ident_b, U_b, ones_b = cstb[:, 0:128], cstb[:, 128:256], cstb[:, 256:384]
    mhalf = S.sb("mhalf", [128, 1], F32)
    b.memset('dve', mhalf, -0.5)

    PS = [S.ps("ps%d" % i, [128, 512], F32) for i in range(7)]
    PTb = S.ps("ptb", [128, 1024], BF16)
    psi = [0]

    def nps(lo=0):
        psi[0] += 1
        return PS[lo + psi[0] % (7 - lo)]

    h4 = lambda t: t.re("p (h i) -> p h i", h=4)

    pools = {}

    def scr(cls, i):
        key = (cls, i)
        if key not in pools:
            pools[key] = S.sb("scr%s%d" % (cls, i), [128, 512], F32 if cls == 'A' else BF16)
        return pools[key]

    FM = S.sb("FM", [128, 16, MT], BF16)
    xts = [S.sb("xt%d" % i, [128, 1024], F32) for i in range(1)]
    hb = S.sb("hb", [128, 1024], BF16)
    junk = hb
    hT = S.sb("hT", [128, 8, MT], BF16)
    wbufs = [S.sb("wb%d" % i, [128, 8, 528], BF16) for i in range(2)]
    wi = [0]
    merged = [S.sb("mg%d" % i, [128, 1024], F32) for i in range(4)]
    gpre = S.sb("gpre", [128, 1024], F32)
    gpost = S.sb("gpost", [128, 1024], F32)
    ssq = S.sb("ssq", [128, 1], F32)
    rstd = S.sb("rstd", [128, 1], F32)
    ybr = [S.sb("ybr%d" % i, [128, 512], BF16) for i in range(4)]
    yT = S.sb("yT", [128, 4, 4, 128], BF16)
    zz = [S.sb("zz%d" % i, [128, 512], BF16) for i in range(4)]
    raw = [S.sb("raw%d" % i, [128, 515], BF16) for i in range(2)]
    dg = [S.sb("dg%d" % i, [128, 4, 128], BF16) for i in range(2)]
    sg = scr('A', 0)
    tmpm = scr('A', 1)
    mgb = hb
    mgT = yT[:, 0:2].re("p a k t -> p (a k) t")

    def bcast_load(tile, src_ap, n):
        b.dma('sp', tile, dt(src_ap.partition_broadcast(128)))

    def wload(w_l, col0, ncols, rows8=True):
        wb = wbufs[wi[0] % 2]
        wi[0] += 1
        src = w_l.rearrange("(k p) n -> p k n", p=128)
        nk = src.shape[1]
        hk = max(nk // 2, 1)
        b.dma('pool', wb[:, 0:hk, 0:ncols], dt(src[:, 0:hk, col0:col0 + ncols]))
        if nk > hk:
            b.dma('pool', wb[:, hk:nk, 0:ncols], dt(src[:, hk:nk, col0:col0 + ncols]))
        return wb

    def proj_tok(ps, st, wb, c0, n, o0=0):
        for kc in range(8):
            b.mm(ps[:, o0:o0 + n], hT[:, kc, st * 128:(st + 1) * 128], wb[:, kc, c0:c0 + n], start=kc == 0, stop=kc == 7)

    def proj_feat(ps, wb, c0, mch):
        for kc in range(8):
            b.mm(ps[0:mch, :], wb[:, kc, c0:c0 + mch], hT[:, kc, :], start=kc == 0, stop=kc == 7)

    def rms_rstd(out1, ss1, n):
        b.ts('dve', out1, ss1, 1.0 / n, ALU.mult, NORM_EPS, ALU.add)
        b.tt('pool', out1, out1, mhalf_k(out1.ap.shape[1]), ALU.pow)

    mh_cache = {}

    def mhalf_k(k):
        if k not in mh_cache:
            t = S.sb("mh%d" % k, [128, k], F32)
            b.memset('dve', t, -0.5)
            mh_cache[k] = t
        return mh_cache[k]

    def emit_yT(st):
        for kc in range(4):
            b.tr(PTb[:, kc * 128:(kc + 1) * 128], ybr[st][:, kc * 128:(kc + 1) * 128], ident_b)
        b.cp('dve', yT[:, st], PTb[:, 0:512].re("p (k t) -> p k t", k=4))

    ctx = dict(jidx=cst[:, 384:448], e0=cst[:, 448:512], half01=cst[:, 512:513], emit_yT=emit_yT, scr=scr, FM=FM, nc=nc, S=S, b=b, dr=dr, dt=dt, PS=PS, PTb=PTb, nps=nps, h4=h4, hT=hT, wload=wload,
               proj_tok=proj_tok, proj_feat=proj_feat, rms_rstd=rms_rstd, ident_f=ident_f, U_f=U_f,
               ones_f=ones_f, ident_b=ident_b, U_b=U_b, ones_b=ones_b, ybr=ybr, zz=zz, raw=raw, dg=dg,
               S_len=S_len, NT=NT, NM=NM, bcast_load=bcast_load, depth=depth, mhalf_k=mhalf_k)
    mixers = {}
    if 0 in branches:
        mixers[0] = GDN(ctx)
    if 1 in branches:
        mixers[1] = GLA(ctx)
    if 2 in branches:
        mixers[2] = SSD(ctx)
    if 3 in branches:
        mixers[3] = NSA(ctx)

    for l in range(depth):
        w_in_l = dr['w_in'][l]
        bcast_load(gpre, dr['norm_pre'][l:l + 1, :], 1024)
        bcast_load(gpost, dr['norm_post'][l:l + 1, :], 1024)
        for n in mixers:
            mixers[n].layer_setup(l)
        for m in range(NM):
            for st in range(4):
                g = m * 4 + st
                xt = xts[0]
                src = dr['x'] if l == 0 else y
                b.dma('sp', xt, dt(src[g * 128:(g + 1) * 128, :], yres[g]))
                b.act(junk, xt, AF.Square, accum=ssq)
                rms_rstd(rstd, ssq, 1024)
                b.stt(hb, xt, rstd[:, 0:1], gpre, ALU.mult, ALU.mult)
                for kc in range(8):
                    b.tr(PTb[:, kc * 128:(kc + 1) * 128], hb[:, kc * 128:(kc + 1) * 128], ident_b)
                b.cp('act', hT[:, :, st * 128:(st + 1) * 128], PTb.re("p (k t) -> p k t", k=8))
            first = True
            for n in (0, 1, 2, 3):
                if n not in mixers:
                    continue
                mixers[n].macro(l, m, w_in_l)
                for half in range(2):
                    wg = wload(w_in_l, OFF['merge_gate'] + n * 1024 + half * 512, 512)
                    wbr = wload(dr['w_br'][l, n], half * 512, 512)
                    for st in range(4):
                        pg = nps()
                        proj_tok(pg, st, wg, 0, 512)
                        b.act(sg, pg, AF.Sigmoid)
                        pb = nps()
                        for kc in range(4):
                            b.mm(pb, yT[:, st, kc, :], wbr[:, kc, 0:512], start=kc == 0, stop=kc == 3)
                        mslice = merged[st][:, half * 512:(half + 1) * 512]
                        if first:
                            b.tt('dve', mslice, sg, pb, ALU.mult)
                        else:
                            b.tt('dve', tmpm, sg, pb, ALU.mult)
                            b.tt('pool', mslice, mslice, tmpm, ALU.add)
                first = False
            wo = [wload(dr['w_out'][l], half * 512, 512) for half in range(2)]
            for st in range(4):
                g = m * 4 + st
                osb = merged[st]
                b.cp('act', mgb, merged[st])
                for kc in range(8):
                    b.tr(PTb[:, kc * 128:(kc + 1) * 128], mgb[:, kc * 128:(kc + 1) * 128], ident_b)
                b.cp('dve', mgT, PTb.re("p (k t) -> p k t", k=8))
                for half in range(2):
                    po = nps()
                    for kc in range(8):
                        b.mm(po, mgT[:, kc, :], wo[half][:, kc, 0:512], start=kc == 0, stop=kc == 7)
                    b.cp('act', osb[:, half * 512:(half + 1) * 512], po)
                b.act(junk, osb, AF.Square, accum=ssq)
                rms_rstd(rstd, ssq, 1024)
                xt = xts[0]
                src = dr['x'] if l == 0 else y
                b.dma('sp', xt, dt(src[g * 128:(g + 1) * 128, :], yres[g]))
                b.stt(osb, osb, rstd[:, 0:1], gpost, ALU.mult, ALU.mult)
                b.tt('dve', osb, osb, xt, ALU.add)
                S.dma('sp', y[g * 128:(g + 1) * 128, :], osb.ap, reads=[osb], writes=[yres[g]], out=(l == depth - 1))
    S.emit()
    return nc, S


class Mixer:
    def __init__(self, ctx):
        self.__dict__.update(ctx)

    def conv_chunk(self, ps_in, convw, c, halo, out_fm, bias=None):
        b = self.b
        k = self.cc
        self.cc += 1
        raw, dg = self.raw[k % 2], self.dg[k % 2]
        b.cp('dve', raw[:, 0:3], halo)
        b.cp('act', raw[:, 3:515], ps_in)
        b.cp('dve', halo, raw[:, 512:515])
        b.tt('dve', dg, self.ident_b.bc(1, [128, 4, 128]), convw[:, c, :].bc(2, [128, 4, 128]), ALU.mult)
        p2 = self.nps()
        for t in range(4):
            b.mm(p2, dg[:, t, :], raw[:, t:t + 512], start=t == 0, stop=t == 3)
        if bias is None:
            b.act(out_fm, p2, AF.Silu)
        else:
            b.act(out_fm, p2, AF.Silu, bias=bias)

    def out_norm_heads(self, po, st, onz):
        b, S = self.b, self.S
        o = self.osb4
        b.cp('act', o, po)
        b.tt('pool', self.sq4, o, o, ALU.mult)
        b.red(self.ss4, self.h4(self.sq4))
        self.rms_rstd(self.rs4, self.ss4, 128)
        b.tt('dve', self.h4(o), self.h4(o), self.rs4.bc(2, [128, 4, 128]), ALU.mult)
        b.tt('dve', self.ybr[st], o, onz, ALU.mult)

    def z_block(self, wb, c0, onorm_b, per_head=True):
        b = self.b
        for st in range(4):
            pz = self.nps()
            self.proj_tok(pz, st, wb, c0, 512)
            b.act(self.zz[st], pz, AF.Silu)
            if onorm_b is not None:
                if per_head:
                    b.tt('pool', self.h4(self.zz[st]), self.h4(self.zz[st]), onorm_b.bc(1, [128, 4, 128]), ALU.mult)
                else:
                    b.tt('pool', self.zz[st], self.zz[st], onorm_b, ALU.mult)


class GDN(Mixer):
    def __init__(self, ctx):
        super().__init__(ctx)
        S = self.S
        self.cc = 0
        scr = self.scr
        A4 = lambda i: scr('A', i).re("p (h i) -> p h i", h=4)
        B4 = lambda i: scr('B', i).re("p (h i) -> p h i", h=4)
        self.fm = self.FM[:, 0:12, :]
        self.sqa, self.sqb = B4(9), B4(10)
        self.halo = [S.sb("gdn_h%d" % c, [128, 3], BF16) for c in range(12)]
        self.convw = S.sb("gdn_cw", [128, 12, 4], F32)
        self.nega = S.sb("gdn_nega", [128, 4], F32)
        self.dtb = S.sb("gdn_dtb", [128, 4], F32)
        self.onorm = S.sb("gdn_on", [128, 128], F32)
        self.Sf = S.sb("gdn_Sf", [128, 4, 128], F32)
        self.Sb = S.sb("gdn_Sb", [128, 4, 128], BF16)
        f = lambda n, k: S.sb("gdn_" + n, [128, k], F32)
        self.lnss, self.lnr, self.ba, self.e1, self.nlb = f("lnss", 8), f("lnr", 8), f("ba", 8), f("e1", 4), f("nlb", 4)
        self.apb, self.e2, self.sp, self.g, self.gcl = f("apb", 4), f("e2", 4), f("sp", 4), f("g", 4), f("gcl", 8)
        self.C3, self.X4, self.EX = f("C3", 12), f("X4", 16), f("EX", 16)
        self.D3 = [A4(2), A4(3), A4(4)]
        self.BJ, self.BA = A4(5), A4(6)
        self.E1, self.E2, self.E3 = A4(7), A4(8), A4(9)
        self.EB = B4(0)
        self.P = [A4(10), A4(11)]
        self.PT = [A4(12), A4(13)]
        self.AT = [A4(14), A4(15)]
        self.ATb, self.At, self.Rk, self.Kd = B4(1), B4(2), B4(3), B4(4)
        self.Vb, self.nW, self.Vn, self.qg = B4(5), B4(6), B4(7), B4(8)
        self.osb4 = scr('A', 0)
        self.sq4 = scr('A', 1)
        self.ss4, self.rs4 = f("ss4", 4), f("rs4", 4)
        self.ea = f("ea", 4)

    def layer_setup(self, l):
        b, dr, dt = self.b, self.dr, self.dt
        b.dma('sp', self.convw, dt(dr['conv_aT'][l]))
        self.bcast_load(self.ea, dr['a_log_a'][l:l + 1, :], 4)
        b.act(self.nega, self.ea, AF.Exp)
        b.ts('dve', self.nega, self.nega, -1.0, ALU.mult)
        self.bcast_load(self.dtb, dr['dt_bias_a'][l:l + 1, :], 4)
        self.bcast_load(self.onorm, dr['onorm_a'][l:l + 1, :], 128)
        b.memset('dve', self.Sf, 0.0)
        b.memset('dve', self.Sb, 0.0)
        for c in range(12):
            b.memset('pool', self.halo[c], 0.0)

    def macro(self, l, m, w_in_l):
        b, nps, h4 = self.b, self.nps, self.h4
        fm = self.fm
        for blk in range(3):
            wb = self.wload(w_in_l, blk * 512, 512)
            for cc in range(4):
                c = blk * 4 + cc
                p = nps()
                self.proj_feat(p, wb, cc * 128, 128)
                self.conv_chunk(p, self.convw, c, self.halo[c], fm[:, c, :])
        wb3 = self.wload(w_in_l, OFF['gdn_beta'], 520)
        self.z_block(wb3, 8, self.onorm)
        for st in range(4):
            self.sub(st, wb3)
            self.emit_yT(st)

    def sub(self, st, wb3):
        b, nps, h4 = self.b, self.nps, self.h4
        fm = self.fm
        tk = slice(st * 128, (st + 1) * 128)
        bc4 = lambda t: t.bc(2, [128, 4, 128])
        b.tt('pool', self.sqa, fm[:, 4:8, tk], fm[:, 4:8, tk], ALU.mult)
        b.tt('pool', self.sqb, fm[:, 0:4, tk], fm[:, 0:4, tk], ALU.mult)
        pq = nps()
        for c in range(8):
            sq_c = self.sqa[:, c, :] if c < 4 else self.sqb[:, c - 4, :]
            b.mm(pq[:, c:c + 1], sq_c, self.ones_b[:, 0:1])
        self.proj_tok(pq, st, wb3, 0, 8, o0=8)
        b.act(self.lnss, pq[:, 0:8], AF.Ln, bias=NORM_EPS)
        b.ts('dve', self.lnr, self.lnss, -0.5, ALU.mult)
        b.cp('dve', self.ba, pq[:, 8:16])
        b.act(self.e1, self.ba[:, 0:4], AF.Exp, scale=-1.0)
        b.act(self.nlb, self.e1, AF.Ln, bias=1.0)
        b.tt('dve', self.apb, self.ba[:, 4:8], self.dtb, ALU.add)
        b.act(self.e2, self.apb, AF.Exp)
        b.act(self.sp, self.e2, AF.Ln, bias=1.0)
        b.tt('dve', self.g, self.sp, self.nega, ALU.mult)
        pg = nps()
        b.mm(pg[:, 0:4], self.U_f, self.g)
        b.mm(pg[:, 4:8], self.ones_f, self.g)
        b.cp('dve', self.gcl, pg[:, 0:8])
        gc, gl = self.gcl[:, 0:4], self.gcl[:, 4:8]
        lnrk, lnrq = self.lnr[:, 0:4], self.lnr[:, 4:8]
        cA, cB, cJ = self.C3[:, 0:4], self.C3[:, 4:8], self.C3[:, 8:12]
        b.tt('dve', cJ, lnrk, gc, ALU.subtract)
        b.tt('dve', cA, gc, self.nlb, ALU.subtract)
        b.tt('dve', cA, cA, lnrk, ALU.add)
        b.stt(cB, gc, float(np.log(128.0 ** -0.5)), lnrq, ALU.add, ALU.add)
        X4 = self.X4
        b.cp('pool', X4[:, 0:4], cA)
        b.tt('pool', X4[:, 4:8], cJ, gl, ALU.add)
        b.ts('pool', X4[:, 8:12], self.nlb, -1.0, ALU.mult)
        b.cp('pool', X4[:, 12:16], gl)
        b.act(self.EX, X4, AF.Exp)
        sRk, sKd, sVb, dec = self.EX[:, 0:4], self.EX[:, 4:8], self.EX[:, 8:12], self.EX[:, 12:16]
        for v3 in range(3):
            b.tt('dve', self.D3[v3], self.ident_f.bc(1, [128, 4, 128]), bc4(self.C3[:, v3 * 4:(v3 + 1) * 4]), ALU.mult)
        b.cp('pool', self.BJ, bc4(cJ))
        b.cp('pool', self.BA, bc4(cA))
        f4 = lambda t: t.re("p h i -> p (h i)")
        pX1, pX2, pX3, pXB = nps(), nps(), nps(), nps()
        b.mm(pX1, self.ones_f, f4(self.D3[0]), start=True, stop=False)
        b.mm(pX1, self.ident_f, f4(self.BJ), start=False, stop=True)
        b.mm(pX2, self.ones_f, f4(self.D3[1]), start=True, stop=False)
        b.mm(pX2, self.ident_f, f4(self.BJ), start=False, stop=True)
        b.mm(pX3, self.ones_f, f4(self.D3[2]), start=True, stop=False)
        b.mm(pX3, self.ident_f, f4(self.BA), start=False, stop=True)
        b.mm(pXB, self.ones_f, f4(self.D3[1]))
        b.act(f4(self.E1), pX1, AF.Exp)
        b.act(f4(self.E2), pX2, AF.Exp)
        b.act(f4(self.E3), pX3, AF.Exp)
        b.act(f4(self.EB), pXB, AF.Exp)
        b.asel(self.E1, self.E1, [[0, 4], [1, 128]], ALU.is_ge, 0.0, -1, -1)
        b.asel(self.E2, self.E2, [[0, 4], [1, 128]], ALU.is_ge, 0.0, 0, -1)
        b.asel(self.E3, self.E3, [[0, 4], [-1, 128]], ALU.is_ge, 0.0, -1, 1)
        pG, pKQ = nps(), nps()
        for h in range(4):
            b.mm(pG[:, h * 128:(h + 1) * 128], fm[:, 4 + h, tk], fm[:, 4 + h, tk])
        for h in range(4):
            b.mm(pKQ[:, h * 128:(h + 1) * 128], fm[:, 4 + h, tk], fm[:, h, tk])
        P, PT, AT = self.P, self.PT, self.AT
        b.stt(f4(PT[0]), f4(self.E1), -1.0, pG, ALU.mult, ALU.mult)
        b.stt(f4(P[0]), f4(self.E3), -1.0, pG, ALU.mult, ALU.mult)
        b.tt('dve', f4(self.At), f4(self.E2), pKQ, ALU.mult)
        b.tt('pool', AT[0], PT[0], self.ident_f.bc(1, [128, 4, 128]), ALU.add)
        cur = 0
        for lev in range(1, 7):
            nxt = 1 - cur
            pP = nps()
            for h in range(4):
                b.mm(pP[:, h * 128:(h + 1) * 128], PT[cur][:, h, :], P[cur][:, h, :])
            if lev < 6:
                pPT = nps()
                for h in range(4):
                    b.mm(pPT[:, h * 128:(h + 1) * 128], P[cur][:, h, :], PT[cur][:, h, :])
            b.cp('act', f4(P[nxt]), pP)
            if lev < 6:
                b.cp('dve', f4(PT[nxt]), pPT)
            pA = nps()
            for h in range(4):
                b.mm(pA[:, h * 128:(h + 1) * 128], P[nxt][:, h, :], AT[cur][:, h, :])
            b.tt('dve', f4(AT[nxt]), f4(AT[cur]), pA, ALU.add)
            cur = nxt
        b.cp('act', self.ATb, AT[cur])
        PTb = self.PTb
        for h in range(4):
            b.tr(PTb[:, h * 128:(h + 1) * 128], fm[:, 4 + h, tk], self.ident_b)
            b.tr(PTb[:, (4 + h) * 128:(5 + h) * 128], fm[:, 8 + h, tk], self.ident_b)
        pk = PTb[:, 0:512].re("p (h d) -> p h d", h=4)
        pv = PTb[:, 512:1024].re("p (h d) -> p h d", h=4)
        b.tt('dve', self.Rk, pk, bc4(sRk), ALU.mult)
        b.tt('dve', self.Kd, pk, bc4(sKd), ALU.mult)
        b.tt('dve', self.Vb, pv, bc4(sVb), ALU.mult)
        pW = nps()
        for h in range(4):
            b.mm(pW[:, h * 128:(h + 1) * 128], self.Rk[:, h, :], self.ATb[:, h, :])
        b.act(f4(self.nW), pW, AF.Copy, scale=-1.0)
        pV = nps()
        for h in range(4):
            b.mm(pV[:, h * 128:(h + 1) * 128], self.ATb[:, h, :], self.Vb[:, h, :], start=(h == 0), stop=False)
            b.mm(pV[:, h * 128:(h + 1) * 128], self.nW[:, h, :], self.Sb[:, h, :], start=False, stop=True)
        b.cp('act', f4(self.Vn), pV)
        b.tt('dve', self.qg, fm[:, 0:4, tk], self.EB, ALU.mult)
        pO = nps()
        for h in range(4):
            b.mm(pO[:, h * 128:(h + 1) * 128], self.qg[:, h, :], self.Sb[:, h, :], start=(h == 0), stop=False)
            b.mm(pO[:, h * 128:(h + 1) * 128], self.At[:, h, :], self.Vn[:, h, :], start=False, stop=True)
        pS = nps()
        for h in range(4):
            b.mm(pS[:, h * 128:(h + 1) * 128], self.Kd[:, h, :], self.Vn[:, h, :])
        b.tt('dve', self.Sf, self.Sf, bc4(dec), ALU.mult)
        b.tt('dve', f4(self.Sf), f4(self.Sf), pS, ALU.add)
        b.cp('act', self.Sb, self.Sf)
        self.out_norm_heads(pO, st, self.zz[st])


class GLA(Mixer):
    def __init__(self, ctx):
        super().__init__(ctx)
        S = self.S
        scr = self.scr
        A2 = lambda i, o: scr('A', i)[:, o * 256:(o + 1) * 256].re("p (h i) -> p h i", h=2)
        B4 = lambda i: scr('B', i).re("p (h i) -> p h i", h=4)
        B2 = lambda i, o: scr('B', i)[:, o * 256:(o + 1) * 256].re("p (h i) -> p h i", h=2)
        self.fm = self.FM[:, 0:4, :]
        self.vt = [scr('B', 4 + i) for i in range(4)]
        self.gklo = scr('A', 10)
        self.wgk = S.sb("gla_wgk", [128, 256], F32)
        self.nbg = S.sb("gla_nbg", [128, 2], F32)
        self.onorm = S.sb("gla_on", [128, 128], F32)
        self.e = scr('A', 2)
        self.sp = [scr('A', 6), scr('A', 7)]
        self.nb = [scr('A', 8), scr('A', 9)]
        self.negc = S.sb("gla_negc", [128, 2, 2], F32)
        self.Eq, self.Ek = A2(3, 0), A2(3, 1)
        self.Eg, self.Ed = A2(4, 0), A2(4, 1)
        self.qt, self.kd = B2(0, 0), B2(0, 1)
        self.kt = B4(1)
        self.qg = B4(2)
        self.kdt = scr('B', 3)[:, 0:256]
        self.At = B4(8)
        self.Sf = S.sb("gla_Sf", [128, 2, 128], F32)
        self.Sb = S.sb("gla_Sb", [128, 2, 128], BF16)
        self.osb4 = scr('A', 0)
        self.sq4 = scr('A', 1)
        self.ss4 = S.sb("gla_ss4", [128, 4], F32)
        self.rs4 = S.sb("gla_rs4", [128, 4], F32)
        self.rm = S.sb("gla_rm", [128, 4], F32)
        self.Sd = scr('A', 5)[:, 0:128]

    def layer_setup(self, l):
        b, dr, dt = self.b, self.dr, self.dt
        b.memset('dve', self.wgk, 0.0)
        b.dma('sp', self.wgk[0:16, :], dt(dr['w_gk'][l]))
        b.dma('sp', self.nbg, dt(dr['b_gkT'][l]))
        b.ts('dve', self.nbg, self.nbg, -1.0, ALU.mult)
        self.bcast_load(self.onorm, dr['onorm_b'][l:l + 1, :], 128)
        b.memset('dve', self.Sf, 0.0)
        b.memset('dve', self.Sb, 0.0)
        if l == 0:
            b.memset('dve', self.rm, 0.0)
            b.memset('dve', self.rm[0:64, 0:1], 1.0)
            b.memset('dve', self.rm[64:128, 1:2], 1.0)
            b.memset('dve', self.rm[0:64, 2:3], 0.125)
            b.memset('dve', self.rm[64:128, 3:4], 0.125)

    def macro(self, l, m, w_in_l):
        b, nps = self.b, self.nps
        wb = self.wload(w_in_l, OFF['gla_q'], 512)
        for c in range(4):
            p = nps()
            self.proj_feat(p, wb, c * 128, 128)
            b.cp('act', self.fm[:, c, :], p)
        wb = self.wload(w_in_l, OFF['gla_v'], 512)
        for st in range(4):
            p = nps()
            self.proj_tok(p, st, wb, 0, 512)
            b.cp('act', self.vt[st], p)
        wb = self.wload(w_in_l, OFF['gla_gk'], 528)
        p = nps()
        self.proj_feat(p, wb, 0, 128)
        b.memset('pool', self.gklo, 0.0)
        b.cp('dve', self.gklo[0:16, :], p[0:16, :])
        for c in range(2):
            p = nps()
            b.mm(p, self.wgk[:, c * 128:(c + 1) * 128], self.gklo)
            b.act(self.e, p, AF.Exp, scale=-1.0, bias=self.nbg[:, c:c + 1])
            b.act(self.sp[c], self.e, AF.Ln, bias=1.0)
            b.ts('dve', self.sp[c], self.sp[c], 1.0 / 16.0, ALU.mult)
            for st in range(4):
                tk = slice(st * 128, (st + 1) * 128)
                b.scan(self.nb[c][:, tk], self.ones_f, self.sp[c][:, tk], 0.0, ALU.mult, ALU.add)
        self.z_block(wb, 16, self.onorm)
        for st in range(4):
            self.sub(st)
            self.emit_yT(st)

    def sub(self, st):
        b, nps = self.b, self.nps
        tk = slice(st * 128, (st + 1) * 128)
        fm = self.fm
        for c in range(2):
            nbs = self.nb[c][:, tk]
            ref = self.nb[c][:, st * 128 + 64:st * 128 + 65]
            last = self.nb[c][:, st * 128 + 127:st * 128 + 128]
            b.ts('dve', self.negc[:, c, 0:1], ref, -1.0, ALU.mult)
            b.ts('dve', self.negc[:, c, 1:2], last, -1.0, ALU.mult)
            b.act(self.Eq[:, c, :], nbs, AF.Exp, scale=-1.0, bias=ref)
            b.act(self.Ek[:, c, :], nbs, AF.Exp, bias=self.negc[:, c, 0:1])
            b.act(self.Eg[:, c, :], nbs, AF.Exp, scale=-1.0)
            b.act(self.Ed[:, c, :], nbs, AF.Exp, bias=self.negc[:, c, 1:2])
        b.stt(self.qt, self.Eq, 0.125, fm[:, 0:2, tk], ALU.mult, ALU.mult)
        b.tt('pool', self.kd, self.Ed, fm[:, 2:4, tk], ALU.mult)
        for h in range(4):
            c, r = h // 2, h % 2
            b.stt(self.kt[:, h, :], self.Ek[:, c, :], self.rm[:, r:r + 1], fm[:, 2 + c, tk], ALU.mult, ALU.mult)
            b.stt(self.qg[:, h, :], self.Eg[:, c, :], self.rm[:, 2 + r:3 + r], fm[:, c, tk], ALU.mult, ALU.mult)
        pA = nps()
        for h in range(4):
            c = h // 2
            b.mm(pA[:, h * 128:(h + 1) * 128], self.kt[:, h, :], self.qt[:, c, :])
        b.tt('dve', self.At, self.h4(pA), self.U_b.bc(1, [128, 4, 128]), ALU.mult)
        for c in range(2):
            b.tr(self.PTb[:, c * 128:(c + 1) * 128], self.kd[:, c, :], self.ident_b)
        b.cp('act', self.kdt, self.PTb[:, 0:256])
        pO = nps()
        for h in range(4):
            c = h // 2
            hs = slice(h * 128, (h + 1) * 128)
            b.mm(pO[:, hs], self.At[:, h, :], self.vt[st][:, hs], start=(h == 0), stop=False)
            b.mm(pO[:, hs], self.qg[:, h, :], self.Sb[:, c, :], start=False, stop=True)
        pS = nps()
        for h in range(4):
            c = h // 2
            b.mm(pS[:, h * 128:(h + 1) * 128], self.kdt[:, c * 128:(c + 1) * 128], self.vt[st][:, h * 128:(h + 1) * 128])
        for c in range(2):
            b.ts('dve', self.Sd, self.Sf[:, c, :], self.Eg[:, c, 127:128], ALU.mult)
            b.stt(self.Sd, pS[:, (2 * c) * 128:(2 * c + 1) * 128], self.rm[:, 0:1], self.Sd, ALU.mult, ALU.add)
            b.stt(self.Sf[:, c, :], pS[:, (2 * c + 1) * 128:(2 * c + 2) * 128], self.rm[:, 1:2], self.Sd, ALU.mult, ALU.add)
        b.cp('act', self.Sb, self.Sf)
        self.out_norm_heads(pO, st, self.zz[st])


class SSD(Mixer):
    def __init__(self, ctx):
        super().__init__(ctx)
        S = self.S
        self.cc = 0
        scr = self.scr
        A4 = lambda i: scr('A', i).re("p (h i) -> p h i", h=4)
        B4 = lambda i: scr('B', i).re("p (h i) -> p h i", h=4)
        self.fm = self.FM[:, 0:8, :]
        self.halo = [S.sb("ssd_h%d" % c, [128, 3], BF16) for c in range(8)]
        self.convw = S.sb("ssd_cw", [128, 8, 4], F32)
        self.convb = S.sb("ssd_cb", [128, 8], F32)
        f = lambda n, k: S.sb("ssd_" + n, [128, k], F32)
        self.nega, self.dtb, self.dsk, self.ea = f("nega", 8), f("dtb", 8), f("dsk", 8), f("ea", 8)
        self.onc = S.sb("ssd_onc", [128, 512], F32)
        self.dtr, self.apb, self.e, self.dtv, self.da, self.acl = f("dtr", 8), f("apb", 8), f("e", 8), f("dtv", 8), f("da", 8), f("acl", 16)
        self.X, self.EX, self.sdtd = f("X", 24), f("EX", 24), f("sdtd", 8)
        self.D8 = [A4(2), A4(3)]
        self.Bn = [A4(4), A4(5)]
        self.E = [A4(6), A4(7)]
        self.Mt = [B4(0), B4(1)]
        self.xdt = scr('B', 2)
        self.xdtd = scr('B', 3)
        self.xsk = scr('A', 8)
        self.Btok = scr('B', 4)[:, 0:256]
        self.yo = scr('A', 9)
        self.Hf = S.sb("ssd_Hf", [128, 512], F32)
        self.Hb = S.sb("ssd_Hb", [128, 512], BF16)
        self.ssq = f("ssq", 1)
        self.rstd = f("rstd", 1)
        self.junk = scr('B', 5)

    def layer_setup(self, l):
        b, dr, dt = self.b, self.dr, self.dt
        b.dma('sp', self.convw, dt(dr['conv_cT'][l]))
        b.dma('sp', self.convb, dt(dr['conv_bias_cT'][l]))
        self.bcast_load(self.ea, dr['a_log_c'][l:l + 1, :], 8)
        b.act(self.nega, self.ea, AF.Exp)
        b.ts('dve', self.nega, self.nega, -1.0, ALU.mult)
        self.bcast_load(self.dtb, dr['dt_bias_c'][l:l + 1, :], 8)
        self.bcast_load(self.dsk, dr['d_skip_c'][l:l + 1, :], 8)
        self.bcast_load(self.onc, dr['onorm_c'][l:l + 1, :], 512)
        b.memset('dve', self.Hf, 0.0)
        b.memset('dve', self.Hb, 0.0)
        for c in range(8):
            b.memset('pool', self.halo[c], 0.0)

    def macro(self, l, m, w_in_l):
        b, nps = self.b, self.nps
        for blk in range(2):
            wb = self.wload(w_in_l, OFF['ssd_x'] + blk * 512, 512)
            for cc in range(4):
                c = blk * 4 + cc
                p = nps()
                self.proj_feat(p, wb, cc * 128, 128)
                self.conv_chunk(p, self.convw, c, self.halo[c], self.fm[:, c, :], bias=self.convb[:, c:c + 1])
        wb3 = self.wload(w_in_l, OFF['ssd_dt'], 520)
        self.z_block(wb3, 8, None)
        for st in range(4):
            self.sub(st, wb3)
            self.emit_yT(st)

    def sub(self, st, wb3):
        b, nps = self.b, self.nps
        fm = self.fm
        tk = slice(st * 128, (st + 1) * 128)
        h8 = lambda t, k=64: t.re("p (h i) -> p h i", h=8)
        bc8 = lambda t, k: t.bc(2, [128, 8, k])
        pq = nps()
        self.proj_tok(pq, st, wb3, 0, 8)
        b.cp('dve', self.dtr, pq[:, 0:8])
        b.tt('dve', self.apb, self.dtr, self.dtb, ALU.add)
        b.act(self.e, self.apb, AF.Exp)
        b.act(self.dtv, self.e, AF.Ln, bias=1.0)
        b.tt('dve', self.da, self.dtv, self.nega, ALU.mult)
        pg = nps()
        b.mm(pg[:, 0:8], self.U_f, self.da)
        b.mm(pg[:, 8:16], self.ones_f, self.da)
        b.cp('dve', self.acl, pg[:, 0:16])
        acs, alast = self.acl[:, 0:8], self.acl[:, 8:16]
        b.cp('pool', self.X[:, 0:8], acs)
        b.tt('pool', self.X[:, 8:16], alast, acs, ALU.subtract)
        b.cp('pool', self.X[:, 16:24], alast)
        b.act(self.EX, self.X, AF.Exp)
        eacs, edst, dec = self.EX[:, 0:8], self.EX[:, 8:16], self.EX[:, 16:24]
        b.tt('dve', self.sdtd, self.dtv, edst, ALU.mult)
        bc4 = lambda t: t.bc(2, [128, 4, 128])
        f4 = lambda t: t.re("p h i -> p (h i)")
        pCB = nps()
        for g in range(2):
            b.mm(pCB[:, g * 128:(g + 1) * 128], fm[:, 4 + g, tk], fm[:, 6 + g, tk])
        for g in range(2):
            hs = slice(g * 4, g * 4 + 4)
            b.tt('dve', self.D8[g], self.ident_f.bc(1, [128, 4, 128]), bc4(acs[:, hs]), ALU.mult)
            b.ts('pool', self.Bn[g], bc4(acs[:, hs]), -1.0, ALU.mult)
            pX = nps()
            b.mm(pX, self.ones_f, f4(self.D8[g]), start=True, stop=False)
            b.mm(pX, self.ident_f, f4(self.Bn[g]), start=False, stop=True)
            b.act(f4(self.E[g]), pX, AF.Exp)
            b.asel(self.E[g], self.E[g], [[0, 4], [1, 128]], ALU.is_ge, 0.0, 0, -1)
            b.tt('dve', self.Mt[g], self.E[g], pCB[:, g * 128:(g + 1) * 128].bc(1, [128, 4, 128]), ALU.mult)
        PTb = self.PTb
        for c in range(4):
            b.tr(PTb[:, c * 128:(c + 1) * 128], fm[:, c, tk], self.ident_b)
        for g in range(2):
            b.tr(PTb[:, 512 + g * 128:512 + (g + 1) * 128], fm[:, 4 + g, tk], self.ident_b)
        xtok = h8(PTb[:, 0:512])
        b.tt('dve', h8(self.xdt), xtok, bc8(self.dtv, 64), ALU.mult)
        b.tt('dve', h8(self.xdtd), xtok, bc8(self.sdtd, 64), ALU.mult)
        b.tt('dve', h8(self.xsk), xtok, bc8(self.dsk, 64), ALU.mult)
        b.cp('act', self.Btok, PTb[:, 512:768])
        pY = nps()
        for h in range(8):
            b.mm(pY[:, h * 64:(h + 1) * 64], self.Mt[h // 4][:, h % 4, :], self.xdt[:, h * 64:(h + 1) * 64])
        pF = nps()
        for g in range(2):
            b.mm(pF[:, g * 256:(g + 1) * 256], fm[:, 6 + g, tk], self.Hb[:, g * 256:(g + 1) * 256])
        b.tt('dve', h8(self.yo), h8(pF), bc8(eacs, 64), ALU.mult)
        b.tt('pool', self.yo, self.yo, self.xsk, ALU.add)
        b.tt('dve', self.yo, self.yo, pY, ALU.add)
        pH = nps()
        for g in range(2):
            b.mm(pH[:, g * 256:(g + 1) * 256], self.Btok[:, g * 128:(g + 1) * 128], self.xdtd[:, g * 256:(g + 1) * 256])
        b.tt('dve', h8(self.Hf), h8(self.Hf), bc8(dec, 64), ALU.mult)
        b.tt('dve', self.Hf, self.Hf, pH, ALU.add)
        b.cp('act', self.Hb, self.Hf)
        b.tt('dve', self.yo, self.yo, self.zz[st], ALU.mult)
        b.act(self.junk, self.yo, AF.Square, accum=self.ssq)
        self.rms_rstd(self.rstd, self.ssq, 512)
        b.stt(self.ybr[st], self.yo, self.rstd[:, 0:1], self.onc, ALU.mult, ALU.mult)


class NSA(Mixer):
    def __init__(self, ctx):
        super().__init__(ctx)
        S, scr, S_len, NT = self.S, self.scr, self.S_len, self.NT
        self.kS = [S.sb("nsa_kS%d" % g, [128, S_len], BF16) for g in range(2)]
        self.kW = [S.sb("nsa_kW%d" % g, [128, 8 * 128], BF16) for g in range(2)]
        self.vS = [S.sb("nsa_vS%d" % g, [128, NT, 66], BF16) for g in range(2)]
        self.vW = [S.sb("nsa_vW%d" % g, [128, 8, 66], BF16) for g in range(2)]
        self.kC = [S.sb("nsa_kC%d" % g, [128, 256], BF16) for g in range(2)]
        self.vcT = [S.sb("nsa_vcT%d" % g, [128, 256], BF16) for g in range(2)]
        self.vC = [S.sb("nsa_vC%d" % g, [128, 2, 130], BF16) for g in range(2)]
        self.EK = S.sb("nsa_EK", [128, S_len], BF16)
        self.EKc = S.sb("nsa_EKc", [128, 256], BF16)
        self.qm = self.FM[:, 0:8, :]
        self.qA = self.FM[:, 8:16, :]
        self.qS = S.sb("nsa_qS", [128, 4, 128], BF16)
        self.rawc = [S.sb("nsa_rawc%d" % g, [128, 528], BF16) for g in range(2)]
        self.W1 = S.sb("nsa_W1", [128, 32, 128], BF16)
        self.W2k = S.sb("nsa_W2k", [128, 128], BF16)
        self.W2v = S.sb("nsa_W2v", [128, 128], BF16)
        self.posT = S.sb("nsa_posT", [128, 32], BF16)
        self.cpos = S.sb("nsa_cpos", [128, 1], F32)
        self.wsel = S.sb("nsa_wsel", [128, 8, 128], BF16)
        self.gsig = [S.sb("nsa_gs%d" % i, [128, 24], F32) for i in range(4)]
        self.rmq = S.sb("nsa_rmq", [128, 2], F32)
        self.hid = S.sb("nsa_hid", [128, 32], BF16)
        f = lambda n, k: S.sb("nsa_" + n, [128, k], F32)
        self.dall = S.sb("nsa_dall", [128, 3, 4], F32)
        self.rall = S.sb("nsa_rall", [128, 3, 4], F32)
        self.coef = S.sb("nsa_coef", [128, 3, 4], F32)
        self.imp, self.imp2, self.m8a, self.m8b, self.thr, self.selb = f("imp", 64), f("imp2", 64), f("m8a", 8), f("m8b", 8), f("thr", 1), f("selb", 64)
        self.selbb = S.sb("nsa_selbb", [128, 128], BF16)
        self.cur, self.val, self.fz = f("cur", 1), f("val", 64), f("fz", 64)
        self.Pb = [scr('B', i).re("p (h i) -> p h i", h=4) for i in range(3)]
        self.pi = 0
        self.on = scr('A', 2).re("p (h d) -> p h d", h=8)
        self.tmp4 = scr('A', 3)[:, 0:256].re("p (h d) -> p h d", h=4)
        self.tmp5 = scr('A', 4)[:, 0:256].re("p (h d) -> p h d", h=4)

    def layer_setup(self, l):
        b, dr, dt, nps = self.b, self.dr, self.dt, self.nps
        if NSTAGE < 0:
            return
        if l == 0:
            for g in range(2):
                for t in (self.kS[g], self.kW[g], self.vS[g], self.vW[g], self.kC[g], self.vcT[g], self.vC[g]):
                    b.memset('pool', t, 0.0)
                b.memset('pool', self.vS[g][:, :, 64:65], 1.0)
                b.memset('pool', self.vW[g][:, :, 64:65], 1.0)
                b.memset('pool', self.vC[g][:, :, 64:65], 1.0)
                b.memset('pool', self.vC[g][0:1, 0, 64:65], 0.0)
                for kt in range(2):
                    b.dma('pool', self.vC[g][:, kt, 65:129], dt(dr['overlap'][kt * 128:(kt + 1) * 128, :]))
            b.memset('pool', self.EK, 0.0)
            for c0 in range(0, self.S_len, 1024):
                c1 = min(c0 + 1024, self.S_len)
                b.dma('pool', self.EK[0:64, c0:c1], dt(dr['esel'][:, c0:c1]))
                b.dma('pool', self.EK[64:68, c0:c1], dt(dr['kaug'][:, c0:c1]))
            b.memset('pool', self.EKc, 0.0)
            b.dma('pool', self.EKc[64:68, :], dt(dr['kaugc']))
            b.memset('pool', self.qS, 0.0)
            b.memset('pool', self.selbb, 0.0)
            b.memset('pool', self.rmq, 0.0)
            b.memset('pool', self.rmq[0:64, 0:1], 0.125)
            b.memset('pool', self.rmq[64:128, 1:2], 0.125)
        b.memset('pool', self.W1, 0.0)
        b.dma('pool', self.W1[0:64, :, 0:64], dt(dr['w_ck1'][l].rearrange("(p d) o -> d p o", d=64)))
        b.dma('pool', self.W1[64:128, :, 64:128], dt(dr['w_cv1'][l].rearrange("(p d) o -> d p o", d=64)))
        b.memset('pool', self.W2k, 0.0)
        b.dma('pool', self.W2k[0:64, 0:64], dt(dr['w_ck2'][l]))
        b.dma('pool', self.W2k[0:64, 64:128], dt(dr['w_ck2'][l]))
        b.memset('pool', self.W2v, 0.0)
        b.dma('pool', self.W2v[64:128, 0:64], dt(dr['w_cv2'][l]))
        b.dma('pool', self.posT[0:64, :], dt(dr['cmp_pos_kT'][l]))
        b.dma('pool', self.posT[64:128, :], dt(dr['cmp_pos_vT'][l]))
        pc = nps()
        for p in range(32):
            b.mm(pc[:, 0:1], self.W1[:, p, :], self.posT[:, p:p + 1], start=p == 0, stop=p == 31)
        b.cp('dve', self.cpos, pc[:, 0:1])
        for g in range(2):
            b.memset('pool', self.rawc[g], 0.0)

    def macro(self, l, m, w_in_l):
        b, nps = self.b, self.nps
        if NSTAGE < 1:
            for st in range(4):
                self.emit_yT(st)
            return
        n0 = 4 * m
        wsel = self.wsel
        wb = self.wload(w_in_l, OFF['nsa_q'], 512)
        for c in range(4):
            p = nps()
            self.proj_feat(p, wb, c * 128, 128)
            b.ts('dve', self.qm[:, 2 * c, :], p, self.rmq[:, 0:1], ALU.mult)
            b.act(self.qm[:, 2 * c + 1, :], p, AF.Copy, scale=self.rmq[:, 1:2])
        def bail():
            for st in range(4):
                self.emit_yT(st)
        if NSTAGE < 1.15:
            return bail()
        b.memset('pool', self.qA, 0.0)
        b.dma('pool', self.qA[64:68, :, :], self.dt(self.dr['qaug'][:, :, m * MT:(m + 1) * MT]))
        if NSTAGE < 1.25:
            return bail()
        wb = self.wload(w_in_l, OFF['nsa_kc'], 512)
        for g in range(2):
            b.cp('dve', wsel[:, :, 0:64], wb[:, :, g * 64:(g + 1) * 64])
            b.cp('dve', wsel[:, :, 64:128], wb[:, :, 128 + g * 64:128 + (g + 1) * 64])
            p = nps()
            self.proj_feat(p, wsel, 0, 128)
            b.cp('dve', self.rawc[g][:, 0:16], self.rawc[g][:, 512:528])
            b.cp('act', self.rawc[g][:, 16:528], p)
        if NSTAGE < 1.27:
            return bail()
        for g in range(2):
            b.cp('dve', wsel[:, :, 0:64], wb[:, :, 256 + g * 64:256 + (g + 1) * 64])
            b.cp('dve', wsel[:, :, 64:128], wb[:, :, 256 + g * 64:256 + (g + 1) * 64])
            p = nps()
            self.proj_feat(p, wsel, 0, 128)
            b.cp('act', self.kS[g][:, m * MT:(m + 1) * MT], p)
        if NSTAGE < 1.29:
            return bail()
        for st in range(4):
            p = nps()
            self.proj_tok(p, st, wb, 384, 128)
            for g in range(2 if NSTAGE >= 1.2915 else 0):
                b.cp('dve', self.vS[g][:, n0 + st, 0:64], p[:, g * 64:(g + 1) * 64])
        if NSTAGE < 1.35:
            return bail()
        wb = self.wload(w_in_l, OFF['nsa_kw'], 280)
        for g in range(2):
            b.cp('dve', wsel[:, :, 0:64], wb[:, :, g * 64:(g + 1) * 64])
            b.cp('dve', wsel[:, :, 64:128], wb[:, :, g * 64:(g + 1) * 64])
            p = nps()
            self.proj_feat(p, wsel, 0, 128)
            s0 = (n0 % 8) * 128
            b.cp('act', self.kW[g][:, s0:s0 + 512], p)
        for st in range(4):
            p = nps()
            self.proj_tok(p, st, wb, 128, 152)
            for g in range(2):
                b.cp('dve', self.vW[g][:, (n0 + st) % 8, 0:64], p[:, g * 64:(g + 1) * 64])
            b.act(self.gsig[st], p[:, 128:152], AF.Sigmoid)
        if NSTAGE < 1.45:
            return bail()
        wb = self.wload(w_in_l, OFF['nsa_z'], 512)
        self.z_block(wb, 0, None)
        for g in range(2 if NSTAGE >= 2 else 0):
            pC = nps()
            for p_ in range(32):
                b.mm(pC[:, 0:32], self.W1[:, p_, :], self.rawc[g][:, p_:p_ + 497:16], start=p_ == 0, stop=p_ == 31)
            b.act(self.hid, pC[:, 0:32], AF.Silu, bias=self.cpos[:, 0:1])
            pK = nps()
            b.mm(pK[:, 0:32], self.W2k, self.hid)
            b.mm(pK[:, 32:64], self.W2v, self.hid)
            c0 = 32 * m
            cnt = 32
            b.cp('dve', self.kC[g][:, c0:c0 + cnt], pK[:, 0:32])
            b.cp('dve', self.vcT[g][:, c0:c0 + cnt], pK[:, 32:64])
            if m == 0:
                b.memset('pool', self.kC[g][:, 0:1], 0.0)
                b.memset('pool', self.vcT[g][:, 0:1], 0.0)
            for kt in sorted({c0 // 128, (c0 + cnt - 1) // 128}):
                b.tr(self.PTb[:, 0:128], self.vcT[g][:, kt * 128:(kt + 1) * 128], self.ident_b)
                b.cp('dve', self.vC[g][:, kt, 0:64], self.PTb[:, 0:64])
        for st in range(4):
            if NSTAGE >= 3:
                self.sub(m, st)
            self.emit_yT(st)

    def pbuf(self):
        self.pi += 1
        return self.Pb[self.pi % 3]

    def sub(self, m, st):
        b, nps, PS = self.b, self.nps, self.PS
        n = 4 * m + st
        tk = slice(st * 128, (st + 1) * 128)
        f4 = lambda t: t.re("p h i -> p (h i)")
        pOc, pOs, pOw = [PS[0], PS[1]], PS[2], PS[3]
        on = self.on
        causal = lambda t: b.asel(t, t, [[0, 4], [1, 128]], ALU.is_ge, 0.0, 0, -1)
        for g in range(2):
            qrhs = self.qm[:, 4 * g:4 * g + 4, tk]
            arhs = self.qA[:, 4 * g:4 * g + 4, tk]
            kts = [kt for kt in (0, 1) if n >= 16 * kt]
            for ki, kt in enumerate(kts):
                ks_ = slice(kt * 128, (kt + 1) * 128)
                pS = nps(4)
                b.mm(pS, self.kC[g][:, ks_], qrhs, start=True, stop=False)
                b.mm(pS, self.EKc[:, ks_], arhs, start=False, stop=True)
                Pt = self.pbuf()
                b.act(f4(Pt), pS, AF.Exp)
                if n < 16 * kt + 16:
                    b.asel(Pt, Pt, [[0, 4], [1, 128]], ALU.is_ge, 0.0, 128 * n - 2048 * kt - 15, -16)
                for hh in range(4):
                    col = (hh % 2) * 129
                    b.mm(pOc[hh // 2][:, col:col + 129], Pt[:, hh, :], self.vC[g][:, kt, 0:129],
                         start=(ki == 0 and hh % 2 == 0), stop=(ki == len(kts) - 1))
            for bnk in range(2):
                v = pOc[bnk][:, 0:258].re("p (h c) -> p h c", h=2)
                b.cp('dve', self.dall[:, 0, 2 * bnk:2 * bnk + 2], v[:, :, 64])
            rcc = self.rall[:, 0, :]
            b.ts('dve', rcc, self.dall[:, 0, :], 1e-30, ALU.max)
            b.recip(rcc, rcc)
            imp = self.imp
            b.ts('dve', imp, pOc[0][:, 65:129], rcc[:, 0:1], ALU.mult)
            b.stt(imp, pOc[0][:, 194:258], rcc[:, 1:2], imp, ALU.mult, ALU.add)
            b.stt(imp, pOc[1][:, 65:129], rcc[:, 2:3], imp, ALU.mult, ALU.add)
            b.stt(imp, pOc[1][:, 194:258], rcc[:, 3:4], imp, ALU.mult, ALU.add)
            cur, val, fz = self.cur, self.val, self.fz
            b.ts('dve', cur, self.half01, float(2 * n), ALU.add)
            b.ts('dve', val, self.jidx, cur[:, 0:1], ALU.is_le)
            b.tt('dve', imp, imp, val, ALU.mult)
            b.ts('dve', val, val, -1.0, ALU.add)
            b.tt('dve', imp, imp, val, ALU.add)
            b.ts('dve', fz, self.jidx, cur[:, 0:1], ALU.is_equal)
            b.ts('dve', val, self.jidx, 1.0, ALU.add, cur[:, 0:1], ALU.is_equal)
            b.tt('dve', fz, fz, val, ALU.add)
            b.tt('dve', fz, fz, self.e0, ALU.add)
            b.stt(imp, fz, 1.0e4, imp, ALU.mult, ALU.max)
            b.max8(self.m8a, imp)
            b.mrep(self.imp2, self.m8a, imp, -2.0)
            b.max8(self.m8b, self.imp2)
            b.ts('dve', self.thr, self.m8b[:, 7:8], 0.0, ALU.max)
            b.ts('dve', self.selb, imp, self.thr[:, 0:1], ALU.is_ge, 30000.0, ALU.mult)
            b.ts('dve', self.selbb[:, 0:64], self.selb, -30000.0, ALU.add)
            b.tr(self.PTb[:, 0:128], self.selbb, self.ident_b)
            b.cp('dve', self.qS[0:64], self.PTb[0:64, 0:128].bc(1, [64, 4, 128]))
            b.cp('act', self.qS[64:68], self.qA[64:68, 4 * g:4 * g + 4, tk])
            for kt in range(n + 1):
                ks_ = slice(kt * 128, (kt + 1) * 128)
                pS = nps(4)
                b.mm(pS, self.kS[g][:, ks_], qrhs, start=True, stop=False)
                b.mm(pS, self.EK[:, ks_], f4(self.qS), start=False, stop=True)
                Pt = self.pbuf()
                b.act(f4(Pt), pS, AF.Exp)
                if kt == n:
                    causal(Pt)
                for hh in range(4):
                    b.mm(pOs[:, hh * 65:(hh + 1) * 65], Pt[:, hh, :], self.vS[g][:, kt, 0:65],
                         start=(kt == 0 and hh == 0), stop=(kt == n))
            kts = list(range(max(0, n - 4), n + 1))
            for kt in kts:
                ks_ = slice(kt * 128, (kt + 1) * 128)
                sl = kt % 8
                pS = nps(4)
                b.mm(pS, self.kW[g][:, sl * 128:(sl + 1) * 128], qrhs, start=True, stop=False)
                b.mm(pS, self.EK[:, ks_], arhs, start=False, stop=True)
                Pt = self.pbuf()
                b.act(f4(Pt), pS, AF.Exp)
                if kt == n:
                    causal(Pt)
                if kt == n - 4:
                    b.asel(Pt, Pt, [[0, 4], [-1, 128]], ALU.is_ge, 0.0, -1, 1)
                for hh in range(4):
                    b.mm(pOw[:, hh * 65:(hh + 1) * 65], Pt[:, hh, :], self.vW[g][:, sl, 0:65],
                         start=(kt == kts[0] and hh == 0), stop=(kt == n))
            vs_ = pOs[:, 0:260].re("p (h c) -> p h c", h=4)
            vw_ = pOw[:, 0:260].re("p (h c) -> p h c", h=4)
            b.cp('dve', self.dall[:, 1, :], vs_[:, :, 64])
            b.cp('dve', self.dall[:, 2, :], vw_[:, :, 64])
            b.ts('dve', self.rall[:, 1:3, :], self.dall[:, 1:3, :], 1e-30, ALU.max)
            b.recip(self.rall[:, 1:3, :], self.rall[:, 1:3, :])
            gv = self.gsig[st][:, 12 * g:12 * g + 12].re("p (h b) -> p b h", b=3)
            b.tt('dve', self.coef, self.rall, gv, ALU.mult)
            for bnk in range(2):
                v = pOc[bnk][:, 0:258].re("p (h c) -> p h c", h=2)
                b.tt('dve', on[:, 4 * g + 2 * bnk:4 * g + 2 * bnk + 2, :], v[:, :, 0:64],
                     self.coef[:, 0, 2 * bnk:2 * bnk + 2].bc(2, [128, 2, 64]), ALU.mult)
            b.tt('dve', self.tmp4, vs_[:, :, 0:64], self.coef[:, 1, :].bc(2, [128, 4, 64]), ALU.mult)
            b.tt('pool', on[:, 4 * g:4 * g + 4, :], on[:, 4 * g:4 * g + 4, :], self.tmp4, ALU.add)
            b.tt('dve', self.tmp5, vw_[:, :, 0:64], self.coef[:, 2, :].bc(2, [128, 4, 64]), ALU.mult)
            b.tt('pool', on[:, 4 * g:4 * g + 4, :], on[:, 4 * g:4 * g + 4, :], self.tmp5, ALU.add)
        b.tt('dve', self.ybr[st], on.re("p h d -> p (h d)"), self.zz[st], ALU.mult)


def kernel(**inputs):
    depth, S_len, n_cores = 4, 4096, 8
    x = np.asarray(inputs['x'], dtype=np.float32)
    nc, _ = build(S_len, depth)
    pin = prep_inputs(inputs, depth)
    hc = host_constants(S_len)
    in_maps = []
    for i in range(n_cores):
        d = dict(pin)
        d.update(hc)
        d['x'] = np.ascontiguousarray(x[i])
        in_maps.append(d)
    res = run_bass_kernel_spmd(nc, in_maps, core_ids=list(range(n_cores)))
    return np.stack([np.asarray(r['y'], dtype=np.float32) for r in res.results], axis=0)
```

```python
from contextlib import ExitStack
import os
NSTAGE = float(os.environ.get('NSTAGE', '99'))
import numpy as np
import concourse.bass as bass
import concourse.mybir as mybir
from concourse.bass_utils import run_bass_kernel_spmd

F32 = mybir.dt.float32
BF16 = mybir.dt.bfloat16
I32 = mybir.dt.int32
ALU = mybir.AluOpType
AF = mybir.ActivationFunctionType
AX = mybir.AxisListType

ENGS = ('pe', 'act', 'dve', 'pool', 'sp')
NDSEM = 12


class Tile:
    def __init__(self, name, ap, rid):
        self.name, self.ap, self.rid = name, ap, rid

    def __getitem__(self, k):
        return Tile(self.name, self.ap[k], self.rid)

    def re(self, s, **kw):
        return Tile(self.name, self.ap.rearrange(s, **kw), self.rid)

    def bc(self, axis, shape):
        return Tile(self.name, self.ap.unsqueeze(axis).to_broadcast(list(shape)), self.rid)

    def v(self, ap):
        return Tile(self.name, ap, self.rid)


class Op:
    __slots__ = ('eng', 'fn', 'deps', 'signal', 'count', 'dma', 'dsem', 'dcount', 'idx')


class Sched:
    def __init__(self, nc):
        self.nc = nc
        self.es = ExitStack()
        self.ops = {e: [] for e in ENGS}
        self.lastw = {}
        self.readers = {}
        self.ndma = {e: 0 for e in ENGS}
        self.dma_ops = {e: [] for e in ENGS}
        self.nres = 0

    def sb(self, name, shape, dtype):
        t = self.es.enter_context(self.nc.sbuf_tensor("sb_" + name, list(shape), dtype))
        self.nres += 1
        return Tile(name, t[:] if hasattr(t, '__getitem__') else t, self.nres)

    def ps(self, name, shape, dtype):
        t = self.es.enter_context(self.nc.psum_tensor("pm_" + name, list(shape), dtype))
        self.nres += 1
        return Tile(name, t[:], self.nres)

    def res(self, name):
        self.nres += 1
        return Tile(name, None, self.nres)

    def view(self, tile, ap, own=False):
        if own:
            self.nres += 1
            return Tile(tile.name, ap, self.nres)
        return Tile(tile.name, ap, tile.rid)

    def _deps(self, reads, writes):
        deps = []
        for r in reads:
            w = self.lastw.get(r.rid)
            if w is not None:
                deps.append(w)
        for r in writes:
            w = self.lastw.get(r.rid)
            if w is not None:
                deps.append(w)
            deps.extend(self.readers.get(r.rid, ()))
        return deps

    def _record(self, op, reads, writes):
        for r in writes:
            self.lastw[r.rid] = op
            self.readers[r.rid] = []
        for r in reads:
            if self.lastw.get(r.rid) is op:
                continue
            self.readers.setdefault(r.rid, []).append(op)

    def op(self, eng, fn, reads=(), writes=()):
        o = Op()
        o.eng, o.fn, o.dma, o.signal, o.count = eng, fn, False, False, 0
        o.deps = self._deps(reads, writes)
        o.idx = len(self.ops[eng])
        self.ops[eng].append(o)
        self._record(o, reads, writes)
        return o

    def dma(self, eng, out_ap, in_ap, reads=(), writes=(), out=False, **kw):
        o = Op()
        o.eng, o.dma, o.signal, o.count = eng, True, True, 0
        oa = out_ap.ap if isinstance(out_ap, Tile) else out_ap
        ia = in_ap.ap if isinstance(in_ap, Tile) else in_ap
        o.fn = lambda e: e.dma_start(out=oa, in_=ia, **kw)
        o.deps = self._deps(reads, writes)
        i = self.ndma[eng]
        self.ndma[eng] += 1
        o.dsem = (eng, i % NDSEM)
        o.dcount = 16 * (i // NDSEM + 1)
        if i >= NDSEM:
            o.deps.append(self.dma_ops[eng][i - NDSEM])
        self.dma_ops[eng].append(o)
        o.idx = len(self.ops[eng])
        self.ops[eng].append(o)
        self._record(o, reads, writes)
        if out:
            self.out_dmas = getattr(self, 'out_dmas', []) + [o]
        return o

    def emit(self):
        nc = self.nc
        fin = Op()
        fin.eng, fin.dma, fin.signal, fin.count, fin.fn = 'sp', False, False, 0, None
        fin.deps = list(getattr(self, 'out_dmas', []))
        self.ops['sp'].append(fin)
        for e in ENGS:
            for o in self.ops[e]:
                for d in o.deps:
                    if not d.dma:
                        if d.eng == 'pe' and o.eng == 'pe':
                            continue
                        d.signal = True
        for e in ENGS:
            c = 0
            for o in self.ops[e]:
                if o.signal and not o.dma:
                    c += 1
                    o.count = c
        sems = {e: self.es.enter_context(nc.semaphore("s_" + e)) for e in ENGS}
        dsems = {}
        for e in ENGS:
            if self.ndma[e]:
                for k in range(min(NDSEM, self.ndma[e])):
                    dsems[(e, k)] = self.es.enter_context(nc.semaphore("d_%s%d" % (e, k)))
        self.stats = {}

        def run(ename, eng):
            known = {}
            nw = 0
            for o in self.ops[ename]:
                need = {}
                for d in o.deps:
                    if d.dma:
                        key, val = ('d',) + d.dsem, d.dcount
                    else:
                        if d.eng == 'pe' and ename == 'pe':
                            continue
                        key, val = ('c', d.eng), d.count
                    if known.get(key, 0) >= val:
                        continue
                    if need.get(key, 0) < val:
                        need[key] = val
                for key, val in need.items():
                    s = sems[key[1]] if key[0] == 'c' else dsems[(key[1], key[2])]
                    eng.wait_ge(s, val)
                    known[key] = val
                    nw += 1
                if o.fn is None:
                    continue
                ins = o.fn(eng)
                if o.dma:
                    ins.then_inc(dsems[o.dsem], 16)
                elif o.signal:
                    ins.then_inc(sems[ename], 1)
            self.stats[ename] = (len(self.ops[ename]), nw)

        with nc.Block() as block:
            @block.sync
            def _(eng):
                run('sp', eng)

            @block.scalar
            def _(eng):
                run('act', eng)

            @block.vector
            def _(eng):
                run('dve', eng)

            @block.gpsimd
            def _(eng):
                run('pool', eng)

            @block.tensor
            def _(eng):
                run('pe', eng)
        self.es.close()


class B:
    def __init__(self, S):
        self.S = S

    @staticmethod
    def _t(xs):
        return [x for x in xs if isinstance(x, Tile)]

    @staticmethod
    def _a(x):
        return x.ap if isinstance(x, Tile) else x

    def mm(self, out, lhsT, rhs, start=True, stop=True):
        o, l, r = out.ap, lhsT.ap, rhs.ap
        self.S.op('pe', lambda e: e.matmul(o, l, r, start=start, stop=stop, skip_group_check=True),
                  reads=[lhsT, rhs], writes=[out])

    def tr(self, out, in_, ident):
        o, i, d = out.ap, in_.ap, ident.ap
        self.S.op('pe', lambda e: e.transpose(o, i, d), reads=[in_, ident], writes=[out])

    def act(self, out, in_, func, bias=None, scale=None, accum=None, eng='act'):
        o, i = out.ap, in_.ap
        kw = {}
        if bias is not None:
            kw['bias'] = self._a(bias)
        if scale is not None:
            kw['scale'] = self._a(scale)
        if accum is not None:
            kw['accum_out'] = accum.ap
        w = [out] + ([accum] if accum is not None else [])
        self.S.op('act', lambda e: e.activation(o, i, func, **kw),
                  reads=self._t([in_, bias, scale]), writes=w)

    def tt(self, eng, out, a, b, op):
        o, x, y = out.ap, a.ap, b.ap
        self.S.op(eng, lambda e: e.tensor_tensor(o, x, y, op), reads=[a, b], writes=[out])

    def ts(self, eng, out, a, s1, op0, s2=None, op1=None):
        o, x = out.ap, a.ap
        a1, a2 = self._a(s1), self._a(s2)
        if op1 is None:
            self.S.op(eng, lambda e: e.tensor_scalar(o, x, a1, None, op0), reads=self._t([a, s1]), writes=[out])
        else:
            self.S.op(eng, lambda e: e.tensor_scalar(o, x, a1, a2, op0, op1), reads=self._t([a, s1, s2]), writes=[out])

    def stt(self, out, a, s, b, op0, op1):
        o, x, y, sc = out.ap, a.ap, b.ap, self._a(s)
        self.S.op('dve', lambda e: e.scalar_tensor_tensor(o, x, sc, y, op0, op1), reads=self._t([a, s, b]), writes=[out])

    def cp(self, eng, out, in_):
        o, i = out.ap, in_.ap
        if eng == 'act':
            self.S.op('act', lambda e: e.copy(o, i), reads=[in_], writes=[out])
        else:
            self.S.op(eng, lambda e: e.tensor_copy(o, i), reads=[in_], writes=[out])

    def red(self, out, in_, op=None, axis=None):
        o, i = out.ap, in_.ap
        op = op or ALU.add
        axis = axis or AX.X
        self.S.op('dve', lambda e: e.tensor_reduce(o, i, axis, op), reads=[in_], writes=[out])

    def memset(self, eng, out, val):
        o = out.ap
        self.S.op(eng, lambda e: e.memset(o, val), reads=[], writes=[out])

    def asel(self, out, in_, pattern, cmp, fill, base, cm):
        o, i = out.ap, in_.ap
        def fn(e):
            try:
                return e.affine_select(o, i, pattern, cmp, fill, base=base, channel_multiplier=cm)
            except Exception:
                print("ASEL FAIL", pattern, base, cm, fill, o)
                raise
        self.S.op('pool', fn, reads=[in_], writes=[out])

    def scan(self, out, d0, d1, init, op0, op1):
        o, x, y, ii = out.ap, d0.ap, d1.ap, self._a(init)
        self.S.op('dve', lambda e: e.tensor_tensor_scan(o, x, y, ii, op0, op1), reads=self._t([d0, d1, init]), writes=[out])

    def recip(self, out, in_):
        o, i = out.ap, in_.ap
        self.S.op('dve', lambda e: e.reciprocal(o, i), reads=[in_], writes=[out])

    def max8(self, out, in_):
        o, i = out.ap, in_.ap
        self.S.op('dve', lambda e: e.max(o, i), reads=[in_], writes=[out])

    def mrep(self, out, rep, vals, imm):
        o, r, v = out.ap, rep.ap, vals.ap
        self.S.op('dve', lambda e: e.match_replace(o, r, v, imm), reads=[rep, vals], writes=[out])

    def dma(self, eng, out, in_, out_final=False, **kw):
        self.S.dma(eng, out, in_, reads=self._t([in_]), writes=self._t([out]), out=out_final, **kw)


D_MODEL = 1024
NORM_EPS = 1e-6
IN_SPLITS = (
    ('gdn_q', 512), ('gdn_k', 512), ('gdn_v', 512), ('gdn_beta', 4), ('gdn_a', 4), ('gdn_z', 512),
    ('gla_q', 256), ('gla_k', 256), ('gla_v', 512), ('gla_gk', 16), ('gla_z', 512),
    ('ssd_x', 512), ('ssd_b', 256), ('ssd_c', 256), ('ssd_dt', 8), ('ssd_z', 512),
    ('nsa_q', 512), ('nsa_kc', 128), ('nsa_vc', 128), ('nsa_ks', 128), ('nsa_vs', 128),
    ('nsa_kw', 128), ('nsa_vw', 128), ('nsa_gate', 24), ('nsa_z', 512),
    ('merge_gate', 4096),
)
OFF = {}
_s = 0
for _n, _w in IN_SPLITS:
    OFF[_n] = _s
    _s += _w
D_IN = _s
MT = 512
NEG = -30000.0


def host_constants(S_len):
    c = {}
    ident = np.eye(128, dtype=np.float32)
    U = np.triu(np.ones((128, 128), np.float32))
    ones = np.ones((128, 128), np.float32)
    jidx = np.tile(np.arange(64, dtype=np.float32)[None, :], (128, 1))
    e0 = np.zeros((128, 64), np.float32)
    e0[:, 0] = 1.0
    half = (np.arange(128) >= 64).astype(np.float32)[:, None]
    c['cst'] = np.concatenate([ident, U, ones, jidx, e0, half], axis=1)
    slopes = 2.0 ** (-np.arange(1, 9, dtype=np.float64))
    t = np.arange(S_len)
    qaug = np.zeros((4, 8, S_len), np.float32)
    for h in range(8):
        qaug[0, h] = slopes[h] * 64
        qaug[1, h] = slopes[h]
        qaug[2, h] = -slopes[h] * 64 * (t // 64)
        qaug[3, h] = -slopes[h] * (t % 64)
    c['qaug'] = qaug
    kaug = np.stack([t // 64, t % 64, np.ones_like(t), np.ones_like(t)]).astype(np.float32)
    c['kaug'] = kaug
    ncp = 256
    cp = np.arange(ncp) * 16 + 31
    kc_ = np.stack([cp // 64, cp % 64, np.ones_like(cp), np.ones_like(cp)]).astype(np.float32)
    c['kaugc'] = np.concatenate([np.zeros((4, 1), np.float32), kc_[:, :-1]], axis=1)
    nsel = S_len // 64
    cs = np.arange(ncp) * 16
    ss = np.arange(64) * 64
    ov = ((cs[:, None] <= ss[None, :] + 63) & (cp[:, None] >= ss[None, :])).astype(np.float32)
    ov[:, nsel:] = 0.0
    ov = np.concatenate([np.zeros((1, 64), np.float32), ov[:-1]], axis=0)
    c['overlap'] = ov
    E = np.zeros((64, S_len), np.float32)
    E[t // 64, t] = 1.0
    c['esel'] = E
    return c


PARAM_SHAPES = {
    'norm_pre': [1024], 'norm_post': [1024],
    'conv_aT': [128, 12, 4], 'a_log_a': [4], 'dt_bias_a': [4], 'onorm_a': [128],
    'w_gk': [16, 256], 'b_gkT': [128, 2], 'onorm_b': [128],
    'conv_cT': [128, 8, 4], 'conv_bias_cT': [128, 8], 'a_log_c': [8], 'dt_bias_c': [8], 'd_skip_c': [8],
    'onorm_c': [512], 'cmp_pos_kT': [64, 32], 'cmp_pos_vT': [64, 32],
    'w_ck1': [2048, 64], 'w_ck2': [64, 64], 'w_cv1': [2048, 64], 'w_cv2': [64, 64],
    'w_br': [4, 512, 1024], 'w_out': [1024, 1024], 'w_in': [1024, D_IN],
}


def prep_inputs(inp, depth):
    o = {}
    f = lambda a: np.ascontiguousarray(np.asarray(a, dtype=np.float32))
    for k in ('norm_pre', 'norm_post', 'a_log_a', 'dt_bias_a', 'onorm_a', 'w_gk', 'onorm_b', 'a_log_c',
              'dt_bias_c', 'd_skip_c', 'onorm_c', 'w_ck1', 'w_ck2', 'w_cv1', 'w_cv2', 'w_br', 'w_out', 'w_in'):
        o[k] = f(inp[k][:depth])
    o['conv_aT'] = f(np.asarray(inp['conv_a'])[:depth].reshape(depth, 4, 12, 128).transpose(0, 3, 2, 1))
    o['conv_cT'] = f(np.asarray(inp['conv_c'])[:depth].reshape(depth, 4, 8, 128).transpose(0, 3, 2, 1))
    o['conv_bias_cT'] = f(np.asarray(inp['conv_bias_c'])[:depth].reshape(depth, 8, 128).transpose(0, 2, 1))
    o['b_gkT'] = f(np.asarray(inp['b_gk'])[:depth].reshape(depth, 2, 128).transpose(0, 2, 1))
    o['cmp_pos_kT'] = f(np.asarray(inp['cmp_pos_k'])[:depth].transpose(0, 2, 1))
    o['cmp_pos_vT'] = f(np.asarray(inp['cmp_pos_v'])[:depth].transpose(0, 2, 1))
    return o


def build(S_len=4096, depth=4, branches=(0, 1, 2, 3)):
    nc = bass.Bass("TRN2", target_bir_lowering=False)
    NT = S_len // 128
    NM = S_len // MT
    S = Sched(nc)
    b = B(S)
    dr = {}
    dr['x'] = nc.dram_tensor("x", [S_len, 1024], F32, kind="ExternalInput").ap()
    for k, shp in PARAM_SHAPES.items():
        dr[k] = nc.dram_tensor(k, [depth] + shp, F32, kind="ExternalInput").ap()
    hc = host_constants(S_len)
    for k, v in hc.items():
        dr[k] = nc.dram_tensor(k, list(v.shape), F32, kind="ExternalInput").ap()
    y = nc.dram_tensor("y", [S_len, 1024], F32, kind="ExternalOutput").ap()
    wq = {'w_in': nc.dram_tensor("wq_in", [depth, 1024, D_IN], BF16, kind="Internal").ap(),
          'w_br': nc.dram_tensor("wq_br", [depth, 2048, 1024], BF16, kind="Internal").ap(),
          'w_out': nc.dram_tensor("wq_out", [depth, 1024, 1024], BF16, kind="Internal").ap()}
    wqres = {}
    yres = [S.res("y%d" % g) for g in range(NT)]

    def dt(ap, res=None):
        return Tile('dram', ap, res.rid if res is not None else 0)

    cst = S.sb("cst", [128, 513], F32)
    b.dma('sp', cst, dt(dr['cst']))
    ident_f, U_f, ones_f = cst[:, 0:128], cst[:, 128:256], cst[:, 256:384]
    cstb = S.sb("cstb", [128, 384], BF16)
    b.cp('dve', cstb, cst[:, 0:384])
    ident_b, U_b, ones_b = cstb[:, 0:128], cstb[:, 128:256], cstb[:, 256:384]
    mhalf = S.sb("mhalf", [128, 1], F32)
    b.memset('dve', mhalf, -0.5)

    PS = [S.ps("ps%d" % i, [128, 512], F32) for i in range(7)]
    PTb = S.ps("ptb", [128, 1024], BF16)
    psi = [0]

    def nps(lo=0):
        psi[0] += 1
        return PS[lo + psi[0] % (7 - lo)]

    h4 = lambda t: t.re("p (h i) -> p h i", h=4)

    pools = {}

    def scr(cls, i):
        key = (cls, i)
        if key not in pools:
            pools[key] = S.sb("scr%s%d" % (cls, i), [128, 512], F32 if cls == 'A' else BF16)
        return pools[key]

    FM = S.sb("FM", [128, 16, MT], BF16)
    xts = [S.sb("xt%d" % i, [128, 1024], F32) for i in range(1)]
    hb = S.sb("hb", [128, 1024], BF16)
    junk = hb
    hT = S.sb("hT", [128, 8, MT], BF16)
    wbufs = [S.sb("wb%d" % i, [128, 8, 528], BF16) for i in range(2)]
    wi = [0]
    merged = [S.sb("mg%d" % i, [128, 1024], F32) for i in range(4)]
    gpre = S.sb("gpre", [128, 1024], F32)
    gpost = S.sb("gpost", [128, 1024], F32)
    ssq = S.sb("ssq", [128, 1], F32)
    rstd = S.sb("rstd", [128, 1], F32)
    ybr = [S.sb("ybr%d" % i, [128, 512], BF16) for i in range(4)]
    yT = S.sb("yT", [128, 4, 4, 128], BF16)
    zz = [S.sb("zz%d" % i, [128, 512], BF16) for i in range(4)]
    raw = [S.sb("raw%d" % i, [128, 515], BF16) for i in range(2)]
    dg = [S.sb("dg%d" % i, [128, 4, 128], BF16) for i in range(2)]
    sg = scr('A', 0)
    tmpm = scr('A', 1)
    mgb = hb
    mgT = yT[:, 0:2].re("p a k t -> p (a k) t")

    def bcast_load(tile, src_ap, n):
        b.dma('pool', tile, dt(src_ap.partition_broadcast(128)))

    def convert_layer(l):
        for name, src2d in (('w_in', dr['w_in'][l]), ('w_br', dr['w_br'][l].rearrange("n k c -> (n k) c")),
                            ('w_out', dr['w_out'][l])):
            r = S.res("wq_%s%d" % (name, l))
            wqres[(name, l)] = r
            ncol = src2d.shape[1]
            nrow = src2d.shape[0]
            for r0 in range(0, nrow, 1024):
                for c0 in range(0, ncol, 2048):
                    c1 = min(c0 + 2048, ncol)
                    S.dma('pool', wq[name][l][r0:r0 + 1024, c0:c1], src2d[r0:r0 + 1024, c0:c1], reads=[], writes=[r])

    def wload(key, col0, ncols, rows8=True):
        name, l, row0, nrows = key
        wb = wbufs[wi[0] % 2]
        wi[0] += 1
        src = wq[name][l][row0:row0 + nrows, :].rearrange("(k p) n -> p k n", p=128)
        nk = src.shape[1]
        b.dma('sp', wb[:, 0:nk, 0:ncols], dt(src[:, :, col0:col0 + ncols], wqres[(name, l)]))
        return wb

    def proj_tok(ps, st, wb, c0, n, o0=0):
        for kc in range(8):
            b.mm(ps[:, o0:o0 + n], hT[:, kc, st * 128:(st + 1) * 128], wb[:, kc, c0:c0 + n], start=kc == 0, stop=kc == 7)

    def proj_feat(ps, wb, c0, mch):
        for kc in range(8):
            b.mm(ps[0:mch, :], wb[:, kc, c0:c0 + mch], hT[:, kc, :], start=kc == 0, stop=kc == 7)

    def rms_rstd(out1, ss1, n):
        b.ts('dve', out1, ss1, 1.0 / n, ALU.mult, NORM_EPS, ALU.add)
        b.tt('pool', out1, out1, mhalf_k(out1.ap.shape[1]), ALU.pow)

    mh_cache = {}

    def mhalf_k(k):
        if k not in mh_cache:
            t = S.sb("mh%d" % k, [128, k], F32)
            b.memset('dve', t, -0.5)
            mh_cache[k] = t
        return mh_cache[k]

    def emit_yT(st):
        for kc in range(4):
            b.tr(PTb[:, kc * 128:(kc + 1) * 128], ybr[st][:, kc * 128:(kc + 1) * 128], ident_b)
        b.cp('dve', yT[:, st], PTb[:, 0:512].re("p (k t) -> p k t", k=4))

    ctx = dict(jidx=cst[:, 384:448], e0=cst[:, 448:512], half01=cst[:, 512:513], emit_yT=emit_yT, scr=scr, FM=FM, nc=nc, S=S, b=b, dr=dr, dt=dt, PS=PS, PTb=PTb, nps=nps, h4=h4, hT=hT, wload=wload,
               proj_tok=proj_tok, proj_feat=proj_feat, rms_rstd=rms_rstd, ident_f=ident_f, U_f=U_f,
               ones_f=ones_f, ident_b=ident_b, U_b=U_b, ones_b=ones_b, ybr=ybr, zz=zz, raw=raw, dg=dg,
               S_len=S_len, NT=NT, NM=NM, bcast_load=bcast_load, depth=depth, mhalf_k=mhalf_k)
    mixers = {}
    if 0 in branches:
        mixers[0] = GDN(ctx)
    if 1 in branches:
        mixers[1] = GLA(ctx)
    if 2 in branches:
        mixers[2] = SSD(ctx)
    if 3 in branches:
        mixers[3] = NSA(ctx)

    convert_layer(0)
    for l in range(depth):
        w_in_l = ('w_in', l, 0, 1024)
        if l + 1 < depth:
            convert_layer(l + 1)
        bcast_load(gpre, dr['norm_pre'][l:l + 1, :], 1024)
        bcast_load(gpost, dr['norm_post'][l:l + 1, :], 1024)
        for n in mixers:
            mixers[n].layer_setup(l)
        for m in range(NM):
            for st in range(4):
                g = m * 4 + st
                xt = xts[0]
                src = dr['x'] if l == 0 else y
                b.dma('pool', xt, dt(src[g * 128:(g + 1) * 128, :], yres[g]))
                b.act(junk, xt, AF.Square, accum=ssq)
                rms_rstd(rstd, ssq, 1024)
                b.stt(hb, xt, rstd[:, 0:1], gpre, ALU.mult, ALU.mult)
                for kc in range(8):
                    b.tr(PTb[:, kc * 128:(kc + 1) * 128], hb[:, kc * 128:(kc + 1) * 128], ident_b)
                b.cp('act', hT[:, :, st * 128:(st + 1) * 128], PTb.re("p (k t) -> p k t", k=8))
            first = True
            for n in (0, 1, 2, 3):
                if n not in mixers:
                    continue
                mixers[n].macro(l, m, w_in_l)
                for half in range(2):
                    wg = wload(w_in_l, OFF['merge_gate'] + n * 1024 + half * 512, 512)
                    wbr = wload(('w_br', l, n * 512, 512), half * 512, 512)
                    for st in range(4):
                        pg = nps()
                        proj_tok(pg, st, wg, 0, 512)
                        b.act(sg, pg, AF.Sigmoid)
                        pb = nps()
                        for kc in range(4):
                            b.mm(pb, yT[:, st, kc, :], wbr[:, kc, 0:512], start=kc == 0, stop=kc == 3)
                        mslice = merged[st][:, half * 512:(half + 1) * 512]
                        if first:
                            b.tt('dve', mslice, sg, pb, ALU.mult)
                        else:
                            b.tt('dve', tmpm, sg, pb, ALU.mult)
                            b.tt('pool', mslice, mslice, tmpm, ALU.add)
                first = False
            wo = [wload(('w_out', l, 0, 1024), half * 512, 512) for half in range(2)]
            for st in range(4):
                g = m * 4 + st
                osb = merged[st]
                b.cp('act', mgb, merged[st])
                for kc in range(8):
                    b.tr(PTb[:, kc * 128:(kc + 1) * 128], mgb[:, kc * 128:(kc + 1) * 128], ident_b)
                b.cp('dve', mgT, PTb.re("p (k t) -> p k t", k=8))
                for half in range(2):
                    po = nps()
                    for kc in range(8):
                        b.mm(po, mgT[:, kc, :], wo[half][:, kc, 0:512], start=kc == 0, stop=kc == 7)
                    b.cp('act', osb[:, half * 512:(half + 1) * 512], po)
                b.act(junk, osb, AF.Square, accum=ssq)
                rms_rstd(rstd, ssq, 1024)
                xt = xts[0]
                src = dr['x'] if l == 0 else y
                b.dma('pool', xt, dt(src[g * 128:(g + 1) * 128, :], yres[g]))
                b.stt(osb, osb, rstd[:, 0:1], gpost, ALU.mult, ALU.mult)
                b.tt('dve', osb, osb, xt, ALU.add)
                S.dma('pool', y[g * 128:(g + 1) * 128, :], osb.ap, reads=[osb], writes=[yres[g]], out=(l == depth - 1))
    S.emit()
    return nc, S


class Mixer:
    def __init__(self, ctx):
        self.__dict__.update(ctx)

    def conv_chunk(self, ps_in, convw, c, halo, out_fm, bias=None):
        b = self.b
        k = self.cc
        self.cc += 1
        raw, dg = self.raw[k % 2], self.dg[k % 2]
        b.cp('dve', raw[:, 0:3], halo)
        b.cp('act', raw[:, 3:515], ps_in)
        b.cp('dve', halo, raw[:, 512:515])
        b.tt('dve', dg, self.ident_b.bc(1, [128, 4, 128]), convw[:, c, :].bc(2, [128, 4, 128]), ALU.mult)
        p2 = self.nps()
        for t in range(4):
            b.mm(p2, dg[:, t, :], raw[:, t:t + 512], start=t == 0, stop=t == 3)
        if bias is None:
            b.act(out_fm, p2, AF.Silu)
        else:
            b.act(out_fm, p2, AF.Silu, bias=bias)

    def out_norm_heads(self, po, st, onz):
        b, S = self.b, self.S
        o = self.osb4
        b.cp('act', o, po)
        b.tt('pool', self.sq4, o, o, ALU.mult)
        b.red(self.ss4, self.h4(self.sq4))
        self.rms_rstd(self.rs4, self.ss4, 128)
        b.tt('dve', self.h4(o), self.h4(o), self.rs4.bc(2, [128, 4, 128]), ALU.mult)
        b.tt('dve', self.ybr[st], o, onz, ALU.mult)

    def z_block(self, wb, c0, onorm_b, per_head=True):
        b = self.b
        for st in range(4):
            pz = self.nps()
            self.proj_tok(pz, st, wb, c0, 512)
            b.act(self.zz[st], pz, AF.Silu)
            if onorm_b is not None:
                if per_head:
                    b.tt('pool', self.h4(self.zz[st]), self.h4(self.zz[st]), onorm_b.bc(1, [128, 4, 128]), ALU.mult)
                else:
                    b.tt('pool', self.zz[st], self.zz[st], onorm_b, ALU.mult)


class GDN(Mixer):
    def __init__(self, ctx):
        super().__init__(ctx)
        S = self.S
        self.cc = 0
        scr = self.scr
        A4 = lambda i: scr('A', i).re("p (h i) -> p h i", h=4)
        B4 = lambda i: scr('B', i).re("p (h i) -> p h i", h=4)
        self.fm = self.FM[:, 0:12, :]
        self.sqa, self.sqb = B4(9), B4(10)
        self.halo = [S.sb("gdn_h%d" % c, [128, 3], BF16) for c in range(12)]
        self.convw = S.sb("gdn_cw", [128, 12, 4], F32)
        self.nega = S.sb("gdn_nega", [128, 4], F32)
        self.dtb = S.sb("gdn_dtb", [128, 4], F32)
        self.onorm = S.sb("gdn_on", [128, 128], F32)
        self.Sf = S.sb("gdn_Sf", [128, 4, 128], F32)
        self.Sb = S.sb("gdn_Sb", [128, 4, 128], BF16)
        f = lambda n, k: S.sb("gdn_" + n, [128, k], F32)
        self.lnss, self.lnr, self.ba, self.e1, self.nlb = f("lnss", 8), f("lnr", 8), f("ba", 8), f("e1", 4), f("nlb", 4)
        self.apb, self.e2, self.sp, self.g, self.gcl = f("apb", 4), f("e2", 4), f("sp", 4), f("g", 4), f("gcl", 8)
        self.C3, self.X4, self.EX = f("C3", 12), f("X4", 16), f("EX", 16)
        self.D3 = [A4(2), A4(3), A4(4)]
        self.BJ, self.BA = A4(5), A4(6)
        self.E1, self.E2, self.E3 = A4(7), A4(8), A4(9)
        self.EB = B4(0)
        self.P = [A4(10), A4(11)]
        self.PT = [A4(12), A4(13)]
        self.AT = [A4(14), A4(15)]
        self.ATb, self.At, self.Rk, self.Kd = B4(1), B4(2), B4(3), B4(4)
        self.Vb, self.nW, self.Vn, self.qg = B4(5), B4(6), B4(7), B4(8)
        self.osb4 = scr('A', 0)
        self.sq4 = scr('A', 1)
        self.ss4, self.rs4 = f("ss4", 4), f("rs4", 4)
        self.ea = f("ea", 4)

    def layer_setup(self, l):
        b, dr, dt = self.b, self.dr, self.dt
        b.dma('pool', self.convw, dt(dr['conv_aT'][l]))
        self.bcast_load(self.ea, dr['a_log_a'][l:l + 1, :], 4)
        b.act(self.nega, self.ea, AF.Exp)
        b.ts('dve', self.nega, self.nega, -1.0, ALU.mult)
        self.bcast_load(self.dtb, dr['dt_bias_a'][l:l + 1, :], 4)
        self.bcast_load(self.onorm, dr['onorm_a'][l:l + 1, :], 128)
        b.memset('dve', self.Sf, 0.0)
        b.memset('dve', self.Sb, 0.0)
        for c in range(12):
            b.memset('pool', self.halo[c], 0.0)

    def macro(self, l, m, w_in_l):
        b, nps, h4 = self.b, self.nps, self.h4
        fm = self.fm
        for blk in range(3):
            wb = self.wload(w_in_l, blk * 512, 512)
            for cc in range(4):
                c = blk * 4 + cc
                p = nps()
                self.proj_feat(p, wb, cc * 128, 128)
                self.conv_chunk(p, self.convw, c, self.halo[c], fm[:, c, :])
        wb3 = self.wload(w_in_l, OFF['gdn_beta'], 520)
        self.z_block(wb3, 8, self.onorm)
        for st in range(4):
            self.sub(st, wb3)
            self.emit_yT(st)

    def sub(self, st, wb3):
        b, nps, h4 = self.b, self.nps, self.h4
        fm = self.fm
        tk = slice(st * 128, (st + 1) * 128)
        bc4 = lambda t: t.bc(2, [128, 4, 128])
        b.tt('pool', self.sqa, fm[:, 4:8, tk], fm[:, 4:8, tk], ALU.mult)
        b.tt('pool', self.sqb, fm[:, 0:4, tk], fm[:, 0:4, tk], ALU.mult)
        pq = nps()
        for c in range(8):
            sq_c = self.sqa[:, c, :] if c < 4 else self.sqb[:, c - 4, :]
            b.mm(pq[:, c:c + 1], sq_c, self.ones_b[:, 0:1])
        self.proj_tok(pq, st, wb3, 0, 8, o0=8)
        b.act(self.lnss, pq[:, 0:8], AF.Ln, bias=NORM_EPS)
        b.ts('dve', self.lnr, self.lnss, -0.5, ALU.mult)
        b.cp('dve', self.ba, pq[:, 8:16])
        b.act(self.e1, self.ba[:, 0:4], AF.Exp, scale=-1.0)
        b.act(self.nlb, self.e1, AF.Ln, bias=1.0)
        b.tt('dve', self.apb, self.ba[:, 4:8], self.dtb, ALU.add)
        b.act(self.e2, self.apb, AF.Exp)
        b.act(self.sp, self.e2, AF.Ln, bias=1.0)
        b.tt('dve', self.g, self.sp, self.nega, ALU.mult)
        pg = nps()
        b.mm(pg[:, 0:4], self.U_f, self.g)
        b.mm(pg[:, 4:8], self.ones_f, self.g)
        b.cp('dve', self.gcl, pg[:, 0:8])
        gc, gl = self.gcl[:, 0:4], self.gcl[:, 4:8]
        lnrk, lnrq = self.lnr[:, 0:4], self.lnr[:, 4:8]
        cA, cB, cJ = self.C3[:, 0:4], self.C3[:, 4:8], self.C3[:, 8:12]
        b.tt('dve', cJ, lnrk, gc, ALU.subtract)
        b.tt('dve', cA, gc, self.nlb, ALU.subtract)
        b.tt('dve', cA, cA, lnrk, ALU.add)
        b.stt(cB, gc, float(np.log(128.0 ** -0.5)), lnrq, ALU.add, ALU.add)
        X4 = self.X4
        b.cp('pool', X4[:, 0:4], cA)
        b.tt('pool', X4[:, 4:8], cJ, gl, ALU.add)
        b.ts('pool', X4[:, 8:12], self.nlb, -1.0, ALU.mult)
        b.cp('pool', X4[:, 12:16], gl)
        b.act(self.EX, X4, AF.Exp)
        sRk, sKd, sVb, dec = self.EX[:, 0:4], self.EX[:, 4:8], self.EX[:, 8:12], self.EX[:, 12:16]
        for v3 in range(3):
            b.tt('dve', self.D3[v3], self.ident_f.bc(1, [128, 4, 128]), bc4(self.C3[:, v3 * 4:(v3 + 1) * 4]), ALU.mult)
        b.cp('pool', self.BJ, bc4(cJ))
        b.cp('pool', self.BA, bc4(cA))
        f4 = lambda t: t.re("p h i -> p (h i)")
        pX1, pX2, pX3, pXB = nps(), nps(), nps(), nps()
        b.mm(pX1, self.ones_f, f4(self.D3[0]), start=True, stop=False)
        b.mm(pX1, self.ident_f, f4(self.BJ), start=False, stop=True)
        b.mm(pX2, self.ones_f, f4(self.D3[1]), start=True, stop=False)
        b.mm(pX2, self.ident_f, f4(self.BJ), start=False, stop=True)
        b.mm(pX3, self.ones_f, f4(self.D3[2]), start=True, stop=False)
        b.mm(pX3, self.ident_f, f4(self.BA), start=False, stop=True)
        b.mm(pXB, self.ones_f, f4(self.D3[1]))
        b.act(f4(self.E1), pX1, AF.Exp)
        b.act(f4(self.E2), pX2, AF.Exp)
        b.act(f4(self.E3), pX3, AF.Exp)
        b.act(f4(self.EB), pXB, AF.Exp)
        b.asel(self.E1, self.E1, [[0, 4], [1, 128]], ALU.is_ge, 0.0, -1, -1)
        b.asel(self.E2, self.E2, [[0, 4], [1, 128]], ALU.is_ge, 0.0, 0, -1)
        b.asel(self.E3, self.E3, [[0, 4], [-1, 128]], ALU.is_ge, 0.0, -1, 1)
        pG, pKQ = nps(), nps()
        for h in range(4):
            b.mm(pG[:, h * 128:(h + 1) * 128], fm[:, 4 + h, tk], fm[:, 4 + h, tk])
        for h in range(4):
            b.mm(pKQ[:, h * 128:(h + 1) * 128], fm[:, 4 + h, tk], fm[:, h, tk])
        P, PT, AT = self.P, self.PT, self.AT
        b.stt(f4(PT[0]), f4(self.E1), -1.0, pG, ALU.mult, ALU.mult)
        b.stt(f4(P[0]), f4(self.E3), -1.0, pG, ALU.mult, ALU.mult)
        b.tt('dve', f4(self.At), f4(self.E2), pKQ, ALU.mult)
        b.tt('pool', AT[0], PT[0], self.ident_f.bc(1, [128, 4, 128]), ALU.add)
        cur = 0
        for lev in range(1, 7):
            nxt = 1 - cur
            pP = nps()
            for h in range(4):
                b.mm(pP[:, h * 128:(h + 1) * 128], PT[cur][:, h, :], P[cur][:, h, :])
            if lev < 6:
                pPT = nps()
                for h in range(4):
                    b.mm(pPT[:, h * 128:(h + 1) * 128], P[cur][:, h, :], PT[cur][:, h, :])
            b.cp('act', f4(P[nxt]), pP)
            if lev < 6:
                b.cp('dve', f4(PT[nxt]), pPT)
            pA = nps()
            for h in range(4):
                b.mm(pA[:, h * 128:(h + 1) * 128], P[nxt][:, h, :], AT[cur][:, h, :])
            b.tt('dve', f4(AT[nxt]), f4(AT[cur]), pA, ALU.add)
            cur = nxt
        b.cp('act', self.ATb, AT[cur])
        PTb = self.PTb
        for h in range(4):
            b.tr(PTb[:, h * 128:(h + 1) * 128], fm[:, 4 + h, tk], self.ident_b)
            b.tr(PTb[:, (4 + h) * 128:(5 + h) * 128], fm[:, 8 + h, tk], self.ident_b)
        pk = PTb[:, 0:512].re("p (h d) -> p h d", h=4)
        pv = PTb[:, 512:1024].re("p (h d) -> p h d", h=4)
        b.tt('dve', self.Rk, pk, bc4(sRk), ALU.mult)
        b.tt('dve', self.Kd, pk, bc4(sKd), ALU.mult)
        b.tt('dve', self.Vb, pv, bc4(sVb), ALU.mult)
        pW = nps()
        for h in range(4):
            b.mm(pW[:, h * 128:(h + 1) * 128], self.Rk[:, h, :], self.ATb[:, h, :])
        b.act(f4(self.nW), pW, AF.Copy, scale=-1.0)
        pV = nps()
        for h in range(4):
            b.mm(pV[:, h * 128:(h + 1) * 128], self.ATb[:, h, :], self.Vb[:, h, :], start=(h == 0), stop=False)
            b.mm(pV[:, h * 128:(h + 1) * 128], self.nW[:, h, :], self.Sb[:, h, :], start=False, stop=True)
        b.cp('act', f4(self.Vn), pV)
        b.tt('dve', self.qg, fm[:, 0:4, tk], self.EB, ALU.mult)
        pO = nps()
        for h in range(4):
            b.mm(pO[:, h * 128:(h + 1) * 128], self.qg[:, h, :], self.Sb[:, h, :], start=(h == 0), stop=False)
            b.mm(pO[:, h * 128:(h + 1) * 128], self.At[:, h, :], self.Vn[:, h, :], start=False, stop=True)
        pS = nps()
        for h in range(4):
            b.mm(pS[:, h * 128:(h + 1) * 128], self.Kd[:, h, :], self.Vn[:, h, :])
        b.tt('dve', self.Sf, self.Sf, bc4(dec), ALU.mult)
        b.tt('dve', f4(self.Sf), f4(self.Sf), pS, ALU.add)
        b.cp('act', self.Sb, self.Sf)
        self.out_norm_heads(pO, st, self.zz[st])


class GLA(Mixer):
    def __init__(self, ctx):
        super().__init__(ctx)
        S = self.S
        scr = self.scr
        A2 = lambda i, o: scr('A', i)[:, o * 256:(o + 1) * 256].re("p (h i) -> p h i", h=2)
        B4 = lambda i: scr('B', i).re("p (h i) -> p h i", h=4)
        B2 = lambda i, o: scr('B', i)[:, o * 256:(o + 1) * 256].re("p (h i) -> p h i", h=2)
        self.fm = self.FM[:, 0:4, :]
        self.vt = [scr('B', 4 + i) for i in range(4)]
        self.gklo = scr('A', 10)
        self.wgk = S.sb("gla_wgk", [128, 256], F32)
        self.nbg = S.sb("gla_nbg", [128, 2], F32)
        self.onorm = S.sb("gla_on", [128, 128], F32)
        self.e = scr('A', 2)
        self.sp = [scr('A', 6), scr('A', 7)]
        self.nb = [scr('A', 8), scr('A', 9)]
        self.negc = S.sb("gla_negc", [128, 2, 2], F32)
        self.Eq, self.Ek = A2(3, 0), A2(3, 1)
        self.Eg, self.Ed = A2(4, 0), A2(4, 1)
        self.qt, self.kd = B2(0, 0), B2(0, 1)
        self.kt = B4(1)
        self.qg = B4(2)
        self.kdt = scr('B', 3)[:, 0:256]
        self.At = B4(8)
        self.Sf = S.sb("gla_Sf", [128, 2, 128], F32)
        self.Sb = S.sb("gla_Sb", [128, 2, 128], BF16)
        self.osb4 = scr('A', 0)
        self.sq4 = scr('A', 1)
        self.ss4 = S.sb("gla_ss4", [128, 4], F32)
        self.rs4 = S.sb("gla_rs4", [128, 4], F32)
        self.rm = S.sb("gla_rm", [128, 4], F32)
        self.Sd = scr('A', 5)[:, 0:128]

    def layer_setup(self, l):
        b, dr, dt = self.b, self.dr, self.dt
        b.memset('dve', self.wgk, 0.0)
        b.dma('pool', self.wgk[0:16, :], dt(dr['w_gk'][l]))
        b.dma('pool', self.nbg, dt(dr['b_gkT'][l]))
        b.ts('dve', self.nbg, self.nbg, -1.0, ALU.mult)
        self.bcast_load(self.onorm, dr['onorm_b'][l:l + 1, :], 128)
        b.memset('dve', self.Sf, 0.0)
        b.memset('dve', self.Sb, 0.0)
        if l == 0:
            b.memset('dve', self.rm, 0.0)
            b.memset('dve', self.rm[0:64, 0:1], 1.0)
            b.memset('dve', self.rm[64:128, 1:2], 1.0)
            b.memset('dve', self.rm[0:64, 2:3], 0.125)
            b.memset('dve', self.rm[64:128, 3:4], 0.125)

    def macro(self, l, m, w_in_l):
        b, nps = self.b, self.nps
        wb = self.wload(w_in_l, OFF['gla_q'], 512)
        for c in range(4):
            p = nps()
            self.proj_feat(p, wb, c * 128, 128)
            b.cp('act', self.fm[:, c, :], p)
        wb = self.wload(w_in_l, OFF['gla_v'], 512)
        for st in range(4):
            p = nps()
            self.proj_tok(p, st, wb, 0, 512)
            b.cp('act', self.vt[st], p)
        wb = self.wload(w_in_l, OFF['gla_gk'], 528)
        p = nps()
        self.proj_feat(p, wb, 0, 128)
        b.memset('pool', self.gklo, 0.0)
        b.cp('dve', self.gklo[0:16, :], p[0:16, :])
        for c in range(2):
            p = nps()
            b.mm(p, self.wgk[:, c * 128:(c + 1) * 128], self.gklo)
            b.act(self.e, p, AF.Exp, scale=-1.0, bias=self.nbg[:, c:c + 1])
            b.act(self.sp[c], self.e, AF.Ln, bias=1.0)
            b.ts('dve', self.sp[c], self.sp[c], 1.0 / 16.0, ALU.mult)
            for st in range(4):
                tk = slice(st * 128, (st + 1) * 128)
                b.scan(self.nb[c][:, tk], self.ones_f, self.sp[c][:, tk], 0.0, ALU.mult, ALU.add)
        self.z_block(wb, 16, self.onorm)
        for st in range(4):
            self.sub(st)
            self.emit_yT(st)

    def sub(self, st):
        b, nps = self.b, self.nps
        tk = slice(st * 128, (st + 1) * 128)
        fm = self.fm
        for c in range(2):
            nbs = self.nb[c][:, tk]
            ref = self.nb[c][:, st * 128 + 64:st * 128 + 65]
            last = self.nb[c][:, st * 128 + 127:st * 128 + 128]
            b.ts('dve', self.negc[:, c, 0:1], ref, -1.0, ALU.mult)
            b.ts('dve', self.negc[:, c, 1:2], last, -1.0, ALU.mult)
            b.act(self.Eq[:, c, :], nbs, AF.Exp, scale=-1.0, bias=ref)
            b.act(self.Ek[:, c, :], nbs, AF.Exp, bias=self.negc[:, c, 0:1])
            b.act(self.Eg[:, c, :], nbs, AF.Exp, scale=-1.0)
            b.act(self.Ed[:, c, :], nbs, AF.Exp, bias=self.negc[:, c, 1:2])
        b.stt(self.qt, self.Eq, 0.125, fm[:, 0:2, tk], ALU.mult, ALU.mult)
        b.tt('pool', self.kd, self.Ed, fm[:, 2:4, tk], ALU.mult)
        for h in range(4):
            c, r = h // 2, h % 2
            b.stt(self.kt[:, h, :], self.Ek[:, c, :], self.rm[:, r:r + 1], fm[:, 2 + c, tk], ALU.mult, ALU.mult)
            b.stt(self.qg[:, h, :], self.Eg[:, c, :], self.rm[:, 2 + r:3 + r], fm[:, c, tk], ALU.mult, ALU.mult)
        pA = nps()
        for h in range(4):
            c = h // 2
            b.mm(pA[:, h * 128:(h + 1) * 128], self.kt[:, h, :], self.qt[:, c, :])
        b.tt('dve', self.At, self.h4(pA), self.U_b.bc(1, [128, 4, 128]), ALU.mult)
        for c in range(2):
            b.tr(self.PTb[:, c * 128:(c + 1) * 128], self.kd[:, c, :], self.ident_b)
        b.cp('act', self.kdt, self.PTb[:, 0:256])
        pO = nps()
        for h in range(4):
            c = h // 2
            hs = slice(h * 128, (h + 1) * 128)
            b.mm(pO[:, hs], self.At[:, h, :], self.vt[st][:, hs], start=(h == 0), stop=False)
            b.mm(pO[:, hs], self.qg[:, h, :], self.Sb[:, c, :], start=False, stop=True)
        pS = nps()
        for h in range(4):
            c = h // 2
            b.mm(pS[:, h * 128:(h + 1) * 128], self.kdt[:, c * 128:(c + 1) * 128], self.vt[st][:, h * 128:(h + 1) * 128])
        for c in range(2):
            b.ts('dve', self.Sd, self.Sf[:, c, :], self.Eg[:, c, 127:128], ALU.mult)
            b.stt(self.Sd, pS[:, (2 * c) * 128:(2 * c + 1) * 128], self.rm[:, 0:1], self.Sd, ALU.mult, ALU.add)
            b.stt(self.Sf[:, c, :], pS[:, (2 * c + 1) * 128:(2 * c + 2) * 128], self.rm[:, 1:2], self.Sd, ALU.mult, ALU.add)
        b.cp('act', self.Sb, self.Sf)
        self.out_norm_heads(pO, st, self.zz[st])


class SSD(Mixer):
    def __init__(self, ctx):
        super().__init__(ctx)
        S = self.S
        self.cc = 0
        scr = self.scr
        A4 = lambda i: scr('A', i).re("p (h i) -> p h i", h=4)
        B4 = lambda i: scr('B', i).re("p (h i) -> p h i", h=4)
        self.fm = self.FM[:, 0:8, :]
        self.halo = [S.sb("ssd_h%d" % c, [128, 3], BF16) for c in range(8)]
        self.convw = S.sb("ssd_cw", [128, 8, 4], F32)
        self.convb = S.sb("ssd_cb", [128, 8], F32)
        f = lambda n, k: S.sb("ssd_" + n, [128, k], F32)
        self.nega, self.dtb, self.dsk, self.ea = f("nega", 8), f("dtb", 8), f("dsk", 8), f("ea", 8)
        self.onc = S.sb("ssd_onc", [128, 512], F32)
        self.dtr, self.apb, self.e, self.dtv, self.da, self.acl = f("dtr", 8), f("apb", 8), f("e", 8), f("dtv", 8), f("da", 8), f("acl", 16)
        self.X, self.EX, self.sdtd = f("X", 24), f("EX", 24), f("sdtd", 8)
        self.D8 = [A4(2), A4(3)]
        self.Bn = [A4(4), A4(5)]
        self.E = [A4(6), A4(7)]
        self.Mt = [B4(0), B4(1)]
        self.xdt = scr('B', 2)
        self.xdtd = scr('B', 3)
        self.xsk = scr('A', 8)
        self.Btok = scr('B', 4)[:, 0:256]
        self.yo = scr('A', 9)
        self.Hf = S.sb("ssd_Hf", [128, 512], F32)
        self.Hb = S.sb("ssd_Hb", [128, 512], BF16)
        self.ssq = f("ssq", 1)
        self.rstd = f("rstd", 1)
        self.junk = scr('B', 5)

    def layer_setup(self, l):
        b, dr, dt = self.b, self.dr, self.dt
        b.dma('pool', self.convw, dt(dr['conv_cT'][l]))
        b.dma('pool', self.convb, dt(dr['conv_bias_cT'][l]))
        self.bcast_load(self.ea, dr['a_log_c'][l:l + 1, :], 8)
        b.act(self.nega, self.ea, AF.Exp)
        b.ts('dve', self.nega, self.nega, -1.0, ALU.mult)
        self.bcast_load(self.dtb, dr['dt_bias_c'][l:l + 1, :], 8)
        self.bcast_load(self.dsk, dr['d_skip_c'][l:l + 1, :], 8)
        self.bcast_load(self.onc, dr['onorm_c'][l:l + 1, :], 512)
        b.memset('dve', self.Hf, 0.0)
        b.memset('dve', self.Hb, 0.0)
        for c in range(8):
            b.memset('pool', self.halo[c], 0.0)

    def macro(self, l, m, w_in_l):
        b, nps = self.b, self.nps
        for blk in range(2):
            wb = self.wload(w_in_l, OFF['ssd_x'] + blk * 512, 512)
            for cc in range(4):
                c = blk * 4 + cc
                p = nps()
                self.proj_feat(p, wb, cc * 128, 128)
                self.conv_chunk(p, self.convw, c, self.halo[c], self.fm[:, c, :], bias=self.convb[:, c:c + 1])
        wb3 = self.wload(w_in_l, OFF['ssd_dt'], 520)
        self.z_block(wb3, 8, None)
        for st in range(4):
            self.sub(st, wb3)
            self.emit_yT(st)

    def sub(self, st, wb3):
        b, nps = self.b, self.nps
        fm = self.fm
        tk = slice(st * 128, (st + 1) * 128)
        h8 = lambda t, k=64: t.re("p (h i) -> p h i", h=8)
        bc8 = lambda t, k: t.bc(2, [128, 8, k])
        pq = nps()
        self.proj_tok(pq, st, wb3, 0, 8)
        b.cp('dve', self.dtr, pq[:, 0:8])
        b.tt('dve', self.apb, self.dtr, self.dtb, ALU.add)
        b.act(self.e, self.apb, AF.Exp)
        b.act(self.dtv, self.e, AF.Ln, bias=1.0)
        b.tt('dve', self.da, self.dtv, self.nega, ALU.mult)
        pg = nps()
        b.mm(pg[:, 0:8], self.U_f, self.da)
        b.mm(pg[:, 8:16], self.ones_f, self.da)
        b.cp('dve', self.acl, pg[:, 0:16])
        acs, alast = self.acl[:, 0:8], self.acl[:, 8:16]
        b.cp('pool', self.X[:, 0:8], acs)
        b.tt('pool', self.X[:, 8:16], alast, acs, ALU.subtract)
        b.cp('pool', self.X[:, 16:24], alast)
        b.act(self.EX, self.X, AF.Exp)
        eacs, edst, dec = self.EX[:, 0:8], self.EX[:, 8:16], self.EX[:, 16:24]
        b.tt('dve', self.sdtd, self.dtv, edst, ALU.mult)
        bc4 = lambda t: t.bc(2, [128, 4, 128])
        f4 = lambda t: t.re("p h i -> p (h i)")
        pCB = nps()
        for g in range(2):
            b.mm(pCB[:, g * 128:(g + 1) * 128], fm[:, 4 + g, tk], fm[:, 6 + g, tk])
        for g in range(2):
            hs = slice(g * 4, g * 4 + 4)
            b.tt('dve', self.D8[g], self.ident_f.bc(1, [128, 4, 128]), bc4(acs[:, hs]), ALU.mult)
            b.ts('pool', self.Bn[g], bc4(acs[:, hs]), -1.0, ALU.mult)
            pX = nps()
            b.mm(pX, self.ones_f, f4(self.D8[g]), start=True, stop=False)
            b.mm(pX, self.ident_f, f4(self.Bn[g]), start=False, stop=True)
            b.act(f4(self.E[g]), pX, AF.Exp)
            b.asel(self.E[g], self.E[g], [[0, 4], [1, 128]], ALU.is_ge, 0.0, 0, -1)
            b.tt('dve', self.Mt[g], self.E[g], pCB[:, g * 128:(g + 1) * 128].bc(1, [128, 4, 128]), ALU.mult)
        PTb = self.PTb
        for c in range(4):
            b.tr(PTb[:, c * 128:(c + 1) * 128], fm[:, c, tk], self.ident_b)
        for g in range(2):
            b.tr(PTb[:, 512 + g * 128:512 + (g + 1) * 128], fm[:, 4 + g, tk], self.ident_b)
        xtok = h8(PTb[:, 0:512])
        b.tt('dve', h8(self.xdt), xtok, bc8(self.dtv, 64), ALU.mult)
        b.tt('dve', h8(self.xdtd), xtok, bc8(self.sdtd, 64), ALU.mult)
        b.tt('dve', h8(self.xsk), xtok, bc8(self.dsk, 64), ALU.mult)
        b.cp('act', self.Btok, PTb[:, 512:768])
        pY = nps()
        for h in range(8):
            b.mm(pY[:, h * 64:(h + 1) * 64], self.Mt[h // 4][:, h % 4, :], self.xdt[:, h * 64:(h + 1) * 64])
        pF = nps()
        for g in range(2):
            b.mm(pF[:, g * 256:(g + 1) * 256], fm[:, 6 + g, tk], self.Hb[:, g * 256:(g + 1) * 256])
        b.tt('dve', h8(self.yo), h8(pF), bc8(eacs, 64), ALU.mult)
        b.tt('pool', self.yo, self.yo, self.xsk, ALU.add)
        b.tt('dve', self.yo, self.yo, pY, ALU.add)
        pH = nps()
        for g in range(2):
            b.mm(pH[:, g * 256:(g + 1) * 256], self.Btok[:, g * 128:(g + 1) * 128], self.xdtd[:, g * 256:(g + 1) * 256])
        b.tt('dve', h8(self.Hf), h8(self.Hf), bc8(dec, 64), ALU.mult)
        b.tt('dve', self.Hf, self.Hf, pH, ALU.add)
        b.cp('act', self.Hb, self.Hf)
        b.tt('dve', self.yo, self.yo, self.zz[st], ALU.mult)
        b.act(self.junk, self.yo, AF.Square, accum=self.ssq)
        self.rms_rstd(self.rstd, self.ssq, 512)
        b.stt(self.ybr[st], self.yo, self.rstd[:, 0:1], self.onc, ALU.mult, ALU.mult)


class NSA(Mixer):
    def __init__(self, ctx):
        super().__init__(ctx)
        S, scr, S_len, NT = self.S, self.scr, self.S_len, self.NT
        self.kS = [S.sb("nsa_kS%d" % g, [128, S_len], BF16) for g in range(2)]
        self.kW = [S.sb("nsa_kW%d" % g, [128, 8 * 128], BF16) for g in range(2)]
        self.vS = [S.sb("nsa_vS%d" % g, [128, NT, 66], BF16) for g in range(2)]
        self.vW = [S.sb("nsa_vW%d" % g, [128, 8, 66], BF16) for g in range(2)]
        self.kC = [S.sb("nsa_kC%d" % g, [128, 256], BF16) for g in range(2)]
        self.vcT = [S.sb("nsa_vcT%d" % g, [128, 256], BF16) for g in range(2)]
        self.vC = [S.sb("nsa_vC%d" % g, [128, 2, 130], BF16) for g in range(2)]
        self.EK = S.sb("nsa_EK", [128, S_len], BF16)
        self.EKc = S.sb("nsa_EKc", [128, 256], BF16)
        self.qm = self.FM[:, 0:8, :]
        self.qA = self.FM[:, 8:16, :]
        self.qS = S.sb("nsa_qS", [128, 4, 128], BF16)
        self.rawc = [S.sb("nsa_rawc%d" % g, [128, 528], BF16) for g in range(2)]
        self.W1 = S.sb("nsa_W1", [128, 32, 128], BF16)
        self.W2k = S.sb("nsa_W2k", [128, 128], BF16)
        self.W2v = S.sb("nsa_W2v", [128, 128], BF16)
        self.posT = S.sb("nsa_posT", [128, 32], BF16)
        self.cpos = S.sb("nsa_cpos", [128, 1], F32)
        self.wsel = S.sb("nsa_wsel", [128, 8, 128], BF16)
        self.gsig = [S.sb("nsa_gs%d" % i, [128, 24], F32) for i in range(4)]
        self.rmq = S.sb("nsa_rmq", [128, 2], F32)
        self.hid = S.sb("nsa_hid", [128, 32], BF16)
        f = lambda n, k: S.sb("nsa_" + n, [128, k], F32)
        self.dall = S.sb("nsa_dall", [128, 3, 4], F32)
        self.rall = S.sb("nsa_rall", [128, 3, 4], F32)
        self.coef = S.sb("nsa_coef", [128, 3, 4], F32)
        self.imp, self.imp2, self.m8a, self.m8b, self.thr, self.selb = f("imp", 64), f("imp2", 64), f("m8a", 8), f("m8b", 8), f("thr", 1), f("selb", 64)
        self.selbb = S.sb("nsa_selbb", [128, 128], BF16)
        self.cur, self.val, self.fz = f("cur", 1), f("val", 64), f("fz", 64)
        self.Pb = [scr('B', i).re("p (h i) -> p h i", h=4) for i in range(3)]
        self.pi = 0
        self.on = scr('A', 2).re("p (h d) -> p h d", h=8)
        self.tmp4 = scr('A', 3)[:, 0:256].re("p (h d) -> p h d", h=4)
        self.tmp5 = scr('A', 4)[:, 0:256].re("p (h d) -> p h d", h=4)

    def layer_setup(self, l):
        b, dr, dt, nps = self.b, self.dr, self.dt, self.nps
        if NSTAGE < 0:
            return
        if l == 0:
            for g in range(2):
                for t in (self.kS[g], self.kW[g], self.vS[g], self.vW[g], self.kC[g], self.vcT[g], self.vC[g]):
                    b.memset('pool', t, 0.0)
                b.memset('pool', self.vS[g][:, :, 64:65], 1.0)
                b.memset('pool', self.vW[g][:, :, 64:65], 1.0)
                b.memset('pool', self.vC[g][:, :, 64:65], 1.0)
                b.memset('pool', self.vC[g][0:1, 0, 64:65], 0.0)
                for kt in range(2):
                    b.dma('pool', self.vC[g][:, kt, 65:129], dt(dr['overlap'][kt * 128:(kt + 1) * 128, :]))
            b.memset('pool', self.EK, 0.0)
            for c0 in range(0, self.S_len, 1024):
                c1 = min(c0 + 1024, self.S_len)
                b.dma('pool', self.EK[0:64, c0:c1], dt(dr['esel'][:, c0:c1]))
                b.dma('pool', self.EK[64:68, c0:c1], dt(dr['kaug'][:, c0:c1]))
            b.memset('pool', self.EKc, 0.0)
            b.dma('pool', self.EKc[64:68, :], dt(dr['kaugc']))
            b.memset('pool', self.qS, 0.0)
            b.memset('pool', self.selbb, 0.0)
            b.memset('pool', self.rmq, 0.0)
            b.memset('pool', self.rmq[0:64, 0:1], 0.125)
            b.memset('pool', self.rmq[64:128, 1:2], 0.125)
        b.memset('pool', self.W1, 0.0)
        b.dma('pool', self.W1[0:64, :, 0:64], dt(dr['w_ck1'][l].rearrange("(p d) o -> d p o", d=64)))
        b.dma('pool', self.W1[64:128, :, 64:128], dt(dr['w_cv1'][l].rearrange("(p d) o -> d p o", d=64)))
        b.memset('pool', self.W2k, 0.0)
        b.dma('pool', self.W2k[0:64, 0:64], dt(dr['w_ck2'][l]))
        b.dma('pool', self.W2k[0:64, 64:128], dt(dr['w_ck2'][l]))
        b.memset('pool', self.W2v, 0.0)
        b.dma('pool', self.W2v[64:128, 0:64], dt(dr['w_cv2'][l]))
        b.dma('pool', self.posT[0:64, :], dt(dr['cmp_pos_kT'][l]))
        b.dma('pool', self.posT[64:128, :], dt(dr['cmp_pos_vT'][l]))
        pc = nps()
        for p in range(32):
            b.mm(pc[:, 0:1], self.W1[:, p, :], self.posT[:, p:p + 1], start=p == 0, stop=p == 31)
        b.cp('dve', self.cpos, pc[:, 0:1])
        for g in range(2):
            b.memset('pool', self.rawc[g], 0.0)

    def macro(self, l, m, w_in_l):
        b, nps = self.b, self.nps
        if NSTAGE < 1:
            for st in range(4):
                self.emit_yT(st)
            return
        n0 = 4 * m
        wsel = self.wsel
        wb = self.wload(w_in_l, OFF['nsa_q'], 512)
        for c in range(4):
            p = nps()
            self.proj_feat(p, wb, c * 128, 128)
            b.ts('dve', self.qm[:, 2 * c, :], p, self.rmq[:, 0:1], ALU.mult)
            b.act(self.qm[:, 2 * c + 1, :], p, AF.Copy, scale=self.rmq[:, 1:2])
        def bail():
            for st in range(4):
                self.emit_yT(st)
        if NSTAGE < 1.15:
            return bail()
        b.memset('pool', self.qA, 0.0)
        b.dma('pool', self.qA[64:68, :, :], self.dt(self.dr['qaug'][:, :, m * MT:(m + 1) * MT]))
        if NSTAGE < 1.25:
            return bail()
        wb = self.wload(w_in_l, OFF['nsa_kc'], 512)
        for g in range(2):
            b.cp('dve', wsel[:, :, 0:64], wb[:, :, g * 64:(g + 1) * 64])
            b.cp('dve', wsel[:, :, 64:128], wb[:, :, 128 + g * 64:128 + (g + 1) * 64])
            p = nps()
            self.proj_feat(p, wsel, 0, 128)
            b.cp('dve', self.rawc[g][:, 0:16], self.rawc[g][:, 512:528])
            b.cp('act', self.rawc[g][:, 16:528], p)
        if NSTAGE < 1.27:
            return bail()
        for g in range(2):
            b.cp('dve', wsel[:, :, 0:64], wb[:, :, 256 + g * 64:256 + (g + 1) * 64])
            b.cp('dve', wsel[:, :, 64:128], wb[:, :, 256 + g * 64:256 + (g + 1) * 64])
            p = nps()
            self.proj_feat(p, wsel, 0, 128)
            b.cp('act', self.kS[g][:, m * MT:(m + 1) * MT], p)
        if NSTAGE < 1.29:
            return bail()
        for st in range(4):
            p = nps()
            self.proj_tok(p, st, wb, 384, 128)
            for g in range(2 if NSTAGE >= 1.2915 else 0):
                b.cp('dve', self.vS[g][:, n0 + st, 0:64], p[:, g * 64:(g + 1) * 64])
        if NSTAGE < 1.35:
            return bail()
        wb = self.wload(w_in_l, OFF['nsa_kw'], 280)
        for g in range(2):
            b.cp('dve', wsel[:, :, 0:64], wb[:, :, g * 64:(g + 1) * 64])
            b.cp('dve', wsel[:, :, 64:128], wb[:, :, g * 64:(g + 1) * 64])
            p = nps()
            self.proj_feat(p, wsel, 0, 128)
            s0 = (n0 % 8) * 128
            b.cp('act', self.kW[g][:, s0:s0 + 512], p)
        for st in range(4):
            p = nps()
            self.proj_tok(p, st, wb, 128, 152)
            for g in range(2):
                b.cp('dve', self.vW[g][:, (n0 + st) % 8, 0:64], p[:, g * 64:(g + 1) * 64])
            b.act(self.gsig[st], p[:, 128:152], AF.Sigmoid)
        if NSTAGE < 1.45:
            return bail()
        wb = self.wload(w_in_l, OFF['nsa_z'], 512)
        self.z_block(wb, 0, None)
        for g in range(2 if NSTAGE >= 2 else 0):
            pC = nps()
            for p_ in range(32):
                b.mm(pC[:, 0:32], self.W1[:, p_, :], self.rawc[g][:, p_:p_ + 497:16], start=p_ == 0, stop=p_ == 31)
            b.act(self.hid, pC[:, 0:32], AF.Silu, bias=self.cpos[:, 0:1])
            pK = nps()
            b.mm(pK[:, 0:32], self.W2k, self.hid)
            b.mm(pK[:, 32:64], self.W2v, self.hid)
            c0 = 32 * m
            cnt = 32
            b.cp('dve', self.kC[g][:, c0:c0 + cnt], pK[:, 0:32])
            b.cp('dve', self.vcT[g][:, c0:c0 + cnt], pK[:, 32:64])
            if m == 0:
                b.memset('pool', self.kC[g][:, 0:1], 0.0)
                b.memset('pool', self.vcT[g][:, 0:1], 0.0)
            for kt in sorted({c0 // 128, (c0 + cnt - 1) // 128}):
                b.tr(self.PTb[:, 0:128], self.vcT[g][:, kt * 128:(kt + 1) * 128], self.ident_b)
                b.cp('dve', self.vC[g][:, kt, 0:64], self.PTb[:, 0:64])
        for st in range(4):
            if NSTAGE >= 3:
                self.sub(m, st)
            self.emit_yT(st)

    def pbuf(self):
        self.pi += 1
        return self.Pb[self.pi % 3]

    def sub(self, m, st):
        b, nps, PS = self.b, self.nps, self.PS
        n = 4 * m + st
        tk = slice(st * 128, (st + 1) * 128)
        f4 = lambda t: t.re("p h i -> p (h i)")
        pOc, pOs, pOw = [PS[0], PS[1]], PS[2], PS[3]
        on = self.on
        causal = lambda t: b.asel(t, t, [[0, 4], [1, 128]], ALU.is_ge, 0.0, 0, -1)
        for g in range(2):
            qrhs = self.qm[:, 4 * g:4 * g + 4, tk]
            arhs = self.qA[:, 4 * g:4 * g + 4, tk]
            kts = [kt for kt in (0, 1) if n >= 16 * kt]
            for ki, kt in enumerate(kts):
                ks_ = slice(kt * 128, (kt + 1) * 128)
                pS = nps(4)
                b.mm(pS, self.kC[g][:, ks_], qrhs, start=True, stop=False)
                b.mm(pS, self.EKc[:, ks_], arhs, start=False, stop=True)
                Pt = self.pbuf()
                b.act(f4(Pt), pS, AF.Exp)
                if n < 16 * kt + 16:
                    b.asel(Pt, Pt, [[0, 4], [1, 128]], ALU.is_ge, 0.0, 128 * n - 2048 * kt - 15, -16)
                for hh in range(4):
                    col = (hh % 2) * 129
                    b.mm(pOc[hh // 2][:, col:col + 129], Pt[:, hh, :], self.vC[g][:, kt, 0:129],
                         start=(ki == 0 and hh % 2 == 0), stop=(ki == len(kts) - 1))
            for bnk in range(2):
                v = pOc[bnk][:, 0:258].re("p (h c) -> p h c", h=2)
                b.cp('dve', self.dall[:, 0, 2 * bnk:2 * bnk + 2], v[:, :, 64])
            rcc = self.rall[:, 0, :]
            b.ts('dve', rcc, self.dall[:, 0, :], 1e-30, ALU.max)
            b.recip(rcc, rcc)
            imp = self.imp
            b.ts('dve', imp, pOc[0][:, 65:129], rcc[:, 0:1], ALU.mult)
            b.stt(imp, pOc[0][:, 194:258], rcc[:, 1:2], imp, ALU.mult, ALU.add)
            b.stt(imp, pOc[1][:, 65:129], rcc[:, 2:3], imp, ALU.mult, ALU.add)
            b.stt(imp, pOc[1][:, 194:258], rcc[:, 3:4], imp, ALU.mult, ALU.add)
            cur, val, fz = self.cur, self.val, self.fz
            b.ts('dve', cur, self.half01, float(2 * n), ALU.add)
            b.ts('dve', val, self.jidx, cur[:, 0:1], ALU.is_le)
            b.tt('dve', imp, imp, val, ALU.mult)
            b.ts('dve', val, val, -1.0, ALU.add)
            b.tt('dve', imp, imp, val, ALU.add)
            b.ts('dve', fz, self.jidx, cur[:, 0:1], ALU.is_equal)
            b.ts('dve', val, self.jidx, 1.0, ALU.add, cur[:, 0:1], ALU.is_equal)
            b.tt('dve', fz, fz, val, ALU.add)
            b.tt('dve', fz, fz, self.e0, ALU.add)
            b.stt(imp, fz, 1.0e4, imp, ALU.mult, ALU.max)
            b.max8(self.m8a, imp)
            b.mrep(self.imp2, self.m8a, imp, -2.0)
            b.max8(self.m8b, self.imp2)
            b.ts('dve', self.thr, self.m8b[:, 7:8], 0.0, ALU.max)
            b.ts('dve', self.selb, imp, self.thr[:, 0:1], ALU.is_ge, 30000.0, ALU.mult)
            b.ts('dve', self.selbb[:, 0:64], self.selb, -30000.0, ALU.add)
            b.tr(self.PTb[:, 0:128], self.selbb, self.ident_b)
            b.cp('dve', self.qS[0:64], self.PTb[0:64, 0:128].bc(1, [64, 4, 128]))
            b.cp('act', self.qS[64:68], self.qA[64:68, 4 * g:4 * g + 4, tk])
            for kt in range(n + 1):
                ks_ = slice(kt * 128, (kt + 1) * 128)
                pS = nps(4)
                b.mm(pS, self.kS[g][:, ks_], qrhs, start=True, stop=False)
                b.mm(pS, self.EK[:, ks_], f4(self.qS), start=False, stop=True)
                Pt = self.pbuf()
                b.act(f4(Pt), pS, AF.Exp)
                if kt == n:
                    causal(Pt)
                for hh in range(4):
                    b.mm(pOs[:, hh * 65:(hh + 1) * 65], Pt[:, hh, :], self.vS[g][:, kt, 0:65],
                         start=(kt == 0 and hh == 0), stop=(kt == n))
            kts = list(range(max(0, n - 4), n + 1))
            for kt in kts:
                ks_ = slice(kt * 128, (kt + 1) * 128)
                sl = kt % 8
                pS = nps(4)
                b.mm(pS, self.kW[g][:, sl * 128:(sl + 1) * 128], qrhs, start=True, stop=False)
                b.mm(pS, self.EK[:, ks_], arhs, start=False, stop=True)
                Pt = self.pbuf()
                b.act(f4(Pt), pS, AF.Exp)
                if kt == n:
                    causal(Pt)
                if kt == n - 4:
                    b.asel(Pt, Pt, [[0, 4], [-1, 128]], ALU.is_ge, 0.0, -1, 1)
                for hh in range(4):
                    b.mm(pOw[:, hh * 65:(hh + 1) * 65], Pt[:, hh, :], self.vW[g][:, sl, 0:65],
                         start=(kt == kts[0] and hh == 0), stop=(kt == n))
            vs_ = pOs[:, 0:260].re("p (h c) -> p h c", h=4)
            vw_ = pOw[:, 0:260].re("p (h c) -> p h c", h=4)
            b.cp('dve', self.dall[:, 1, :], vs_[:, :, 64])
            b.cp('dve', self.dall[:, 2, :], vw_[:, :, 64])
            b.ts('dve', self.rall[:, 1:3, :], self.dall[:, 1:3, :], 1e-30, ALU.max)
            b.recip(self.rall[:, 1:3, :], self.rall[:, 1:3, :])
            gv = self.gsig[st][:, 12 * g:12 * g + 12].re("p (h b) -> p b h", b=3)
            b.tt('dve', self.coef, self.rall, gv, ALU.mult)
            for bnk in range(2):
                v = pOc[bnk][:, 0:258].re("p (h c) -> p h c", h=2)
                b.tt('dve', on[:, 4 * g + 2 * bnk:4 * g + 2 * bnk + 2, :], v[:, :, 0:64],
                     self.coef[:, 0, 2 * bnk:2 * bnk + 2].bc(2, [128, 2, 64]), ALU.mult)
            b.tt('dve', self.tmp4, vs_[:, :, 0:64], self.coef[:, 1, :].bc(2, [128, 4, 64]), ALU.mult)
            b.tt('pool', on[:, 4 * g:4 * g + 4, :], on[:, 4 * g:4 * g + 4, :], self.tmp4, ALU.add)
            b.tt('dve', self.tmp5, vw_[:, :, 0:64], self.coef[:, 2, :].bc(2, [128, 4, 64]), ALU.mult)
            b.tt('pool', on[:, 4 * g:4 * g + 4, :], on[:, 4 * g:4 * g + 4, :], self.tmp5, ALU.add)
        b.tt('dve', self.ybr[st], on.re("p h d -> p (h d)"), self.zz[st], ALU.mult)


def kernel(**inputs):
    depth, S_len, n_cores = 4, 4096, 8
    x = np.asarray(inputs['x'], dtype=np.float32)
    nc, _ = build(S_len, depth)
    pin = prep_inputs(inputs, depth)
    hc = host_constants(S_len)
    in_maps = []
    for i in range(n_cores):
        d = dict(pin)
        d.update(hc)
        d['x'] = np.ascontiguousarray(x[i])
        in_maps.append(d)
    res = run_bass_kernel_spmd(nc, in_maps, core_ids=list(range(n_cores)))
    return np.stack([np.asarray(r['y'], dtype=np.float32) for r in res.results], axis=0)
```

```python
from contextlib import ExitStack
import os
NSTAGE = float(os.environ.get('NSTAGE', '99'))
SENG = os.environ.get('SENG', '').split(',')
import numpy as np
import concourse.bass as bass
import concourse.mybir as mybir
from concourse.bass_utils import run_bass_kernel_spmd

F32 = mybir.dt.float32
BF16 = mybir.dt.bfloat16
I32 = mybir.dt.int32
ALU = mybir.AluOpType
AF = mybir.ActivationFunctionType
AX = mybir.AxisListType

ENGS = ('pe', 'act', 'dve', 'pool', 'sp')
NDSEM = 12


class Tile:
    def __init__(self, name, ap, rid):
        self.name, self.ap, self.rid = name, ap, rid

    def __getitem__(self, k):
        return Tile(self.name, self.ap[k], self.rid)

    def re(self, s, **kw):
        return Tile(self.name, self.ap.rearrange(s, **kw), self.rid)

    def bc(self, axis, shape):
        return Tile(self.name, self.ap.unsqueeze(axis).to_broadcast(list(shape)), self.rid)

    def v(self, ap):
        return Tile(self.name, ap, self.rid)


class Op:
    __slots__ = ('eng', 'fn', 'deps', 'signal', 'count', 'dma', 'dsem', 'dcount', 'idx', 'cost', 'lat', 'seq', 'nrem', 'users', 'rt', 'fin')


class Sched:
    def __init__(self, nc):
        self.nc = nc
        self.es = ExitStack()
        self.ops = {e: [] for e in ENGS}
        self.lastw = {}
        self.readers = {}
        self.ndma = {e: 0 for e in ENGS}
        self.dma_ops = {e: [] for e in ENGS}
        self.nres = 0

    def sb(self, name, shape, dtype):
        t = self.es.enter_context(self.nc.sbuf_tensor("sb_" + name, list(shape), dtype))
        self.nres += 1
        return Tile(name, t[:] if hasattr(t, '__getitem__') else t, self.nres)

    def ps(self, name, shape, dtype):
        t = self.es.enter_context(self.nc.psum_tensor("pm_" + name, list(shape), dtype))
        self.nres += 1
        return Tile(name, t[:], self.nres)

    def res(self, name):
        self.nres += 1
        return Tile(name, None, self.nres)

    def view(self, tile, ap, own=False):
        if own:
            self.nres += 1
            return Tile(tile.name, ap, self.nres)
        return Tile(tile.name, ap, tile.rid)

    def _deps(self, reads, writes):
        deps = []
        for r in reads:
            w = self.lastw.get(r.rid)
            if w is not None:
                deps.append(w)
        for r in writes:
            w = self.lastw.get(r.rid)
            if w is not None:
                deps.append(w)
            deps.extend(self.readers.get(r.rid, ()))
        return deps

    def _record(self, op, reads, writes):
        for r in writes:
            self.lastw[r.rid] = op
            self.readers[r.rid] = []
        for r in reads:
            if self.lastw.get(r.rid) is op:
                continue
            self.readers.setdefault(r.rid, []).append(op)

    def op(self, eng, fn, reads=(), writes=(), cost=300.0):
        o = Op()
        o.eng, o.fn, o.dma, o.signal, o.count = eng, fn, False, False, 0
        o.cost = o.lat = cost
        self.nseq = getattr(self, 'nseq', 0) + 1
        o.seq = self.nseq
        o.deps = self._deps(reads, writes)
        o.idx = len(self.ops[eng])
        self.ops[eng].append(o)
        self._record(o, reads, writes)
        return o

    def dma(self, eng, out_ap, in_ap, reads=(), writes=(), out=False, **kw):
        o = Op()
        o.eng, o.dma, o.signal, o.count = eng, True, True, 0
        oa = out_ap.ap if isinstance(out_ap, Tile) else out_ap
        ia = in_ap.ap if isinstance(in_ap, Tile) else in_ap
        o.fn = lambda e: e.dma_start(out=oa, in_=ia, **kw)
        o.deps = self._deps(reads, writes)
        self.ndma[eng] += 1
        nbytes = 1
        for d_ in oa.shape:
            nbytes *= d_
        nbytes *= 2 if oa.dtype == BF16 else 4
        o.cost = 150.0 if eng == 'sp' else 1500.0
        o.lat = 2500.0 + nbytes / 60.0
        self.nseq = getattr(self, 'nseq', 0) + 1
        o.seq = self.nseq
        o.idx = len(self.ops[eng])
        self.ops[eng].append(o)
        self._record(o, reads, writes)
        if out:
            self.out_dmas = getattr(self, 'out_dmas', []) + [o]
        return o

    def schedule(self, window=int(os.environ.get("SWIN", "40"))):
        allops = [o for e in ENGS for o in self.ops[e]]
        for o in allops:
            o.users = []
            o.rt = 0.0
            o.fin = None
        for o in allops:
            ds = set(id(d) for d in o.deps)
            o.deps = [d for d in {id(d): d for d in o.deps}.values()]
            o.nrem = len(o.deps)
            for d in o.deps:
                d.users.append(o)
        pend = {e: list(self.ops[e]) for e in ENGS}
        new = {e: [] for e in ENGS}
        free_t = {e: 0.0 for e in ENGS}
        remaining = len(allops)
        while remaining:
            best = None
            bkey = None
            for e in ENGS:
                pe_ = pend[e]
                ft = free_t[e]
                for k in range(min(window if e in SENG else 1, len(pe_))):
                    o = pe_[k]
                    if o.nrem:
                        continue
                    st = o.rt if o.rt > ft else ft
                    key = (st, o.seq)
                    if bkey is None or key < bkey:
                        bkey, best, bk = key, o, k
                    if st <= ft:
                        break
            o = best
            e = o.eng
            pend[e].pop(bk) if pend[e][bk] is o else pend[e].remove(o)
            st = bkey[0]
            o.fin = st + o.lat
            free_t[e] = st + o.cost
            new[e].append(o)
            for u in o.users:
                u.nrem -= 1
                if o.fin > u.rt:
                    u.rt = o.fin
            remaining -= 1
        self.ops = new
        self.sim_time = max(free_t.values())

    def emit(self):
        nc = self.nc
        if SENG != ['']:
            self.schedule()
        for e in ENGS:
            i = 0
            prev = []
            for o in self.ops[e]:
                if o.dma:
                    o.dsem = (e, i % NDSEM)
                    o.dcount = 16 * (i // NDSEM + 1)
                    if i >= NDSEM:
                        o.deps.append(prev[i - NDSEM])
                    prev.append(o)
                    i += 1
        fin = Op()
        fin.eng, fin.dma, fin.signal, fin.count, fin.fn = 'sp', False, False, 0, None
        fin.deps = list(getattr(self, 'out_dmas', []))
        self.ops['sp'].append(fin)
        for e in ENGS:
            for o in self.ops[e]:
                for d in o.deps:
                    if not d.dma:
                        if d.eng == 'pe' and o.eng == 'pe':
                            continue
                        d.signal = True
        for e in ENGS:
            c = 0
            for o in self.ops[e]:
                if o.signal and not o.dma:
                    c += 1
                    o.count = c
        sems = {e: self.es.enter_context(nc.semaphore("s_" + e)) for e in ENGS}
        dsems = {}
        for e in ENGS:
            if self.ndma[e]:
                for k in range(min(NDSEM, self.ndma[e])):
                    dsems[(e, k)] = self.es.enter_context(nc.semaphore("d_%s%d" % (e, k)))
        self.stats = {}

        def run(ename, eng):
            known = {}
            nw = 0
            for o in self.ops[ename]:
                need = {}
                for d in o.deps:
                    if d.dma:
                        key, val = ('d',) + d.dsem, d.dcount
                    else:
                        if d.eng == 'pe' and ename == 'pe':
                            continue
                        key, val = ('c', d.eng), d.count
                    if known.get(key, 0) >= val:
                        continue
                    if need.get(key, 0) < val:
                        need[key] = val
                for key, val in need.items():
                    s = sems[key[1]] if key[0] == 'c' else dsems[(key[1], key[2])]
                    eng.wait_ge(s, val)
                    known[key] = val
                    nw += 1
                if o.fn is None:
                    continue
                ins = o.fn(eng)
                if o.dma:
                    ins.then_inc(dsems[o.dsem], 16)
                elif o.signal:
                    ins.then_inc(sems[ename], 1)
            self.stats[ename] = (len(self.ops[ename]), nw)

        with nc.Block() as block:
            @block.sync
            def _(eng):
                run('sp', eng)

            @block.scalar
            def _(eng):
                run('act', eng)

            @block.vector
            def _(eng):
                run('dve', eng)

            @block.gpsimd
            def _(eng):
                run('pool', eng)

            @block.tensor
            def _(eng):
                run('pe', eng)
        self.es.close()


def _fsz(ap):
    p = 1
    for d in ap.shape[1:]:
        p *= d
    return p


class B:
    def __init__(self, S):
        self.S = S

    @staticmethod
    def _t(xs):
        return [x for x in xs if isinstance(x, Tile)]

    @staticmethod
    def _a(x):
        return x.ap if isinstance(x, Tile) else x

    def mm(self, out, lhsT, rhs, start=True, stop=True):
        o, l, r = out.ap, lhsT.ap, rhs.ap
        f32 = 4.0 if l.dtype == F32 else 1.0
        self.S.op('pe', lambda e: e.matmul(o, l, r, start=start, stop=stop, skip_group_check=True),
                  reads=[lhsT, rhs], writes=[out], cost=30.0 + f32 * (max(_fsz(r), 32) + 100) / 2.4)

    def tr(self, out, in_, ident):
        o, i, d = out.ap, in_.ap, ident.ap
        self.S.op('pe', lambda e: e.transpose(o, i, d), reads=[in_, ident], writes=[out], cost=120.0)

    def act(self, out, in_, func, bias=None, scale=None, accum=None, eng='act'):
        o, i = out.ap, in_.ap
        kw = {}
        if bias is not None:
            kw['bias'] = self._a(bias)
        if scale is not None:
            kw['scale'] = self._a(scale)
        if accum is not None:
            kw['accum_out'] = accum.ap
        w = [out] + ([accum] if accum is not None else [])
        self.S.op('act', lambda e: e.activation(o, i, func, **kw),
                  reads=self._t([in_, bias, scale]), writes=w, cost=230.0 + _fsz(o) / 1.2)

    def tt(self, eng, out, a, b, op):
        o, x, y = out.ap, a.ap, b.ap
        self.S.op(eng, lambda e: e.tensor_tensor(o, x, y, op), reads=[a, b], writes=[out], cost=(120.0 + _fsz(o) / 0.96) if eng == 'dve' else (250.0 + _fsz(o) / 0.6))

    def ts(self, eng, out, a, s1, op0, s2=None, op1=None):
        o, x = out.ap, a.ap
        a1, a2 = self._a(s1), self._a(s2)
        if op1 is None:
            self.S.op(eng, lambda e: e.tensor_scalar(o, x, a1, None, op0), reads=self._t([a, s1]), writes=[out], cost=(120.0 + _fsz(o) / 1.5) if eng == 'dve' else (250.0 + _fsz(o) / 0.6))
        else:
            self.S.op(eng, lambda e: e.tensor_scalar(o, x, a1, a2, op0, op1), reads=self._t([a, s1, s2]), writes=[out], cost=(120.0 + _fsz(o) / 1.5) if eng == 'dve' else (250.0 + _fsz(o) / 0.6))

    def stt(self, out, a, s, b, op0, op1):
        o, x, y, sc = out.ap, a.ap, b.ap, self._a(s)
        self.S.op('dve', lambda e: e.scalar_tensor_tensor(o, x, sc, y, op0, op1), reads=self._t([a, s, b]), writes=[out], cost=120.0 + _fsz(o) / 0.96)

    def cp(self, eng, out, in_):
        o, i = out.ap, in_.ap
        if eng == 'act':
            self.S.op('act', lambda e: e.copy(o, i), reads=[in_], writes=[out], cost=230.0 + _fsz(o) / 1.2)
        else:
            self.S.op(eng, lambda e: e.tensor_copy(o, i), reads=[in_], writes=[out], cost=(120.0 + _fsz(o) / 1.5) if eng == 'dve' else (250.0 + _fsz(o) / 0.6))

    def red(self, out, in_, op=None, axis=None):
        o, i = out.ap, in_.ap
        op = op or ALU.add
        axis = axis or AX.X
        self.S.op('dve', lambda e: e.tensor_reduce(o, i, axis, op), reads=[in_], writes=[out], cost=120.0 + _fsz(i) / 0.96)

    def memset(self, eng, out, val):
        o = out.ap
        self.S.op(eng, lambda e: e.memset(o, val), reads=[], writes=[out], cost=150.0 + _fsz(o) / 0.96)

    def asel(self, out, in_, pattern, cmp, fill, base, cm):
        o, i = out.ap, in_.ap
        def fn(e):
            try:
                return e.affine_select(o, i, pattern, cmp, fill, base=base, channel_multiplier=cm)
            except Exception:
                print("ASEL FAIL", pattern, base, cm, fill, o)
                raise
        self.S.op('pool', fn, reads=[in_], writes=[out], cost=300.0 + _fsz(o) / 0.9)

    def scan(self, out, d0, d1, init, op0, op1):
        o, x, y, ii = out.ap, d0.ap, d1.ap, self._a(init)
        self.S.op('dve', lambda e: e.tensor_tensor_scan(o, x, y, ii, op0, op1), reads=self._t([d0, d1, init]), writes=[out], cost=120.0 + _fsz(o) * 2 / 0.96)

    def recip(self, out, in_):
        o, i = out.ap, in_.ap
        self.S.op('dve', lambda e: e.reciprocal(o, i), reads=[in_], writes=[out])

    def max8(self, out, in_):
        o, i = out.ap, in_.ap
        self.S.op('dve', lambda e: e.max(o, i), reads=[in_], writes=[out])

    def mrep(self, out, rep, vals, imm):
        o, r, v = out.ap, rep.ap, vals.ap
        self.S.op('dve', lambda e: e.match_replace(o, r, v, imm), reads=[rep, vals], writes=[out])

    def dma(self, eng, out, in_, out_final=False, **kw):
        self.S.dma(eng, out, in_, reads=self._t([in_]), writes=self._t([out]), out=out_final, **kw)


D_MODEL = 1024
NORM_EPS = 1e-6
IN_SPLITS = (
    ('gdn_q', 512), ('gdn_k', 512), ('gdn_v', 512), ('gdn_beta', 4), ('gdn_a', 4), ('gdn_z', 512),
    ('gla_q', 256), ('gla_k', 256), ('gla_v', 512), ('gla_gk', 16), ('gla_z', 512),
    ('ssd_x', 512), ('ssd_b', 256), ('ssd_c', 256), ('ssd_dt', 8), ('ssd_z', 512),
    ('nsa_q', 512), ('nsa_kc', 128), ('nsa_vc', 128), ('nsa_ks', 128), ('nsa_vs', 128),
    ('nsa_kw', 128), ('nsa_vw', 128), ('nsa_gate', 24), ('nsa_z', 512),
    ('merge_gate', 4096),
)
OFF = {}
_s = 0
for _n, _w in IN_SPLITS:
    OFF[_n] = _s
    _s += _w
D_IN = _s
MT = 512
NEG = -30000.0


def host_constants(S_len):
    c = {}
    ident = np.eye(128, dtype=np.float32)
    U = np.triu(np.ones((128, 128), np.float32))
    ones = np.ones((128, 128), np.float32)
    jidx = np.tile(np.arange(64, dtype=np.float32)[None, :], (128, 1))
    e0 = np.zeros((128, 64), np.float32)
    e0[:, 0] = 1.0
    half = (np.arange(128) >= 64).astype(np.float32)[:, None]
    c['cst'] = np.concatenate([ident, U, ones, jidx, e0, half], axis=1)
    slopes = 2.0 ** (-np.arange(1, 9, dtype=np.float64))
    t = np.arange(S_len)
    qaug = np.zeros((4, 8, S_len), np.float32)
    for h in range(8):
        qaug[0, h] = slopes[h] * 64
        qaug[1, h] = slopes[h]
        qaug[2, h] = -slopes[h] * 64 * (t // 64)
        qaug[3, h] = -slopes[h] * (t % 64)
    c['qaug'] = qaug
    kaug = np.stack([t // 64, t % 64, np.ones_like(t), np.ones_like(t)]).astype(np.float32)
    c['kaug'] = kaug
    ncp = 256
    cp = np.arange(ncp) * 16 + 31
    kc_ = np.stack([cp // 64, cp % 64, np.ones_like(cp), np.ones_like(cp)]).astype(np.float32)
    c['kaugc'] = np.concatenate([np.zeros((4, 1), np.float32), kc_[:, :-1]], axis=1)
    nsel = S_len // 64
    cs = np.arange(ncp) * 16
    ss = np.arange(64) * 64
    ov = ((cs[:, None] <= ss[None, :] + 63) & (cp[:, None] >= ss[None, :])).astype(np.float32)
    ov[:, nsel:] = 0.0
    ov = np.concatenate([np.zeros((1, 64), np.float32), ov[:-1]], axis=0)
    c['overlap'] = ov
    E = np.zeros((64, S_len), np.float32)
    E[t // 64, t] = 1.0
    c['esel'] = E
    return c


PARAM_SHAPES = {
    'norm_pre': [1024], 'norm_post': [1024],
    'conv_aT': [128, 12, 4], 'a_log_a': [4], 'dt_bias_a': [4], 'onorm_a': [128],
    'w_gk': [16, 256], 'b_gkT': [128, 2], 'onorm_b': [128],
    'conv_cT': [128, 8, 4], 'conv_bias_cT': [128, 8], 'a_log_c': [8], 'dt_bias_c': [8], 'd_skip_c': [8],
    'onorm_c': [512], 'cmp_pos_kT': [64, 32], 'cmp_pos_vT': [64, 32],
    'w_ck1': [2048, 64], 'w_ck2': [64, 64], 'w_cv1': [2048, 64], 'w_cv2': [64, 64],
    'w_br': [4, 512, 1024], 'w_out': [1024, 1024], 'w_in': [1024, D_IN],
}


def prep_inputs(inp, depth):
    o = {}
    f = lambda a: np.ascontiguousarray(np.asarray(a, dtype=np.float32))
    for k in ('norm_pre', 'norm_post', 'a_log_a', 'dt_bias_a', 'onorm_a', 'w_gk', 'onorm_b', 'a_log_c',
              'dt_bias_c', 'd_skip_c', 'onorm_c', 'w_ck1', 'w_ck2', 'w_cv1', 'w_cv2', 'w_br', 'w_out', 'w_in'):
        o[k] = f(inp[k][:depth])
    o['conv_aT'] = f(np.asarray(inp['conv_a'])[:depth].reshape(depth, 4, 12, 128).transpose(0, 3, 2, 1))
    o['conv_cT'] = f(np.asarray(inp['conv_c'])[:depth].reshape(depth, 4, 8, 128).transpose(0, 3, 2, 1))
    o['conv_bias_cT'] = f(np.asarray(inp['conv_bias_c'])[:depth].reshape(depth, 8, 128).transpose(0, 2, 1))
    o['b_gkT'] = f(np.asarray(inp['b_gk'])[:depth].reshape(depth, 2, 128).transpose(0, 2, 1))
    o['cmp_pos_kT'] = f(np.asarray(inp['cmp_pos_k'])[:depth].transpose(0, 2, 1))
    o['cmp_pos_vT'] = f(np.asarray(inp['cmp_pos_v'])[:depth].transpose(0, 2, 1))
    return o


def build(S_len=4096, depth=4, branches=(0, 1, 2, 3)):
    nc = bass.Bass("TRN2", target_bir_lowering=False)
    NT = S_len // 128
    NM = S_len // MT
    S = Sched(nc)
    b = B(S)
    dr = {}
    dr['x'] = nc.dram_tensor("x", [S_len, 1024], F32, kind="ExternalInput").ap()
    for k, shp in PARAM_SHAPES.items():
        dr[k] = nc.dram_tensor(k, [depth] + shp, F32, kind="ExternalInput").ap()
    hc = host_constants(S_len)
    for k, v in hc.items():
        dr[k] = nc.dram_tensor(k, list(v.shape), F32, kind="ExternalInput").ap()
    y = nc.dram_tensor("y", [S_len, 1024], F32, kind="ExternalOutput").ap()
    wq = {'w_in': nc.dram_tensor("wq_in", [depth, 1024, D_IN], BF16, kind="Internal").ap(),
          'w_br': nc.dram_tensor("wq_br", [depth, 2048, 1024], BF16, kind="Internal").ap(),
          'w_out': nc.dram_tensor("wq_out", [depth, 1024, 1024], BF16, kind="Internal").ap()}
    wqres = {}
    yres = [S.res("y%d" % g) for g in range(NT)]

    def dt(ap, res=None):
        return Tile('dram', ap, res.rid if res is not None else 0)

    cst = S.sb("cst", [128, 513], F32)
    b.dma('sp', cst, dt(dr['cst']))
    ident_f, U_f, ones_f = cst[:, 0:128], cst[:, 128:256], cst[:, 256:384]
    cstb = S.sb("cstb", [128, 384], BF16)
    b.cp('dve', cstb, cst[:, 0:384])
    ident_b, U_b, ones_b = cstb[:, 0:128], cstb[:, 128:256], cstb[:, 256:384]
    mhalf = S.sb("mhalf", [128, 1], F32)
    b.memset('dve', mhalf, -0.5)

    PS = [S.ps("ps%d" % i, [128, 512], F32) for i in range(7)]
    PTb = S.ps("ptb", [128, 1024], BF16)
    psi = [0]

    def nps(lo=0):
        psi[0] += 1
        return PS[lo + psi[0] % (7 - lo)]

    h4 = lambda t: t.re("p (h i) -> p h i", h=4)

    pools = {}

    def scr(cls, i):
        key = (cls, i)
        if key not in pools:
            pools[key] = S.sb("scr%s%d" % (cls, i), [128, 512], F32 if cls == 'A' else BF16)
        return pools[key]

    FM = S.sb("FM", [128, 16, MT], BF16)
    xts = [S.sb("xt%d" % i, [128, 1024], F32) for i in range(1)]
    hb = S.sb("hb", [128, 1024], BF16)
    junk = hb
    hT = S.sb("hT", [128, 8, MT], BF16)
    wbufs = [S.sb("wb%d" % i, [128, 8, 528], BF16) for i in range(2)]
    wi = [0]
    merged = [S.sb("mg%d" % i, [128, 1024], F32) for i in range(4)]
    gpre = S.sb("gpre", [128, 1024], F32)
    gpost = S.sb("gpost", [128, 1024], F32)
    ssq = S.sb("ssq", [128, 1], F32)
    rstd = S.sb("rstd", [128, 1], F32)
    ybr = [S.sb("ybr%d" % i, [128, 512], BF16) for i in range(4)]
    yT = S.sb("yT", [128, 4, 4, 128], BF16)
    zz = [S.sb("zz%d" % i, [128, 512], BF16) for i in range(4)]
    raw = [S.sb("raw%d" % i, [128, 515], BF16) for i in range(2)]
    dg = [S.sb("dg%d" % i, [128, 4, 128], BF16) for i in range(2)]
    sg = scr('A', 0)
    tmpm = scr('A', 1)
    mgb = hb
    mgT = yT[:, 0:2].re("p a k t -> p (a k) t")

    def bcast_load(tile, src_ap, n):
        b.dma('pool', tile, dt(src_ap.partition_broadcast(128)))

    def convert_layer(l):
        for name, src2d in (('w_in', dr['w_in'][l]), ('w_br', dr['w_br'][l].rearrange("n k c -> (n k) c")),
                            ('w_out', dr['w_out'][l])):
            r = S.res("wq_%s%d" % (name, l))
            wqres[(name, l)] = r
            ncol = src2d.shape[1]
            nrow = src2d.shape[0]
            for r0 in range(0, nrow, 1024):
                for c0 in range(0, ncol, 2048):
                    c1 = min(c0 + 2048, ncol)
                    S.dma('pool', wq[name][l][r0:r0 + 1024, c0:c1], src2d[r0:r0 + 1024, c0:c1], reads=[], writes=[r])

    def wload(key, col0, ncols, rows8=True):
        name, l, row0, nrows = key
        wb = wbufs[wi[0] % 2]
        wi[0] += 1
        src = wq[name][l][row0:row0 + nrows, :].rearrange("(k p) n -> p k n", p=128)
        nk = src.shape[1]
        b.dma('sp', wb[:, 0:nk, 0:ncols], dt(src[:, :, col0:col0 + ncols], wqres[(name, l)]))
        return wb

    def proj_tok(ps, st, wb, c0, n, o0=0):
        for kc in range(8):
            b.mm(ps[:, o0:o0 + n], hT[:, kc, st * 128:(st + 1) * 128], wb[:, kc, c0:c0 + n], start=kc == 0, stop=kc == 7)

    def proj_feat(ps, wb, c0, mch):
        for kc in range(8):
            b.mm(ps[0:mch, :], wb[:, kc, c0:c0 + mch], hT[:, kc, :], start=kc == 0, stop=kc == 7)

    def rms_rstd(out1, ss1, n):
        b.ts('dve', out1, ss1, 1.0 / n, ALU.mult, NORM_EPS, ALU.add)
        b.tt('pool', out1, out1, mhalf_k(out1.ap.shape[1]), ALU.pow)

    mh_cache = {}

    def mhalf_k(k):
        if k not in mh_cache:
            t = S.sb("mh%d" % k, [128, k], F32)
            b.memset('dve', t, -0.5)
            mh_cache[k] = t
        return mh_cache[k]

    def emit_yT(st):
        for kc in range(4):
            b.tr(PTb[:, kc * 128:(kc + 1) * 128], ybr[st][:, kc * 128:(kc + 1) * 128], ident_b)
        b.cp('dve', yT[:, st], PTb[:, 0:512].re("p (k t) -> p k t", k=4))

    ctx = dict(jidx=cst[:, 384:448], e0=cst[:, 448:512], half01=cst[:, 512:513], emit_yT=emit_yT, scr=scr, FM=FM, nc=nc, S=S, b=b, dr=dr, dt=dt, PS=PS, PTb=PTb, nps=nps, h4=h4, hT=hT, wload=wload,
               proj_tok=proj_tok, proj_feat=proj_feat, rms_rstd=rms_rstd, ident_f=ident_f, U_f=U_f,
               ones_f=ones_f, ident_b=ident_b, U_b=U_b, ones_b=ones_b, ybr=ybr, zz=zz, raw=raw, dg=dg,
               S_len=S_len, NT=NT, NM=NM, bcast_load=bcast_load, depth=depth, mhalf_k=mhalf_k)
    mixers = {}
    if 0 in branches:
        mixers[0] = GDN(ctx)
    if 1 in branches:
        mixers[1] = GLA(ctx)
    if 2 in branches:
        mixers[2] = SSD(ctx)
    if 3 in branches:
        mixers[3] = NSA(ctx)

    convert_layer(0)
    for l in range(depth):
        w_in_l = ('w_in', l, 0, 1024)
        if l + 1 < depth:
            convert_layer(l + 1)
        bcast_load(gpre, dr['norm_pre'][l:l + 1, :], 1024)
        bcast_load(gpost, dr['norm_post'][l:l + 1, :], 1024)
        for n in mixers:
            mixers[n].layer_setup(l)
        for m in range(NM):
            for st in range(4):
                g = m * 4 + st
                xt = xts[0]
                src = dr['x'] if l == 0 else y
                b.dma('pool', xt, dt(src[g * 128:(g + 1) * 128, :], yres[g]))
                b.act(junk, xt, AF.Square, accum=ssq)
                rms_rstd(rstd, ssq, 1024)
                b.stt(hb, xt, rstd[:, 0:1], gpre, ALU.mult, ALU.mult)
                for kc in range(8):
                    b.tr(PTb[:, kc * 128:(kc + 1) * 128], hb[:, kc * 128:(kc + 1) * 128], ident_b)
                b.cp('act', hT[:, :, st * 128:(st + 1) * 128], PTb.re("p (k t) -> p k t", k=8))
            first = True
            for n in (0, 1, 2, 3):
                if n not in mixers:
                    continue
                mixers[n].macro(l, m, w_in_l)
                for half in range(2):
                    wg = wload(w_in_l, OFF['merge_gate'] + n * 1024 + half * 512, 512)
                    wbr = wload(('w_br', l, n * 512, 512), half * 512, 512)
                    for st in range(4):
                        pg = nps()
                        proj_tok(pg, st, wg, 0, 512)
                        b.act(sg, pg, AF.Sigmoid)
                        pb = nps()
                        for kc in range(4):
                            b.mm(pb, yT[:, st, kc, :], wbr[:, kc, 0:512], start=kc == 0, stop=kc == 3)
                        mslice = merged[st][:, half * 512:(half + 1) * 512]
                        if first:
                            b.tt('dve', mslice, sg, pb, ALU.mult)
                        else:
                            b.tt('dve', tmpm, sg, pb, ALU.mult)
                            b.tt('pool', mslice, mslice, tmpm, ALU.add)
                first = False
            wo = [wload(('w_out', l, 0, 1024), half * 512, 512) for half in range(2)]
            for st in range(4):
                g = m * 4 + st
                osb = merged[st]
                b.cp('act', mgb, merged[st])
                for kc in range(8):
                    b.tr(PTb[:, kc * 128:(kc + 1) * 128], mgb[:, kc * 128:(kc + 1) * 128], ident_b)
                b.cp('dve', mgT, PTb.re("p (k t) -> p k t", k=8))
                for half in range(2):
                    po = nps()
                    for kc in range(8):
                        b.mm(po, mgT[:, kc, :], wo[half][:, kc, 0:512], start=kc == 0, stop=kc == 7)
                    b.cp('act', osb[:, half * 512:(half + 1) * 512], po)
                b.act(junk, osb, AF.Square, accum=ssq)
                rms_rstd(rstd, ssq, 1024)
                xt = xts[0]
                src = dr['x'] if l == 0 else y
                b.dma('pool', xt, dt(src[g * 128:(g + 1) * 128, :], yres[g]))
                b.stt(osb, osb, rstd[:, 0:1], gpost, ALU.mult, ALU.mult)
                b.tt('dve', osb, osb, xt, ALU.add)
                S.dma('pool', y[g * 128:(g + 1) * 128, :], osb.ap, reads=[osb], writes=[yres[g]], out=(l == depth - 1))
    S.emit()
    return nc, S


class Mixer:
    def __init__(self, ctx):
        self.__dict__.update(ctx)

    def conv_chunk(self, ps_in, convw, c, halo, out_fm, bias=None):
        b = self.b
        k = self.cc
        self.cc += 1
        raw, dg = self.raw[k % 2], self.dg[k % 2]
        b.cp('dve', raw[:, 0:3], halo)
        b.cp('act', raw[:, 3:515], ps_in)
        b.cp('dve', halo, raw[:, 512:515])
        b.tt('dve', dg, self.ident_b.bc(1, [128, 4, 128]), convw[:, c, :].bc(2, [128, 4, 128]), ALU.mult)
        p2 = self.nps()
        for t in range(4):
            b.mm(p2, dg[:, t, :], raw[:, t:t + 512], start=t == 0, stop=t == 3)
        if bias is None:
            b.act(out_fm, p2, AF.Silu)
        else:
            b.act(out_fm, p2, AF.Silu, bias=bias)

    def out_norm_heads(self, po, st, onz):
        b, S = self.b, self.S
        o = self.osb4
        b.cp('act', o, po)
        b.tt('pool', self.sq4, o, o, ALU.mult)
        b.red(self.ss4, self.h4(self.sq4))
        self.rms_rstd(self.rs4, self.ss4, 128)
        b.tt('dve', self.h4(o), self.h4(o), self.rs4.bc(2, [128, 4, 128]), ALU.mult)
        b.tt('dve', self.ybr[st], o, onz, ALU.mult)

    def z_block(self, wb, c0, onorm_b, per_head=True):
        b = self.b
        for st in range(4):
            pz = self.nps()
            self.proj_tok(pz, st, wb, c0, 512)
            b.act(self.zz[st], pz, AF.Silu)
            if onorm_b is not None:
                if per_head:
                    b.tt('pool', self.h4(self.zz[st]), self.h4(self.zz[st]), onorm_b.bc(1, [128, 4, 128]), ALU.mult)
                else:
                    b.tt('pool', self.zz[st], self.zz[st], onorm_b, ALU.mult)


class GDN(Mixer):
    def __init__(self, ctx):
        super().__init__(ctx)
        S = self.S
        self.cc = 0
        scr = self.scr
        A4 = lambda i: scr('A', i).re("p (h i) -> p h i", h=4)
        B4 = lambda i: scr('B', i).re("p (h i) -> p h i", h=4)
        self.fm = self.FM[:, 0:12, :]
        self.sqa, self.sqb = B4(9), B4(10)
        self.halo = [S.sb("gdn_h%d" % c, [128, 3], BF16) for c in range(12)]
        self.convw = S.sb("gdn_cw", [128, 12, 4], F32)
        self.nega = S.sb("gdn_nega", [128, 4], F32)
        self.dtb = S.sb("gdn_dtb", [128, 4], F32)
        self.onorm = S.sb("gdn_on", [128, 128], F32)
        self.Sf = S.sb("gdn_Sf", [128, 4, 128], F32)
        self.Sb = S.sb("gdn_Sb", [128, 4, 128], BF16)
        f = lambda n, k: S.sb("gdn_" + n, [128, k], F32)
        self.lnss, self.lnr, self.ba, self.e1, self.nlb = f("lnss", 8), f("lnr", 8), f("ba", 8), f("e1", 4), f("nlb", 4)
        self.apb, self.e2, self.sp, self.g, self.gcl = f("apb", 4), f("e2", 4), f("sp", 4), f("g", 4), f("gcl", 8)
        self.C3, self.X4, self.EX = f("C3", 12), f("X4", 16), f("EX", 16)
        self.D3 = [A4(2), A4(3), A4(4)]
        self.BJ, self.BA = A4(5), A4(6)
        self.E1, self.E2, self.E3 = A4(7), A4(8), A4(9)
        self.EB = B4(0)
        self.P = [A4(10), A4(11)]
        self.PT = [A4(12), A4(13)]
        self.AT = [A4(14), A4(15)]
        self.ATb, self.At, self.Rk, self.Kd = B4(1), B4(2), B4(3), B4(4)
        self.Vb, self.nW, self.Vn, self.qg = B4(5), B4(6), B4(7), B4(8)
        self.osb4 = scr('A', 0)
        self.sq4 = scr('A', 1)
        self.ss4, self.rs4 = f("ss4", 4), f("rs4", 4)
        self.ea = f("ea", 4)

    def layer_setup(self, l):
        b, dr, dt = self.b, self.dr, self.dt
        b.dma('pool', self.convw, dt(dr['conv_aT'][l]))
        self.bcast_load(self.ea, dr['a_log_a'][l:l + 1, :], 4)
        b.act(self.nega, self.ea, AF.Exp)
        b.ts('dve', self.nega, self.nega, -1.0, ALU.mult)
        self.bcast_load(self.dtb, dr['dt_bias_a'][l:l + 1, :], 4)
        self.bcast_load(self.onorm, dr['onorm_a'][l:l + 1, :], 128)
        b.memset('dve', self.Sf, 0.0)
        b.memset('dve', self.Sb, 0.0)
        for c in range(12):
            b.memset('pool', self.halo[c], 0.0)

    def macro(self, l, m, w_in_l):
        b, nps, h4 = self.b, self.nps, self.h4
        fm = self.fm
        for blk in range(3):
            wb = self.wload(w_in_l, blk * 512, 512)
            for cc in range(4):
                c = blk * 4 + cc
                p = nps()
                self.proj_feat(p, wb, cc * 128, 128)
                self.conv_chunk(p, self.convw, c, self.halo[c], fm[:, c, :])
        wb3 = self.wload(w_in_l, OFF['gdn_beta'], 520)
        self.z_block(wb3, 8, self.onorm)
        for st in range(4):
            self.sub(st, wb3)
            self.emit_yT(st)

    def sub(self, st, wb3):
        b, nps, h4 = self.b, self.nps, self.h4
        fm = self.fm
        tk = slice(st * 128, (st + 1) * 128)
        bc4 = lambda t: t.bc(2, [128, 4, 128])
        b.tt('pool', self.sqa, fm[:, 4:8, tk], fm[:, 4:8, tk], ALU.mult)
        b.tt('pool', self.sqb, fm[:, 0:4, tk], fm[:, 0:4, tk], ALU.mult)
        pq = nps()
        for c in range(8):
            sq_c = self.sqa[:, c, :] if c < 4 else self.sqb[:, c - 4, :]
            b.mm(pq[:, c:c + 1], sq_c, self.ones_b[:, 0:1])
        self.proj_tok(pq, st, wb3, 0, 8, o0=8)
        b.act(self.lnss, pq[:, 0:8], AF.Ln, bias=NORM_EPS)
        b.ts('dve', self.lnr, self.lnss, -0.5, ALU.mult)
        b.cp('dve', self.ba, pq[:, 8:16])
        b.act(self.e1, self.ba[:, 0:4], AF.Exp, scale=-1.0)
        b.act(self.nlb, self.e1, AF.Ln, bias=1.0)
        b.tt('dve', self.apb, self.ba[:, 4:8], self.dtb, ALU.add)
        b.act(self.e2, self.apb, AF.Exp)
        b.act(self.sp, self.e2, AF.Ln, bias=1.0)
        b.tt('dve', self.g, self.sp, self.nega, ALU.mult)
        pg = nps()
        b.mm(pg[:, 0:4], self.U_f, self.g)
        b.mm(pg[:, 4:8], self.ones_f, self.g)
        b.cp('dve', self.gcl, pg[:, 0:8])
        gc, gl = self.gcl[:, 0:4], self.gcl[:, 4:8]
        lnrk, lnrq = self.lnr[:, 0:4], self.lnr[:, 4:8]
        cA, cB, cJ = self.C3[:, 0:4], self.C3[:, 4:8], self.C3[:, 8:12]
        b.tt('dve', cJ, lnrk, gc, ALU.subtract)
        b.tt('dve', cA, gc, self.nlb, ALU.subtract)
        b.tt('dve', cA, cA, lnrk, ALU.add)
        b.stt(cB, gc, float(np.log(128.0 ** -0.5)), lnrq, ALU.add, ALU.add)
        X4 = self.X4
        b.cp('pool', X4[:, 0:4], cA)
        b.tt('pool', X4[:, 4:8], cJ, gl, ALU.add)
        b.ts('pool', X4[:, 8:12], self.nlb, -1.0, ALU.mult)
        b.cp('pool', X4[:, 12:16], gl)
        b.act(self.EX, X4, AF.Exp)
        sRk, sKd, sVb, dec = self.EX[:, 0:4], self.EX[:, 4:8], self.EX[:, 8:12], self.EX[:, 12:16]
        for v3 in range(3):
            b.tt('dve', self.D3[v3], self.ident_f.bc(1, [128, 4, 128]), bc4(self.C3[:, v3 * 4:(v3 + 1) * 4]), ALU.mult)
        b.cp('pool', self.BJ, bc4(cJ))
        b.cp('pool', self.BA, bc4(cA))
        f4 = lambda t: t.re("p h i -> p (h i)")
        pX1, pX2, pX3, pXB = nps(), nps(), nps(), nps()
        b.mm(pX1, self.ones_f, f4(self.D3[0]), start=True, stop=False)
        b.mm(pX1, self.ident_f, f4(self.BJ), start=False, stop=True)
        b.mm(pX2, self.ones_f, f4(self.D3[1]), start=True, stop=False)
        b.mm(pX2, self.ident_f, f4(self.BJ), start=False, stop=True)
        b.mm(pX3, self.ones_f, f4(self.D3[2]), start=True, stop=False)
        b.mm(pX3, self.ident_f, f4(self.BA), start=False, stop=True)
        b.mm(pXB, self.ones_f, f4(self.D3[1]))
        b.act(f4(self.E1), pX1, AF.Exp)
        b.act(f4(self.E2), pX2, AF.Exp)
        b.act(f4(self.E3), pX3, AF.Exp)
        b.act(f4(self.EB), pXB, AF.Exp)
        b.asel(self.E1, self.E1, [[0, 4], [1, 128]], ALU.is_ge, 0.0, -1, -1)
        b.asel(self.E2, self.E2, [[0, 4], [1, 128]], ALU.is_ge, 0.0, 0, -1)
        b.asel(self.E3, self.E3, [[0, 4], [-1, 128]], ALU.is_ge, 0.0, -1, 1)
        pG, pKQ = nps(), nps()
        for h in range(4):
            b.mm(pG[:, h * 128:(h + 1) * 128], fm[:, 4 + h, tk], fm[:, 4 + h, tk])
        for h in range(4):
            b.mm(pKQ[:, h * 128:(h + 1) * 128], fm[:, 4 + h, tk], fm[:, h, tk])
        P, PT, AT = self.P, self.PT, self.AT
        b.stt(f4(PT[0]), f4(self.E1), -1.0, pG, ALU.mult, ALU.mult)
        b.stt(f4(P[0]), f4(self.E3), -1.0, pG, ALU.mult, ALU.mult)
        b.tt('dve', f4(self.At), f4(self.E2), pKQ, ALU.mult)
        b.tt('pool', AT[0], PT[0], self.ident_f.bc(1, [128, 4, 128]), ALU.add)
        cur = 0
        for lev in range(1, 7):
            nxt = 1 - cur
            pP = nps()
            for h in range(4):
                b.mm(pP[:, h * 128:(h + 1) * 128], PT[cur][:, h, :], P[cur][:, h, :])
            if lev < 6:
                pPT = nps()
                for h in range(4):
                    b.mm(pPT[:, h * 128:(h + 1) * 128], P[cur][:, h, :], PT[cur][:, h, :])
            b.cp('act', f4(P[nxt]), pP)
            if lev < 6:
                b.cp('dve', f4(PT[nxt]), pPT)
            pA = nps()
            for h in range(4):
                b.mm(pA[:, h * 128:(h + 1) * 128], P[nxt][:, h, :], AT[cur][:, h, :])
            b.tt('dve', f4(AT[nxt]), f4(AT[cur]), pA, ALU.add)
            cur = nxt
        b.cp('act', self.ATb, AT[cur])
        PTb = self.PTb
        for h in range(4):
            b.tr(PTb[:, h * 128:(h + 1) * 128], fm[:, 4 + h, tk], self.ident_b)
            b.tr(PTb[:, (4 + h) * 128:(5 + h) * 128], fm[:, 8 + h, tk], self.ident_b)
        pk = PTb[:, 0:512].re("p (h d) -> p h d", h=4)
        pv = PTb[:, 512:1024].re("p (h d) -> p h d", h=4)
        b.tt('dve', self.Rk, pk, bc4(sRk), ALU.mult)
        b.tt('dve', self.Kd, pk, bc4(sKd), ALU.mult)
        b.tt('dve', self.Vb, pv, bc4(sVb), ALU.mult)
        pW = nps()
        for h in range(4):
            b.mm(pW[:, h * 128:(h + 1) * 128], self.Rk[:, h, :], self.ATb[:, h, :])
        b.act(f4(self.nW), pW, AF.Copy, scale=-1.0)
        pV = nps()
        for h in range(4):
            b.mm(pV[:, h * 128:(h + 1) * 128], self.ATb[:, h, :], self.Vb[:, h, :], start=(h == 0), stop=False)
            b.mm(pV[:, h * 128:(h + 1) * 128], self.nW[:, h, :], self.Sb[:, h, :], start=False, stop=True)
        b.cp('act', f4(self.Vn), pV)
        b.tt('dve', self.qg, fm[:, 0:4, tk], self.EB, ALU.mult)
        pO = nps()
        for h in range(4):
            b.mm(pO[:, h * 128:(h + 1) * 128], self.qg[:, h, :], self.Sb[:, h, :], start=(h == 0), stop=False)
            b.mm(pO[:, h * 128:(h + 1) * 128], self.At[:, h, :], self.Vn[:, h, :], start=False, stop=True)
        pS = nps()
        for h in range(4):
            b.mm(pS[:, h * 128:(h + 1) * 128], self.Kd[:, h, :], self.Vn[:, h, :])
        b.tt('dve', self.Sf, self.Sf, bc4(dec), ALU.mult)
        b.tt('dve', f4(self.Sf), f4(self.Sf), pS, ALU.add)
        b.cp('act', self.Sb, self.Sf)
        self.out_norm_heads(pO, st, self.zz[st])


class GLA(Mixer):
    def __init__(self, ctx):
        super().__init__(ctx)
        S = self.S
        scr = self.scr
        A2 = lambda i, o: scr('A', i)[:, o * 256:(o + 1) * 256].re("p (h i) -> p h i", h=2)
        B4 = lambda i: scr('B', i).re("p (h i) -> p h i", h=4)
        B2 = lambda i, o: scr('B', i)[:, o * 256:(o + 1) * 256].re("p (h i) -> p h i", h=2)
        self.fm = self.FM[:, 0:4, :]
        self.vt = [scr('B', 4 + i) for i in range(4)]
        self.gklo = scr('A', 10)
        self.wgk = S.sb("gla_wgk", [128, 256], F32)
        self.nbg = S.sb("gla_nbg", [128, 2], F32)
        self.onorm = S.sb("gla_on", [128, 128], F32)
        self.e = scr('A', 2)
        self.sp = [scr('A', 6), scr('A', 7)]
        self.nb = [scr('A', 8), scr('A', 9)]
        self.negc = S.sb("gla_negc", [128, 2, 2], F32)
        self.Eq, self.Ek = A2(3, 0), A2(3, 1)
        self.Eg, self.Ed = A2(4, 0), A2(4, 1)
        self.qt, self.kd = B2(0, 0), B2(0, 1)
        self.kt = B4(1)
        self.qg = B4(2)
        self.kdt = scr('B', 3)[:, 0:256]
        self.At = B4(8)
        self.Sf = S.sb("gla_Sf", [128, 2, 128], F32)
        self.Sb = S.sb("gla_Sb", [128, 2, 128], BF16)
        self.osb4 = scr('A', 0)
        self.sq4 = scr('A', 1)
        self.ss4 = S.sb("gla_ss4", [128, 4], F32)
        self.rs4 = S.sb("gla_rs4", [128, 4], F32)
        self.rm = S.sb("gla_rm", [128, 4], F32)
        self.Sd = scr('A', 5)[:, 0:128]

    def layer_setup(self, l):
        b, dr, dt = self.b, self.dr, self.dt
        b.memset('dve', self.wgk, 0.0)
        b.dma('pool', self.wgk[0:16, :], dt(dr['w_gk'][l]))
        b.dma('pool', self.nbg, dt(dr['b_gkT'][l]))
        b.ts('dve', self.nbg, self.nbg, -1.0, ALU.mult)
        self.bcast_load(self.onorm, dr['onorm_b'][l:l + 1, :], 128)
        b.memset('dve', self.Sf, 0.0)
        b.memset('dve', self.Sb, 0.0)
        if l == 0:
            b.memset('dve', self.rm, 0.0)
            b.memset('dve', self.rm[0:64, 0:1], 1.0)
            b.memset('dve', self.rm[64:128, 1:2], 1.0)
            b.memset('dve', self.rm[0:64, 2:3], 0.125)
            b.memset('dve', self.rm[64:128, 3:4], 0.125)

    def macro(self, l, m, w_in_l):
        b, nps = self.b, self.nps
        wb = self.wload(w_in_l, OFF['gla_q'], 512)
        for c in range(4):
            p = nps()
            self.proj_feat(p, wb, c * 128, 128)
            b.cp('act', self.fm[:, c, :], p)
        wb = self.wload(w_in_l, OFF['gla_v'], 512)
        for st in range(4):
            p = nps()
            self.proj_tok(p, st, wb, 0, 512)
            b.cp('act', self.vt[st], p)
        wb = self.wload(w_in_l, OFF['gla_gk'], 528)
        p = nps()
        self.proj_feat(p, wb, 0, 128)
        b.memset('pool', self.gklo, 0.0)
        b.cp('dve', self.gklo[0:16, :], p[0:16, :])
        for c in range(2):
            p = nps()
            b.mm(p, self.wgk[:, c * 128:(c + 1) * 128], self.gklo)
            b.act(self.e, p, AF.Exp, scale=-1.0, bias=self.nbg[:, c:c + 1])
            b.act(self.sp[c], self.e, AF.Ln, bias=1.0)
            b.ts('dve', self.sp[c], self.sp[c], 1.0 / 16.0, ALU.mult)
            for st in range(4):
                tk = slice(st * 128, (st + 1) * 128)
                b.scan(self.nb[c][:, tk], self.ones_f, self.sp[c][:, tk], 0.0, ALU.mult, ALU.add)
        self.z_block(wb, 16, self.onorm)
        for st in range(4):
            self.sub(st)
            self.emit_yT(st)

    def sub(self, st):
        b, nps = self.b, self.nps
        tk = slice(st * 128, (st + 1) * 128)
        fm = self.fm
        for c in range(2):
            nbs = self.nb[c][:, tk]
            ref = self.nb[c][:, st * 128 + 64:st * 128 + 65]
            last = self.nb[c][:, st * 128 + 127:st * 128 + 128]
            b.ts('dve', self.negc[:, c, 0:1], ref, -1.0, ALU.mult)
            b.ts('dve', self.negc[:, c, 1:2], last, -1.0, ALU.mult)
            b.act(self.Eq[:, c, :], nbs, AF.Exp, scale=-1.0, bias=ref)
            b.act(self.Ek[:, c, :], nbs, AF.Exp, bias=self.negc[:, c, 0:1])
            b.act(self.Eg[:, c, :], nbs, AF.Exp, scale=-1.0)
            b.act(self.Ed[:, c, :], nbs, AF.Exp, bias=self.negc[:, c, 1:2])
        b.stt(self.qt, self.Eq, 0.125, fm[:, 0:2, tk], ALU.mult, ALU.mult)
        b.tt('pool', self.kd, self.Ed, fm[:, 2:4, tk], ALU.mult)
        for h in range(4):
            c, r = h // 2, h % 2
            b.stt(self.kt[:, h, :], self.Ek[:, c, :], self.rm[:, r:r + 1], fm[:, 2 + c, tk], ALU.mult, ALU.mult)
            b.stt(self.qg[:, h, :], self.Eg[:, c, :], self.rm[:, 2 + r:3 + r], fm[:, c, tk], ALU.mult, ALU.mult)
        pA = nps()
        for h in range(4):
            c = h // 2
            b.mm(pA[:, h * 128:(h + 1) * 128], self.kt[:, h, :], self.qt[:, c, :])
        b.tt('dve', self.At, self.h4(pA), self.U_b.bc(1, [128, 4, 128]), ALU.mult)
        for c in range(2):
            b.tr(self.PTb[:, c * 128:(c + 1) * 128], self.kd[:, c, :], self.ident_b)
        b.cp('act', self.kdt, self.PTb[:, 0:256])
        pO = nps()
        for h in range(4):
            c = h // 2
            hs = slice(h * 128, (h + 1) * 128)
            b.mm(pO[:, hs], self.At[:, h, :], self.vt[st][:, hs], start=(h == 0), stop=False)
            b.mm(pO[:, hs], self.qg[:, h, :], self.Sb[:, c, :], start=False, stop=True)
        pS = nps()
        for h in range(4):
            c = h // 2
            b.mm(pS[:, h * 128:(h + 1) * 128], self.kdt[:, c * 128:(c + 1) * 128], self.vt[st][:, h * 128:(h + 1) * 128])
        for c in range(2):
            b.ts('dve', self.Sd, self.Sf[:, c, :], self.Eg[:, c, 127:128], ALU.mult)
            b.stt(self.Sd, pS[:, (2 * c) * 128:(2 * c + 1) * 128], self.rm[:, 0:1], self.Sd, ALU.mult, ALU.add)
            b.stt(self.Sf[:, c, :], pS[:, (2 * c + 1) * 128:(2 * c + 2) * 128], self.rm[:, 1:2], self.Sd, ALU.mult, ALU.add)
        b.cp('act', self.Sb, self.Sf)
        self.out_norm_heads(pO, st, self.zz[st])


class SSD(Mixer):
    def __init__(self, ctx):
        super().__init__(ctx)
        S = self.S
        self.cc = 0
        scr = self.scr
        A4 = lambda i: scr('A', i).re("p (h i) -> p h i", h=4)
        B4 = lambda i: scr('B', i).re("p (h i) -> p h i", h=4)
        self.fm = self.FM[:, 0:8, :]
        self.halo = [S.sb("ssd_h%d" % c, [128, 3], BF16) for c in range(8)]
        self.convw = S.sb("ssd_cw", [128, 8, 4], F32)
        self.convb = S.sb("ssd_cb", [128, 8], F32)
        f = lambda n, k: S.sb("ssd_" + n, [128, k], F32)
        self.nega, self.dtb, self.dsk, self.ea = f("nega", 8), f("dtb", 8), f("dsk", 8), f("ea", 8)
        self.onc = S.sb("ssd_onc", [128, 512], F32)
        self.dtr, self.apb, self.e, self.dtv, self.da, self.acl = f("dtr", 8), f("apb", 8), f("e", 8), f("dtv", 8), f("da", 8), f("acl", 16)
        self.X, self.EX, self.sdtd = f("X", 24), f("EX", 24), f("sdtd", 8)
        self.D8 = [A4(2), A4(3)]
        self.Bn = [A4(4), A4(5)]
        self.E = [A4(6), A4(7)]
        self.Mt = [B4(0), B4(1)]
        self.xdt = scr('B', 2)
        self.xdtd = scr('B', 3)
        self.xsk = scr('A', 8)
        self.Btok = scr('B', 4)[:, 0:256]
        self.yo = scr('A', 9)
        self.Hf = S.sb("ssd_Hf", [128, 512], F32)
        self.Hb = S.sb("ssd_Hb", [128, 512], BF16)
        self.ssq = f("ssq", 1)
        self.rstd = f("rstd", 1)
        self.junk = scr('B', 5)

    def layer_setup(self, l):
        b, dr, dt = self.b, self.dr, self.dt
        b.dma('pool', self.convw, dt(dr['conv_cT'][l]))
        b.dma('pool', self.convb, dt(dr['conv_bias_cT'][l]))
        self.bcast_load(self.ea, dr['a_log_c'][l:l + 1, :], 8)
        b.act(self.nega, self.ea, AF.Exp)
        b.ts('dve', self.nega, self.nega, -1.0, ALU.mult)
        self.bcast_load(self.dtb, dr['dt_bias_c'][l:l + 1, :], 8)
        self.bcast_load(self.dsk, dr['d_skip_c'][l:l + 1, :], 8)
        self.bcast_load(self.onc, dr['onorm_c'][l:l + 1, :], 512)
        b.memset('dve', self.Hf, 0.0)
        b.memset('dve', self.Hb, 0.0)
        for c in range(8):
            b.memset('pool', self.halo[c], 0.0)

    def macro(self, l, m, w_in_l):
        b, nps = self.b, self.nps
        for blk in range(2):
            wb = self.wload(w_in_l, OFF['ssd_x'] + blk * 512, 512)
            for cc in range(4):
                c = blk * 4 + cc
                p = nps()
                self.proj_feat(p, wb, cc * 128, 128)
                self.conv_chunk(p, self.convw, c, self.halo[c], self.fm[:, c, :], bias=self.convb[:, c:c + 1])
        wb3 = self.wload(w_in_l, OFF['ssd_dt'], 520)
        self.z_block(wb3, 8, None)
        for st in range(4):
            self.sub(st, wb3)
            self.emit_yT(st)

    def sub(self, st, wb3):
        b, nps = self.b, self.nps
        fm = self.fm
        tk = slice(st * 128, (st + 1) * 128)
        h8 = lambda t, k=64: t.re("p (h i) -> p h i", h=8)
        bc8 = lambda t, k: t.bc(2, [128, 8, k])
        pq = nps()
        self.proj_tok(pq, st, wb3, 0, 8)
        b.cp('dve', self.dtr, pq[:, 0:8])
        b.tt('dve', self.apb, self.dtr, self.dtb, ALU.add)
        b.act(self.e, self.apb, AF.Exp)
        b.act(self.dtv, self.e, AF.Ln, bias=1.0)
        b.tt('dve', self.da, self.dtv, self.nega, ALU.mult)
        pg = nps()
        b.mm(pg[:, 0:8], self.U_f, self.da)
        b.mm(pg[:, 8:16], self.ones_f, self.da)
        b.cp('dve', self.acl, pg[:, 0:16])
        acs, alast = self.acl[:, 0:8], self.acl[:, 8:16]
        b.cp('pool', self.X[:, 0:8], acs)
        b.tt('pool', self.X[:, 8:16], alast, acs, ALU.subtract)
        b.cp('pool', self.X[:, 16:24], alast)
        b.act(self.EX, self.X, AF.Exp)
        eacs, edst, dec = self.EX[:, 0:8], self.EX[:, 8:16], self.EX[:, 16:24]
        b.tt('dve', self.sdtd, self.dtv, edst, ALU.mult)
        bc4 = lambda t: t.bc(2, [128, 4, 128])
        f4 = lambda t: t.re("p h i -> p (h i)")
        pCB = nps()
        for g in range(2):
            b.mm(pCB[:, g * 128:(g + 1) * 128], fm[:, 4 + g, tk], fm[:, 6 + g, tk])
        for g in range(2):
            hs = slice(g * 4, g * 4 + 4)
            b.tt('dve', self.D8[g], self.ident_f.bc(1, [128, 4, 128]), bc4(acs[:, hs]), ALU.mult)
            b.ts('pool', self.Bn[g], bc4(acs[:, hs]), -1.0, ALU.mult)
            pX = nps()
            b.mm(pX, self.ones_f, f4(self.D8[g]), start=True, stop=False)
            b.mm(pX, self.ident_f, f4(self.Bn[g]), start=False, stop=True)
            b.act(f4(self.E[g]), pX, AF.Exp)
            b.asel(self.E[g], self.E[g], [[0, 4], [1, 128]], ALU.is_ge, 0.0, 0, -1)
            b.tt('dve', self.Mt[g], self.E[g], pCB[:, g * 128:(g + 1) * 128].bc(1, [128, 4, 128]), ALU.mult)
        PTb = self.PTb
        for c in range(4):
            b.tr(PTb[:, c * 128:(c + 1) * 128], fm[:, c, tk], self.ident_b)
        for g in range(2):
            b.tr(PTb[:, 512 + g * 128:512 + (g + 1) * 128], fm[:, 4 + g, tk], self.ident_b)
        xtok = h8(PTb[:, 0:512])
        b.tt('dve', h8(self.xdt), xtok, bc8(self.dtv, 64), ALU.mult)
        b.tt('dve', h8(self.xdtd), xtok, bc8(self.sdtd, 64), ALU.mult)
        b.tt('dve', h8(self.xsk), xtok, bc8(self.dsk, 64), ALU.mult)
        b.cp('act', self.Btok, PTb[:, 512:768])
        pY = nps()
        for h in range(8):
            b.mm(pY[:, h * 64:(h + 1) * 64], self.Mt[h // 4][:, h % 4, :], self.xdt[:, h * 64:(h + 1) * 64])
        pF = nps()
        for g in range(2):
            b.mm(pF[:, g * 256:(g + 1) * 256], fm[:, 6 + g, tk], self.Hb[:, g * 256:(g + 1) * 256])
        b.tt('dve', h8(self.yo), h8(pF), bc8(eacs, 64), ALU.mult)
        b.tt('pool', self.yo, self.yo, self.xsk, ALU.add)
        b.tt('dve', self.yo, self.yo, pY, ALU.add)
        pH = nps()
        for g in range(2):
            b.mm(pH[:, g * 256:(g + 1) * 256], self.Btok[:, g * 128:(g + 1) * 128], self.xdtd[:, g * 256:(g + 1) * 256])
        b.tt('dve', h8(self.Hf), h8(self.Hf), bc8(dec, 64), ALU.mult)
        b.tt('dve', self.Hf, self.Hf, pH, ALU.add)
        b.cp('act', self.Hb, self.Hf)
        b.tt('dve', self.yo, self.yo, self.zz[st], ALU.mult)
        b.act(self.junk, self.yo, AF.Square, accum=self.ssq)
        self.rms_rstd(self.rstd, self.ssq, 512)
        b.stt(self.ybr[st], self.yo, self.rstd[:, 0:1], self.onc, ALU.mult, ALU.mult)


class NSA(Mixer):
    def __init__(self, ctx):
        super().__init__(ctx)
        S, scr, S_len, NT = self.S, self.scr, self.S_len, self.NT
        self.kS = [S.sb("nsa_kS%d" % g, [128, S_len], BF16) for g in range(2)]
        self.kW = [S.sb("nsa_kW%d" % g, [128, 8 * 128], BF16) for g in range(2)]
        self.vS = [S.sb("nsa_vS%d" % g, [128, NT, 66], BF16) for g in range(2)]
        self.vW = [S.sb("nsa_vW%d" % g, [128, 8, 66], BF16) for g in range(2)]
        self.kC = [S.sb("nsa_kC%d" % g, [128, 256], BF16) for g in range(2)]
        self.vcT = [S.sb("nsa_vcT%d" % g, [128, 256], BF16) for g in range(2)]
        self.vC = [S.sb("nsa_vC%d" % g, [128, 2, 130], BF16) for g in range(2)]
        self.EK = S.sb("nsa_EK", [128, S_len], BF16)
        self.EKc = S.sb("nsa_EKc", [128, 256], BF16)
        self.qm = self.FM[:, 0:8, :]
        self.qA = self.FM[:, 8:16, :]
        self.qS = S.sb("nsa_qS", [128, 4, 128], BF16)
        self.rawc = [S.sb("nsa_rawc%d" % g, [128, 528], BF16) for g in range(2)]
        self.W1 = S.sb("nsa_W1", [128, 32, 128], BF16)
        self.W2k = S.sb("nsa_W2k", [128, 128], BF16)
        self.W2v = S.sb("nsa_W2v", [128, 128], BF16)
        self.posT = S.sb("nsa_posT", [128, 32], BF16)
        self.cpos = S.sb("nsa_cpos", [128, 1], F32)
        self.wsel = S.sb("nsa_wsel", [128, 8, 128], BF16)
        self.gsig = [S.sb("nsa_gs%d" % i, [128, 24], F32) for i in range(4)]
        self.rmq = S.sb("nsa_rmq", [128, 2], F32)
        self.hid = S.sb("nsa_hid", [128, 32], BF16)
        f = lambda n, k: S.sb("nsa_" + n, [128, k], F32)
        self.dall = S.sb("nsa_dall", [128, 3, 4], F32)
        self.rall = S.sb("nsa_rall", [128, 3, 4], F32)
        self.coef = S.sb("nsa_coef", [128, 3, 4], F32)
        self.imp, self.imp2, self.m8a, self.m8b, self.thr, self.selb = f("imp", 64), f("imp2", 64), f("m8a", 8), f("m8b", 8), f("thr", 1), f("selb", 64)
        self.selbb = S.sb("nsa_selbb", [128, 128], BF16)
        self.cur, self.val, self.fz = f("cur", 1), f("val", 64), f("fz", 64)
        self.Pb = [scr('B', i).re("p (h i) -> p h i", h=4) for i in range(3)]
        self.pi = 0
        self.on = scr('A', 2).re("p (h d) -> p h d", h=8)
        self.tmp4 = scr('A', 3)[:, 0:256].re("p (h d) -> p h d", h=4)
        self.tmp5 = scr('A', 4)[:, 0:256].re("p (h d) -> p h d", h=4)

    def layer_setup(self, l):
        b, dr, dt, nps = self.b, self.dr, self.dt, self.nps
        if NSTAGE < 0:
            return
        if l == 0:
            for g in range(2):
                for t in (self.kS[g], self.kW[g], self.vS[g], self.vW[g], self.kC[g], self.vcT[g], self.vC[g]):
                    b.memset('pool', t, 0.0)
                b.memset('pool', self.vS[g][:, :, 64:65], 1.0)
                b.memset('pool', self.vW[g][:, :, 64:65], 1.0)
                b.memset('pool', self.vC[g][:, :, 64:65], 1.0)
                b.memset('pool', self.vC[g][0:1, 0, 64:65], 0.0)
                for kt in range(2):
                    b.dma('pool', self.vC[g][:, kt, 65:129], dt(dr['overlap'][kt * 128:(kt + 1) * 128, :]))
            b.memset('pool', self.EK, 0.0)
            for c0 in range(0, self.S_len, 1024):
                c1 = min(c0 + 1024, self.S_len)
                b.dma('pool', self.EK[0:64, c0:c1], dt(dr['esel'][:, c0:c1]))
                b.dma('pool', self.EK[64:68, c0:c1], dt(dr['kaug'][:, c0:c1]))
            b.memset('pool', self.EKc, 0.0)
            b.dma('pool', self.EKc[64:68, :], dt(dr['kaugc']))
            b.memset('pool', self.qS, 0.0)
            b.memset('pool', self.selbb, 0.0)
            b.memset('pool', self.rmq, 0.0)
            b.memset('pool', self.rmq[0:64, 0:1], 0.125)
            b.memset('pool', self.rmq[64:128, 1:2], 0.125)
        b.memset('pool', self.W1, 0.0)
        b.dma('pool', self.W1[0:64, :, 0:64], dt(dr['w_ck1'][l].rearrange("(p d) o -> d p o", d=64)))
        b.dma('pool', self.W1[64:128, :, 64:128], dt(dr['w_cv1'][l].rearrange("(p d) o -> d p o", d=64)))
        b.memset('pool', self.W2k, 0.0)
        b.dma('pool', self.W2k[0:64, 0:64], dt(dr['w_ck2'][l]))
        b.dma('pool', self.W2k[0:64, 64:128], dt(dr['w_ck2'][l]))
        b.memset('pool', self.W2v, 0.0)
        b.dma('pool', self.W2v[64:128, 0:64], dt(dr['w_cv2'][l]))
        b.dma('pool', self.posT[0:64, :], dt(dr['cmp_pos_kT'][l]))
        b.dma('pool', self.posT[64:128, :], dt(dr['cmp_pos_vT'][l]))
        pc = nps()
        for p in range(32):
            b.mm(pc[:, 0:1], self.W1[:, p, :], self.posT[:, p:p + 1], start=p == 0, stop=p == 31)
        b.cp('dve', self.cpos, pc[:, 0:1])
        for g in range(2):
            b.memset('pool', self.rawc[g], 0.0)

    def macro(self, l, m, w_in_l):
        b, nps = self.b, self.nps
        if NSTAGE < 1:
            for st in range(4):
                self.emit_yT(st)
            return
        n0 = 4 * m
        wsel = self.wsel
        wb = self.wload(w_in_l, OFF['nsa_q'], 512)
        for c in range(4):
            p = nps()
            self.proj_feat(p, wb, c * 128, 128)
            b.ts('dve', self.qm[:, 2 * c, :], p, self.rmq[:, 0:1], ALU.mult)
            b.act(self.qm[:, 2 * c + 1, :], p, AF.Copy, scale=self.rmq[:, 1:2])
        def bail():
            for st in range(4):
                self.emit_yT(st)
        if NSTAGE < 1.15:
            return bail()
        b.memset('pool', self.qA, 0.0)
        b.dma('pool', self.qA[64:68, :, :], self.dt(self.dr['qaug'][:, :, m * MT:(m + 1) * MT]))
        if NSTAGE < 1.25:
            return bail()
        wb = self.wload(w_in_l, OFF['nsa_kc'], 512)
        for g in range(2):
            b.cp('dve', wsel[:, :, 0:64], wb[:, :, g * 64:(g + 1) * 64])
            b.cp('dve', wsel[:, :, 64:128], wb[:, :, 128 + g * 64:128 + (g + 1) * 64])
            p = nps()
            self.proj_feat(p, wsel, 0, 128)
            b.cp('dve', self.rawc[g][:, 0:16], self.rawc[g][:, 512:528])
            b.cp('act', self.rawc[g][:, 16:528], p)
        if NSTAGE < 1.27:
            return bail()
        for g in range(2):
            b.cp('dve', wsel[:, :, 0:64], wb[:, :, 256 + g * 64:256 + (g + 1) * 64])
            b.cp('dve', wsel[:, :, 64:128], wb[:, :, 256 + g * 64:256 + (g + 1) * 64])
            p = nps()
            self.proj_feat(p, wsel, 0, 128)
            b.cp('act', self.kS[g][:, m * MT:(m + 1) * MT], p)
        if NSTAGE < 1.29:
            return bail()
        for st in range(4):
            p = nps()
            self.proj_tok(p, st, wb, 384, 128)
            for g in range(2 if NSTAGE >= 1.2915 else 0):
                b.cp('dve', self.vS[g][:, n0 + st, 0:64], p[:, g * 64:(g + 1) * 64])
        if NSTAGE < 1.35:
            return bail()
        wb = self.wload(w_in_l, OFF['nsa_kw'], 280)
        for g in range(2):
            b.cp('dve', wsel[:, :, 0:64], wb[:, :, g * 64:(g + 1) * 64])
            b.cp('dve', wsel[:, :, 64:128], wb[:, :, g * 64:(g + 1) * 64])
            p = nps()
            self.proj_feat(p, wsel, 0, 128)
            s0 = (n0 % 8) * 128
            b.cp('act', self.kW[g][:, s0:s0 + 512], p)
        for st in range(4):
            p = nps()
            self.proj_tok(p, st, wb, 128, 152)
            for g in range(2):
                b.cp('dve', self.vW[g][:, (n0 + st) % 8, 0:64], p[:, g * 64:(g + 1) * 64])
            b.act(self.gsig[st], p[:, 128:152], AF.Sigmoid)
        if NSTAGE < 1.45:
            return bail()
        wb = self.wload(w_in_l, OFF['nsa_z'], 512)
        self.z_block(wb, 0, None)
        for g in range(2 if NSTAGE >= 2 else 0):
            pC = nps()
            for p_ in range(32):
                b.mm(pC[:, 0:32], self.W1[:, p_, :], self.rawc[g][:, p_:p_ + 497:16], start=p_ == 0, stop=p_ == 31)
            b.act(self.hid, pC[:, 0:32], AF.Silu, bias=self.cpos[:, 0:1])
            pK = nps()
            b.mm(pK[:, 0:32], self.W2k, self.hid)
            b.mm(pK[:, 32:64], self.W2v, self.hid)
            c0 = 32 * m
            cnt = 32
            b.cp('dve', self.kC[g][:, c0:c0 + cnt], pK[:, 0:32])
            b.cp('dve', self.vcT[g][:, c0:c0 + cnt], pK[:, 32:64])
            if m == 0:
                b.memset('pool', self.kC[g][:, 0:1], 0.0)
                b.memset('pool', self.vcT[g][:, 0:1], 0.0)
            for kt in sorted({c0 // 128, (c0 + cnt - 1) // 128}):
                b.tr(self.PTb[:, 0:128], self.vcT[g][:, kt * 128:(kt + 1) * 128], self.ident_b)
                b.cp('dve', self.vC[g][:, kt, 0:64], self.PTb[:, 0:64])
        for st in range(4):
            if NSTAGE >= 3:
                self.sub(m, st)
            self.emit_yT(st)

    def pbuf(self):
        self.pi += 1
        return self.Pb[self.pi % 3]

    def sub(self, m, st):
        b, nps, PS = self.b, self.nps, self.PS
        n = 4 * m + st
        tk = slice(st * 128, (st + 1) * 128)
        f4 = lambda t: t.re("p h i -> p (h i)")
        pOc, pOs, pOw = [PS[0], PS[1]], PS[2], PS[3]
        on = self.on
        causal = lambda t: b.asel(t, t, [[0, 4], [1, 128]], ALU.is_ge, 0.0, 0, -1)
        for g in range(2):
            qrhs = self.qm[:, 4 * g:4 * g + 4, tk]
            arhs = self.qA[:, 4 * g:4 * g + 4, tk]
            kts = [kt for kt in (0, 1) if n >= 16 * kt]
            for ki, kt in enumerate(kts):
                ks_ = slice(kt * 128, (kt + 1) * 128)
                pS = nps(4)
                b.mm(pS, self.kC[g][:, ks_], qrhs, start=True, stop=False)
                b.mm(pS, self.EKc[:, ks_], arhs, start=False, stop=True)
                Pt = self.pbuf()
                b.act(f4(Pt), pS, AF.Exp)
                if n < 16 * kt + 16:
                    b.asel(Pt, Pt, [[0, 4], [1, 128]], ALU.is_ge, 0.0, 128 * n - 2048 * kt - 15, -16)
                for hh in range(4):
                    col = (hh % 2) * 129
                    b.mm(pOc[hh // 2][:, col:col + 129], Pt[:, hh, :], self.vC[g][:, kt, 0:129],
                         start=(ki == 0 and hh % 2 == 0), stop=(ki == len(kts) - 1))
            for bnk in range(2):
                v = pOc[bnk][:, 0:258].re("p (h c) -> p h c", h=2)
                b.cp('dve', self.dall[:, 0, 2 * bnk:2 * bnk + 2], v[:, :, 64])
            rcc = self.rall[:, 0, :]
            b.ts('dve', rcc, self.dall[:, 0, :], 1e-30, ALU.max)
            b.recip(rcc, rcc)
            imp = self.imp
            b.ts('dve', imp, pOc[0][:, 65:129], rcc[:, 0:1], ALU.mult)
            b.stt(imp, pOc[0][:, 194:258], rcc[:, 1:2], imp, ALU.mult, ALU.add)
            b.stt(imp, pOc[1][:, 65:129], rcc[:, 2:3], imp, ALU.mult, ALU.add)
            b.stt(imp, pOc[1][:, 194:258], rcc[:, 3:4], imp, ALU.mult, ALU.add)
            cur, val, fz = self.cur, self.val, self.fz
            b.ts('dve', cur, self.half01, float(2 * n), ALU.add)
            b.ts('dve', val, self.jidx, cur[:, 0:1], ALU.is_le)
            b.tt('dve', imp, imp, val, ALU.mult)
            b.ts('dve', val, val, -1.0, ALU.add)
            b.tt('dve', imp, imp, val, ALU.add)
            b.ts('dve', fz, self.jidx, cur[:, 0:1], ALU.is_equal)
            b.ts('dve', val, self.jidx, 1.0, ALU.add, cur[:, 0:1], ALU.is_equal)
            b.tt('dve', fz, fz, val, ALU.add)
            b.tt('dve', fz, fz, self.e0, ALU.add)
            b.stt(imp, fz, 1.0e4, imp, ALU.mult, ALU.max)
            b.max8(self.m8a, imp)
            b.mrep(self.imp2, self.m8a, imp, -2.0)
            b.max8(self.m8b, self.imp2)
            b.ts('dve', self.thr, self.m8b[:, 7:8], 0.0, ALU.max)
            b.ts('dve', self.selb, imp, self.thr[:, 0:1], ALU.is_ge, 30000.0, ALU.mult)
            b.ts('dve', self.selbb[:, 0:64], self.selb, -30000.0, ALU.add)
            b.tr(self.PTb[:, 0:128], self.selbb, self.ident_b)
            b.cp('dve', self.qS[0:64], self.PTb[0:64, 0:128].bc(1, [64, 4, 128]))
            b.cp('act', self.qS[64:68], self.qA[64:68, 4 * g:4 * g + 4, tk])
            def scores(kind, kt):
                ks_ = slice(kt * 128, (kt + 1) * 128)
                pS = nps(4)
                if kind == 's':
                    b.mm(pS, self.kS[g][:, ks_], qrhs, start=True, stop=False)
                    b.mm(pS, self.EK[:, ks_], f4(self.qS), start=False, stop=True)
                else:
                    sl = kt % 8
                    b.mm(pS, self.kW[g][:, sl * 128:(sl + 1) * 128], qrhs, start=True, stop=False)
                    b.mm(pS, self.EK[:, ks_], arhs, start=False, stop=True)
                Pt = self.pbuf()
                b.act(f4(Pt), pS, AF.Exp)
                if kt == n:
                    causal(Pt)
                if kind == 'w' and kt == n - 4:
                    b.asel(Pt, Pt, [[0, 4], [-1, 128]], ALU.is_ge, 0.0, -1, 1)
                return Pt

            def pv(kind, kt, Pt, first, last):
                for hh in range(4):
                    if kind == 's':
                        b.mm(pOs[:, hh * 65:(hh + 1) * 65], Pt[:, hh, :], self.vS[g][:, kt, 0:65],
                             start=(first and hh == 0), stop=last)
                    else:
                        b.mm(pOw[:, hh * 65:(hh + 1) * 65], Pt[:, hh, :], self.vW[g][:, kt % 8, 0:65],
                             start=(first and hh == 0), stop=last)

            wk0 = max(0, n - 4)
            items = [('s', kt, kt == 0, kt == n) for kt in range(n + 1)] + \
                    [('w', kt, kt == wk0, kt == n) for kt in range(wk0, n + 1)]
            prev = None
            for it in items:
                Pt = scores(it[0], it[1])
                if prev is not None:
                    pv(prev[0][0], prev[0][1], prev[1], prev[0][2], prev[0][3])
                prev = (it, Pt)
            pv(prev[0][0], prev[0][1], prev[1], prev[0][2], prev[0][3])
            vs_ = pOs[:, 0:260].re("p (h c) -> p h c", h=4)
            vw_ = pOw[:, 0:260].re("p (h c) -> p h c", h=4)
            b.cp('dve', self.dall[:, 1, :], vs_[:, :, 64])
            b.cp('dve', self.dall[:, 2, :], vw_[:, :, 64])
            b.ts('dve', self.rall[:, 1:3, :], self.dall[:, 1:3, :], 1e-30, ALU.max)
            b.recip(self.rall[:, 1:3, :], self.rall[:, 1:3, :])
            gv = self.gsig[st][:, 12 * g:12 * g + 12].re("p (h b) -> p b h", b=3)
            b.tt('dve', self.coef, self.rall, gv, ALU.mult)
            for bnk in range(2):
                v = pOc[bnk][:, 0:258].re("p (h c) -> p h c", h=2)
                b.tt('dve', on[:, 4 * g + 2 * bnk:4 * g + 2 * bnk + 2, :], v[:, :, 0:64],
                     self.coef[:, 0, 2 * bnk:2 * bnk + 2].bc(2, [128, 2, 64]), ALU.mult)
            b.tt('dve', self.tmp4, vs_[:, :, 0:64], self.coef[:, 1, :].bc(2, [128, 4, 64]), ALU.mult)
            b.tt('pool', on[:, 4 * g:4 * g + 4, :], on[:, 4 * g:4 * g + 4, :], self.tmp4, ALU.add)
            b.tt('dve', self.tmp5, vw_[:, :, 0:64], self.coef[:, 2, :].bc(2, [128, 4, 64]), ALU.mult)
            b.tt('pool', on[:, 4 * g:4 * g + 4, :], on[:, 4 * g:4 * g + 4, :], self.tmp5, ALU.add)
        b.tt('dve', self.ybr[st], on.re("p h d -> p (h d)"), self.zz[st], ALU.mult)


def kernel(**inputs):
    depth, S_len, n_cores = 4, 4096, 8
    x = np.asarray(inputs['x'], dtype=np.float32)
    nc, _ = build(S_len, depth)
    pin = prep_inputs(inputs, depth)
    hc = host_constants(S_len)
    in_maps = []
    for i in range(n_cores):
        d = dict(pin)
        d.update(hc)
        d['x'] = np.ascontiguousarray(x[i])
        in_maps.append(d)
    res = run_bass_kernel_spmd(nc, in_maps, core_ids=list(range(n_cores)))
    return np.stack([np.asarray(r['y'], dtype=np.float32) for r in res.results], axis=0)
```

```python
from contextlib import ExitStack
import os
NSTAGE = float(os.environ.get('NSTAGE', '99'))
SENG = os.environ.get('SENG', '').split(',')
import numpy as np
import concourse.bass as bass
import concourse.mybir as mybir
from concourse.bass_utils import run_bass_kernel_spmd

F32 = mybir.dt.float32
BF16 = mybir.dt.bfloat16
I32 = mybir.dt.int32
ALU = mybir.AluOpType
AF = mybir.ActivationFunctionType
AX = mybir.AxisListType

ENGS = ('pe', 'act', 'dve', 'pool', 'sp')
NDSEM = 12


class Tile:
    def __init__(self, name, ap, rid):
        self.name, self.ap, self.rid = name, ap, rid

    def __getitem__(self, k):
        return Tile(self.name, self.ap[k], self.rid)

    def re(self, s, **kw):
        return Tile(self.name, self.ap.rearrange(s, **kw), self.rid)

    def bc(self, axis, shape):
        return Tile(self.name, self.ap.unsqueeze(axis).to_broadcast(list(shape)), self.rid)

    def v(self, ap):
        return Tile(self.name, ap, self.rid)


class Op:
    __slots__ = ('eng', 'fn', 'deps', 'signal', 'count', 'dma', 'dsem', 'dcount', 'idx', 'cost', 'lat', 'seq', 'nrem', 'users', 'rt', 'fin')


class Sched:
    def __init__(self, nc):
        self.nc = nc
        self.es = ExitStack()
        self.ops = {e: [] for e in ENGS}
        self.lastw = {}
        self.readers = {}
        self.ndma = {e: 0 for e in ENGS}
        self.dma_ops = {e: [] for e in ENGS}
        self.nres = 0

    def sb(self, name, shape, dtype):
        t = self.es.enter_context(self.nc.sbuf_tensor("sb_" + name, list(shape), dtype))
        self.nres += 1
        return Tile(name, t[:] if hasattr(t, '__getitem__') else t, self.nres)

    def ps(self, name, shape, dtype):
        t = self.es.enter_context(self.nc.psum_tensor("pm_" + name, list(shape), dtype))
        self.nres += 1
        return Tile(name, t[:], self.nres)

    def res(self, name):
        self.nres += 1
        return Tile(name, None, self.nres)

    def view(self, tile, ap, own=False):
        if own:
            self.nres += 1
            return Tile(tile.name, ap, self.nres)
        return Tile(tile.name, ap, tile.rid)

    def _deps(self, reads, writes):
        deps = []
        for r in reads:
            w = self.lastw.get(r.rid)
            if w is not None:
                deps.append(w)
        for r in writes:
            w = self.lastw.get(r.rid)
            if w is not None:
                deps.append(w)
            deps.extend(self.readers.get(r.rid, ()))
        return deps

    def _record(self, op, reads, writes):
        for r in writes:
            self.lastw[r.rid] = op
            self.readers[r.rid] = []
        for r in reads:
            if self.lastw.get(r.rid) is op:
                continue
            self.readers.setdefault(r.rid, []).append(op)

    def op(self, eng, fn, reads=(), writes=(), cost=300.0):
        o = Op()
        o.eng, o.fn, o.dma, o.signal, o.count = eng, fn, False, False, 0
        o.cost = o.lat = cost
        self.nseq = getattr(self, 'nseq', 0) + 1
        o.seq = self.nseq
        o.deps = self._deps(reads, writes)
        o.idx = len(self.ops[eng])
        self.ops[eng].append(o)
        self._record(o, reads, writes)
        return o

    def dma(self, eng, out_ap, in_ap, reads=(), writes=(), out=False, **kw):
        o = Op()
        o.eng, o.dma, o.signal, o.count = eng, True, True, 0
        oa = out_ap.ap if isinstance(out_ap, Tile) else out_ap
        ia = in_ap.ap if isinstance(in_ap, Tile) else in_ap
        o.fn = lambda e: e.dma_start(out=oa, in_=ia, **kw)
        o.deps = self._deps(reads, writes)
        self.ndma[eng] += 1
        nbytes = 1
        for d_ in oa.shape:
            nbytes *= d_
        nbytes *= 2 if oa.dtype == BF16 else 4
        o.cost = 150.0 if eng == 'sp' else 1500.0
        o.lat = 2500.0 + nbytes / 60.0
        self.nseq = getattr(self, 'nseq', 0) + 1
        o.seq = self.nseq
        o.idx = len(self.ops[eng])
        self.ops[eng].append(o)
        self._record(o, reads, writes)
        if out:
            self.out_dmas = getattr(self, 'out_dmas', []) + [o]
        return o

    def schedule(self, window=int(os.environ.get("SWIN", "40"))):
        allops = [o for e in ENGS for o in self.ops[e]]
        for o in allops:
            o.users = []
            o.rt = 0.0
            o.fin = None
        for o in allops:
            ds = set(id(d) for d in o.deps)
            o.deps = [d for d in {id(d): d for d in o.deps}.values()]
            o.nrem = len(o.deps)
            for d in o.deps:
                d.users.append(o)
        pend = {e: list(self.ops[e]) for e in ENGS}
        new = {e: [] for e in ENGS}
        free_t = {e: 0.0 for e in ENGS}
        remaining = len(allops)
        while remaining:
            best = None
            bkey = None
            for e in ENGS:
                pe_ = pend[e]
                ft = free_t[e]
                for k in range(min(window if e in SENG else 1, len(pe_))):
                    o = pe_[k]
                    if o.nrem:
                        continue
                    st = o.rt if o.rt > ft else ft
                    key = (st, o.seq)
                    if bkey is None or key < bkey:
                        bkey, best, bk = key, o, k
                    if st <= ft:
                        break
            o = best
            e = o.eng
            pend[e].pop(bk) if pend[e][bk] is o else pend[e].remove(o)
            st = bkey[0]
            o.fin = st + o.lat
            free_t[e] = st + o.cost
            new[e].append(o)
            for u in o.users:
                u.nrem -= 1
                if o.fin > u.rt:
                    u.rt = o.fin
            remaining -= 1
        self.ops = new
        self.sim_time = max(free_t.values())

    def emit(self):
        nc = self.nc
        if SENG != ['']:
            self.schedule()
        for e in ENGS:
            i = 0
            prev = []
            for o in self.ops[e]:
                if o.dma:
                    o.dsem = (e, i % NDSEM)
                    o.dcount = 16 * (i // NDSEM + 1)
                    if i >= NDSEM:
                        o.deps.append(prev[i - NDSEM])
                    prev.append(o)
                    i += 1
        fin = Op()
        fin.eng, fin.dma, fin.signal, fin.count, fin.fn = 'sp', False, False, 0, None
        fin.deps = list(getattr(self, 'out_dmas', []))
        self.ops['sp'].append(fin)
        for e in ENGS:
            for o in self.ops[e]:
                for d in o.deps:
                    if not d.dma:
                        if d.eng == 'pe' and o.eng == 'pe':
                            continue
                        d.signal = True
        for e in ENGS:
            c = 0
            for o in self.ops[e]:
                if o.signal and not o.dma:
                    c += 1
                    o.count = c
        sems = {e: self.es.enter_context(nc.semaphore("s_" + e)) for e in ENGS}
        dsems = {}
        for e in ENGS:
            if self.ndma[e]:
                for k in range(min(NDSEM, self.ndma[e])):
                    dsems[(e, k)] = self.es.enter_context(nc.semaphore("d_%s%d" % (e, k)))
        self.stats = {}

        def run(ename, eng):
            known = {}
            nw = 0
            for o in self.ops[ename]:
                need = {}
                for d in o.deps:
                    if d.dma:
                        key, val = ('d',) + d.dsem, d.dcount
                    else:
                        if d.eng == 'pe' and ename == 'pe':
                            continue
                        key, val = ('c', d.eng), d.count
                    if known.get(key, 0) >= val:
                        continue
                    if need.get(key, 0) < val:
                        need[key] = val
                for key, val in need.items():
                    s = sems[key[1]] if key[0] == 'c' else dsems[(key[1], key[2])]
                    eng.wait_ge(s, val)
                    known[key] = val
                    nw += 1
                if o.fn is None:
                    continue
                ins = o.fn(eng)
                if o.dma:
                    ins.then_inc(dsems[o.dsem], 16)
                elif o.signal:
                    ins.then_inc(sems[ename], 1)
            self.stats[ename] = (len(self.ops[ename]), nw)

        with nc.Block() as block:
            @block.sync
            def _(eng):
                run('sp', eng)

            @block.scalar
            def _(eng):
                run('act', eng)

            @block.vector
            def _(eng):
                run('dve', eng)

            @block.gpsimd
            def _(eng):
                run('pool', eng)

            @block.tensor
            def _(eng):
                run('pe', eng)
        self.es.close()


def _fsz(ap):
    p = 1
    for d in ap.shape[1:]:
        p *= d
    return p


class B:
    def __init__(self, S):
        self.S = S

    @staticmethod
    def _t(xs):
        return [x for x in xs if isinstance(x, Tile)]

    @staticmethod
    def _a(x):
        return x.ap if isinstance(x, Tile) else x

    def mm(self, out, lhsT, rhs, start=True, stop=True):
        o, l, r = out.ap, lhsT.ap, rhs.ap
        f32 = 4.0 if l.dtype == F32 else 1.0
        self.S.op('pe', lambda e: e.matmul(o, l, r, start=start, stop=stop, skip_group_check=True),
                  reads=[lhsT, rhs], writes=[out], cost=30.0 + f32 * (max(_fsz(r), 32) + 100) / 2.4)

    def tr(self, out, in_, ident):
        o, i, d = out.ap, in_.ap, ident.ap
        self.S.op('pe', lambda e: e.transpose(o, i, d), reads=[in_, ident], writes=[out], cost=120.0)

    def act(self, out, in_, func, bias=None, scale=None, accum=None, eng='act'):
        o, i = out.ap, in_.ap
        kw = {}
        if bias is not None:
            kw['bias'] = self._a(bias)
        if scale is not None:
            kw['scale'] = self._a(scale)
        if accum is not None:
            kw['accum_out'] = accum.ap
        w = [out] + ([accum] if accum is not None else [])
        self.S.op('act', lambda e: e.activation(o, i, func, **kw),
                  reads=self._t([in_, bias, scale]), writes=w, cost=230.0 + _fsz(o) / 1.2)

    def tt(self, eng, out, a, b, op):
        o, x, y = out.ap, a.ap, b.ap
        self.S.op(eng, lambda e: e.tensor_tensor(o, x, y, op), reads=[a, b], writes=[out], cost=(120.0 + _fsz(o) / 0.96) if eng == 'dve' else (250.0 + _fsz(o) / 0.6))

    def ts(self, eng, out, a, s1, op0, s2=None, op1=None):
        o, x = out.ap, a.ap
        a1, a2 = self._a(s1), self._a(s2)
        if op1 is None:
            self.S.op(eng, lambda e: e.tensor_scalar(o, x, a1, None, op0), reads=self._t([a, s1]), writes=[out], cost=(120.0 + _fsz(o) / 1.5) if eng == 'dve' else (250.0 + _fsz(o) / 0.6))
        else:
            self.S.op(eng, lambda e: e.tensor_scalar(o, x, a1, a2, op0, op1), reads=self._t([a, s1, s2]), writes=[out], cost=(120.0 + _fsz(o) / 1.5) if eng == 'dve' else (250.0 + _fsz(o) / 0.6))

    def stt(self, out, a, s, b, op0, op1):
        o, x, y, sc = out.ap, a.ap, b.ap, self._a(s)
        self.S.op('dve', lambda e: e.scalar_tensor_tensor(o, x, sc, y, op0, op1), reads=self._t([a, s, b]), writes=[out], cost=120.0 + _fsz(o) / 0.96)

    def cp(self, eng, out, in_):
        o, i = out.ap, in_.ap
        if eng == 'act':
            self.S.op('act', lambda e: e.copy(o, i), reads=[in_], writes=[out], cost=230.0 + _fsz(o) / 1.2)
        else:
            self.S.op(eng, lambda e: e.tensor_copy(o, i), reads=[in_], writes=[out], cost=(120.0 + _fsz(o) / 1.5) if eng == 'dve' else (250.0 + _fsz(o) / 0.6))

    def red(self, out, in_, op=None, axis=None):
        o, i = out.ap, in_.ap
        op = op or ALU.add
        axis = axis or AX.X
        self.S.op('dve', lambda e: e.tensor_reduce(o, i, axis, op), reads=[in_], writes=[out], cost=120.0 + _fsz(i) / 0.96)

    def memset(self, eng, out, val):
        o = out.ap
        self.S.op(eng, lambda e: e.memset(o, val), reads=[], writes=[out], cost=150.0 + _fsz(o) / 0.96)

    def asel(self, out, in_, pattern, cmp, fill, base, cm):
        o, i = out.ap, in_.ap
        def fn(e):
            try:
                return e.affine_select(o, i, pattern, cmp, fill, base=base, channel_multiplier=cm)
            except Exception:
                print("ASEL FAIL", pattern, base, cm, fill, o)
                raise
        self.S.op('pool', fn, reads=[in_], writes=[out], cost=300.0 + _fsz(o) / 0.9)

    def scan(self, out, d0, d1, init, op0, op1):
        o, x, y, ii = out.ap, d0.ap, d1.ap, self._a(init)
        self.S.op('dve', lambda e: e.tensor_tensor_scan(o, x, y, ii, op0, op1), reads=self._t([d0, d1, init]), writes=[out], cost=120.0 + _fsz(o) * 2 / 0.96)

    def recip(self, out, in_):
        o, i = out.ap, in_.ap
        self.S.op('dve', lambda e: e.reciprocal(o, i), reads=[in_], writes=[out])

    def max8(self, out, in_):
        o, i = out.ap, in_.ap
        self.S.op('dve', lambda e: e.max(o, i), reads=[in_], writes=[out])

    def mrep(self, out, rep, vals, imm):
        o, r, v = out.ap, rep.ap, vals.ap
        self.S.op('dve', lambda e: e.match_replace(o, r, v, imm), reads=[rep, vals], writes=[out])

    def dma(self, eng, out, in_, out_final=False, **kw):
        self.S.dma(eng, out, in_, reads=self._t([in_]), writes=self._t([out]), out=out_final, **kw)


D_MODEL = 1024
NORM_EPS = 1e-6
IN_SPLITS = (
    ('gdn_q', 512), ('gdn_k', 512), ('gdn_v', 512), ('gdn_beta', 4), ('gdn_a', 4), ('gdn_z', 512),
    ('gla_q', 256), ('gla_k', 256), ('gla_v', 512), ('gla_gk', 16), ('gla_z', 512),
    ('ssd_x', 512), ('ssd_b', 256), ('ssd_c', 256), ('ssd_dt', 8), ('ssd_z', 512),
    ('nsa_q', 512), ('nsa_kc', 128), ('nsa_vc', 128), ('nsa_ks', 128), ('nsa_vs', 128),
    ('nsa_kw', 128), ('nsa_vw', 128), ('nsa_gate', 24), ('nsa_z', 512),
    ('merge_gate', 4096),
)
OFF = {}
_s = 0
for _n, _w in IN_SPLITS:
    OFF[_n] = _s
    _s += _w
D_IN = _s
MT = 512
NEG = -30000.0


def host_constants(S_len):
    c = {}
    ident = np.eye(128, dtype=np.float32)
    U = np.triu(np.ones((128, 128), np.float32))
    ones = np.ones((128, 128), np.float32)
    jidx = np.tile(np.arange(64, dtype=np.float32)[None, :], (128, 1))
    e0 = np.zeros((128, 64), np.float32)
    e0[:, 0] = 1.0
    half = (np.arange(128) >= 64).astype(np.float32)[:, None]
    c['cst'] = np.concatenate([ident, U, ones, jidx, e0, half], axis=1)
    slopes = 2.0 ** (-np.arange(1, 9, dtype=np.float64))
    t = np.arange(S_len)
    qaug = np.zeros((4, 8, S_len), np.float32)
    for h in range(8):
        qaug[0, h] = slopes[h] * 64
        qaug[1, h] = slopes[h]
        qaug[2, h] = -slopes[h] * 64 * (t // 64)
        qaug[3, h] = -slopes[h] * (t % 64)
    c['qaug'] = qaug
    kaug = np.stack([t // 64, t % 64, np.ones_like(t), np.ones_like(t)]).astype(np.float32)
    c['kaug'] = kaug
    ncp = 256
    cp = np.arange(ncp) * 16 + 31
    kc_ = np.stack([cp // 64, cp % 64, np.ones_like(cp), np.ones_like(cp)]).astype(np.float32)
    c['kaugc'] = np.concatenate([np.zeros((4, 1), np.float32), kc_[:, :-1]], axis=1)
    nsel = S_len // 64
    cs = np.arange(ncp) * 16
    ss = np.arange(64) * 64
    ov = ((cs[:, None] <= ss[None, :] + 63) & (cp[:, None] >= ss[None, :])).astype(np.float32)
    ov[:, nsel:] = 0.0
    ov = np.concatenate([np.zeros((1, 64), np.float32), ov[:-1]], axis=0)
    c['overlap'] = ov
    E = np.zeros((64, S_len), np.float32)
    E[t // 64, t] = 1.0
    c['esel'] = E
    return c


PARAM_SHAPES = {
    'norm_pre': [1024], 'norm_post': [1024],
    'conv_aT': [128, 12, 4], 'a_log_a': [4], 'dt_bias_a': [4], 'onorm_a': [128],
    'w_gk': [16, 256], 'b_gkT': [128, 2], 'onorm_b': [128],
    'conv_cT': [128, 8, 4], 'conv_bias_cT': [128, 8], 'a_log_c': [8], 'dt_bias_c': [8], 'd_skip_c': [8],
    'onorm_c': [512], 'cmp_pos_kT': [64, 32], 'cmp_pos_vT': [64, 32],
    'w_ck1': [2048, 64], 'w_ck2': [64, 64], 'w_cv1': [2048, 64], 'w_cv2': [64, 64],
    'w_br': [4, 512, 1024], 'w_out': [1024, 1024], 'w_in': [1024, D_IN],
}


def prep_inputs(inp, depth):
    o = {}
    f = lambda a: np.ascontiguousarray(np.asarray(a, dtype=np.float32))
    for k in ('norm_pre', 'norm_post', 'a_log_a', 'dt_bias_a', 'onorm_a', 'w_gk', 'onorm_b', 'a_log_c',
              'dt_bias_c', 'd_skip_c', 'onorm_c', 'w_ck1', 'w_ck2', 'w_cv1', 'w_cv2', 'w_br', 'w_out', 'w_in'):
        o[k] = f(inp[k][:depth])
    o['conv_aT'] = f(np.asarray(inp['conv_a'])[:depth].reshape(depth, 4, 12, 128).transpose(0, 3, 2, 1))
    o['conv_cT'] = f(np.asarray(inp['conv_c'])[:depth].reshape(depth, 4, 8, 128).transpose(0, 3, 2, 1))
    o['conv_bias_cT'] = f(np.asarray(inp['conv_bias_c'])[:depth].reshape(depth, 8, 128).transpose(0, 2, 1))
    o['b_gkT'] = f(np.asarray(inp['b_gk'])[:depth].reshape(depth, 2, 128).transpose(0, 2, 1))
    o['cmp_pos_kT'] = f(np.asarray(inp['cmp_pos_k'])[:depth].transpose(0, 2, 1))
    o['cmp_pos_vT'] = f(np.asarray(inp['cmp_pos_v'])[:depth].transpose(0, 2, 1))
    return o


def build(S_len=4096, depth=4, branches=(0, 1, 2, 3)):
    nc = bass.Bass("TRN2", target_bir_lowering=False)
    NT = S_len // 128
    NM = S_len // MT
    S = Sched(nc)
    b = B(S)
    dr = {}
    dr['x'] = nc.dram_tensor("x", [S_len, 1024], F32, kind="ExternalInput").ap()
    for k, shp in PARAM_SHAPES.items():
        dr[k] = nc.dram_tensor(k, [depth] + shp, F32, kind="ExternalInput").ap()
    hc = host_constants(S_len)
    for k, v in hc.items():
        dr[k] = nc.dram_tensor(k, list(v.shape), F32, kind="ExternalInput").ap()
    y = nc.dram_tensor("y", [S_len, 1024], F32, kind="ExternalOutput").ap()
    wq = {'w_in': nc.dram_tensor("wq_in", [depth, 1024, D_IN], BF16, kind="Internal").ap(),
          'w_br': nc.dram_tensor("wq_br", [depth, 2048, 1024], BF16, kind="Internal").ap(),
          'w_out': nc.dram_tensor("wq_out", [depth, 1024, 1024], BF16, kind="Internal").ap()}
    wqres = {}
    yres = [S.res("y%d" % g) for g in range(NT)]

    def dt(ap, res=None):
        return Tile('dram', ap, res.rid if res is not None else 0)

    cst = S.sb("cst", [128, 513], F32)
    b.dma('sp', cst, dt(dr['cst']))
    ident_f, U_f, ones_f = cst[:, 0:128], cst[:, 128:256], cst[:, 256:384]
    cstb = S.sb("cstb", [128, 384], BF16)
    b.cp('dve', cstb, cst[:, 0:384])
    ident_b, U_b, ones_b = cstb[:, 0:128], cstb[:, 128:256], cstb[:, 256:384]
    mhalf = S.sb("mhalf", [128, 1], F32)
    b.memset('dve', mhalf, -0.5)

    PS = [S.ps("ps%d" % i, [128, 512], F32) for i in range(7)]
    PTb = S.ps("ptb", [128, 1024], BF16)
    psi = [0]

    def nps(lo=0):
        psi[0] += 1
        return PS[lo + psi[0] % (7 - lo)]

    h4 = lambda t: t.re("p (h i) -> p h i", h=4)

    pools = {}

    def scr(cls, i):
        key = (cls, i)
        if key not in pools:
            pools[key] = S.sb("scr%s%d" % (cls, i), [128, 512], F32 if cls == 'A' else BF16)
        return pools[key]

    FM = S.sb("FM", [128, 16, MT], BF16)
    xts = [S.sb("xt%d" % i, [128, 1024], F32) for i in range(1)]
    hb = S.sb("hb", [128, 1024], BF16)
    junk = hb
    hT = S.sb("hT", [128, 8, MT], BF16)
    wbufs = [S.sb("wb%d" % i, [128, 8, 528], BF16) for i in range(2)]
    wi = [0]
    merged = [S.sb("mg%d" % i, [128, 1024], F32) for i in range(4)]
    gpre = S.sb("gpre", [128, 1024], F32)
    gpost = S.sb("gpost", [128, 1024], F32)
    ssq = S.sb("ssq", [128, 1], F32)
    rstd = S.sb("rstd", [128, 1], F32)
    ybr = [S.sb("ybr%d" % i, [128, 512], BF16) for i in range(4)]
    yT = S.sb("yT", [128, 4, 4, 128], BF16)
    zz = [S.sb("zz%d" % i, [128, 512], BF16) for i in range(4)]
    raw = [S.sb("raw%d" % i, [128, 515], BF16) for i in range(2)]
    dg = [S.sb("dg%d" % i, [128, 4, 128], BF16) for i in range(2)]
    sg = scr('A', 0)
    tmpm = scr('A', 1)
    mgb = hb
    mgT = yT[:, 0:2].re("p a k t -> p (a k) t")

    def bcast_load(tile, src_ap, n):
        b.dma('pool', tile, dt(src_ap.partition_broadcast(128)))

    def convert_layer(l):
        for name, src2d in (('w_in', dr['w_in'][l]), ('w_br', dr['w_br'][l].rearrange("n k c -> (n k) c")),
                            ('w_out', dr['w_out'][l])):
            r = S.res("wq_%s%d" % (name, l))
            wqres[(name, l)] = r
            ncol = src2d.shape[1]
            nrow = src2d.shape[0]
            for r0 in range(0, nrow, 1024):
                for c0 in range(0, ncol, 2048):
                    c1 = min(c0 + 2048, ncol)
                    S.dma('pool', wq[name][l][r0:r0 + 1024, c0:c1], src2d[r0:r0 + 1024, c0:c1], reads=[], writes=[r])

    def wload(key, col0, ncols, rows8=True):
        name, l, row0, nrows = key
        wb = wbufs[wi[0] % 2]
        wi[0] += 1
        src = wq[name][l][row0:row0 + nrows, :].rearrange("(k p) n -> p k n", p=128)
        nk = src.shape[1]
        b.dma('sp', wb[:, 0:nk, 0:ncols], dt(src[:, :, col0:col0 + ncols], wqres[(name, l)]))
        return wb

    def proj_tok(ps, st, wb, c0, n, o0=0):
        for kc in range(8):
            b.mm(ps[:, o0:o0 + n], hT[:, kc, st * 128:(st + 1) * 128], wb[:, kc, c0:c0 + n], start=kc == 0, stop=kc == 7)

    def proj_feat(ps, wb, c0, mch):
        for kc in range(8):
            b.mm(ps[0:mch, :], wb[:, kc, c0:c0 + mch], hT[:, kc, :], start=kc == 0, stop=kc == 7)

    def rms_rstd(out1, ss1, n):
        b.ts('dve', out1, ss1, 1.0 / n, ALU.mult, NORM_EPS, ALU.add)
        b.tt('pool', out1, out1, mhalf_k(out1.ap.shape[1]), ALU.pow)

    mh_cache = {}

    def mhalf_k(k):
        if k not in mh_cache:
            t = S.sb("mh%d" % k, [128, k], F32)
            b.memset('dve', t, -0.5)
            mh_cache[k] = t
        return mh_cache[k]

    def emit_yT(st):
        for kc in range(4):
            b.tr(PTb[:, kc * 128:(kc + 1) * 128], ybr[st][:, kc * 128:(kc + 1) * 128], ident_b)
        b.cp('dve', yT[:, st], PTb[:, 0:512].re("p (k t) -> p k t", k=4))

    ctx = dict(jidx=cst[:, 384:448], e0=cst[:, 448:512], half01=cst[:, 512:513], emit_yT=emit_yT, scr=scr, FM=FM, nc=nc, S=S, b=b, dr=dr, dt=dt, PS=PS, PTb=PTb, nps=nps, h4=h4, hT=hT, wload=wload,
               proj_tok=proj_tok, proj_feat=proj_feat, rms_rstd=rms_rstd, ident_f=ident_f, U_f=U_f,
               ones_f=ones_f, ident_b=ident_b, U_b=U_b, ones_b=ones_b, ybr=ybr, zz=zz, raw=raw, dg=dg,
               S_len=S_len, NT=NT, NM=NM, bcast_load=bcast_load, depth=depth, mhalf_k=mhalf_k)
    mixers = {}
    if 0 in branches:
        mixers[0] = GDN(ctx)
    if 1 in branches:
        mixers[1] = GLA(ctx)
    if 2 in branches:
        mixers[2] = SSD(ctx)
    if 3 in branches:
        mixers[3] = NSA(ctx)

    convert_layer(0)
    for l in range(depth):
        w_in_l = ('w_in', l, 0, 1024)
        if l + 1 < depth:
            convert_layer(l + 1)
        bcast_load(gpre, dr['norm_pre'][l:l + 1, :], 1024)
        bcast_load(gpost, dr['norm_post'][l:l + 1, :], 1024)
        for n in mixers:
            mixers[n].layer_setup(l)
        for m in range(NM):
            for st in range(4):
                g = m * 4 + st
                xt = xts[0]
                src = dr['x'] if l == 0 else y
                b.dma('pool', xt, dt(src[g * 128:(g + 1) * 128, :], yres[g]))
                b.act(junk, xt, AF.Square, accum=ssq)
                rms_rstd(rstd, ssq, 1024)
                b.stt(hb, xt, rstd[:, 0:1], gpre, ALU.mult, ALU.mult)
                for kc in range(8):
                    b.tr(PTb[:, kc * 128:(kc + 1) * 128], hb[:, kc * 128:(kc + 1) * 128], ident_b)
                b.cp('act', hT[:, :, st * 128:(st + 1) * 128], PTb.re("p (k t) -> p k t", k=8))
            first = True
            for n in (0, 1, 2, 3):
                if n not in mixers:
                    continue
                mixers[n].macro(l, m, w_in_l)
                for half in range(2):
                    wg = wload(w_in_l, OFF['merge_gate'] + n * 1024 + half * 512, 512)
                    wbr = wload(('w_br', l, n * 512, 512), half * 512, 512)
                    for st in range(4):
                        pg = nps()
                        proj_tok(pg, st, wg, 0, 512)
                        b.act(sg, pg, AF.Sigmoid)
                        pb = nps()
                        for kc in range(4):
                            b.mm(pb, yT[:, st, kc, :], wbr[:, kc, 0:512], start=kc == 0, stop=kc == 3)
                        mslice = merged[st][:, half * 512:(half + 1) * 512]
                        if first:
                            b.tt('dve', mslice, sg, pb, ALU.mult)
                        else:
                            b.tt('dve', tmpm, sg, pb, ALU.mult)
                            b.tt('pool', mslice, mslice, tmpm, ALU.add)
                first = False
            wo = [wload(('w_out', l, 0, 1024), half * 512, 512) for half in range(2)]
            for st in range(4):
                g = m * 4 + st
                osb = merged[st]
                b.cp('act', mgb, merged[st])
                for kc in range(8):
                    b.tr(PTb[:, kc * 128:(kc + 1) * 128], mgb[:, kc * 128:(kc + 1) * 128], ident_b)
                b.cp('dve', mgT, PTb.re("p (k t) -> p k t", k=8))
                for half in range(2):
                    po = nps()
                    for kc in range(8):
                        b.mm(po, mgT[:, kc, :], wo[half][:, kc, 0:512], start=kc == 0, stop=kc == 7)
                    b.cp('act', osb[:, half * 512:(half + 1) * 512], po)
                b.act(junk, osb, AF.Square, accum=ssq)
                rms_rstd(rstd, ssq, 1024)
                xt = xts[0]
                src = dr['x'] if l == 0 else y
                b.dma('pool', xt, dt(src[g * 128:(g + 1) * 128, :], yres[g]))
                b.stt(osb, osb, rstd[:, 0:1], gpost, ALU.mult, ALU.mult)
                b.tt('dve', osb, osb, xt, ALU.add)
                S.dma('pool', y[g * 128:(g + 1) * 128, :], osb.ap, reads=[osb], writes=[yres[g]], out=(l == depth - 1))
    S.emit()
    return nc, S


class Mixer:
    def __init__(self, ctx):
        self.__dict__.update(ctx)

    def conv_chunk(self, ps_in, convw, c, halo, out_fm, bias=None):
        b = self.b
        k = self.cc
        self.cc += 1
        raw, dg = self.raw[k % 2], self.dg[k % 2]
        b.cp('dve', raw[:, 0:3], halo)
        b.cp('act', raw[:, 3:515], ps_in)
        b.cp('dve', halo, raw[:, 512:515])
        b.tt('dve', dg, self.ident_b.bc(1, [128, 4, 128]), convw[:, c, :].bc(2, [128, 4, 128]), ALU.mult)
        p2 = self.nps()
        for t in range(4):
            b.mm(p2, dg[:, t, :], raw[:, t:t + 512], start=t == 0, stop=t == 3)
        if bias is None:
            b.act(out_fm, p2, AF.Silu)
        else:
            b.act(out_fm, p2, AF.Silu, bias=bias)

    def out_norm_heads(self, po, st, onz):
        b, S = self.b, self.S
        o = self.osb4
        b.cp('act', o, po)
        b.tt('pool', self.sq4, o, o, ALU.mult)
        b.red(self.ss4, self.h4(self.sq4))
        self.rms_rstd(self.rs4, self.ss4, 128)
        b.tt('dve', self.h4(o), self.h4(o), self.rs4.bc(2, [128, 4, 128]), ALU.mult)
        b.tt('dve', self.ybr[st], o, onz, ALU.mult)

    def z_block(self, wb, c0, onorm_b, per_head=True):
        b = self.b
        for st in range(4):
            pz = self.nps()
            self.proj_tok(pz, st, wb, c0, 512)
            b.act(self.zz[st], pz, AF.Silu)
            if onorm_b is not None:
                if per_head:
                    b.tt('pool', self.h4(self.zz[st]), self.h4(self.zz[st]), onorm_b.bc(1, [128, 4, 128]), ALU.mult)
                else:
                    b.tt('pool', self.zz[st], self.zz[st], onorm_b, ALU.mult)


class GDN(Mixer):
    def __init__(self, ctx):
        super().__init__(ctx)
        S = self.S
        self.cc = 0
        scr = self.scr
        A4 = lambda i: scr('A', i).re("p (h i) -> p h i", h=4)
        B4 = lambda i: scr('B', i).re("p (h i) -> p h i", h=4)
        self.fm = self.FM[:, 0:12, :]
        self.sqa, self.sqb = B4(9), B4(10)
        self.halo = [S.sb("gdn_h%d" % c, [128, 3], BF16) for c in range(12)]
        self.convw = S.sb("gdn_cw", [128, 12, 4], F32)
        self.nega = S.sb("gdn_nega", [128, 4], F32)
        self.dtb = S.sb("gdn_dtb", [128, 4], F32)
        self.onorm = S.sb("gdn_on", [128, 128], F32)
        self.Sf = S.sb("gdn_Sf", [128, 4, 128], F32)
        self.Sb = S.sb("gdn_Sb", [128, 4, 128], BF16)
        f = lambda n, k: S.sb("gdn_" + n, [128, k], F32)
        self.lnss, self.lnr, self.ba, self.e1, self.nlb = f("lnss", 8), f("lnr", 8), f("ba", 8), f("e1", 4), f("nlb", 4)
        self.apb, self.e2, self.sp, self.g, self.gcl = f("apb", 4), f("e2", 4), f("sp", 4), f("g", 4), f("gcl", 8)
        self.C3, self.X4, self.EX = f("C3", 12), f("X4", 16), f("EX", 16)
        self.D3 = [A4(2), A4(3), A4(4)]
        self.BJ, self.BA = A4(5), A4(6)
        self.E1, self.E2, self.E3 = A4(7), A4(8), A4(9)
        self.EB = B4(0)
        self.P = [A4(10), A4(11)]
        self.PT = [A4(12), A4(13)]
        self.AT = [A4(14), A4(15)]
        self.ATb, self.At, self.Rk, self.Kd = B4(1), B4(2), B4(3), B4(4)
        self.Vb, self.nW, self.Vn, self.qg = B4(5), B4(6), B4(7), B4(8)
        self.osb4 = scr('A', 0)
        self.sq4 = scr('A', 1)
        self.ss4, self.rs4 = f("ss4", 4), f("rs4", 4)
        self.ea = f("ea", 4)

    def layer_setup(self, l):
        b, dr, dt = self.b, self.dr, self.dt
        b.dma('pool', self.convw, dt(dr['conv_aT'][l]))
        self.bcast_load(self.ea, dr['a_log_a'][l:l + 1, :], 4)
        b.act(self.nega, self.ea, AF.Exp)
        b.ts('dve', self.nega, self.nega, -1.0, ALU.mult)
        self.bcast_load(self.dtb, dr['dt_bias_a'][l:l + 1, :], 4)
        self.bcast_load(self.onorm, dr['onorm_a'][l:l + 1, :], 128)
        b.memset('dve', self.Sf, 0.0)
        b.memset('dve', self.Sb, 0.0)
        for c in range(12):
            b.memset('pool', self.halo[c], 0.0)

    def macro(self, l, m, w_in_l):
        b, nps, h4 = self.b, self.nps, self.h4
        fm = self.fm
        for blk in range(3):
            wb = self.wload(w_in_l, blk * 512, 512)
            for cc in range(4):
                c = blk * 4 + cc
                p = nps()
                self.proj_feat(p, wb, cc * 128, 128)
                self.conv_chunk(p, self.convw, c, self.halo[c], fm[:, c, :])
        wb3 = self.wload(w_in_l, OFF['gdn_beta'], 520)
        self.z_block(wb3, 8, self.onorm)
        for st in range(4):
            self.sub(st, wb3)
            self.emit_yT(st)

    def sub(self, st, wb3):
        b, nps, h4 = self.b, self.nps, self.h4
        fm = self.fm
        tk = slice(st * 128, (st + 1) * 128)
        bc4 = lambda t: t.bc(2, [128, 4, 128])
        b.tt('pool', self.sqa, fm[:, 4:8, tk], fm[:, 4:8, tk], ALU.mult)
        b.tt('pool', self.sqb, fm[:, 0:4, tk], fm[:, 0:4, tk], ALU.mult)
        pq = nps()
        for c in range(8):
            sq_c = self.sqa[:, c, :] if c < 4 else self.sqb[:, c - 4, :]
            b.mm(pq[:, c:c + 1], sq_c, self.ones_b[:, 0:1])
        self.proj_tok(pq, st, wb3, 0, 8, o0=8)
        b.act(self.lnss, pq[:, 0:8], AF.Ln, bias=NORM_EPS)
        b.ts('dve', self.lnr, self.lnss, -0.5, ALU.mult)
        b.cp('dve', self.ba, pq[:, 8:16])
        b.act(self.e1, self.ba[:, 0:4], AF.Exp, scale=-1.0)
        b.act(self.nlb, self.e1, AF.Ln, bias=1.0)
        b.tt('dve', self.apb, self.ba[:, 4:8], self.dtb, ALU.add)
        b.act(self.e2, self.apb, AF.Exp)
        b.act(self.sp, self.e2, AF.Ln, bias=1.0)
        b.tt('dve', self.g, self.sp, self.nega, ALU.mult)
        pg = nps()
        b.mm(pg[:, 0:4], self.U_f, self.g)
        b.mm(pg[:, 4:8], self.ones_f, self.g)
        b.cp('dve', self.gcl, pg[:, 0:8])
        gc, gl = self.gcl[:, 0:4], self.gcl[:, 4:8]
        lnrk, lnrq = self.lnr[:, 0:4], self.lnr[:, 4:8]
        cA, cB, cJ = self.C3[:, 0:4], self.C3[:, 4:8], self.C3[:, 8:12]
        b.tt('dve', cJ, lnrk, gc, ALU.subtract)
        b.tt('dve', cA, gc, self.nlb, ALU.subtract)
        b.tt('dve', cA, cA, lnrk, ALU.add)
        b.stt(cB, gc, float(np.log(128.0 ** -0.5)), lnrq, ALU.add, ALU.add)
        X4 = self.X4
        b.cp('pool', X4[:, 0:4], cA)
        b.tt('pool', X4[:, 4:8], cJ, gl, ALU.add)
        b.ts('pool', X4[:, 8:12], self.nlb, -1.0, ALU.mult)
        b.cp('pool', X4[:, 12:16], gl)
        b.act(self.EX, X4, AF.Exp)
        sRk, sKd, sVb, dec = self.EX[:, 0:4], self.EX[:, 4:8], self.EX[:, 8:12], self.EX[:, 12:16]
        for v3 in range(3):
            b.tt('dve', self.D3[v3], self.ident_f.bc(1, [128, 4, 128]), bc4(self.C3[:, v3 * 4:(v3 + 1) * 4]), ALU.mult)
        b.cp('pool', self.BJ, bc4(cJ))
        b.cp('pool', self.BA, bc4(cA))
        f4 = lambda t: t.re("p h i -> p (h i)")
        pX1, pX2, pX3, pXB = nps(), nps(), nps(), nps()
        b.mm(pX1, self.ones_f, f4(self.D3[0]), start=True, stop=False)
        b.mm(pX1, self.ident_f, f4(self.BJ), start=False, stop=True)
        b.mm(pX2, self.ones_f, f4(self.D3[1]), start=True, stop=False)
        b.mm(pX2, self.ident_f, f4(self.BJ), start=False, stop=True)
        b.mm(pX3, self.ones_f, f4(self.D3[2]), start=True, stop=False)
        b.mm(pX3, self.ident_f, f4(self.BA), start=False, stop=True)
        b.mm(pXB, self.ones_f, f4(self.D3[1]))
        b.act(f4(self.E1), pX1, AF.Exp)
        b.act(f4(self.E2), pX2, AF.Exp)
        b.act(f4(self.E3), pX3, AF.Exp)
        b.act(f4(self.EB), pXB, AF.Exp)
        b.asel(self.E1, self.E1, [[0, 4], [1, 128]], ALU.is_ge, 0.0, -1, -1)
        b.asel(self.E2, self.E2, [[0, 4], [1, 128]], ALU.is_ge, 0.0, 0, -1)
        b.asel(self.E3, self.E3, [[0, 4], [-1, 128]], ALU.is_ge, 0.0, -1, 1)
        pG, pKQ = nps(), nps()
        for h in range(4):
            b.mm(pG[:, h * 128:(h + 1) * 128], fm[:, 4 + h, tk], fm[:, 4 + h, tk])
        for h in range(4):
            b.mm(pKQ[:, h * 128:(h + 1) * 128], fm[:, 4 + h, tk], fm[:, h, tk])
        P, PT, AT = self.P, self.PT, self.AT
        b.stt(f4(PT[0]), f4(self.E1), -1.0, pG, ALU.mult, ALU.mult)
        b.stt(f4(P[0]), f4(self.E3), -1.0, pG, ALU.mult, ALU.mult)
        b.tt('dve', f4(self.At), f4(self.E2), pKQ, ALU.mult)
        b.tt('pool', AT[0], PT[0], self.ident_f.bc(1, [128, 4, 128]), ALU.add)
        cur = 0
        for lev in range(1, 7):
            nxt = 1 - cur
            pP = nps()
            for h in range(4):
                b.mm(pP[:, h * 128:(h + 1) * 128], PT[cur][:, h, :], P[cur][:, h, :])
            if lev < 6:
                pPT = nps()
                for h in range(4):
                    b.mm(pPT[:, h * 128:(h + 1) * 128], P[cur][:, h, :], PT[cur][:, h, :])
            b.cp('act', f4(P[nxt]), pP)
            if lev < 6:
                b.cp('dve', f4(PT[nxt]), pPT)
            pA = nps()
            for h in range(4):
                b.mm(pA[:, h * 128:(h + 1) * 128], P[nxt][:, h, :], AT[cur][:, h, :])
            b.tt('dve', f4(AT[nxt]), f4(AT[cur]), pA, ALU.add)
            cur = nxt
        b.cp('act', self.ATb, AT[cur])
        PTb = self.PTb
        for h in range(4):
            b.tr(PTb[:, h * 128:(h + 1) * 128], fm[:, 4 + h, tk], self.ident_b)
            b.tr(PTb[:, (4 + h) * 128:(5 + h) * 128], fm[:, 8 + h, tk], self.ident_b)
        pk = PTb[:, 0:512].re("p (h d) -> p h d", h=4)
        pv = PTb[:, 512:1024].re("p (h d) -> p h d", h=4)
        b.tt('dve', self.Rk, pk, bc4(sRk), ALU.mult)
        b.tt('dve', self.Kd, pk, bc4(sKd), ALU.mult)
        b.tt('dve', self.Vb, pv, bc4(sVb), ALU.mult)
        pW = nps()
        for h in range(4):
            b.mm(pW[:, h * 128:(h + 1) * 128], self.Rk[:, h, :], self.ATb[:, h, :])
        b.act(f4(self.nW), pW, AF.Copy, scale=-1.0)
        pV = nps()
        for h in range(4):
            b.mm(pV[:, h * 128:(h + 1) * 128], self.ATb[:, h, :], self.Vb[:, h, :], start=(h == 0), stop=False)
            b.mm(pV[:, h * 128:(h + 1) * 128], self.nW[:, h, :], self.Sb[:, h, :], start=False, stop=True)
        b.cp('act', f4(self.Vn), pV)
        b.tt('dve', self.qg, fm[:, 0:4, tk], self.EB, ALU.mult)
        pO = nps()
        for h in range(4):
            b.mm(pO[:, h * 128:(h + 1) * 128], self.qg[:, h, :], self.Sb[:, h, :], start=(h == 0), stop=False)
            b.mm(pO[:, h * 128:(h + 1) * 128], self.At[:, h, :], self.Vn[:, h, :], start=False, stop=True)
        pS = nps()
        for h in range(4):
            b.mm(pS[:, h * 128:(h + 1) * 128], self.Kd[:, h, :], self.Vn[:, h, :])
        b.tt('dve', self.Sf, self.Sf, bc4(dec), ALU.mult)
        b.tt('dve', f4(self.Sf), f4(self.Sf), pS, ALU.add)
        b.cp('act', self.Sb, self.Sf)
        self.out_norm_heads(pO, st, self.zz[st])


class GLA(Mixer):
    def __init__(self, ctx):
        super().__init__(ctx)
        S = self.S
        scr = self.scr
        A2 = lambda i, o: scr('A', i)[:, o * 256:(o + 1) * 256].re("p (h i) -> p h i", h=2)
        B4 = lambda i: scr('B', i).re("p (h i) -> p h i", h=4)
        B2 = lambda i, o: scr('B', i)[:, o * 256:(o + 1) * 256].re("p (h i) -> p h i", h=2)
        self.fm = self.FM[:, 0:4, :]
        self.vt = [scr('B', 4 + i) for i in range(4)]
        self.gklo = scr('A', 10)
        self.wgk = S.sb("gla_wgk", [128, 256], F32)
        self.nbg = S.sb("gla_nbg", [128, 2], F32)
        self.onorm = S.sb("gla_on", [128, 128], F32)
        self.e = scr('A', 2)
        self.sp = [scr('A', 6), scr('A', 7)]
        self.nb = [scr('A', 8), scr('A', 9)]
        self.negc = S.sb("gla_negc", [128, 2, 2], F32)
        self.Eq, self.Ek = A2(3, 0), A2(3, 1)
        self.Eg, self.Ed = A2(4, 0), A2(4, 1)
        self.qt, self.kd = B2(0, 0), B2(0, 1)
        self.kt = B4(1)
        self.qg = B4(2)
        self.kdt = scr('B', 3)[:, 0:256]
        self.At = B4(8)
        self.Sf = S.sb("gla_Sf", [128, 2, 128], F32)
        self.Sb = S.sb("gla_Sb", [128, 2, 128], BF16)
        self.osb4 = scr('A', 0)
        self.sq4 = scr('A', 1)
        self.ss4 = S.sb("gla_ss4", [128, 4], F32)
        self.rs4 = S.sb("gla_rs4", [128, 4], F32)
        self.rm = S.sb("gla_rm", [128, 4], F32)
        self.Sd = scr('A', 5)[:, 0:128]

    def layer_setup(self, l):
        b, dr, dt = self.b, self.dr, self.dt
        b.memset('dve', self.wgk, 0.0)
        b.dma('pool', self.wgk[0:16, :], dt(dr['w_gk'][l]))
        b.dma('pool', self.nbg, dt(dr['b_gkT'][l]))
        b.ts('dve', self.nbg, self.nbg, -1.0, ALU.mult)
        self.bcast_load(self.onorm, dr['onorm_b'][l:l + 1, :], 128)
        b.memset('dve', self.Sf, 0.0)
        b.memset('dve', self.Sb, 0.0)
        if l == 0:
            b.memset('dve', self.rm, 0.0)
            b.memset('dve', self.rm[0:64, 0:1], 1.0)
            b.memset('dve', self.rm[64:128, 1:2], 1.0)
            b.memset('dve', self.rm[0:64, 2:3], 0.125)
            b.memset('dve', self.rm[64:128, 3:4], 0.125)

    def macro(self, l, m, w_in_l):
        b, nps = self.b, self.nps
        wb = self.wload(w_in_l, OFF['gla_q'], 512)
        for c in range(4):
            p = nps()
            self.proj_feat(p, wb, c * 128, 128)
            b.cp('act', self.fm[:, c, :], p)
        wb = self.wload(w_in_l, OFF['gla_v'], 512)
        for st in range(4):
            p = nps()
            self.proj_tok(p, st, wb, 0, 512)
            b.cp('act', self.vt[st], p)
        wb = self.wload(w_in_l, OFF['gla_gk'], 528)
        p = nps()
        self.proj_feat(p, wb, 0, 128)
        b.memset('pool', self.gklo, 0.0)
        b.cp('dve', self.gklo[0:16, :], p[0:16, :])
        for c in range(2):
            p = nps()
            b.mm(p, self.wgk[:, c * 128:(c + 1) * 128], self.gklo)
            b.act(self.e, p, AF.Exp, scale=-1.0, bias=self.nbg[:, c:c + 1])
            b.act(self.sp[c], self.e, AF.Ln, bias=1.0)
            b.ts('dve', self.sp[c], self.sp[c], 1.0 / 16.0, ALU.mult)
            for st in range(4):
                tk = slice(st * 128, (st + 1) * 128)
                b.scan(self.nb[c][:, tk], self.ones_f, self.sp[c][:, tk], 0.0, ALU.mult, ALU.add)
        self.z_block(wb, 16, self.onorm)
        for st in range(4):
            self.sub(st)
            self.emit_yT(st)

    def sub(self, st):
        b, nps = self.b, self.nps
        tk = slice(st * 128, (st + 1) * 128)
        fm = self.fm
        for c in range(2):
            nbs = self.nb[c][:, tk]
            ref = self.nb[c][:, st * 128 + 64:st * 128 + 65]
            last = self.nb[c][:, st * 128 + 127:st * 128 + 128]
            b.ts('dve', self.negc[:, c, 0:1], ref, -1.0, ALU.mult)
            b.ts('dve', self.negc[:, c, 1:2], last, -1.0, ALU.mult)
            b.act(self.Eq[:, c, :], nbs, AF.Exp, scale=-1.0, bias=ref)
            b.act(self.Ek[:, c, :], nbs, AF.Exp, bias=self.negc[:, c, 0:1])
            b.act(self.Eg[:, c, :], nbs, AF.Exp, scale=-1.0)
            b.act(self.Ed[:, c, :], nbs, AF.Exp, bias=self.negc[:, c, 1:2])
        b.stt(self.qt, self.Eq, 0.125, fm[:, 0:2, tk], ALU.mult, ALU.mult)
        b.tt('pool', self.kd, self.Ed, fm[:, 2:4, tk], ALU.mult)
        for h in range(4):
            c, r = h // 2, h % 2
            b.stt(self.kt[:, h, :], self.Ek[:, c, :], self.rm[:, r:r + 1], fm[:, 2 + c, tk], ALU.mult, ALU.mult)
            b.stt(self.qg[:, h, :], self.Eg[:, c, :], self.rm[:, 2 + r:3 + r], fm[:, c, tk], ALU.mult, ALU.mult)
        pA = nps()
        for h in range(4):
            c = h // 2
            b.mm(pA[:, h * 128:(h + 1) * 128], self.kt[:, h, :], self.qt[:, c, :])
        b.tt('dve', self.At, self.h4(pA), self.U_b.bc(1, [128, 4, 128]), ALU.mult)
        for c in range(2):
            b.tr(self.PTb[:, c * 128:(c + 1) * 128], self.kd[:, c, :], self.ident_b)
        b.cp('act', self.kdt, self.PTb[:, 0:256])
        pO = nps()
        for h in range(4):
            c = h // 2
            hs = slice(h * 128, (h + 1) * 128)
            b.mm(pO[:, hs], self.At[:, h, :], self.vt[st][:, hs], start=(h == 0), stop=False)
            b.mm(pO[:, hs], self.qg[:, h, :], self.Sb[:, c, :], start=False, stop=True)
        pS = nps()
        for h in range(4):
            c = h // 2
            b.mm(pS[:, h * 128:(h + 1) * 128], self.kdt[:, c * 128:(c + 1) * 128], self.vt[st][:, h * 128:(h + 1) * 128])
        for c in range(2):
            b.ts('dve', self.Sd, self.Sf[:, c, :], self.Eg[:, c, 127:128], ALU.mult)
            b.stt(self.Sd, pS[:, (2 * c) * 128:(2 * c + 1) * 128], self.rm[:, 0:1], self.Sd, ALU.mult, ALU.add)
            b.stt(self.Sf[:, c, :], pS[:, (2 * c + 1) * 128:(2 * c + 2) * 128], self.rm[:, 1:2], self.Sd, ALU.mult, ALU.add)
        b.cp('act', self.Sb, self.Sf)
        self.out_norm_heads(pO, st, self.zz[st])


class SSD(Mixer):
    def __init__(self, ctx):
        super().__init__(ctx)
        S = self.S
        self.cc = 0
        scr = self.scr
        A4 = lambda i: scr('A', i).re("p (h i) -> p h i", h=4)
        B4 = lambda i: scr('B', i).re("p (h i) -> p h i", h=4)
        self.fm = self.FM[:, 0:8, :]
        self.halo = [S.sb("ssd_h%d" % c, [128, 3], BF16) for c in range(8)]
        self.convw = S.sb("ssd_cw", [128, 8, 4], F32)
        self.convb = S.sb("ssd_cb", [128, 8], F32)
        f = lambda n, k: S.sb("ssd_" + n, [128, k], F32)
        self.nega, self.dtb, self.dsk, self.ea = f("nega", 8), f("dtb", 8), f("dsk", 8), f("ea", 8)
        self.onc = S.sb("ssd_onc", [128, 512], F32)
        self.dtr, self.apb, self.e, self.dtv, self.da, self.acl = f("dtr", 8), f("apb", 8), f("e", 8), f("dtv", 8), f("da", 8), f("acl", 16)
        self.X, self.EX, self.sdtd = f("X", 24), f("EX", 24), f("sdtd", 8)
        self.D8 = [A4(2), A4(3)]
        self.Bn = [A4(4), A4(5)]
        self.E = [A4(6), A4(7)]
        self.Mt = [B4(0), B4(1)]
        self.xdt = scr('B', 2)
        self.xdtd = scr('B', 3)
        self.xsk = scr('A', 8)
        self.Btok = scr('B', 4)[:, 0:256]
        self.yo = scr('A', 9)
        self.Hf = S.sb("ssd_Hf", [128, 512], F32)
        self.Hb = S.sb("ssd_Hb", [128, 512], BF16)
        self.ssq = f("ssq", 1)
        self.rstd = f("rstd", 1)
        self.junk = scr('B', 5)

    def layer_setup(self, l):
        b, dr, dt = self.b, self.dr, self.dt
        b.dma('pool', self.convw, dt(dr['conv_cT'][l]))
        b.dma('pool', self.convb, dt(dr['conv_bias_cT'][l]))
        self.bcast_load(self.ea, dr['a_log_c'][l:l + 1, :], 8)
        b.act(self.nega, self.ea, AF.Exp)
        b.ts('dve', self.nega, self.nega, -1.0, ALU.mult)
        self.bcast_load(self.dtb, dr['dt_bias_c'][l:l + 1, :], 8)
        self.bcast_load(self.dsk, dr['d_skip_c'][l:l + 1, :], 8)
        self.bcast_load(self.onc, dr['onorm_c'][l:l + 1, :], 512)
        b.memset('dve', self.Hf, 0.0)
        b.memset('dve', self.Hb, 0.0)
        for c in range(8):
            b.memset('pool', self.halo[c], 0.0)

    def macro(self, l, m, w_in_l):
        b, nps = self.b, self.nps
        for blk in range(2):
            wb = self.wload(w_in_l, OFF['ssd_x'] + blk * 512, 512)
            for cc in range(4):
                c = blk * 4 + cc
                p = nps()
                self.proj_feat(p, wb, cc * 128, 128)
                self.conv_chunk(p, self.convw, c, self.halo[c], self.fm[:, c, :], bias=self.convb[:, c:c + 1])
        wb3 = self.wload(w_in_l, OFF['ssd_dt'], 520)
        self.z_block(wb3, 8, None)
        for st in range(4):
            self.sub(st, wb3)
            self.emit_yT(st)

    def sub(self, st, wb3):
        b, nps = self.b, self.nps
        fm = self.fm
        tk = slice(st * 128, (st + 1) * 128)
        h8 = lambda t, k=64: t.re("p (h i) -> p h i", h=8)
        bc8 = lambda t, k: t.bc(2, [128, 8, k])
        pq = nps()
        self.proj_tok(pq, st, wb3, 0, 8)
        b.cp('dve', self.dtr, pq[:, 0:8])
        b.tt('dve', self.apb, self.dtr, self.dtb, ALU.add)
        b.act(self.e, self.apb, AF.Exp)
        b.act(self.dtv, self.e, AF.Ln, bias=1.0)
        b.tt('dve', self.da, self.dtv, self.nega, ALU.mult)
        pg = nps()
        b.mm(pg[:, 0:8], self.U_f, self.da)
        b.mm(pg[:, 8:16], self.ones_f, self.da)
        b.cp('dve', self.acl, pg[:, 0:16])
        acs, alast = self.acl[:, 0:8], self.acl[:, 8:16]
        b.cp('pool', self.X[:, 0:8], acs)
        b.tt('pool', self.X[:, 8:16], alast, acs, ALU.subtract)
        b.cp('pool', self.X[:, 16:24], alast)
        b.act(self.EX, self.X, AF.Exp)
        eacs, edst, dec = self.EX[:, 0:8], self.EX[:, 8:16], self.EX[:, 16:24]
        b.tt('dve', self.sdtd, self.dtv, edst, ALU.mult)
        bc4 = lambda t: t.bc(2, [128, 4, 128])
        f4 = lambda t: t.re("p h i -> p (h i)")
        pCB = nps()
        for g in range(2):
            b.mm(pCB[:, g * 128:(g + 1) * 128], fm[:, 4 + g, tk], fm[:, 6 + g, tk])
        for g in range(2):
            hs = slice(g * 4, g * 4 + 4)
            b.tt('dve', self.D8[g], self.ident_f.bc(1, [128, 4, 128]), bc4(acs[:, hs]), ALU.mult)
            b.ts('pool', self.Bn[g], bc4(acs[:, hs]), -1.0, ALU.mult)
            pX = nps()
            b.mm(pX, self.ones_f, f4(self.D8[g]), start=True, stop=False)
            b.mm(pX, self.ident_f, f4(self.Bn[g]), start=False, stop=True)
            b.act(f4(self.E[g]), pX, AF.Exp)
            b.asel(self.E[g], self.E[g], [[0, 4], [1, 128]], ALU.is_ge, 0.0, 0, -1)
            b.tt('dve', self.Mt[g], self.E[g], pCB[:, g * 128:(g + 1) * 128].bc(1, [128, 4, 128]), ALU.mult)
        PTb = self.PTb
        for c in range(4):
            b.tr(PTb[:, c * 128:(c + 1) * 128], fm[:, c, tk], self.ident_b)
        for g in range(2):
            b.tr(PTb[:, 512 + g * 128:512 + (g + 1) * 128], fm[:, 4 + g, tk], self.ident_b)
        xtok = h8(PTb[:, 0:512])
        b.tt('dve', h8(self.xdt), xtok, bc8(self.dtv, 64), ALU.mult)
        b.tt('dve', h8(self.xdtd), xtok, bc8(self.sdtd, 64), ALU.mult)
        b.tt('dve', h8(self.xsk), xtok, bc8(self.dsk, 64), ALU.mult)
        b.cp('act', self.Btok, PTb[:, 512:768])
        pY = nps()
        for h in range(8):
            b.mm(pY[:, h * 64:(h + 1) * 64], self.Mt[h // 4][:, h % 4, :], self.xdt[:, h * 64:(h + 1) * 64])
        pF = nps()
        for g in range(2):
            b.mm(pF[:, g * 256:(g + 1) * 256], fm[:, 6 + g, tk], self.Hb[:, g * 256:(g + 1) * 256])
        b.tt('dve', h8(self.yo), h8(pF), bc8(eacs, 64), ALU.mult)
        b.tt('pool', self.yo, self.yo, self.xsk, ALU.add)
        b.tt('dve', self.yo, self.yo, pY, ALU.add)
        pH = nps()
        for g in range(2):
            b.mm(pH[:, g * 256:(g + 1) * 256], self.Btok[:, g * 128:(g + 1) * 128], self.xdtd[:, g * 256:(g + 1) * 256])
        b.tt('dve', h8(self.Hf), h8(self.Hf), bc8(dec, 64), ALU.mult)
        b.tt('dve', self.Hf, self.Hf, pH, ALU.add)
        b.cp('act', self.Hb, self.Hf)
        b.tt('dve', self.yo, self.yo, self.zz[st], ALU.mult)
        b.act(self.junk, self.yo, AF.Square, accum=self.ssq)
        self.rms_rstd(self.rstd, self.ssq, 512)
        b.stt(self.ybr[st], self.yo, self.rstd[:, 0:1], self.onc, ALU.mult, ALU.mult)


class NSA(Mixer):
    def __init__(self, ctx):
        super().__init__(ctx)
        S, scr, S_len, NT = self.S, self.scr, self.S_len, self.NT
        self.kS = [S.sb("nsa_kS%d" % g, [128, S_len], BF16) for g in range(2)]
        self.kW = [S.sb("nsa_kW%d" % g, [128, 8 * 128], BF16) for g in range(2)]
        self.vS = [S.sb("nsa_vS%d" % g, [128, NT, 66], BF16) for g in range(2)]
        self.vW = [S.sb("nsa_vW%d" % g, [128, 8, 66], BF16) for g in range(2)]
        self.kC = [S.sb("nsa_kC%d" % g, [128, 256], BF16) for g in range(2)]
        self.vcT = [S.sb("nsa_vcT%d" % g, [128, 256], BF16) for g in range(2)]
        self.vC = [S.sb("nsa_vC%d" % g, [128, 2, 130], BF16) for g in range(2)]
        self.EK = S.sb("nsa_EK", [128, S_len], BF16)
        self.EKc = S.sb("nsa_EKc", [128, 256], BF16)
        self.qm = self.FM[:, 0:8, :]
        self.qA = self.FM[:, 8:16, :]
        self.qS = S.sb("nsa_qS", [128, 4, 128], BF16)
        self.rawc = [S.sb("nsa_rawc%d" % g, [128, 528], BF16) for g in range(2)]
        self.W1 = S.sb("nsa_W1", [128, 32, 128], BF16)
        self.W2k = S.sb("nsa_W2k", [128, 128], BF16)
        self.W2v = S.sb("nsa_W2v", [128, 128], BF16)
        self.posT = S.sb("nsa_posT", [128, 32], BF16)
        self.cpos = S.sb("nsa_cpos", [128, 1], F32)
        self.wsel = S.sb("nsa_wsel", [128, 8, 128], BF16)
        self.gsig = [S.sb("nsa_gs%d" % i, [128, 24], F32) for i in range(4)]
        self.rmq = S.sb("nsa_rmq", [128, 2], F32)
        self.hid = S.sb("nsa_hid", [128, 32], BF16)
        f = lambda n, k: S.sb("nsa_" + n, [128, k], F32)
        self.dall = S.sb("nsa_dall", [128, 3, 4], F32)
        self.rall = S.sb("nsa_rall", [128, 3, 4], F32)
        self.coef = S.sb("nsa_coef", [128, 3, 4], F32)
        self.imp, self.imp2, self.m8a, self.m8b, self.thr, self.selb = f("imp", 64), f("imp2", 64), f("m8a", 8), f("m8b", 8), f("thr", 1), f("selb", 64)
        self.selbb = S.sb("nsa_selbb", [128, 128], BF16)
        self.cur, self.val, self.fz = f("cur", 1), f("val", 64), f("fz", 64)
        self.Pb = [scr('B', i).re("p (h i) -> p h i", h=4) for i in range(3)]
        self.pi = 0
        self.on = scr('A', 2).re("p (h d) -> p h d", h=8)
        self.tmp4 = scr('A', 3)[:, 0:256].re("p (h d) -> p h d", h=4)
        self.tmp5 = scr('A', 4)[:, 0:256].re("p (h d) -> p h d", h=4)

    def layer_setup(self, l):
        b, dr, dt, nps = self.b, self.dr, self.dt, self.nps
        if NSTAGE < 0:
            return
        if l == 0:
            for g in range(2):
                for t in (self.kS[g], self.kW[g], self.vS[g], self.vW[g], self.kC[g], self.vcT[g], self.vC[g]):
                    b.memset('pool', t, 0.0)
                b.memset('pool', self.vS[g][:, :, 64:65], 1.0)
                b.memset('pool', self.vW[g][:, :, 64:65], 1.0)
                b.memset('pool', self.vC[g][:, :, 64:65], 1.0)
                b.memset('pool', self.vC[g][0:1, 0, 64:65], 0.0)
                for kt in range(2):
                    b.dma('pool', self.vC[g][:, kt, 65:129], dt(dr['overlap'][kt * 128:(kt + 1) * 128, :]))
            b.memset('pool', self.EK, 0.0)
            for c0 in range(0, self.S_len, 1024):
                c1 = min(c0 + 1024, self.S_len)
                b.dma('pool', self.EK[0:64, c0:c1], dt(dr['esel'][:, c0:c1]))
                b.dma('pool', self.EK[64:68, c0:c1], dt(dr['kaug'][:, c0:c1]))
            b.memset('pool', self.EKc, 0.0)
            b.dma('pool', self.EKc[64:68, :], dt(dr['kaugc']))
            b.memset('pool', self.qS, 0.0)
            b.memset('pool', self.selbb, 0.0)
            b.memset('pool', self.rmq, 0.0)
            b.memset('pool', self.rmq[0:64, 0:1], 0.125)
            b.memset('pool', self.rmq[64:128, 1:2], 0.125)
        b.memset('pool', self.W1, 0.0)
        b.dma('pool', self.W1[0:64, :, 0:64], dt(dr['w_ck1'][l].rearrange("(p d) o -> d p o", d=64)))
        b.dma('pool', self.W1[64:128, :, 64:128], dt(dr['w_cv1'][l].rearrange("(p d) o -> d p o", d=64)))
        b.memset('pool', self.W2k, 0.0)
        b.dma('pool', self.W2k[0:64, 0:64], dt(dr['w_ck2'][l]))
        b.dma('pool', self.W2k[0:64, 64:128], dt(dr['w_ck2'][l]))
        b.memset('pool', self.W2v, 0.0)
        b.dma('pool', self.W2v[64:128, 0:64], dt(dr['w_cv2'][l]))
        b.dma('pool', self.posT[0:64, :], dt(dr['cmp_pos_kT'][l]))
        b.dma('pool', self.posT[64:128, :], dt(dr['cmp_pos_vT'][l]))
        pc = nps()
        for p in range(32):
            b.mm(pc[:, 0:1], self.W1[:, p, :], self.posT[:, p:p + 1], start=p == 0, stop=p == 31)
        b.cp('dve', self.cpos, pc[:, 0:1])
        for g in range(2):
            b.memset('pool', self.rawc[g], 0.0)

    def macro(self, l, m, w_in_l):
        b, nps = self.b, self.nps
        if NSTAGE < 1:
            for st in range(4):
                self.emit_yT(st)
            return
        n0 = 4 * m
        wsel = self.wsel
        wb = self.wload(w_in_l, OFF['nsa_q'], 512)
        for c in range(4):
            p = nps()
            self.proj_feat(p, wb, c * 128, 128)
            b.ts('dve', self.qm[:, 2 * c, :], p, self.rmq[:, 0:1], ALU.mult)
            b.act(self.qm[:, 2 * c + 1, :], p, AF.Copy, scale=self.rmq[:, 1:2])
        def bail():
            for st in range(4):
                self.emit_yT(st)
        if NSTAGE < 1.15:
            return bail()
        b.memset('pool', self.qA, 0.0)
        b.dma('pool', self.qA[64:68, :, :], self.dt(self.dr['qaug'][:, :, m * MT:(m + 1) * MT]))
        if NSTAGE < 1.25:
            return bail()
        wb = self.wload(w_in_l, OFF['nsa_kc'], 512)
        for g in range(2):
            b.cp('dve', wsel[:, :, 0:64], wb[:, :, g * 64:(g + 1) * 64])
            b.cp('dve', wsel[:, :, 64:128], wb[:, :, 128 + g * 64:128 + (g + 1) * 64])
            p = nps()
            self.proj_feat(p, wsel, 0, 128)
            b.cp('dve', self.rawc[g][:, 0:16], self.rawc[g][:, 512:528])
            b.cp('act', self.rawc[g][:, 16:528], p)
        if NSTAGE < 1.27:
            return bail()
        for g in range(2):
            b.cp('dve', wsel[:, :, 0:64], wb[:, :, 256 + g * 64:256 + (g + 1) * 64])
            b.cp('dve', wsel[:, :, 64:128], wb[:, :, 256 + g * 64:256 + (g + 1) * 64])
            p = nps()
            self.proj_feat(p, wsel, 0, 128)
            b.cp('act', self.kS[g][:, m * MT:(m + 1) * MT], p)
        if NSTAGE < 1.29:
            return bail()
        for st in range(4):
            p = nps()
            self.proj_tok(p, st, wb, 384, 128)
            for g in range(2 if NSTAGE >= 1.2915 else 0):
                b.cp('dve', self.vS[g][:, n0 + st, 0:64], p[:, g * 64:(g + 1) * 64])
        if NSTAGE < 1.35:
            return bail()
        wb = self.wload(w_in_l, OFF['nsa_kw'], 280)
        for g in range(2):
            b.cp('dve', wsel[:, :, 0:64], wb[:, :, g * 64:(g + 1) * 64])
            b.cp('dve', wsel[:, :, 64:128], wb[:, :, g * 64:(g + 1) * 64])
            p = nps()
            self.proj_feat(p, wsel, 0, 128)
            s0 = (n0 % 8) * 128
            b.cp('act', self.kW[g][:, s0:s0 + 512], p)
        for st in range(4):
            p = nps()
            self.proj_tok(p, st, wb, 128, 152)
            for g in range(2):
                b.cp('dve', self.vW[g][:, (n0 + st) % 8, 0:64], p[:, g * 64:(g + 1) * 64])
            b.act(self.gsig[st], p[:, 128:152], AF.Sigmoid)
        if NSTAGE < 1.45:
            return bail()
        wb = self.wload(w_in_l, OFF['nsa_z'], 512)
        self.z_block(wb, 0, None)
        for g in range(2 if NSTAGE >= 2 else 0):
            pC = nps()
            for p_ in range(32):
                b.mm(pC[:, 0:32], self.W1[:, p_, :], self.rawc[g][:, p_:p_ + 497:16], start=p_ == 0, stop=p_ == 31)
            b.act(self.hid, pC[:, 0:32], AF.Silu, bias=self.cpos[:, 0:1])
            pK = nps()
            b.mm(pK[:, 0:32], self.W2k, self.hid)
            b.mm(pK[:, 32:64], self.W2v, self.hid)
            c0 = 32 * m
            cnt = 32
            b.cp('dve', self.kC[g][:, c0:c0 + cnt], pK[:, 0:32])
            b.cp('dve', self.vcT[g][:, c0:c0 + cnt], pK[:, 32:64])
            if m == 0:
                b.memset('pool', self.kC[g][:, 0:1], 0.0)
                b.memset('pool', self.vcT[g][:, 0:1], 0.0)
            for kt in sorted({c0 // 128, (c0 + cnt - 1) // 128}):
                b.tr(self.PTb[:, 0:128], self.vcT[g][:, kt * 128:(kt + 1) * 128], self.ident_b)
                b.cp('dve', self.vC[g][:, kt, 0:64], self.PTb[:, 0:64])
        for st in range(4):
            if NSTAGE >= 3:
                self.sub(m, st)
            self.emit_yT(st)

    def pbuf(self):
        self.pi += 1
        return self.Pb[self.pi % 3]

    def sub(self, m, st):
        b, nps, PS = self.b, self.nps, self.PS
        n = 4 * m + st
        tk = slice(st * 128, (st + 1) * 128)
        f4 = lambda t: t.re("p h i -> p (h i)")
        pOc, pOs, pOw = [PS[0], PS[1]], PS[2], PS[3]
        on = self.on
        causal = lambda t: b.asel(t, t, [[0, 4], [1, 128]], ALU.is_ge, 0.0, 0, -1)
        for g in range(2):
            qrhs = self.qm[:, 4 * g:4 * g + 4, tk]
            arhs = self.qA[:, 4 * g:4 * g + 4, tk]
            kts = [kt for kt in (0, 1) if n >= 16 * kt]
            for ki, kt in enumerate(kts):
                ks_ = slice(kt * 128, (kt + 1) * 128)
                pS = nps(4)
                b.mm(pS, self.kC[g][:, ks_], qrhs, start=True, stop=False)
                b.mm(pS, self.EKc[:, ks_], arhs, start=False, stop=True)
                Pt = self.pbuf()
                b.act(f4(Pt), pS, AF.Exp)
                if n < 16 * kt + 16:
                    b.asel(Pt, Pt, [[0, 4], [1, 128]], ALU.is_ge, 0.0, 128 * n - 2048 * kt - 15, -16)
                for hh in range(4):
                    col = (hh % 2) * 129
                    b.mm(pOc[hh // 2][:, col:col + 129], Pt[:, hh, :], self.vC[g][:, kt, 0:129],
                         start=(ki == 0 and hh % 2 == 0), stop=(ki == len(kts) - 1))
            for bnk in range(2):
                v = pOc[bnk][:, 0:258].re("p (h c) -> p h c", h=2)
                b.cp('dve', self.dall[:, 0, 2 * bnk:2 * bnk + 2], v[:, :, 64])
            rcc = self.rall[:, 0, :]
            b.ts('dve', rcc, self.dall[:, 0, :], 1e-30, ALU.max)
            b.recip(rcc, rcc)
            imp = self.imp
            b.ts('dve', imp, pOc[0][:, 65:129], rcc[:, 0:1], ALU.mult)
            b.stt(imp, pOc[0][:, 194:258], rcc[:, 1:2], imp, ALU.mult, ALU.add)
            b.stt(imp, pOc[1][:, 65:129], rcc[:, 2:3], imp, ALU.mult, ALU.add)
            b.stt(imp, pOc[1][:, 194:258], rcc[:, 3:4], imp, ALU.mult, ALU.add)
            cur, val, fz = self.cur, self.val, self.fz
            b.ts('dve', cur, self.half01, float(2 * n), ALU.add)
            b.ts('dve', val, self.jidx, cur[:, 0:1], ALU.is_le)
            b.tt('dve', imp, imp, val, ALU.mult)
            b.ts('dve', val, val, -1.0, ALU.add)
            b.tt('dve', imp, imp, val, ALU.add)
            b.ts('dve', fz, self.jidx, cur[:, 0:1], ALU.is_equal)
            b.ts('dve', val, self.jidx, 1.0, ALU.add, cur[:, 0:1], ALU.is_equal)
            b.tt('dve', fz, fz, val, ALU.add)
            b.tt('dve', fz, fz, self.e0, ALU.add)
            b.stt(imp, fz, 1.0e4, imp, ALU.mult, ALU.max)
            b.max8(self.m8a, imp)
            b.mrep(self.imp2, self.m8a, imp, -2.0)
            b.max8(self.m8b, self.imp2)
            b.ts('dve', self.thr, self.m8b[:, 7:8], 0.0, ALU.max)
            b.ts('dve', self.selb, imp, self.thr[:, 0:1], ALU.is_ge, 30000.0, ALU.mult)
            b.ts('dve', self.selbb[:, 0:64], self.selb, -30000.0, ALU.add)
            def scores(kind, kt):
                ks_ = slice(kt * 128, (kt + 1) * 128)
                pS = nps(4)
                if kind == 's':
                    b.mm(pS, self.kS[g][:, ks_], qrhs, start=True, stop=False)
                    b.mm(pS, self.EK[:, ks_], f4(self.qS), start=False, stop=True)
                else:
                    sl = kt % 8
                    b.mm(pS, self.kW[g][:, sl * 128:(sl + 1) * 128], qrhs, start=True, stop=False)
                    b.mm(pS, self.EK[:, ks_], arhs, start=False, stop=True)
                Pt = self.pbuf()
                b.act(f4(Pt), pS, AF.Exp)
                if kt == n:
                    causal(Pt)
                if kind == 'w' and kt == n - 4:
                    b.asel(Pt, Pt, [[0, 4], [-1, 128]], ALU.is_ge, 0.0, -1, 1)
                return Pt

            def pv(kind, kt, Pt, first, last):
                for hh in range(4):
                    if kind == 's':
                        b.mm(pOs[:, hh * 65:(hh + 1) * 65], Pt[:, hh, :], self.vS[g][:, kt, 0:65],
                             start=(first and hh == 0), stop=last)
                    else:
                        b.mm(pOw[:, hh * 65:(hh + 1) * 65], Pt[:, hh, :], self.vW[g][:, kt % 8, 0:65],
                             start=(first and hh == 0), stop=last)

            wk0 = max(0, n - 4)

            def run_items(items):
                prev = None
                for it in items:
                    Pt = scores(it[0], it[1])
                    if prev is not None:
                        pv(prev[0][0], prev[0][1], prev[1], prev[0][2], prev[0][3])
                    prev = (it, Pt)
                pv(prev[0][0], prev[0][1], prev[1], prev[0][2], prev[0][3])

            run_items([('w', kt, kt == wk0, kt == n) for kt in range(wk0, n + 1)])
            b.tr(self.PTb[:, 0:128], self.selbb, self.ident_b)
            b.cp('dve', self.qS[0:64], self.PTb[0:64, 0:128].bc(1, [64, 4, 128]))
            b.cp('act', self.qS[64:68], self.qA[64:68, 4 * g:4 * g + 4, tk])
            run_items([('s', kt, kt == 0, kt == n) for kt in range(n + 1)])
            vs_ = pOs[:, 0:260].re("p (h c) -> p h c", h=4)
            vw_ = pOw[:, 0:260].re("p (h c) -> p h c", h=4)
            b.cp('dve', self.dall[:, 1, :], vs_[:, :, 64])
            b.cp('dve', self.dall[:, 2, :], vw_[:, :, 64])
            b.ts('dve', self.rall[:, 1:3, :], self.dall[:, 1:3, :], 1e-30, ALU.max)
            b.recip(self.rall[:, 1:3, :], self.rall[:, 1:3, :])
            gv = self.gsig[st][:, 12 * g:12 * g + 12].re("p (h b) -> p b h", b=3)
            b.tt('dve', self.coef, self.rall, gv, ALU.mult)
            for bnk in range(2):
                v = pOc[bnk][:, 0:258].re("p (h c) -> p h c", h=2)
                b.tt('dve', on[:, 4 * g + 2 * bnk:4 * g + 2 * bnk + 2, :], v[:, :, 0:64],
                     self.coef[:, 0, 2 * bnk:2 * bnk + 2].bc(2, [128, 2, 64]), ALU.mult)
            b.tt('dve', self.tmp4, vs_[:, :, 0:64], self.coef[:, 1, :].bc(2, [128, 4, 64]), ALU.mult)
            b.tt('pool', on[:, 4 * g:4 * g + 4, :], on[:, 4 * g:4 * g + 4, :], self.tmp4, ALU.add)
            b.tt('dve', self.tmp5, vw_[:, :, 0:64], self.coef[:, 2, :].bc(2, [128, 4, 64]), ALU.mult)
            b.tt('pool', on[:, 4 * g:4 * g + 4, :], on[:, 4 * g:4 * g + 4, :], self.tmp5, ALU.add)
        b.tt('dve', self.ybr[st], on.re("p h d -> p (h d)"), self.zz[st], ALU.mult)


def kernel(**inputs):
    depth, S_len, n_cores = 4, 4096, 8
    x = np.asarray(inputs['x'], dtype=np.float32)
    nc, _ = build(S_len, depth)
    pin = prep_inputs(inputs, depth)
    hc = host_constants(S_len)
    in_maps = []
    for i in range(n_cores):
        d = dict(pin)
        d.update(hc)
        d['x'] = np.ascontiguousarray(x[i])
        in_maps.append(d)
    res = run_bass_kernel_spmd(nc, in_maps, core_ids=list(range(n_cores)))
    return np.stack([np.asarray(r['y'], dtype=np.float32) for r in res.results], axis=0)
```

```python
from contextlib import ExitStack
import os
NSTAGE = float(os.environ.get('NSTAGE', '99'))
SENG = os.environ.get('SENG', '').split(',')
import numpy as np
import concourse.bass as bass
import concourse.mybir as mybir
from concourse.bass_utils import run_bass_kernel_spmd

F32 = mybir.dt.float32
BF16 = mybir.dt.bfloat16
I32 = mybir.dt.int32
ALU = mybir.AluOpType
AF = mybir.ActivationFunctionType
AX = mybir.AxisListType

ENGS = ('pe', 'act', 'dve', 'pool', 'sp')
NDSEM = 12


class Tile:
    def __init__(self, name, ap, rid):
        self.name, self.ap, self.rid = name, ap, rid

    def __getitem__(self, k):
        return Tile(self.name, self.ap[k], self.rid)

    def re(self, s, **kw):
        return Tile(self.name, self.ap.rearrange(s, **kw), self.rid)

    def bc(self, axis, shape):
        return Tile(self.name, self.ap.unsqueeze(axis).to_broadcast(list(shape)), self.rid)

    def v(self, ap):
        return Tile(self.name, ap, self.rid)


class Op:
    __slots__ = ('eng', 'fn', 'deps', 'signal', 'count', 'dma', 'dsem', 'dcount', 'idx', 'cost', 'lat', 'seq', 'nrem', 'users', 'rt', 'fin')


class Sched:
    def __init__(self, nc):
        self.nc = nc
        self.es = ExitStack()
        self.ops = {e: [] for e in ENGS}
        self.lastw = {}
        self.readers = {}
        self.ndma = {e: 0 for e in ENGS}
        self.dma_ops = {e: [] for e in ENGS}
        self.nres = 0

    def sb(self, name, shape, dtype):
        t = self.es.enter_context(self.nc.sbuf_tensor("sb_" + name, list(shape), dtype))
        self.nres += 1
        return Tile(name, t[:] if hasattr(t, '__getitem__') else t, self.nres)

    def ps(self, name, shape, dtype):
        t = self.es.enter_context(self.nc.psum_tensor("pm_" + name, list(shape), dtype))
        self.nres += 1
        return Tile(name, t[:], self.nres)

    def res(self, name):
        self.nres += 1
        return Tile(name, None, self.nres)

    def view(self, tile, ap, own=False):
        if own:
            self.nres += 1
            return Tile(tile.name, ap, self.nres)
        return Tile(tile.name, ap, tile.rid)

    def _deps(self, reads, writes):
        deps = []
        for r in reads:
            w = self.lastw.get(r.rid)
            if w is not None:
                deps.append(w)
        for r in writes:
            w = self.lastw.get(r.rid)
            if w is not None:
                deps.append(w)
            deps.extend(self.readers.get(r.rid, ()))
        return deps

    def _record(self, op, reads, writes):
        for r in writes:
            self.lastw[r.rid] = op
            self.readers[r.rid] = []
        for r in reads:
            if self.lastw.get(r.rid) is op:
                continue
            self.readers.setdefault(r.rid, []).append(op)

    def op(self, eng, fn, reads=(), writes=(), cost=300.0):
        o = Op()
        o.eng, o.fn, o.dma, o.signal, o.count = eng, fn, False, False, 0
        o.cost = o.lat = cost
        self.nseq = getattr(self, 'nseq', 0) + 1
        o.seq = self.nseq
        o.deps = self._deps(reads, writes)
        o.idx = len(self.ops[eng])
        self.ops[eng].append(o)
        self._record(o, reads, writes)
        return o

    def dma(self, eng, out_ap, in_ap, reads=(), writes=(), out=False, **kw):
        o = Op()
        o.eng, o.dma, o.signal, o.count = eng, True, True, 0
        oa = out_ap.ap if isinstance(out_ap, Tile) else out_ap
        ia = in_ap.ap if isinstance(in_ap, Tile) else in_ap
        o.fn = lambda e: e.dma_start(out=oa, in_=ia, **kw)
        o.deps = self._deps(reads, writes)
        self.ndma[eng] += 1
        nbytes = 1
        for d_ in oa.shape:
            nbytes *= d_
        nbytes *= 2 if oa.dtype == BF16 else 4
        o.cost = 150.0 if eng == 'sp' else 1500.0
        o.lat = 2500.0 + nbytes / 60.0
        self.nseq = getattr(self, 'nseq', 0) + 1
        o.seq = self.nseq
        o.idx = len(self.ops[eng])
        self.ops[eng].append(o)
        self._record(o, reads, writes)
        if out:
            self.out_dmas = getattr(self, 'out_dmas', []) + [o]
        return o

    def schedule(self, window=int(os.environ.get("SWIN", "40"))):
        allops = [o for e in ENGS for o in self.ops[e]]
        for o in allops:
            o.users = []
            o.rt = 0.0
            o.fin = None
        for o in allops:
            ds = set(id(d) for d in o.deps)
            o.deps = [d for d in {id(d): d for d in o.deps}.values()]
            o.nrem = len(o.deps)
            for d in o.deps:
                d.users.append(o)
        pend = {e: list(self.ops[e]) for e in ENGS}
        new = {e: [] for e in ENGS}
        free_t = {e: 0.0 for e in ENGS}
        remaining = len(allops)
        while remaining:
            best = None
            bkey = None
            for e in ENGS:
                pe_ = pend[e]
                ft = free_t[e]
                for k in range(min(window if e in SENG else 1, len(pe_))):
                    o = pe_[k]
                    if o.nrem:
                        continue
                    st = o.rt if o.rt > ft else ft
                    key = (st, o.seq)
                    if bkey is None or key < bkey:
                        bkey, best, bk = key, o, k
                    if st <= ft:
                        break
            o = best
            e = o.eng
            pend[e].pop(bk) if pend[e][bk] is o else pend[e].remove(o)
            st = bkey[0]
            o.fin = st + o.lat
            free_t[e] = st + o.cost
            new[e].append(o)
            for u in o.users:
                u.nrem -= 1
                if o.fin > u.rt:
                    u.rt = o.fin
            remaining -= 1
        self.ops = new
        self.sim_time = max(free_t.values())

    def emit(self):
        nc = self.nc
        if SENG != ['']:
            self.schedule()
        for e in ENGS:
            i = 0
            prev = []
            for o in self.ops[e]:
                if o.dma:
                    o.dsem = (e, i % NDSEM)
                    o.dcount = 16 * (i // NDSEM + 1)
                    if i >= NDSEM:
                        o.deps.append(prev[i - NDSEM])
                    prev.append(o)
                    i += 1
        fin = Op()
        fin.eng, fin.dma, fin.signal, fin.count, fin.fn = 'sp', False, False, 0, None
        fin.deps = list(getattr(self, 'out_dmas', []))
        self.ops['sp'].append(fin)
        for e in ENGS:
            for o in self.ops[e]:
                for d in o.deps:
                    if not d.dma:
                        if d.eng == 'pe' and o.eng == 'pe':
                            continue
                        d.signal = True
        for e in ENGS:
            c = 0
            for o in self.ops[e]:
                if o.signal and not o.dma:
                    c += 1
                    o.count = c
        sems = {e: self.es.enter_context(nc.semaphore("s_" + e)) for e in ENGS}
        dsems = {}
        for e in ENGS:
            if self.ndma[e]:
                for k in range(min(NDSEM, self.ndma[e])):
                    dsems[(e, k)] = self.es.enter_context(nc.semaphore("d_%s%d" % (e, k)))
        self.stats = {}

        def run(ename, eng):
            known = {}
            nw = 0
            for o in self.ops[ename]:
                need = {}
                for d in o.deps:
                    if d.dma:
                        key, val = ('d',) + d.dsem, d.dcount
                    else:
                        if d.eng == 'pe' and ename == 'pe':
                            continue
                        key, val = ('c', d.eng), d.count
                    if known.get(key, 0) >= val:
                        continue
                    if need.get(key, 0) < val:
                        need[key] = val
                for key, val in need.items():
                    s = sems[key[1]] if key[0] == 'c' else dsems[(key[1], key[2])]
                    eng.wait_ge(s, val)
                    known[key] = val
                    nw += 1
                if o.fn is None:
                    continue
                ins = o.fn(eng)
                if o.dma:
                    ins.then_inc(dsems[o.dsem], 16)
                elif o.signal:
                    ins.then_inc(sems[ename], 1)
            self.stats[ename] = (len(self.ops[ename]), nw)

        with nc.Block() as block:
            @block.sync
            def _(eng):
                run('sp', eng)

            @block.scalar
            def _(eng):
                run('act', eng)

            @block.vector
            def _(eng):
                run('dve', eng)

            @block.gpsimd
            def _(eng):
                run('pool', eng)

            @block.tensor
            def _(eng):
                run('pe', eng)
        self.es.close()


def _fsz(ap):
    p = 1
    for d in ap.shape[1:]:
        p *= d
    return p


class B:
    def __init__(self, S):
        self.S = S

    @staticmethod
    def _t(xs):
        return [x for x in xs if isinstance(x, Tile)]

    @staticmethod
    def _a(x):
        return x.ap if isinstance(x, Tile) else x

    def mm(self, out, lhsT, rhs, start=True, stop=True):
        o, l, r = out.ap, lhsT.ap, rhs.ap
        f32 = 4.0 if l.dtype == F32 else 1.0
        self.S.op('pe', lambda e: e.matmul(o, l, r, start=start, stop=stop, skip_group_check=True),
                  reads=[lhsT, rhs], writes=[out], cost=30.0 + f32 * (max(_fsz(r), 32) + 100) / 2.4)

    def tr(self, out, in_, ident):
        o, i, d = out.ap, in_.ap, ident.ap
        self.S.op('pe', lambda e: e.transpose(o, i, d), reads=[in_, ident], writes=[out], cost=120.0)

    def act(self, out, in_, func, bias=None, scale=None, accum=None, eng='act'):
        o, i = out.ap, in_.ap
        kw = {}
        if bias is not None:
            kw['bias'] = self._a(bias)
        if scale is not None:
            kw['scale'] = self._a(scale)
        if accum is not None:
            kw['accum_out'] = accum.ap
        w = [out] + ([accum] if accum is not None else [])
        self.S.op('act', lambda e: e.activation(o, i, func, **kw),
                  reads=self._t([in_, bias, scale]), writes=w, cost=230.0 + _fsz(o) / 1.2)

    def tt(self, eng, out, a, b, op):
        o, x, y = out.ap, a.ap, b.ap
        self.S.op(eng, lambda e: e.tensor_tensor(o, x, y, op), reads=[a, b], writes=[out], cost=(120.0 + _fsz(o) / 0.96) if eng == 'dve' else (250.0 + _fsz(o) / 0.6))

    def ts(self, eng, out, a, s1, op0, s2=None, op1=None):
        o, x = out.ap, a.ap
        a1, a2 = self._a(s1), self._a(s2)
        if op1 is None:
            self.S.op(eng, lambda e: e.tensor_scalar(o, x, a1, None, op0), reads=self._t([a, s1]), writes=[out], cost=(120.0 + _fsz(o) / 1.5) if eng == 'dve' else (250.0 + _fsz(o) / 0.6))
        else:
            self.S.op(eng, lambda e: e.tensor_scalar(o, x, a1, a2, op0, op1), reads=self._t([a, s1, s2]), writes=[out], cost=(120.0 + _fsz(o) / 1.5) if eng == 'dve' else (250.0 + _fsz(o) / 0.6))

    def stt(self, out, a, s, b, op0, op1):
        o, x, y, sc = out.ap, a.ap, b.ap, self._a(s)
        self.S.op('dve', lambda e: e.scalar_tensor_tensor(o, x, sc, y, op0, op1), reads=self._t([a, s, b]), writes=[out], cost=120.0 + _fsz(o) / 0.96)

    def cp(self, eng, out, in_):
        o, i = out.ap, in_.ap
        if eng == 'act':
            self.S.op('act', lambda e: e.copy(o, i), reads=[in_], writes=[out], cost=230.0 + _fsz(o) / 1.2)
        else:
            self.S.op(eng, lambda e: e.tensor_copy(o, i), reads=[in_], writes=[out], cost=(120.0 + _fsz(o) / 1.5) if eng == 'dve' else (250.0 + _fsz(o) / 0.6))

    def red(self, out, in_, op=None, axis=None):
        o, i = out.ap, in_.ap
        op = op or ALU.add
        axis = axis or AX.X
        self.S.op('dve', lambda e: e.tensor_reduce(o, i, axis, op), reads=[in_], writes=[out], cost=120.0 + _fsz(i) / 0.96)

    def memset(self, eng, out, val):
        o = out.ap
        self.S.op(eng, lambda e: e.memset(o, val), reads=[], writes=[out], cost=150.0 + _fsz(o) / 0.96)

    def asel(self, out, in_, pattern, cmp, fill, base, cm):
        o, i = out.ap, in_.ap
        def fn(e):
            try:
                return e.affine_select(o, i, pattern, cmp, fill, base=base, channel_multiplier=cm)
            except Exception:
                print("ASEL FAIL", pattern, base, cm, fill, o)
                raise
        self.S.op('pool', fn, reads=[in_], writes=[out], cost=300.0 + _fsz(o) / 0.9)

    def scan(self, out, d0, d1, init, op0, op1):
        o, x, y, ii = out.ap, d0.ap, d1.ap, self._a(init)
        self.S.op('dve', lambda e: e.tensor_tensor_scan(o, x, y, ii, op0, op1), reads=self._t([d0, d1, init]), writes=[out], cost=120.0 + _fsz(o) * 2 / 0.96)

    def recip(self, out, in_):
        o, i = out.ap, in_.ap
        self.S.op('dve', lambda e: e.reciprocal(o, i), reads=[in_], writes=[out])

    def max8(self, out, in_):
        o, i = out.ap, in_.ap
        self.S.op('dve', lambda e: e.max(o, i), reads=[in_], writes=[out])

    def mrep(self, out, rep, vals, imm):
        o, r, v = out.ap, rep.ap, vals.ap
        self.S.op('dve', lambda e: e.match_replace(o, r, v, imm), reads=[rep, vals], writes=[out])

    def dma(self, eng, out, in_, out_final=False, **kw):
        self.S.dma(eng, out, in_, reads=self._t([in_]), writes=self._t([out]), out=out_final, **kw)


D_MODEL = 1024
NORM_EPS = 1e-6
IN_SPLITS = (
    ('gdn_q', 512), ('gdn_k', 512), ('gdn_v', 512), ('gdn_beta', 4), ('gdn_a', 4), ('gdn_z', 512),
    ('gla_q', 256), ('gla_k', 256), ('gla_v', 512), ('gla_gk', 16), ('gla_z', 512),
    ('ssd_x', 512), ('ssd_b', 256), ('ssd_c', 256), ('ssd_dt', 8), ('ssd_z', 512),
    ('nsa_q', 512), ('nsa_kc', 128), ('nsa_vc', 128), ('nsa_ks', 128), ('nsa_vs', 128),
    ('nsa_kw', 128), ('nsa_vw', 128), ('nsa_gate', 24), ('nsa_z', 512),
    ('merge_gate', 4096),
)
OFF = {}
_s = 0
for _n, _w in IN_SPLITS:
    OFF[_n] = _s
    _s += _w
D_IN = _s
MT = 512
NEG = -30000.0


def host_constants(S_len):
    c = {}
    ident = np.eye(128, dtype=np.float32)
    U = np.triu(np.ones((128, 128), np.float32))
    ones = np.ones((128, 128), np.float32)
    jidx = np.tile(np.arange(64, dtype=np.float32)[None, :], (128, 1))
    e0 = np.zeros((128, 64), np.float32)
    e0[:, 0] = 1.0
    half = (np.arange(128) >= 64).astype(np.float32)[:, None]
    c['cst'] = np.concatenate([ident, U, ones, jidx, e0, half], axis=1)
    slopes = 2.0 ** (-np.arange(1, 9, dtype=np.float64))
    t = np.arange(S_len)
    qaug = np.zeros((4, 8, S_len), np.float32)
    for h in range(8):
        qaug[0, h] = slopes[h] * 64
        qaug[1, h] = slopes[h]
        qaug[2, h] = -slopes[h] * 64 * (t // 64)
        qaug[3, h] = -slopes[h] * (t % 64)
    c['qaug'] = qaug
    kaug = np.stack([t // 64, t % 64, np.ones_like(t), np.ones_like(t)]).astype(np.float32)
    c['kaug'] = kaug
    ncp = 256
    cp = np.arange(ncp) * 16 + 31
    kc_ = np.stack([cp // 64, cp % 64, np.ones_like(cp), np.ones_like(cp)]).astype(np.float32)
    c['kaugc'] = np.concatenate([np.zeros((4, 1), np.float32), kc_[:, :-1]], axis=1)
    nsel = S_len // 64
    cs = np.arange(ncp) * 16
    ss = np.arange(64) * 64
    ov = ((cs[:, None] <= ss[None, :] + 63) & (cp[:, None] >= ss[None, :])).astype(np.float32)
    ov[:, nsel:] = 0.0
    ov = np.concatenate([np.zeros((1, 64), np.float32), ov[:-1]], axis=0)
    c['overlap'] = ov
    E = np.zeros((64, S_len), np.float32)
    E[t // 64, t] = 1.0
    c['esel'] = E
    return c


PARAM_SHAPES = {
    'norm_pre': [1024], 'norm_post': [1024],
    'conv_aT': [128, 12, 4], 'a_log_a': [4], 'dt_bias_a': [4], 'onorm_a': [128],
    'w_gk': [16, 256], 'b_gkT': [128, 2], 'onorm_b': [128],
    'conv_cT': [128, 8, 4], 'conv_bias_cT': [128, 8], 'a_log_c': [8], 'dt_bias_c': [8], 'd_skip_c': [8],
    'onorm_c': [512], 'cmp_pos_kT': [64, 32], 'cmp_pos_vT': [64, 32],
    'w_ck1': [2048, 64], 'w_ck2': [64, 64], 'w_cv1': [2048, 64], 'w_cv2': [64, 64],
    'w_br': [4, 512, 1024], 'w_out': [1024, 1024], 'w_in': [1024, D_IN],
}


def prep_inputs(inp, depth):
    o = {}
    f = lambda a: np.ascontiguousarray(np.asarray(a, dtype=np.float32))
    for k in ('norm_pre', 'norm_post', 'a_log_a', 'dt_bias_a', 'onorm_a', 'w_gk', 'onorm_b', 'a_log_c',
              'dt_bias_c', 'd_skip_c', 'onorm_c', 'w_ck1', 'w_ck2', 'w_cv1', 'w_cv2', 'w_br', 'w_out', 'w_in'):
        o[k] = f(inp[k][:depth])
    o['conv_aT'] = f(np.asarray(inp['conv_a'])[:depth].reshape(depth, 4, 12, 128).transpose(0, 3, 2, 1))
    o['conv_cT'] = f(np.asarray(inp['conv_c'])[:depth].reshape(depth, 4, 8, 128).transpose(0, 3, 2, 1))
    o['conv_bias_cT'] = f(np.asarray(inp['conv_bias_c'])[:depth].reshape(depth, 8, 128).transpose(0, 2, 1))
    o['b_gkT'] = f(np.asarray(inp['b_gk'])[:depth].reshape(depth, 2, 128).transpose(0, 2, 1))
    o['cmp_pos_kT'] = f(np.asarray(inp['cmp_pos_k'])[:depth].transpose(0, 2, 1))
    o['cmp_pos_vT'] = f(np.asarray(inp['cmp_pos_v'])[:depth].transpose(0, 2, 1))
    return o


def build(S_len=4096, depth=4, branches=(0, 1, 2, 3)):
    nc = bass.Bass("TRN2", target_bir_lowering=False)
    NT = S_len // 128
    NM = S_len // MT
    S = Sched(nc)
    b = B(S)
    dr = {}
    dr['x'] = nc.dram_tensor("x", [S_len, 1024], F32, kind="ExternalInput").ap()
    for k, shp in PARAM_SHAPES.items():
        dr[k] = nc.dram_tensor(k, [depth] + shp, F32, kind="ExternalInput").ap()
    hc = host_constants(S_len)
    for k, v in hc.items():
        dr[k] = nc.dram_tensor(k, list(v.shape), F32, kind="ExternalInput").ap()
    y = nc.dram_tensor("y", [S_len, 1024], F32, kind="ExternalOutput").ap()
    wq = {'w_in': nc.dram_tensor("wq_in", [depth, 1024, D_IN], BF16, kind="Internal").ap(),
          'w_br': nc.dram_tensor("wq_br", [depth, 2048, 1024], BF16, kind="Internal").ap(),
          'w_out': nc.dram_tensor("wq_out", [depth, 1024, 1024], BF16, kind="Internal").ap()}
    wqres = {}
    yres = [S.res("y%d" % g) for g in range(NT)]

    def dt(ap, res=None):
        return Tile('dram', ap, res.rid if res is not None else 0)

    cst = S.sb("cst", [128, 513], F32)
    b.dma('sp', cst, dt(dr['cst']))
    ident_f, U_f, ones_f = cst[:, 0:128], cst[:, 128:256], cst[:, 256:384]
    cstb = S.sb("cstb", [128, 384], BF16)
    b.cp('dve', cstb, cst[:, 0:384])
    ident_b, U_b, ones_b = cstb[:, 0:128], cstb[:, 128:256], cstb[:, 256:384]
    mhalf = S.sb("mhalf", [128, 1], F32)
    b.memset('dve', mhalf, -0.5)

    PS = [S.ps("ps%d" % i, [128, 512], F32) for i in range(7)]
    PTb = S.ps("ptb", [128, 1024], BF16)
    psi = [0]

    def nps(lo=0):
        psi[0] += 1
        return PS[lo + psi[0] % (7 - lo)]

    h4 = lambda t: t.re("p (h i) -> p h i", h=4)

    pools = {}

    def scr(cls, i):
        key = (cls, i)
        if key not in pools:
            pools[key] = S.sb("scr%s%d" % (cls, i), [128, 512], F32 if cls == 'A' else BF16)
        return pools[key]

    FM = S.sb("FM", [128, 16, MT], BF16)
    xts = [S.sb("xt%d" % i, [128, 1024], F32) for i in range(1)]
    hb = S.sb("hb", [128, 1024], BF16)
    junk = hb
    hT = S.sb("hT", [128, 8, MT], BF16)
    wbufs = [S.sb("wb%d" % i, [128, 8, 528], BF16) for i in range(2)]
    wi = [0]
    merged = [S.sb("mg%d" % i, [128, 1024], F32) for i in range(4)]
    gpre = S.sb("gpre", [128, 1024], F32)
    gpost = S.sb("gpost", [128, 1024], F32)
    ssq = S.sb("ssq", [128, 1], F32)
    rstd = S.sb("rstd", [128, 1], F32)
    ybr = [S.sb("ybr%d" % i, [128, 512], BF16) for i in range(4)]
    yT = S.sb("yT", [128, 4, 4, 128], BF16)
    zz = [S.sb("zz%d" % i, [128, 512], BF16) for i in range(4)]
    raw = [S.sb("raw%d" % i, [128, 515], BF16) for i in range(2)]
    dg = [S.sb("dg%d" % i, [128, 4, 128], BF16) for i in range(2)]
    sg = scr('A', 0)
    tmpm = scr('A', 1)
    mgb = hb
    mgT = yT[:, 0:2].re("p a k t -> p (a k) t")

    def bcast_load(tile, src_ap, n):
        b.dma('pool', tile, dt(src_ap.partition_broadcast(128)))

    def convert_layer(l):
        for name, src2d in (('w_in', dr['w_in'][l]), ('w_br', dr['w_br'][l].rearrange("n k c -> (n k) c")),
                            ('w_out', dr['w_out'][l])):
            r = S.res("wq_%s%d" % (name, l))
            wqres[(name, l)] = r
            ncol = src2d.shape[1]
            nrow = src2d.shape[0]
            for r0 in range(0, nrow, 1024):
                for c0 in range(0, ncol, 2048):
                    c1 = min(c0 + 2048, ncol)
                    S.dma('pool', wq[name][l][r0:r0 + 1024, c0:c1], src2d[r0:r0 + 1024, c0:c1], reads=[], writes=[r])

    def wload(key, col0, ncols, rows8=True):
        name, l, row0, nrows = key
        wb = wbufs[wi[0] % 2]
        wi[0] += 1
        src = wq[name][l][row0:row0 + nrows, :].rearrange("(k p) n -> p k n", p=128)
        nk = src.shape[1]
        b.dma('sp', wb[:, 0:nk, 0:ncols], dt(src[:, :, col0:col0 + ncols], wqres[(name, l)]))
        return wb

    def proj_tok(ps, st, wb, c0, n, o0=0):
        for kc in range(8):
            b.mm(ps[:, o0:o0 + n], hT[:, kc, st * 128:(st + 1) * 128], wb[:, kc, c0:c0 + n], start=kc == 0, stop=kc == 7)

    def proj_feat(ps, wb, c0, mch):
        for kc in range(8):
            b.mm(ps[0:mch, :], wb[:, kc, c0:c0 + mch], hT[:, kc, :], start=kc == 0, stop=kc == 7)

    def rms_rstd(out1, ss1, n):
        b.ts('dve', out1, ss1, 1.0 / n, ALU.mult, NORM_EPS, ALU.add)
        b.tt('pool', out1, out1, mhalf_k(out1.ap.shape[1]), ALU.pow)

    mh_cache = {}

    def mhalf_k(k):
        if k not in mh_cache:
            t = S.sb("mh%d" % k, [128, k], F32)
            b.memset('dve', t, -0.5)
            mh_cache[k] = t
        return mh_cache[k]

    def emit_yT(st):
        for kc in range(4):
            b.tr(PTb[:, kc * 128:(kc + 1) * 128], ybr[st][:, kc * 128:(kc + 1) * 128], ident_b)
        b.cp('dve', yT[:, st], PTb[:, 0:512].re("p (k t) -> p k t", k=4))

    ctx = dict(jidx=cst[:, 384:448], e0=cst[:, 448:512], half01=cst[:, 512:513], emit_yT=emit_yT, scr=scr, FM=FM, nc=nc, S=S, b=b, dr=dr, dt=dt, PS=PS, PTb=PTb, nps=nps, h4=h4, hT=hT, wload=wload,
               proj_tok=proj_tok, proj_feat=proj_feat, rms_rstd=rms_rstd, ident_f=ident_f, U_f=U_f,
               ones_f=ones_f, ident_b=ident_b, U_b=U_b, ones_b=ones_b, ybr=ybr, zz=zz, raw=raw, dg=dg,
               S_len=S_len, NT=NT, NM=NM, bcast_load=bcast_load, depth=depth, mhalf_k=mhalf_k)
    mixers = {}
    if 0 in branches:
        mixers[0] = GDN(ctx)
    if 1 in branches:
        mixers[1] = GLA(ctx)
    if 2 in branches:
        mixers[2] = SSD(ctx)
    if 3 in branches:
        mixers[3] = NSA(ctx)

    convert_layer(0)
    for l in range(depth):
        w_in_l = ('w_in', l, 0, 1024)
        if l + 1 < depth:
            convert_layer(l + 1)
        bcast_load(gpre, dr['norm_pre'][l:l + 1, :], 1024)
        bcast_load(gpost, dr['norm_post'][l:l + 1, :], 1024)
        for n in mixers:
            mixers[n].layer_setup(l)
        for m in range(NM):
            for st in range(4):
                g = m * 4 + st
                xt = xts[0]
                src = dr['x'] if l == 0 else y
                b.dma('pool', xt, dt(src[g * 128:(g + 1) * 128, :], yres[g]))
                b.act(junk, xt, AF.Square, accum=ssq)
                rms_rstd(rstd, ssq, 1024)
                b.stt(hb, xt, rstd[:, 0:1], gpre, ALU.mult, ALU.mult)
                for kc in range(8):
                    b.tr(PTb[:, kc * 128:(kc + 1) * 128], hb[:, kc * 128:(kc + 1) * 128], ident_b)
                b.cp('act', hT[:, :, st * 128:(st + 1) * 128], PTb.re("p (k t) -> p k t", k=8))
            first = True
            for n in (0, 1, 2, 3):
                if n not in mixers:
                    continue
                mixers[n].macro(l, m, w_in_l)
                for half in range(2):
                    wg = wload(w_in_l, OFF['merge_gate'] + n * 1024 + half * 512, 512)
                    wbr = wload(('w_br', l, n * 512, 512), half * 512, 512)
                    for st in range(4):
                        sg = scr('A', 0) if st % 2 == 0 else scr('A', 2)
                        tmpm = scr('A', 1) if st % 2 == 0 else scr('A', 3)
                        pg = nps()
                        proj_tok(pg, st, wg, 0, 512)
                        b.act(sg, pg, AF.Sigmoid)
                        pb = nps()
                        for kc in range(4):
                            b.mm(pb, yT[:, st, kc, :], wbr[:, kc, 0:512], start=kc == 0, stop=kc == 3)
                        mslice = merged[st][:, half * 512:(half + 1) * 512]
                        if first:
                            b.tt('dve', mslice, sg, pb, ALU.mult)
                        else:
                            b.tt('dve', tmpm, sg, pb, ALU.mult)
                            b.tt('pool', mslice, mslice, tmpm, ALU.add)
                first = False
            wo = [wload(('w_out', l, 0, 1024), half * 512, 512) for half in range(2)]
            for st in range(4):
                g = m * 4 + st
                osb = merged[st]
                b.cp('act', mgb, merged[st])
                for kc in range(8):
                    b.tr(PTb[:, kc * 128:(kc + 1) * 128], mgb[:, kc * 128:(kc + 1) * 128], ident_b)
                b.cp('dve', mgT, PTb.re("p (k t) -> p k t", k=8))
                for half in range(2):
                    po = nps()
                    for kc in range(8):
                        b.mm(po, mgT[:, kc, :], wo[half][:, kc, 0:512], start=kc == 0, stop=kc == 7)
                    b.cp('act', osb[:, half * 512:(half + 1) * 512], po)
                b.act(junk, osb, AF.Square, accum=ssq)
                rms_rstd(rstd, ssq, 1024)
                xt = xts[0]
                src = dr['x'] if l == 0 else y
                b.dma('pool', xt, dt(src[g * 128:(g + 1) * 128, :], yres[g]))
                b.stt(osb, osb, rstd[:, 0:1], gpost, ALU.mult, ALU.mult)
                b.tt('dve', osb, osb, xt, ALU.add)
                S.dma('pool', y[g * 128:(g + 1) * 128, :], osb.ap, reads=[osb], writes=[yres[g]], out=(l == depth - 1))
    S.emit()
    return nc, S


class Mixer:
    def __init__(self, ctx):
        self.__dict__.update(ctx)

    def conv_chunk(self, ps_in, convw, c, halo, out_fm, bias=None):
        b = self.b
        k = self.cc
        self.cc += 1
        raw, dg = self.raw[k % 2], self.dg[k % 2]
        b.cp('dve', raw[:, 0:3], halo)
        b.cp('act', raw[:, 3:515], ps_in)
        b.cp('dve', halo, raw[:, 512:515])
        b.tt('dve', dg, self.ident_b.bc(1, [128, 4, 128]), convw[:, c, :].bc(2, [128, 4, 128]), ALU.mult)
        p2 = self.nps()
        for t in range(4):
            b.mm(p2, dg[:, t, :], raw[:, t:t + 512], start=t == 0, stop=t == 3)
        if bias is None:
            b.act(out_fm, p2, AF.Silu)
        else:
            b.act(out_fm, p2, AF.Silu, bias=bias)

    def out_norm_heads(self, po, st, onz):
        b, S = self.b, self.S
        o = self.osb4
        b.cp('act', o, po)
        b.tt('pool', self.sq4, o, o, ALU.mult)
        b.red(self.ss4, self.h4(self.sq4))
        self.rms_rstd(self.rs4, self.ss4, 128)
        b.tt('dve', self.h4(o), self.h4(o), self.rs4.bc(2, [128, 4, 128]), ALU.mult)
        b.tt('dve', self.ybr[st], o, onz, ALU.mult)

    def z_block(self, wb, c0, onorm_b, per_head=True):
        b = self.b
        for st in range(4):
            pz = self.nps()
            self.proj_tok(pz, st, wb, c0, 512)
            b.act(self.zz[st], pz, AF.Silu)
            if onorm_b is not None:
                if per_head:
                    b.tt('pool', self.h4(self.zz[st]), self.h4(self.zz[st]), onorm_b.bc(1, [128, 4, 128]), ALU.mult)
                else:
                    b.tt('pool', self.zz[st], self.zz[st], onorm_b, ALU.mult)


class GDN(Mixer):
    def __init__(self, ctx):
        super().__init__(ctx)
        S = self.S
        self.cc = 0
        scr = self.scr
        A4 = lambda i: scr('A', i).re("p (h i) -> p h i", h=4)
        B4 = lambda i: scr('B', i).re("p (h i) -> p h i", h=4)
        self.fm = self.FM[:, 0:12, :]
        self.sqa, self.sqb = B4(9), B4(10)
        self.halo = [S.sb("gdn_h%d" % c, [128, 3], BF16) for c in range(12)]
        self.convw = S.sb("gdn_cw", [128, 12, 4], F32)
        self.nega = S.sb("gdn_nega", [128, 4], F32)
        self.dtb = S.sb("gdn_dtb", [128, 4], F32)
        self.onorm = S.sb("gdn_on", [128, 128], F32)
        self.Sf = S.sb("gdn_Sf", [128, 4, 128], F32)
        self.Sb = S.sb("gdn_Sb", [128, 4, 128], BF16)
        f = lambda n, k: S.sb("gdn_" + n, [128, k], F32)
        self.lnss, self.lnr, self.ba, self.e1, self.nlb = f("lnss", 8), f("lnr", 8), f("ba", 8), f("e1", 4), f("nlb", 4)
        self.apb, self.e2, self.sp, self.g, self.gcl = f("apb", 4), f("e2", 4), f("sp", 4), f("g", 4), f("gcl", 8)
        self.C3, self.X4, self.EX = f("C3", 12), f("X4", 16), f("EX", 16)
        self.D3 = [A4(2), A4(3), A4(4)]
        self.BJ, self.BA = A4(5), A4(6)
        self.E1, self.E2, self.E3 = A4(7), A4(8), A4(9)
        self.EB = B4(0)
        self.P = [A4(10), A4(11)]
        self.PT = [A4(12), A4(13)]
        self.AT = [A4(14), A4(15)]
        self.ATb, self.At, self.Rk, self.Kd = B4(1), B4(2), B4(3), B4(4)
        self.Vb, self.nW, self.Vn, self.qg = B4(5), B4(6), B4(7), B4(8)
        self.osb4 = scr('A', 0)
        self.sq4 = scr('A', 1)
        self.ss4, self.rs4 = f("ss4", 4), f("rs4", 4)
        self.ea = f("ea", 4)

    def layer_setup(self, l):
        b, dr, dt = self.b, self.dr, self.dt
        b.dma('pool', self.convw, dt(dr['conv_aT'][l]))
        self.bcast_load(self.ea, dr['a_log_a'][l:l + 1, :], 4)
        b.act(self.nega, self.ea, AF.Exp)
        b.ts('dve', self.nega, self.nega, -1.0, ALU.mult)
        self.bcast_load(self.dtb, dr['dt_bias_a'][l:l + 1, :], 4)
        self.bcast_load(self.onorm, dr['onorm_a'][l:l + 1, :], 128)
        b.memset('dve', self.Sf, 0.0)
        b.memset('dve', self.Sb, 0.0)
        for c in range(12):
            b.memset('pool', self.halo[c], 0.0)

    def macro(self, l, m, w_in_l):
        b, nps, h4 = self.b, self.nps, self.h4
        fm = self.fm
        for blk in range(3):
            wb = self.wload(w_in_l, blk * 512, 512)
            for cc in range(4):
                c = blk * 4 + cc
                p = nps()
                self.proj_feat(p, wb, cc * 128, 128)
                self.conv_chunk(p, self.convw, c, self.halo[c], fm[:, c, :])
        wb3 = self.wload(w_in_l, OFF['gdn_beta'], 520)
        self.z_block(wb3, 8, self.onorm)
        for st in range(4):
            self.sub(st, wb3)
            self.emit_yT(st)

    def sub(self, st, wb3):
        b, nps, h4 = self.b, self.nps, self.h4
        fm = self.fm
        tk = slice(st * 128, (st + 1) * 128)
        bc4 = lambda t: t.bc(2, [128, 4, 128])
        b.tt('pool', self.sqa, fm[:, 4:8, tk], fm[:, 4:8, tk], ALU.mult)
        b.tt('pool', self.sqb, fm[:, 0:4, tk], fm[:, 0:4, tk], ALU.mult)
        pq = nps()
        for c in range(8):
            sq_c = self.sqa[:, c, :] if c < 4 else self.sqb[:, c - 4, :]
            b.mm(pq[:, c:c + 1], sq_c, self.ones_b[:, 0:1])
        self.proj_tok(pq, st, wb3, 0, 8, o0=8)
        b.act(self.lnss, pq[:, 0:8], AF.Ln, bias=NORM_EPS)
        b.ts('dve', self.lnr, self.lnss, -0.5, ALU.mult)
        b.cp('dve', self.ba, pq[:, 8:16])
        b.act(self.e1, self.ba[:, 0:4], AF.Exp, scale=-1.0)
        b.act(self.nlb, self.e1, AF.Ln, bias=1.0)
        b.tt('dve', self.apb, self.ba[:, 4:8], self.dtb, ALU.add)
        b.act(self.e2, self.apb, AF.Exp)
        b.act(self.sp, self.e2, AF.Ln, bias=1.0)
        b.tt('dve', self.g, self.sp, self.nega, ALU.mult)
        pg = nps()
        b.mm(pg[:, 0:4], self.U_f, self.g)
        b.mm(pg[:, 4:8], self.ones_f, self.g)
        b.cp('dve', self.gcl, pg[:, 0:8])
        gc, gl = self.gcl[:, 0:4], self.gcl[:, 4:8]
        lnrk, lnrq = self.lnr[:, 0:4], self.lnr[:, 4:8]
        cA, cB, cJ = self.C3[:, 0:4], self.C3[:, 4:8], self.C3[:, 8:12]
        b.tt('dve', cJ, lnrk, gc, ALU.subtract)
        b.tt('dve', cA, gc, self.nlb, ALU.subtract)
        b.tt('dve', cA, cA, lnrk, ALU.add)
        b.stt(cB, gc, float(np.log(128.0 ** -0.5)), lnrq, ALU.add, ALU.add)
        X4 = self.X4
        b.cp('pool', X4[:, 0:4], cA)
        b.tt('pool', X4[:, 4:8], cJ, gl, ALU.add)
        b.ts('pool', X4[:, 8:12], self.nlb, -1.0, ALU.mult)
        b.cp('pool', X4[:, 12:16], gl)
        b.act(self.EX, X4, AF.Exp)
        sRk, sKd, sVb, dec = self.EX[:, 0:4], self.EX[:, 4:8], self.EX[:, 8:12], self.EX[:, 12:16]
        for v3 in range(3):
            b.tt('dve', self.D3[v3], self.ident_f.bc(1, [128, 4, 128]), bc4(self.C3[:, v3 * 4:(v3 + 1) * 4]), ALU.mult)
        b.cp('pool', self.BJ, bc4(cJ))
        b.cp('pool', self.BA, bc4(cA))
        f4 = lambda t: t.re("p h i -> p (h i)")
        pX1, pX2, pX3, pXB = nps(), nps(), nps(), nps()
        b.mm(pX1, self.ones_f, f4(self.D3[0]), start=True, stop=False)
        b.mm(pX1, self.ident_f, f4(self.BJ), start=False, stop=True)
        b.mm(pX2, self.ones_f, f4(self.D3[1]), start=True, stop=False)
        b.mm(pX2, self.ident_f, f4(self.BJ), start=False, stop=True)
        b.mm(pX3, self.ones_f, f4(self.D3[2]), start=True, stop=False)
        b.mm(pX3, self.ident_f, f4(self.BA), start=False, stop=True)
        b.mm(pXB, self.ones_f, f4(self.D3[1]))
        b.act(f4(self.E1), pX1, AF.Exp)
        b.act(f4(self.E2), pX2, AF.Exp)
        b.act(f4(self.E3), pX3, AF.Exp)
        b.act(f4(self.EB), pXB, AF.Exp)
        b.asel(self.E1, self.E1, [[0, 4], [1, 128]], ALU.is_ge, 0.0, -1, -1)
        b.asel(self.E2, self.E2, [[0, 4], [1, 128]], ALU.is_ge, 0.0, 0, -1)
        b.asel(self.E3, self.E3, [[0, 4], [-1, 128]], ALU.is_ge, 0.0, -1, 1)
        pG, pKQ = nps(), nps()
        for h in range(4):
            b.mm(pG[:, h * 128:(h + 1) * 128], fm[:, 4 + h, tk], fm[:, 4 + h, tk])
        for h in range(4):
            b.mm(pKQ[:, h * 128:(h + 1) * 128], fm[:, 4 + h, tk], fm[:, h, tk])
        P, PT, AT = self.P, self.PT, self.AT
        b.stt(f4(PT[0]), f4(self.E1), -1.0, pG, ALU.mult, ALU.mult)
        b.stt(f4(P[0]), f4(self.E3), -1.0, pG, ALU.mult, ALU.mult)
        b.tt('dve', f4(self.At), f4(self.E2), pKQ, ALU.mult)
        b.tt('pool', AT[0], PT[0], self.ident_f.bc(1, [128, 4, 128]), ALU.add)
        cur = 0
        for lev in range(1, 7):
            nxt = 1 - cur
            pP = nps()
            for h in range(4):
                b.mm(pP[:, h * 128:(h + 1) * 128], PT[cur][:, h, :], P[cur][:, h, :])
            if lev < 6:
                pPT = nps()
                for h in range(4):
                    b.mm(pPT[:, h * 128:(h + 1) * 128], P[cur][:, h, :], PT[cur][:, h, :])
            b.cp('act', f4(P[nxt]), pP)
            if lev < 6:
                b.cp('dve', f4(PT[nxt]), pPT)
            pA = nps()
            for h in range(4):
                b.mm(pA[:, h * 128:(h + 1) * 128], P[nxt][:, h, :], AT[cur][:, h, :])
            b.tt('dve', f4(AT[nxt]), f4(AT[cur]), pA, ALU.add)
            cur = nxt
        b.cp('act', self.ATb, AT[cur])
        PTb = self.PTb
        for h in range(4):
            b.tr(PTb[:, h * 128:(h + 1) * 128], fm[:, 4 + h, tk], self.ident_b)
            b.tr(PTb[:, (4 + h) * 128:(5 + h) * 128], fm[:, 8 + h, tk], self.ident_b)
        pk = PTb[:, 0:512].re("p (h d) -> p h d", h=4)
        pv = PTb[:, 512:1024].re("p (h d) -> p h d", h=4)
        b.tt('dve', self.Rk, pk, bc4(sRk), ALU.mult)
        b.tt('dve', self.Kd, pk, bc4(sKd), ALU.mult)
        b.tt('dve', self.Vb, pv, bc4(sVb), ALU.mult)
        pW = nps()
        for h in range(4):
            b.mm(pW[:, h * 128:(h + 1) * 128], self.Rk[:, h, :], self.ATb[:, h, :])
        b.act(f4(self.nW), pW, AF.Copy, scale=-1.0)
        pV = nps()
        for h in range(4):
            b.mm(pV[:, h * 128:(h + 1) * 128], self.ATb[:, h, :], self.Vb[:, h, :], start=(h == 0), stop=False)
            b.mm(pV[:, h * 128:(h + 1) * 128], self.nW[:, h, :], self.Sb[:, h, :], start=False, stop=True)
        b.cp('act', f4(self.Vn), pV)
        b.tt('dve', self.qg, fm[:, 0:4, tk], self.EB, ALU.mult)
        pO = nps()
        for h in range(4):
            b.mm(pO[:, h * 128:(h + 1) * 128], self.qg[:, h, :], self.Sb[:, h, :], start=(h == 0), stop=False)
            b.mm(pO[:, h * 128:(h + 1) * 128], self.At[:, h, :], self.Vn[:, h, :], start=False, stop=True)
        pS = nps()
        for h in range(4):
            b.mm(pS[:, h * 128:(h + 1) * 128], self.Kd[:, h, :], self.Vn[:, h, :])
        b.tt('dve', self.Sf, self.Sf, bc4(dec), ALU.mult)
        b.tt('dve', f4(self.Sf), f4(self.Sf), pS, ALU.add)
        b.cp('act', self.Sb, self.Sf)
        self.out_norm_heads(pO, st, self.zz[st])


class GLA(Mixer):
    def __init__(self, ctx):
        super().__init__(ctx)
        S = self.S
        scr = self.scr
        A2 = lambda i, o: scr('A', i)[:, o * 256:(o + 1) * 256].re("p (h i) -> p h i", h=2)
        B4 = lambda i: scr('B', i).re("p (h i) -> p h i", h=4)
        B2 = lambda i, o: scr('B', i)[:, o * 256:(o + 1) * 256].re("p (h i) -> p h i", h=2)
        self.fm = self.FM[:, 0:4, :]
        self.vt = [scr('B', 4 + i) for i in range(4)]
        self.gklo = scr('A', 10)
        self.wgk = S.sb("gla_wgk", [128, 256], F32)
        self.nbg = S.sb("gla_nbg", [128, 2], F32)
        self.onorm = S.sb("gla_on", [128, 128], F32)
        self.e = scr('A', 2)
        self.sp = [scr('A', 6), scr('A', 7)]
        self.nb = [scr('A', 8), scr('A', 9)]
        self.negc = S.sb("gla_negc", [128, 2, 2], F32)
        self.Eq, self.Ek = A2(3, 0), A2(3, 1)
        self.Eg, self.Ed = A2(4, 0), A2(4, 1)
        self.qt, self.kd = B2(0, 0), B2(0, 1)
        self.kt = B4(1)
        self.qg = B4(2)
        self.kdt = scr('B', 3)[:, 0:256]
        self.At = B4(8)
        self.Sf = S.sb("gla_Sf", [128, 2, 128], F32)
        self.Sb = S.sb("gla_Sb", [128, 2, 128], BF16)
        self.osb4 = scr('A', 0)
        self.sq4 = scr('A', 1)
        self.ss4 = S.sb("gla_ss4", [128, 4], F32)
        self.rs4 = S.sb("gla_rs4", [128, 4], F32)
        self.rm = S.sb("gla_rm", [128, 4], F32)
        self.Sd = scr('A', 5)[:, 0:128]

    def layer_setup(self, l):
        b, dr, dt = self.b, self.dr, self.dt
        b.memset('dve', self.wgk, 0.0)
        b.dma('pool', self.wgk[0:16, :], dt(dr['w_gk'][l]))
        b.dma('pool', self.nbg, dt(dr['b_gkT'][l]))
        b.ts('dve', self.nbg, self.nbg, -1.0, ALU.mult)
        self.bcast_load(self.onorm, dr['onorm_b'][l:l + 1, :], 128)
        b.memset('dve', self.Sf, 0.0)
        b.memset('dve', self.Sb, 0.0)
        if l == 0:
            b.memset('dve', self.rm, 0.0)
            b.memset('dve', self.rm[0:64, 0:1], 1.0)
            b.memset('dve', self.rm[64:128, 1:2], 1.0)
            b.memset('dve', self.rm[0:64, 2:3], 0.125)
            b.memset('dve', self.rm[64:128, 3:4], 0.125)

    def macro(self, l, m, w_in_l):
        b, nps = self.b, self.nps
        wb = self.wload(w_in_l, OFF['gla_q'], 512)
        for c in range(4):
            p = nps()
            self.proj_feat(p, wb, c * 128, 128)
            b.cp('act', self.fm[:, c, :], p)
        wb = self.wload(w_in_l, OFF['gla_v'], 512)
        for st in range(4):
            p = nps()
            self.proj_tok(p, st, wb, 0, 512)
            b.cp('act', self.vt[st], p)
        wb = self.wload(w_in_l, OFF['gla_gk'], 528)
        p = nps()
        self.proj_feat(p, wb, 0, 128)
        b.memset('pool', self.gklo, 0.0)
        b.cp('dve', self.gklo[0:16, :], p[0:16, :])
        for c in range(2):
            p = nps()
            b.mm(p, self.wgk[:, c * 128:(c + 1) * 128], self.gklo)
            b.act(self.e, p, AF.Exp, scale=-1.0, bias=self.nbg[:, c:c + 1])
            b.act(self.sp[c], self.e, AF.Ln, bias=1.0)
            b.ts('dve', self.sp[c], self.sp[c], 1.0 / 16.0, ALU.mult)
            for st in range(4):
                tk = slice(st * 128, (st + 1) * 128)
                b.scan(self.nb[c][:, tk], self.ones_f, self.sp[c][:, tk], 0.0, ALU.mult, ALU.add)
        self.z_block(wb, 16, self.onorm)
        for st in range(4):
            self.sub(st)
            self.emit_yT(st)

    def sub(self, st):
        b, nps = self.b, self.nps
        tk = slice(st * 128, (st + 1) * 128)
        fm = self.fm
        for c in range(2):
            nbs = self.nb[c][:, tk]
            ref = self.nb[c][:, st * 128 + 64:st * 128 + 65]
            last = self.nb[c][:, st * 128 + 127:st * 128 + 128]
            b.ts('dve', self.negc[:, c, 0:1], ref, -1.0, ALU.mult)
            b.ts('dve', self.negc[:, c, 1:2], last, -1.0, ALU.mult)
            b.act(self.Eq[:, c, :], nbs, AF.Exp, scale=-1.0, bias=ref)
            b.act(self.Ek[:, c, :], nbs, AF.Exp, bias=self.negc[:, c, 0:1])
            b.act(self.Eg[:, c, :], nbs, AF.Exp, scale=-1.0)
            b.act(self.Ed[:, c, :], nbs, AF.Exp, bias=self.negc[:, c, 1:2])
        b.stt(self.qt, self.Eq, 0.125, fm[:, 0:2, tk], ALU.mult, ALU.mult)
        b.tt('pool', self.kd, self.Ed, fm[:, 2:4, tk], ALU.mult)
        for h in range(4):
            c, r = h // 2, h % 2
            b.stt(self.kt[:, h, :], self.Ek[:, c, :], self.rm[:, r:r + 1], fm[:, 2 + c, tk], ALU.mult, ALU.mult)
            b.stt(self.qg[:, h, :], self.Eg[:, c, :], self.rm[:, 2 + r:3 + r], fm[:, c, tk], ALU.mult, ALU.mult)
        pA = nps()
        for h in range(4):
            c = h // 2
            b.mm(pA[:, h * 128:(h + 1) * 128], self.kt[:, h, :], self.qt[:, c, :])
        b.tt('dve', self.At, self.h4(pA), self.U_b.bc(1, [128, 4, 128]), ALU.mult)
        for c in range(2):
            b.tr(self.PTb[:, c * 128:(c + 1) * 128], self.kd[:, c, :], self.ident_b)
        b.cp('act', self.kdt, self.PTb[:, 0:256])
        pO = nps()
        for h in range(4):
            c = h // 2
            hs = slice(h * 128, (h + 1) * 128)
            b.mm(pO[:, hs], self.At[:, h, :], self.vt[st][:, hs], start=(h == 0), stop=False)
            b.mm(pO[:, hs], self.qg[:, h, :], self.Sb[:, c, :], start=False, stop=True)
        pS = nps()
        for h in range(4):
            c = h // 2
            b.mm(pS[:, h * 128:(h + 1) * 128], self.kdt[:, c * 128:(c + 1) * 128], self.vt[st][:, h * 128:(h + 1) * 128])
        for c in range(2):
            b.ts('dve', self.Sd, self.Sf[:, c, :], self.Eg[:, c, 127:128], ALU.mult)
            b.stt(self.Sd, pS[:, (2 * c) * 128:(2 * c + 1) * 128], self.rm[:, 0:1], self.Sd, ALU.mult, ALU.add)
            b.stt(self.Sf[:, c, :], pS[:, (2 * c + 1) * 128:(2 * c + 2) * 128], self.rm[:, 1:2], self.Sd, ALU.mult, ALU.add)
        b.cp('act', self.Sb, self.Sf)
        self.out_norm_heads(pO, st, self.zz[st])


class SSD(Mixer):
    def __init__(self, ctx):
        super().__init__(ctx)
        S = self.S
        self.cc = 0
        scr = self.scr
        A4 = lambda i: scr('A', i).re("p (h i) -> p h i", h=4)
        B4 = lambda i: scr('B', i).re("p (h i) -> p h i", h=4)
        self.fm = self.FM[:, 0:8, :]
        self.halo = [S.sb("ssd_h%d" % c, [128, 3], BF16) for c in range(8)]
        self.convw = S.sb("ssd_cw", [128, 8, 4], F32)
        self.convb = S.sb("ssd_cb", [128, 8], F32)
        f = lambda n, k: S.sb("ssd_" + n, [128, k], F32)
        self.nega, self.dtb, self.dsk, self.ea = f("nega", 8), f("dtb", 8), f("dsk", 8), f("ea", 8)
        self.onc = S.sb("ssd_onc", [128, 512], F32)
        self.dtr, self.apb, self.e, self.dtv, self.da, self.acl = f("dtr", 8), f("apb", 8), f("e", 8), f("dtv", 8), f("da", 8), f("acl", 16)
        self.X, self.EX, self.sdtd = f("X", 24), f("EX", 24), f("sdtd", 8)
        self.D8 = [A4(2), A4(3)]
        self.Bn = [A4(4), A4(5)]
        self.E = [A4(6), A4(7)]
        self.Mt = [B4(0), B4(1)]
        self.xdt = scr('B', 2)
        self.xdtd = scr('B', 3)
        self.xsk = scr('A', 8)
        self.Btok = scr('B', 4)[:, 0:256]
        self.yo = scr('A', 9)
        self.Hf = S.sb("ssd_Hf", [128, 512], F32)
        self.Hb = S.sb("ssd_Hb", [128, 512], BF16)
        self.ssq = f("ssq", 1)
        self.rstd = f("rstd", 1)
        self.junk = scr('B', 5)

    def layer_setup(self, l):
        b, dr, dt = self.b, self.dr, self.dt
        b.dma('pool', self.convw, dt(dr['conv_cT'][l]))
        b.dma('pool', self.convb, dt(dr['conv_bias_cT'][l]))
        self.bcast_load(self.ea, dr['a_log_c'][l:l + 1, :], 8)
        b.act(self.nega, self.ea, AF.Exp)
        b.ts('dve', self.nega, self.nega, -1.0, ALU.mult)
        self.bcast_load(self.dtb, dr['dt_bias_c'][l:l + 1, :], 8)
        self.bcast_load(self.dsk, dr['d_skip_c'][l:l + 1, :], 8)
        self.bcast_load(self.onc, dr['onorm_c'][l:l + 1, :], 512)
        b.memset('dve', self.Hf, 0.0)
        b.memset('dve', self.Hb, 0.0)
        for c in range(8):
            b.memset('pool', self.halo[c], 0.0)

    def macro(self, l, m, w_in_l):
        b, nps = self.b, self.nps
        for blk in range(2):
            wb = self.wload(w_in_l, OFF['ssd_x'] + blk * 512, 512)
            for cc in range(4):
                c = blk * 4 + cc
                p = nps()
                self.proj_feat(p, wb, cc * 128, 128)
                self.conv_chunk(p, self.convw, c, self.halo[c], self.fm[:, c, :], bias=self.convb[:, c:c + 1])
        wb3 = self.wload(w_in_l, OFF['ssd_dt'], 520)
        self.z_block(wb3, 8, None)
        for st in range(4):
            self.sub(st, wb3)
            self.emit_yT(st)

    def sub(self, st, wb3):
        b, nps = self.b, self.nps
        fm = self.fm
        tk = slice(st * 128, (st + 1) * 128)
        h8 = lambda t, k=64: t.re("p (h i) -> p h i", h=8)
        bc8 = lambda t, k: t.bc(2, [128, 8, k])
        pq = nps()
        self.proj_tok(pq, st, wb3, 0, 8)
        b.cp('dve', self.dtr, pq[:, 0:8])
        b.tt('dve', self.apb, self.dtr, self.dtb, ALU.add)
        b.act(self.e, self.apb, AF.Exp)
        b.act(self.dtv, self.e, AF.Ln, bias=1.0)
        b.tt('dve', self.da, self.dtv, self.nega, ALU.mult)
        pg = nps()
        b.mm(pg[:, 0:8], self.U_f, self.da)
        b.mm(pg[:, 8:16], self.ones_f, self.da)
        b.cp('dve', self.acl, pg[:, 0:16])
        acs, alast = self.acl[:, 0:8], self.acl[:, 8:16]
        b.cp('pool', self.X[:, 0:8], acs)
        b.tt('pool', self.X[:, 8:16], alast, acs, ALU.subtract)
        b.cp('pool', self.X[:, 16:24], alast)
        b.act(self.EX, self.X, AF.Exp)
        eacs, edst, dec = self.EX[:, 0:8], self.EX[:, 8:16], self.EX[:, 16:24]
        b.tt('dve', self.sdtd, self.dtv, edst, ALU.mult)
        bc4 = lambda t: t.bc(2, [128, 4, 128])
        f4 = lambda t: t.re("p h i -> p (h i)")
        pCB = nps()
        for g in range(2):
            b.mm(pCB[:, g * 128:(g + 1) * 128], fm[:, 4 + g, tk], fm[:, 6 + g, tk])
        for g in range(2):
            hs = slice(g * 4, g * 4 + 4)
            b.tt('dve', self.D8[g], self.ident_f.bc(1, [128, 4, 128]), bc4(acs[:, hs]), ALU.mult)
            b.ts('pool', self.Bn[g], bc4(acs[:, hs]), -1.0, ALU.mult)
            pX = nps()
            b.mm(pX, self.ones_f, f4(self.D8[g]), start=True, stop=False)
            b.mm(pX, self.ident_f, f4(self.Bn[g]), start=False, stop=True)
            b.act(f4(self.E[g]), pX, AF.Exp)
            b.asel(self.E[g], self.E[g], [[0, 4], [1, 128]], ALU.is_ge, 0.0, 0, -1)
            b.tt('dve', self.Mt[g], self.E[g], pCB[:, g * 128:(g + 1) * 128].bc(1, [128, 4, 128]), ALU.mult)
        PTb = self.PTb
        for c in range(4):
            b.tr(PTb[:, c * 128:(c + 1) * 128], fm[:, c, tk], self.ident_b)
        for g in range(2):
            b.tr(PTb[:, 512 + g * 128:512 + (g + 1) * 128], fm[:, 4 + g, tk], self.ident_b)
        xtok = h8(PTb[:, 0:512])
        b.tt('dve', h8(self.xdt), xtok, bc8(self.dtv, 64), ALU.mult)
        b.tt('dve', h8(self.xdtd), xtok, bc8(self.sdtd, 64), ALU.mult)
        b.tt('dve', h8(self.xsk), xtok, bc8(self.dsk, 64), ALU.mult)
        b.cp('act', self.Btok, PTb[:, 512:768])
        pY = nps()
        for h in range(8):
            b.mm(pY[:, h * 64:(h + 1) * 64], self.Mt[h // 4][:, h % 4, :], self.xdt[:, h * 64:(h + 1) * 64])
        pF = nps()
        for g in range(2):
            b.mm(pF[:, g * 256:(g + 1) * 256], fm[:, 6 + g, tk], self.Hb[:, g * 256:(g + 1) * 256])
        b.tt('dve', h8(self.yo), h8(pF), bc8(eacs, 64), ALU.mult)
        b.tt('pool', self.yo, self.yo, self.xsk, ALU.add)
        b.tt('dve', self.yo, self.yo, pY, ALU.add)
        pH = nps()
        for g in range(2):
            b.mm(pH[:, g * 256:(g + 1) * 256], self.Btok[:, g * 128:(g + 1) * 128], self.xdtd[:, g * 256:(g + 1) * 256])
        b.tt('dve', h8(self.Hf), h8(self.Hf), bc8(dec, 64), ALU.mult)
        b.tt('dve', self.Hf, self.Hf, pH, ALU.add)
        b.cp('act', self.Hb, self.Hf)
        b.tt('dve', self.yo, self.yo, self.zz[st], ALU.mult)
        b.act(self.junk, self.yo, AF.Square, accum=self.ssq)
        self.rms_rstd(self.rstd, self.ssq, 512)
        b.stt(self.ybr[st], self.yo, self.rstd[:, 0:1], self.onc, ALU.mult, ALU.mult)


class NSA(Mixer):
    def __init__(self, ctx):
        super().__init__(ctx)
        S, scr, S_len, NT = self.S, self.scr, self.S_len, self.NT
        self.kS = [S.sb("nsa_kS%d" % g, [128, S_len], BF16) for g in range(2)]
        self.kW = [S.sb("nsa_kW%d" % g, [128, 8 * 128], BF16) for g in range(2)]
        self.vS = [S.sb("nsa_vS%d" % g, [128, NT, 66], BF16) for g in range(2)]
        self.vW = [S.sb("nsa_vW%d" % g, [128, 8, 66], BF16) for g in range(2)]
        self.kC = [S.sb("nsa_kC%d" % g, [128, 256], BF16) for g in range(2)]
        self.vcT = [S.sb("nsa_vcT%d" % g, [128, 256], BF16) for g in range(2)]
        self.vC = [S.sb("nsa_vC%d" % g, [128, 2, 130], BF16) for g in range(2)]
        self.EK = S.sb("nsa_EK", [128, S_len], BF16)
        self.EKc = S.sb("nsa_EKc", [128, 256], BF16)
        self.qm = self.FM[:, 0:8, :]
        self.qA = self.FM[:, 8:16, :]
        self.qS = S.sb("nsa_qS", [128, 4, 128], BF16)
        self.rawc = [S.sb("nsa_rawc%d" % g, [128, 528], BF16) for g in range(2)]
        self.W1 = S.sb("nsa_W1", [128, 32, 128], BF16)
        self.W2k = S.sb("nsa_W2k", [128, 128], BF16)
        self.W2v = S.sb("nsa_W2v", [128, 128], BF16)
        self.posT = S.sb("nsa_posT", [128, 32], BF16)
        self.cpos = S.sb("nsa_cpos", [128, 1], F32)
        self.wsel = S.sb("nsa_wsel", [128, 8, 128], BF16)
        self.gsig = [S.sb("nsa_gs%d" % i, [128, 24], F32) for i in range(4)]
        self.rmq = S.sb("nsa_rmq", [128, 2], F32)
        self.hid = S.sb("nsa_hid", [128, 32], BF16)
        f = lambda n, k: S.sb("nsa_" + n, [128, k], F32)
        self.dall = S.sb("nsa_dall", [128, 3, 4], F32)
        self.rall = S.sb("nsa_rall", [128, 3, 4], F32)
        self.coef = S.sb("nsa_coef", [128, 3, 4], F32)
        self.imp, self.imp2, self.m8a, self.m8b, self.thr, self.selb = f("imp", 64), f("imp2", 64), f("m8a", 8), f("m8b", 8), f("thr", 1), f("selb", 64)
        self.selbb = S.sb("nsa_selbb", [128, 128], BF16)
        self.cur, self.val, self.fz = f("cur", 1), f("val", 64), f("fz", 64)
        self.Pb = [scr('B', i).re("p (h i) -> p h i", h=4) for i in range(3)]
        self.pi = 0
        self.on = scr('A', 2).re("p (h d) -> p h d", h=8)
        self.tmp4 = scr('A', 3)[:, 0:256].re("p (h d) -> p h d", h=4)
        self.tmp5 = scr('A', 4)[:, 0:256].re("p (h d) -> p h d", h=4)

    def layer_setup(self, l):
        b, dr, dt, nps = self.b, self.dr, self.dt, self.nps
        if NSTAGE < 0:
            return
        if l == 0:
            for g in range(2):
                for t in (self.kS[g], self.kW[g], self.vS[g], self.vW[g], self.kC[g], self.vcT[g], self.vC[g]):
                    b.memset('pool', t, 0.0)
                b.memset('pool', self.vS[g][:, :, 64:65], 1.0)
                b.memset('pool', self.vW[g][:, :, 64:65], 1.0)
                b.memset('pool', self.vC[g][:, :, 64:65], 1.0)
                b.memset('pool', self.vC[g][0:1, 0, 64:65], 0.0)
                for kt in range(2):
                    b.dma('pool', self.vC[g][:, kt, 65:129], dt(dr['overlap'][kt * 128:(kt + 1) * 128, :]))
            b.memset('pool', self.EK, 0.0)
            for c0 in range(0, self.S_len, 1024):
                c1 = min(c0 + 1024, self.S_len)
                b.dma('pool', self.EK[0:64, c0:c1], dt(dr['esel'][:, c0:c1]))
                b.dma('pool', self.EK[64:68, c0:c1], dt(dr['kaug'][:, c0:c1]))
            b.memset('pool', self.EKc, 0.0)
            b.dma('pool', self.EKc[64:68, :], dt(dr['kaugc']))
            b.memset('pool', self.qS, 0.0)
            b.memset('pool', self.selbb, 0.0)
            b.memset('pool', self.rmq, 0.0)
            b.memset('pool', self.rmq[0:64, 0:1], 0.125)
            b.memset('pool', self.rmq[64:128, 1:2], 0.125)
        b.memset('pool', self.W1, 0.0)
        b.dma('pool', self.W1[0:64, :, 0:64], dt(dr['w_ck1'][l].rearrange("(p d) o -> d p o", d=64)))
        b.dma('pool', self.W1[64:128, :, 64:128], dt(dr['w_cv1'][l].rearrange("(p d) o -> d p o", d=64)))
        b.memset('pool', self.W2k, 0.0)
        b.dma('pool', self.W2k[0:64, 0:64], dt(dr['w_ck2'][l]))
        b.dma('pool', self.W2k[0:64, 64:128], dt(dr['w_ck2'][l]))
        b.memset('pool', self.W2v, 0.0)
        b.dma('pool', self.W2v[64:128, 0:64], dt(dr['w_cv2'][l]))
        b.dma('pool', self.posT[0:64, :], dt(dr['cmp_pos_kT'][l]))
        b.dma('pool', self.posT[64:128, :], dt(dr['cmp_pos_vT'][l]))
        pc = nps()
        for p in range(32):
            b.mm(pc[:, 0:1], self.W1[:, p, :], self.posT[:, p:p + 1], start=p == 0, stop=p == 31)
        b.cp('dve', self.cpos, pc[:, 0:1])
        for g in range(2):
            b.memset('pool', self.rawc[g], 0.0)

    def macro(self, l, m, w_in_l):
        b, nps = self.b, self.nps
        if NSTAGE < 1:
            for st in range(4):
                self.emit_yT(st)
            return
        n0 = 4 * m
        wsel = self.wsel
        wb = self.wload(w_in_l, OFF['nsa_q'], 512)
        for c in range(4):
            p = nps()
            self.proj_feat(p, wb, c * 128, 128)
            b.ts('dve', self.qm[:, 2 * c, :], p, self.rmq[:, 0:1], ALU.mult)
            b.act(self.qm[:, 2 * c + 1, :], p, AF.Copy, scale=self.rmq[:, 1:2])
        def bail():
            for st in range(4):
                self.emit_yT(st)
        if NSTAGE < 1.15:
            return bail()
        b.memset('pool', self.qA, 0.0)
        b.dma('pool', self.qA[64:68, :, :], self.dt(self.dr['qaug'][:, :, m * MT:(m + 1) * MT]))
        if NSTAGE < 1.25:
            return bail()
        wb = self.wload(w_in_l, OFF['nsa_kc'], 512)
        for g in range(2):
            b.cp('dve', wsel[:, :, 0:64], wb[:, :, g * 64:(g + 1) * 64])
            b.cp('dve', wsel[:, :, 64:128], wb[:, :, 128 + g * 64:128 + (g + 1) * 64])
            p = nps()
            self.proj_feat(p, wsel, 0, 128)
            b.cp('dve', self.rawc[g][:, 0:16], self.rawc[g][:, 512:528])
            b.cp('act', self.rawc[g][:, 16:528], p)
        if NSTAGE < 1.27:
            return bail()
        for g in range(2):
            b.cp('dve', wsel[:, :, 0:64], wb[:, :, 256 + g * 64:256 + (g + 1) * 64])
            b.cp('dve', wsel[:, :, 64:128], wb[:, :, 256 + g * 64:256 + (g + 1) * 64])
            p = nps()
            self.proj_feat(p, wsel, 0, 128)
            b.cp('act', self.kS[g][:, m * MT:(m + 1) * MT], p)
        if NSTAGE < 1.29:
            return bail()
        for st in range(4):
            p = nps()
            self.proj_tok(p, st, wb, 384, 128)
            for g in range(2 if NSTAGE >= 1.2915 else 0):
                b.cp('dve', self.vS[g][:, n0 + st, 0:64], p[:, g * 64:(g + 1) * 64])
        if NSTAGE < 1.35:
            return bail()
        wb = self.wload(w_in_l, OFF['nsa_kw'], 280)
        for g in range(2):
            b.cp('dve', wsel[:, :, 0:64], wb[:, :, g * 64:(g + 1) * 64])
            b.cp('dve', wsel[:, :, 64:128], wb[:, :, g * 64:(g + 1) * 64])
            p = nps()
            self.proj_feat(p, wsel, 0, 128)
            s0 = (n0 % 8) * 128
            b.cp('act', self.kW[g][:, s0:s0 + 512], p)
        for st in range(4):
            p = nps()
            self.proj_tok(p, st, wb, 128, 152)
            for g in range(2):
                b.cp('dve', self.vW[g][:, (n0 + st) % 8, 0:64], p[:, g * 64:(g + 1) * 64])
            b.act(self.gsig[st], p[:, 128:152], AF.Sigmoid)
        if NSTAGE < 1.45:
            return bail()
        wb = self.wload(w_in_l, OFF['nsa_z'], 512)
        self.z_block(wb, 0, None)
        for g in range(2 if NSTAGE >= 2 else 0):
            pC = nps()
            for p_ in range(32):
                b.mm(pC[:, 0:32], self.W1[:, p_, :], self.rawc[g][:, p_:p_ + 497:16], start=p_ == 0, stop=p_ == 31)
            b.act(self.hid, pC[:, 0:32], AF.Silu, bias=self.cpos[:, 0:1])
            pK = nps()
            b.mm(pK[:, 0:32], self.W2k, self.hid)
            b.mm(pK[:, 32:64], self.W2v, self.hid)
            c0 = 32 * m
            cnt = 32
            b.cp('dve', self.kC[g][:, c0:c0 + cnt], pK[:, 0:32])
            b.cp('dve', self.vcT[g][:, c0:c0 + cnt], pK[:, 32:64])
            if m == 0:
                b.memset('pool', self.kC[g][:, 0:1], 0.0)
                b.memset('pool', self.vcT[g][:, 0:1], 0.0)
            for kt in sorted({c0 // 128, (c0 + cnt - 1) // 128}):
                b.tr(self.PTb[:, 0:128], self.vcT[g][:, kt * 128:(kt + 1) * 128], self.ident_b)
                b.cp('dve', self.vC[g][:, kt, 0:64], self.PTb[:, 0:64])
        for st in range(4):
            if NSTAGE >= 3:
                self.sub(m, st)
            self.emit_yT(st)

    def pbuf(self):
        self.pi += 1
        return self.Pb[self.pi % 3]

    def sub(self, m, st):
        b, nps, PS = self.b, self.nps, self.PS
        n = 4 * m + st
        tk = slice(st * 128, (st + 1) * 128)
        f4 = lambda t: t.re("p h i -> p (h i)")
        pOc, pOs, pOw = [PS[0], PS[1]], PS[2], PS[3]
        on = self.on
        causal = lambda t: b.asel(t, t, [[0, 4], [1, 128]], ALU.is_ge, 0.0, 0, -1)
        for g in range(2):
            qrhs = self.qm[:, 4 * g:4 * g + 4, tk]
            arhs = self.qA[:, 4 * g:4 * g + 4, tk]
            kts = [kt for kt in (0, 1) if n >= 16 * kt]
            for ki, kt in enumerate(kts):
                ks_ = slice(kt * 128, (kt + 1) * 128)
                pS = nps(4)
                b.mm(pS, self.kC[g][:, ks_], qrhs, start=True, stop=False)
                b.mm(pS, self.EKc[:, ks_], arhs, start=False, stop=True)
                Pt = self.pbuf()
                b.act(f4(Pt), pS, AF.Exp)
                if n < 16 * kt + 16:
                    b.asel(Pt, Pt, [[0, 4], [1, 128]], ALU.is_ge, 0.0, 128 * n - 2048 * kt - 15, -16)
                for hh in range(4):
                    col = (hh % 2) * 129
                    b.mm(pOc[hh // 2][:, col:col + 129], Pt[:, hh, :], self.vC[g][:, kt, 0:129],
                         start=(ki == 0 and hh % 2 == 0), stop=(ki == len(kts) - 1))
            for bnk in range(2):
                v = pOc[bnk][:, 0:258].re("p (h c) -> p h c", h=2)
                b.cp('dve', self.dall[:, 0, 2 * bnk:2 * bnk + 2], v[:, :, 64])
            rcc = self.rall[:, 0, :]
            b.ts('dve', rcc, self.dall[:, 0, :], 1e-30, ALU.max)
            b.recip(rcc, rcc)
            imp = self.imp
            b.ts('dve', imp, pOc[0][:, 65:129], rcc[:, 0:1], ALU.mult)
            b.stt(imp, pOc[0][:, 194:258], rcc[:, 1:2], imp, ALU.mult, ALU.add)
            b.stt(imp, pOc[1][:, 65:129], rcc[:, 2:3], imp, ALU.mult, ALU.add)
            b.stt(imp, pOc[1][:, 194:258], rcc[:, 3:4], imp, ALU.mult, ALU.add)
            cur, val, fz = self.cur, self.val, self.fz
            b.ts('dve', cur, self.half01, float(2 * n), ALU.add)
            b.ts('dve', val, self.jidx, cur[:, 0:1], ALU.is_le)
            b.tt('dve', imp, imp, val, ALU.mult)
            b.ts('dve', val, val, -1.0, ALU.add)
            b.tt('dve', imp, imp, val, ALU.add)
            b.ts('dve', fz, self.jidx, cur[:, 0:1], ALU.is_equal)
            b.ts('dve', val, self.jidx, 1.0, ALU.add, cur[:, 0:1], ALU.is_equal)
            b.tt('dve', fz, fz, val, ALU.add)
            b.tt('dve', fz, fz, self.e0, ALU.add)
            b.stt(imp, fz, 1.0e4, imp, ALU.mult, ALU.max)
            b.max8(self.m8a, imp)
            b.mrep(self.imp2, self.m8a, imp, -2.0)
            b.max8(self.m8b, self.imp2)
            b.ts('dve', self.thr, self.m8b[:, 7:8], 0.0, ALU.max)
            b.ts('dve', self.selb, imp, self.thr[:, 0:1], ALU.is_ge, 30000.0, ALU.mult)
            b.ts('dve', self.selbb[:, 0:64], self.selb, -30000.0, ALU.add)
            def scores(kind, kt):
                ks_ = slice(kt * 128, (kt + 1) * 128)
                pS = nps(4)
                if kind == 's':
                    b.mm(pS, self.kS[g][:, ks_], qrhs, start=True, stop=False)
                    b.mm(pS, self.EK[:, ks_], f4(self.qS), start=False, stop=True)
                else:
                    sl = kt % 8
                    b.mm(pS, self.kW[g][:, sl * 128:(sl + 1) * 128], qrhs, start=True, stop=False)
                    b.mm(pS, self.EK[:, ks_], arhs, start=False, stop=True)
                Pt = self.pbuf()
                b.act(f4(Pt), pS, AF.Exp)
                if kt == n:
                    causal(Pt)
                if kind == 'w' and kt == n - 4:
                    b.asel(Pt, Pt, [[0, 4], [-1, 128]], ALU.is_ge, 0.0, -1, 1)
                return Pt

            def pv(kind, kt, Pt, first, last):
                for hh in range(4):
                    if kind == 's':
                        b.mm(pOs[:, hh * 65:(hh + 1) * 65], Pt[:, hh, :], self.vS[g][:, kt, 0:65],
                             start=(first and hh == 0), stop=last)
                    else:
                        b.mm(pOw[:, hh * 65:(hh + 1) * 65], Pt[:, hh, :], self.vW[g][:, kt % 8, 0:65],
                             start=(first and hh == 0), stop=last)

            wk0 = max(0, n - 4)

            def run_items(items):
                pend = []
                for it in items:
                    Pt = scores(it[0], it[1])
                    pend.append((it, Pt))
                    if len(pend) > 2:
                        i0, P0 = pend.pop(0)
                        pv(i0[0], i0[1], P0, i0[2], i0[3])
                for i0, P0 in pend:
                    pv(i0[0], i0[1], P0, i0[2], i0[3])

            run_items([('w', kt, kt == wk0, kt == n) for kt in range(wk0, n + 1)])
            b.tr(self.PTb[:, 0:128], self.selbb, self.ident_b)
            b.cp('dve', self.qS[0:64], self.PTb[0:64, 0:128].bc(1, [64, 4, 128]))
            b.cp('act', self.qS[64:68], self.qA[64:68, 4 * g:4 * g + 4, tk])
            run_items([('s', kt, kt == 0, kt == n) for kt in range(n + 1)])
            vs_ = pOs[:, 0:260].re("p (h c) -> p h c", h=4)
            vw_ = pOw[:, 0:260].re("p (h c) -> p h c", h=4)
            b.cp('dve', self.dall[:, 1, :], vs_[:, :, 64])
            b.cp('dve', self.dall[:, 2, :], vw_[:, :, 64])
            b.ts('dve', self.rall[:, 1:3, :], self.dall[:, 1:3, :], 1e-30, ALU.max)
            b.recip(self.rall[:, 1:3, :], self.rall[:, 1:3, :])
            gv = self.gsig[st][:, 12 * g:12 * g + 12].re("p (h b) -> p b h", b=3)
            b.tt('dve', self.coef, self.rall, gv, ALU.mult)
            for bnk in range(2):
                v = pOc[bnk][:, 0:258].re("p (h c) -> p h c", h=2)
                b.tt('dve', on[:, 4 * g + 2 * bnk:4 * g + 2 * bnk + 2, :], v[:, :, 0:64],
                     self.coef[:, 0, 2 * bnk:2 * bnk + 2].bc(2, [128, 2, 64]), ALU.mult)
            b.tt('dve', self.tmp4, vs_[:, :, 0:64], self.coef[:, 1, :].bc(2, [128, 4, 64]), ALU.mult)
            b.tt('pool', on[:, 4 * g:4 * g + 4, :], on[:, 4 * g:4 * g + 4, :], self.tmp4, ALU.add)
            b.tt('dve', self.tmp5, vw_[:, :, 0:64], self.coef[:, 2, :].bc(2, [128, 4, 64]), ALU.mult)
            b.tt('pool', on[:, 4 * g:4 * g + 4, :], on[:, 4 * g:4 * g + 4, :], self.tmp5, ALU.add)
        b.tt('dve', self.ybr[st], on.re("p h d -> p (h d)"), self.zz[st], ALU.mult)


def kernel(**inputs):
    depth, S_len, n_cores = 4, 4096, 8
    x = np.asarray(inputs['x'], dtype=np.float32)
    nc, _ = build(S_len, depth)
    pin = prep_inputs(inputs, depth)
    hc = host_constants(S_len)
    in_maps = []
    for i in range(n_cores):
        d = dict(pin)
        d.update(hc)
        d['x'] = np.ascontiguousarray(x[i])
        in_maps.append(d)
    res = run_bass_kernel_spmd(nc, in_maps, core_ids=list(range(n_cores)))
    return np.stack([np.asarray(r['y'], dtype=np.float32) for r in res.results], axis=0)
```

```python
from contextlib import ExitStack
import os
NSTAGE = float(os.environ.get('NSTAGE', '99'))
SENG = os.environ.get('SENG', '').split(',')
import numpy as np
import concourse.bass as bass
import concourse.mybir as mybir
from concourse.bass_utils import run_bass_kernel_spmd

F32 = mybir.dt.float32
BF16 = mybir.dt.bfloat16
I32 = mybir.dt.int32
ALU = mybir.AluOpType
AF = mybir.ActivationFunctionType
AX = mybir.AxisListType

ENGS = ('pe', 'act', 'dve', 'pool', 'sp')
NDSEM = 12


class Tile:
    def __init__(self, name, ap, rid):
        self.name, self.ap, self.rid = name, ap, rid

    def __getitem__(self, k):
        return Tile(self.name, self.ap[k], self.rid)

    def re(self, s, **kw):
        return Tile(self.name, self.ap.rearrange(s, **kw), self.rid)

    def bc(self, axis, shape):
        return Tile(self.name, self.ap.unsqueeze(axis).to_broadcast(list(shape)), self.rid)

    def v(self, ap):
        return Tile(self.name, ap, self.rid)


class Op:
    __slots__ = ('eng', 'fn', 'deps', 'signal', 'count', 'dma', 'dsem', 'dcount', 'idx', 'cost', 'lat', 'seq', 'nrem', 'users', 'rt', 'fin')


class Sched:
    def __init__(self, nc):
        self.nc = nc
        self.es = ExitStack()
        self.ops = {e: [] for e in ENGS}
        self.lastw = {}
        self.readers = {}
        self.ndma = {e: 0 for e in ENGS}
        self.dma_ops = {e: [] for e in ENGS}
        self.nres = 0

    def sb(self, name, shape, dtype):
        t = self.es.enter_context(self.nc.sbuf_tensor("sb_" + name, list(shape), dtype))
        self.nres += 1
        return Tile(name, t[:] if hasattr(t, '__getitem__') else t, self.nres)

    def ps(self, name, shape, dtype):
        t = self.es.enter_context(self.nc.psum_tensor("pm_" + name, list(shape), dtype))
        self.nres += 1
        return Tile(name, t[:], self.nres)

    def res(self, name):
        self.nres += 1
        return Tile(name, None, self.nres)

    def view(self, tile, ap, own=False):
        if own:
            self.nres += 1
            return Tile(tile.name, ap, self.nres)
        return Tile(tile.name, ap, tile.rid)

    def _deps(self, reads, writes):
        deps = []
        for r in reads:
            w = self.lastw.get(r.rid)
            if w is not None:
                deps.append(w)
        for r in writes:
            w = self.lastw.get(r.rid)
            if w is not None:
                deps.append(w)
            deps.extend(self.readers.get(r.rid, ()))
        return deps

    def _record(self, op, reads, writes):
        for r in writes:
            self.lastw[r.rid] = op
            self.readers[r.rid] = []
        for r in reads:
            if self.lastw.get(r.rid) is op:
                continue
            self.readers.setdefault(r.rid, []).append(op)

    def op(self, eng, fn, reads=(), writes=(), cost=300.0):
        o = Op()
        o.eng, o.fn, o.dma, o.signal, o.count = eng, fn, False, False, 0
        o.cost = o.lat = cost
        self.nseq = getattr(self, 'nseq', 0) + 1
        o.seq = self.nseq
        o.deps = self._deps(reads, writes)
        o.idx = len(self.ops[eng])
        self.ops[eng].append(o)
        self._record(o, reads, writes)
        return o

    def dma(self, eng, out_ap, in_ap, reads=(), writes=(), out=False, **kw):
        o = Op()
        o.eng, o.dma, o.signal, o.count = eng, True, True, 0
        oa = out_ap.ap if isinstance(out_ap, Tile) else out_ap
        ia = in_ap.ap if isinstance(in_ap, Tile) else in_ap
        o.fn = lambda e: e.dma_start(out=oa, in_=ia, **kw)
        o.deps = self._deps(reads, writes)
        self.ndma[eng] += 1
        nbytes = 1
        for d_ in oa.shape:
            nbytes *= d_
        nbytes *= 2 if oa.dtype == BF16 else 4
        o.cost = 150.0 if eng == 'sp' else 1500.0
        o.lat = 2500.0 + nbytes / 60.0
        self.nseq = getattr(self, 'nseq', 0) + 1
        o.seq = self.nseq
        o.idx = len(self.ops[eng])
        self.ops[eng].append(o)
        self._record(o, reads, writes)
        if out:
            self.out_dmas = getattr(self, 'out_dmas', []) + [o]
        return o

    def schedule(self, window=int(os.environ.get("SWIN", "40"))):
        allops = [o for e in ENGS for o in self.ops[e]]
        for o in allops:
            o.users = []
            o.rt = 0.0
            o.fin = None
        for o in allops:
            ds = set(id(d) for d in o.deps)
            o.deps = [d for d in {id(d): d for d in o.deps}.values()]
            o.nrem = len(o.deps)
            for d in o.deps:
                d.users.append(o)
        pend = {e: list(self.ops[e]) for e in ENGS}
        new = {e: [] for e in ENGS}
        free_t = {e: 0.0 for e in ENGS}
        remaining = len(allops)
        while remaining:
            best = None
            bkey = None
            for e in ENGS:
                pe_ = pend[e]
                ft = free_t[e]
                for k in range(min(window if e in SENG else 1, len(pe_))):
                    o = pe_[k]
                    if o.nrem:
                        continue
                    st = o.rt if o.rt > ft else ft
                    key = (st, o.seq)
                    if bkey is None or key < bkey:
                        bkey, best, bk = key, o, k
                    if st <= ft:
                        break
            o = best
            e = o.eng
            pend[e].pop(bk) if pend[e][bk] is o else pend[e].remove(o)
            st = bkey[0]
            o.fin = st + o.lat
            free_t[e] = st + o.cost
            new[e].append(o)
            for u in o.users:
                u.nrem -= 1
                if o.fin > u.rt:
                    u.rt = o.fin
            remaining -= 1
        self.ops = new
        self.sim_time = max(free_t.values())

    def emit(self):
        nc = self.nc
        if SENG != ['']:
            self.schedule()
        for e in ENGS:
            i = 0
            prev = []
            for o in self.ops[e]:
                if o.dma:
                    o.dsem = (e, i % NDSEM)
                    o.dcount = 16 * (i // NDSEM + 1)
                    if i >= NDSEM:
                        o.deps.append(prev[i - NDSEM])
                    prev.append(o)
                    i += 1
        fin = Op()
        fin.eng, fin.dma, fin.signal, fin.count, fin.fn = 'sp', False, False, 0, None
        fin.deps = list(getattr(self, 'out_dmas', []))
        self.ops['sp'].append(fin)
        for e in ENGS:
            for o in self.ops[e]:
                for d in o.deps:
                    if not d.dma:
                        if d.eng == 'pe' and o.eng == 'pe':
                            continue
                        d.signal = True
        for e in ENGS:
            c = 0
            for o in self.ops[e]:
                if o.signal and not o.dma:
                    c += 1
                    o.count = c
        sems = {e: self.es.enter_context(nc.semaphore("s_" + e)) for e in ENGS}
        dsems = {}
        for e in ENGS:
            if self.ndma[e]:
                for k in range(min(NDSEM, self.ndma[e])):
                    dsems[(e, k)] = self.es.enter_context(nc.semaphore("d_%s%d" % (e, k)))
        self.stats = {}

        def run(ename, eng):
            known = {}
            nw = 0
            for o in self.ops[ename]:
                need = {}
                for d in o.deps:
                    if d.dma:
                        key, val = ('d',) + d.dsem, d.dcount
                    else:
                        if d.eng == 'pe' and ename == 'pe':
                            continue
                        key, val = ('c', d.eng), d.count
                    if known.get(key, 0) >= val:
                        continue
                    if need.get(key, 0) < val:
                        need[key] = val
                for key, val in need.items():
                    s = sems[key[1]] if key[0] == 'c' else dsems[(key[1], key[2])]
                    eng.wait_ge(s, val)
                    known[key] = val
                    nw += 1
                if o.fn is None:
                    continue
                ins = o.fn(eng)
                if o.dma:
                    ins.then_inc(dsems[o.dsem], 16)
                elif o.signal:
                    ins.then_inc(sems[ename], 1)
            self.stats[ename] = (len(self.ops[ename]), nw)

        with nc.Block() as block:
            @block.sync
            def _(eng):
                run('sp', eng)

            @block.scalar
            def _(eng):
                run('act', eng)

            @block.vector
            def _(eng):
                run('dve', eng)

            @block.gpsimd
            def _(eng):
                run('pool', eng)

            @block.tensor
            def _(eng):
                run('pe', eng)
        self.es.close()


def _fsz(ap):
    p = 1
    for d in ap.shape[1:]:
        p *= d
    return p


class B:
    def __init__(self, S):
        self.S = S

    @staticmethod
    def _t(xs):
        return [x for x in xs if isinstance(x, Tile)]

    @staticmethod
    def _a(x):
        return x.ap if isinstance(x, Tile) else x

    def mm(self, out, lhsT, rhs, start=True, stop=True):
        o, l, r = out.ap, lhsT.ap, rhs.ap
        f32 = 4.0 if l.dtype == F32 else 1.0
        self.S.op('pe', lambda e: e.matmul(o, l, r, start=start, stop=stop, skip_group_check=True),
                  reads=[lhsT, rhs], writes=[out], cost=30.0 + f32 * (max(_fsz(r), 32) + 100) / 2.4)

    def tr(self, out, in_, ident):
        o, i, d = out.ap, in_.ap, ident.ap
        self.S.op('pe', lambda e: e.transpose(o, i, d), reads=[in_, ident], writes=[out], cost=120.0)

    def act(self, out, in_, func, bias=None, scale=None, accum=None, eng='act'):
        o, i = out.ap, in_.ap
        kw = {}
        if bias is not None:
            kw['bias'] = self._a(bias)
        if scale is not None:
            kw['scale'] = self._a(scale)
        if accum is not None:
            kw['accum_out'] = accum.ap
        w = [out] + ([accum] if accum is not None else [])
        self.S.op('act', lambda e: e.activation(o, i, func, **kw),
                  reads=self._t([in_, bias, scale]), writes=w, cost=230.0 + _fsz(o) / 1.2)

    def tt(self, eng, out, a, b, op):
        o, x, y = out.ap, a.ap, b.ap
        self.S.op(eng, lambda e: e.tensor_tensor(o, x, y, op), reads=[a, b], writes=[out], cost=(120.0 + _fsz(o) / 0.96) if eng == 'dve' else (250.0 + _fsz(o) / 0.6))

    def ts(self, eng, out, a, s1, op0, s2=None, op1=None):
        o, x = out.ap, a.ap
        a1, a2 = self._a(s1), self._a(s2)
        if op1 is None:
            self.S.op(eng, lambda e: e.tensor_scalar(o, x, a1, None, op0), reads=self._t([a, s1]), writes=[out], cost=(120.0 + _fsz(o) / 1.5) if eng == 'dve' else (250.0 + _fsz(o) / 0.6))
        else:
            self.S.op(eng, lambda e: e.tensor_scalar(o, x, a1, a2, op0, op1), reads=self._t([a, s1, s2]), writes=[out], cost=(120.0 + _fsz(o) / 1.5) if eng == 'dve' else (250.0 + _fsz(o) / 0.6))

    def stt(self, out, a, s, b, op0, op1):
        o, x, y, sc = out.ap, a.ap, b.ap, self._a(s)
        self.S.op('dve', lambda e: e.scalar_tensor_tensor(o, x, sc, y, op0, op1), reads=self._t([a, s, b]), writes=[out], cost=120.0 + _fsz(o) / 0.96)

    def cp(self, eng, out, in_):
        o, i = out.ap, in_.ap
        if eng == 'act':
            self.S.op('act', lambda e: e.copy(o, i), reads=[in_], writes=[out], cost=230.0 + _fsz(o) / 1.2)
        else:
            self.S.op(eng, lambda e: e.tensor_copy(o, i), reads=[in_], writes=[out], cost=(120.0 + _fsz(o) / 1.5) if eng == 'dve' else (250.0 + _fsz(o) / 0.6))

    def red(self, out, in_, op=None, axis=None):
        o, i = out.ap, in_.ap
        op = op or ALU.add
        axis = axis or AX.X
        self.S.op('dve', lambda e: e.tensor_reduce(o, i, axis, op), reads=[in_], writes=[out], cost=120.0 + _fsz(i) / 0.96)

    def memset(self, eng, out, val):
        o = out.ap
        self.S.op(eng, lambda e: e.memset(o, val), reads=[], writes=[out], cost=150.0 + _fsz(o) / 0.96)

    def asel(self, out, in_, pattern, cmp, fill, base, cm):
        o, i = out.ap, in_.ap
        def fn(e):
            try:
                return e.affine_select(o, i, pattern, cmp, fill, base=base, channel_multiplier=cm)
            except Exception:
                print("ASEL FAIL", pattern, base, cm, fill, o)
                raise
        self.S.op('pool', fn, reads=[in_], writes=[out], cost=300.0 + _fsz(o) / 0.9)

    def scan(self, out, d0, d1, init, op0, op1):
        o, x, y, ii = out.ap, d0.ap, d1.ap, self._a(init)
        self.S.op('dve', lambda e: e.tensor_tensor_scan(o, x, y, ii, op0, op1), reads=self._t([d0, d1, init]), writes=[out], cost=120.0 + _fsz(o) * 2 / 0.96)

    def recip(self, out, in_):
        o, i = out.ap, in_.ap
        self.S.op('dve', lambda e: e.reciprocal(o, i), reads=[in_], writes=[out])

    def max8(self, out, in_):
        o, i = out.ap, in_.ap
        self.S.op('dve', lambda e: e.max(o, i), reads=[in_], writes=[out])

    def mrep(self, out, rep, vals, imm):
        o, r, v = out.ap, rep.ap, vals.ap
        self.S.op('dve', lambda e: e.match_replace(o, r, v, imm), reads=[rep, vals], writes=[out])

    def dma(self, eng, out, in_, out_final=False, **kw):
        self.S.dma(eng, out, in_, reads=self._t([in_]), writes=self._t([out]), out=out_final, **kw)


D_MODEL = 1024
NORM_EPS = 1e-6
IN_SPLITS = (
    ('gdn_q', 512), ('gdn_k', 512), ('gdn_v', 512), ('gdn_beta', 4), ('gdn_a', 4), ('gdn_z', 512),
    ('gla_q', 256), ('gla_k', 256), ('gla_v', 512), ('gla_gk', 16), ('gla_z', 512),
    ('ssd_x', 512), ('ssd_b', 256), ('ssd_c', 256), ('ssd_dt', 8), ('ssd_z', 512),
    ('nsa_q', 512), ('nsa_kc', 128), ('nsa_vc', 128), ('nsa_ks', 128), ('nsa_vs', 128),
    ('nsa_kw', 128), ('nsa_vw', 128), ('nsa_gate', 24), ('nsa_z', 512),
    ('merge_gate', 4096),
)
OFF = {}
_s = 0
for _n, _w in IN_SPLITS:
    OFF[_n] = _s
    _s += _w
D_IN = _s
MT = 512
NEG = -30000.0


def host_constants(S_len):
    c = {}
    ident = np.eye(128, dtype=np.float32)
    U = np.triu(np.ones((128, 128), np.float32))
    ones = np.ones((128, 128), np.float32)
    jidx = np.tile(np.arange(64, dtype=np.float32)[None, :], (128, 1))
    e0 = np.zeros((128, 64), np.float32)
    e0[:, 0] = 1.0
    half = (np.arange(128) >= 64).astype(np.float32)[:, None]
    c['cst'] = np.concatenate([ident, U, ones, jidx, e0, half], axis=1)
    slopes = 2.0 ** (-np.arange(1, 9, dtype=np.float64))
    t = np.arange(S_len)
    qaug = np.zeros((4, 8, S_len), np.float32)
    for h in range(8):
        qaug[0, h] = slopes[h] * 64
        qaug[1, h] = slopes[h]
        qaug[2, h] = -slopes[h] * 64 * (t // 64)
        qaug[3, h] = -slopes[h] * (t % 64)
    c['qaug'] = qaug
    kaug = np.stack([t // 64, t % 64, np.ones_like(t), np.ones_like(t)]).astype(np.float32)
    c['kaug'] = kaug
    ncp = 256
    cp = np.arange(ncp) * 16 + 31
    kc_ = np.stack([cp // 64, cp % 64, np.ones_like(cp), np.ones_like(cp)]).astype(np.float32)
    c['kaugc'] = np.concatenate([np.zeros((4, 1), np.float32), kc_[:, :-1]], axis=1)
    nsel = S_len // 64
    cs = np.arange(ncp) * 16
    ss = np.arange(64) * 64
    ov = ((cs[:, None] <= ss[None, :] + 63) & (cp[:, None] >= ss[None, :])).astype(np.float32)
    ov[:, nsel:] = 0.0
    ov = np.concatenate([np.zeros((1, 64), np.float32), ov[:-1]], axis=0)
    c['overlap'] = ov
    E = np.zeros((64, S_len), np.float32)
    E[t // 64, t] = 1.0
    c['esel'] = E
    return c


PARAM_SHAPES = {
    'norm_pre': [1024], 'norm_post': [1024],
    'conv_aT': [128, 12, 4], 'a_log_a': [4], 'dt_bias_a': [4], 'onorm_a': [128],
    'w_gk': [16, 256], 'b_gkT': [128, 2], 'onorm_b': [128],
    'conv_cT': [128, 8, 4], 'conv_bias_cT': [128, 8], 'a_log_c': [8], 'dt_bias_c': [8], 'd_skip_c': [8],
    'onorm_c': [512], 'cmp_pos_kT': [64, 32], 'cmp_pos_vT': [64, 32],
    'w_ck1': [2048, 64], 'w_ck2': [64, 64], 'w_cv1': [2048, 64], 'w_cv2': [64, 64],
    'w_br': [4, 512, 1024], 'w_out': [1024, 1024], 'w_in': [1024, D_IN],
}


def prep_inputs(inp, depth):
    o = {}
    f = lambda a: np.ascontiguousarray(np.asarray(a, dtype=np.float32))
    for k in ('norm_pre', 'norm_post', 'a_log_a', 'dt_bias_a', 'onorm_a', 'w_gk', 'onorm_b', 'a_log_c',
              'dt_bias_c', 'd_skip_c', 'onorm_c', 'w_ck1', 'w_ck2', 'w_cv1', 'w_cv2', 'w_br', 'w_out', 'w_in'):
        o[k] = f(inp[k][:depth])
    o['conv_aT'] = f(np.asarray(inp['conv_a'])[:depth].reshape(depth, 4, 12, 128).transpose(0, 3, 2, 1))
    o['conv_cT'] = f(np.asarray(inp['conv_c'])[:depth].reshape(depth, 4, 8, 128).transpose(0, 3, 2, 1))
    o['conv_bias_cT'] = f(np.asarray(inp['conv_bias_c'])[:depth].reshape(depth, 8, 128).transpose(0, 2, 1))
    o['b_gkT'] = f(np.asarray(inp['b_gk'])[:depth].reshape(depth, 2, 128).transpose(0, 2, 1))
    o['cmp_pos_kT'] = f(np.asarray(inp['cmp_pos_k'])[:depth].transpose(0, 2, 1))
    o['cmp_pos_vT'] = f(np.asarray(inp['cmp_pos_v'])[:depth].transpose(0, 2, 1))
    return o


def build(S_len=4096, depth=4, branches=(0, 1, 2, 3)):
    nc = bass.Bass("TRN2", target_bir_lowering=False)
    NT = S_len // 128
    NM = S_len // MT
    S = Sched(nc)
    b = B(S)
    dr = {}
    dr['x'] = nc.dram_tensor("x", [S_len, 1024], F32, kind="ExternalInput").ap()
    for k, shp in PARAM_SHAPES.items():
        dr[k] = nc.dram_tensor(k, [depth] + shp, F32, kind="ExternalInput").ap()
    hc = host_constants(S_len)
    for k, v in hc.items():
        dr[k] = nc.dram_tensor(k, list(v.shape), F32, kind="ExternalInput").ap()
    y = nc.dram_tensor("y", [S_len, 1024], F32, kind="ExternalOutput").ap()
    wq = {'w_in': nc.dram_tensor("wq_in", [depth, 1024, D_IN], BF16, kind="Internal").ap(),
          'w_br': nc.dram_tensor("wq_br", [depth, 2048, 1024], BF16, kind="Internal").ap(),
          'w_out': nc.dram_tensor("wq_out", [depth, 1024, 1024], BF16, kind="Internal").ap()}
    wqres = {}
    yres = [S.res("y%d" % g) for g in range(NT)]

    def dt(ap, res=None):
        return Tile('dram', ap, res.rid if res is not None else 0)

    cst = S.sb("cst", [128, 513], F32)
    b.dma('sp', cst, dt(dr['cst']))
    ident_f, U_f, ones_f = cst[:, 0:128], cst[:, 128:256], cst[:, 256:384]
    cstb = S.sb("cstb", [128, 384], BF16)
    b.cp('dve', cstb, cst[:, 0:384])
    ident_b, U_b, ones_b = cstb[:, 0:128], cstb[:, 128:256], cstb[:, 256:384]
    mhalf = S.sb("mhalf", [128, 1], F32)
    b.memset('dve', mhalf, -0.5)

    PS = [S.ps("ps%d" % i, [128, 512], F32) for i in range(7)]
    PTb = S.ps("ptb", [128, 1024], BF16)
    psi = [0]

    def nps(lo=0):
        psi[0] += 1
        return PS[lo + psi[0] % (7 - lo)]

    h4 = lambda t: t.re("p (h i) -> p h i", h=4)

    pools = {}

    def scr(cls, i):
        key = (cls, i)
        if key not in pools:
            pools[key] = S.sb("scr%s%d" % (cls, i), [128, 512], F32 if cls == 'A' else BF16)
        return pools[key]

    FM = S.sb("FM", [128, 16, MT], BF16)
    xts = [S.sb("xt%d" % i, [128, 1024], F32) for i in range(1)]
    hb = S.sb("hb", [128, 1024], BF16)
    junk = hb
    hT = S.sb("hT", [128, 8, MT], BF16)
    wbufs = [S.sb("wb%d" % i, [128, 8, 528], BF16) for i in range(2)]
    wi = [0]
    merged = [S.sb("mg%d" % i, [128, 1024], F32) for i in range(4)]
    gpre = S.sb("gpre", [128, 1024], F32)
    gpost = S.sb("gpost", [128, 1024], F32)
    ssq = S.sb("ssq", [128, 1], F32)
    rstd = S.sb("rstd", [128, 1], F32)
    ybr = [S.sb("ybr%d" % i, [128, 512], BF16) for i in range(4)]
    yT = S.sb("yT", [128, 4, 4, 128], BF16)
    zz = [S.sb("zz%d" % i, [128, 512], BF16) for i in range(4)]
    raw = [S.sb("raw%d" % i, [128, 515], BF16) for i in range(2)]
    dg = [S.sb("dg%d" % i, [128, 4, 128], BF16) for i in range(2)]
    sg = scr('A', 0)
    tmpm = scr('A', 1)
    mgb = hb
    mgT = yT[:, 0:2].re("p a k t -> p (a k) t")

    def bcast_load(tile, src_ap, n):
        b.dma('pool', tile, dt(src_ap.partition_broadcast(128)))

    def convert_layer(l):
        for name, src2d in (('w_in', dr['w_in'][l]), ('w_br', dr['w_br'][l].rearrange("n k c -> (n k) c")),
                            ('w_out', dr['w_out'][l])):
            r = S.res("wq_%s%d" % (name, l))
            wqres[(name, l)] = r
            ncol = src2d.shape[1]
            nrow = src2d.shape[0]
            for r0 in range(0, nrow, 1024):
                for c0 in range(0, ncol, 2048):
                    c1 = min(c0 + 2048, ncol)
                    S.dma('pool', wq[name][l][r0:r0 + 1024, c0:c1], src2d[r0:r0 + 1024, c0:c1], reads=[], writes=[r])

    def wload(key, col0, ncols, rows8=True):
        name, l, row0, nrows = key
        wb = wbufs[wi[0] % 2]
        wi[0] += 1
        src = wq[name][l][row0:row0 + nrows, :].rearrange("(k p) n -> p k n", p=128)
        nk = src.shape[1]
        b.dma('sp', wb[:, 0:nk, 0:ncols], dt(src[:, :, col0:col0 + ncols], wqres[(name, l)]))
        return wb

    def proj_tok(ps, st, wb, c0, n, o0=0):
        for kc in range(8):
            b.mm(ps[:, o0:o0 + n], hT[:, kc, st * 128:(st + 1) * 128], wb[:, kc, c0:c0 + n], start=kc == 0, stop=kc == 7)

    def proj_feat(ps, wb, c0, mch):
        for kc in range(8):
            b.mm(ps[0:mch, :], wb[:, kc, c0:c0 + mch], hT[:, kc, :], start=kc == 0, stop=kc == 7)

    def rms_rstd(out1, ss1, n):
        b.ts('dve', out1, ss1, 1.0 / n, ALU.mult, NORM_EPS, ALU.add)
        b.tt('pool', out1, out1, mhalf_k(out1.ap.shape[1]), ALU.pow)

    mh_cache = {}

    def mhalf_k(k):
        if k not in mh_cache:
            t = S.sb("mh%d" % k, [128, k], F32)
            b.memset('dve', t, -0.5)
            mh_cache[k] = t
        return mh_cache[k]

    def emit_yT(st):
        for kc in range(4):
            b.tr(PTb[:, kc * 128:(kc + 1) * 128], ybr[st][:, kc * 128:(kc + 1) * 128], ident_b)
        b.cp('dve', yT[:, st], PTb[:, 0:512].re("p (k t) -> p k t", k=4))

    ctx = dict(jidx=cst[:, 384:448], e0=cst[:, 448:512], half01=cst[:, 512:513], emit_yT=emit_yT, scr=scr, FM=FM, nc=nc, S=S, b=b, dr=dr, dt=dt, PS=PS, PTb=PTb, nps=nps, h4=h4, hT=hT, wload=wload,
               proj_tok=proj_tok, proj_feat=proj_feat, rms_rstd=rms_rstd, ident_f=ident_f, U_f=U_f,
               ones_f=ones_f, ident_b=ident_b, U_b=U_b, ones_b=ones_b, ybr=ybr, zz=zz, raw=raw, dg=dg,
               S_len=S_len, NT=NT, NM=NM, bcast_load=bcast_load, depth=depth, mhalf_k=mhalf_k)
    mixers = {}
    if 0 in branches:
        mixers[0] = GDN(ctx)
    if 1 in branches:
        mixers[1] = GLA(ctx)
    if 2 in branches:
        mixers[2] = SSD(ctx)
    if 3 in branches:
        mixers[3] = NSA(ctx)

    convert_layer(0)
    for l in range(depth):
        w_in_l = ('w_in', l, 0, 1024)
        if l + 1 < depth:
            convert_layer(l + 1)
        bcast_load(gpre, dr['norm_pre'][l:l + 1, :], 1024)
        bcast_load(gpost, dr['norm_post'][l:l + 1, :], 1024)
        for n in mixers:
            mixers[n].layer_setup(l)
        for m in range(NM):
            xt = xts[0]
            src = dr['x'] if l == 0 else y
            b.dma('pool', xt, dt(src[m * 512:m * 512 + 128, :], yres[m * 4]))
            for st in range(4):
                g = m * 4 + st
                b.act(junk, xt, AF.Square, accum=ssq)
                rms_rstd(rstd, ssq, 1024)
                b.stt(hb, xt, rstd[:, 0:1], gpre, ALU.mult, ALU.mult)
                if st < 3:
                    b.dma('pool', xt, dt(src[(g + 1) * 128:(g + 2) * 128, :], yres[g + 1]))
                for kc in range(8):
                    b.tr(PTb[:, kc * 128:(kc + 1) * 128], hb[:, kc * 128:(kc + 1) * 128], ident_b)
                b.cp('act', hT[:, :, st * 128:(st + 1) * 128], PTb.re("p (k t) -> p k t", k=8))
            first = True
            for n in (0, 1, 2, 3):
                if n not in mixers:
                    continue
                mixers[n].macro(l, m, w_in_l)
                for half in range(2):
                    wg = wload(w_in_l, OFF['merge_gate'] + n * 1024 + half * 512, 512)
                    wbr = wload(('w_br', l, n * 512, 512), half * 512, 512)
                    for st in range(4):
                        sg = scr('A', 0) if st % 2 == 0 else scr('A', 2)
                        tmpm = scr('A', 1) if st % 2 == 0 else scr('A', 3)
                        pg = nps()
                        proj_tok(pg, st, wg, 0, 512)
                        b.act(sg, pg, AF.Sigmoid)
                        pb = nps()
                        for kc in range(4):
                            b.mm(pb, yT[:, st, kc, :], wbr[:, kc, 0:512], start=kc == 0, stop=kc == 3)
                        mslice = merged[st][:, half * 512:(half + 1) * 512]
                        if first:
                            b.tt('dve', mslice, sg, pb, ALU.mult)
                        else:
                            b.tt('dve', tmpm, sg, pb, ALU.mult)
                            b.tt('pool', mslice, mslice, tmpm, ALU.add)
                first = False
            wo = [wload(('w_out', l, 0, 1024), half * 512, 512) for half in range(2)]
            for st in range(4):
                g = m * 4 + st
                osb = merged[st]
                xt = xts[0]
                src = dr['x'] if l == 0 else y
                b.dma('pool', xt, dt(src[g * 128:(g + 1) * 128, :], yres[g]))
                b.cp('act', mgb, merged[st])
                for kc in range(8):
                    b.tr(PTb[:, kc * 128:(kc + 1) * 128], mgb[:, kc * 128:(kc + 1) * 128], ident_b)
                b.cp('dve', mgT, PTb.re("p (k t) -> p k t", k=8))
                for half in range(2):
                    po = nps()
                    for kc in range(8):
                        b.mm(po, mgT[:, kc, :], wo[half][:, kc, 0:512], start=kc == 0, stop=kc == 7)
                    b.cp('act', osb[:, half * 512:(half + 1) * 512], po)
                b.act(junk, osb, AF.Square, accum=ssq)
                rms_rstd(rstd, ssq, 1024)
                b.stt(osb, osb, rstd[:, 0:1], gpost, ALU.mult, ALU.mult)
                b.tt('dve', osb, osb, xt, ALU.add)
                S.dma('pool', y[g * 128:(g + 1) * 128, :], osb.ap, reads=[osb], writes=[yres[g]], out=(l == depth - 1))
    S.emit()
    return nc, S


class Mixer:
    def __init__(self, ctx):
        self.__dict__.update(ctx)

    def conv_chunk(self, ps_in, convw, c, halo, out_fm, bias=None):
        b = self.b
        k = self.cc
        self.cc += 1
        raw, dg = self.raw[k % 2], self.dg[k % 2]
        b.cp('dve', raw[:, 0:3], halo)
        b.cp('act', raw[:, 3:515], ps_in)
        b.cp('dve', halo, raw[:, 512:515])
        b.tt('dve', dg, self.ident_b.bc(1, [128, 4, 128]), convw[:, c, :].bc(2, [128, 4, 128]), ALU.mult)
        p2 = self.nps()
        for t in range(4):
            b.mm(p2, dg[:, t, :], raw[:, t:t + 512], start=t == 0, stop=t == 3)
        if bias is None:
            b.act(out_fm, p2, AF.Silu)
        else:
            b.act(out_fm, p2, AF.Silu, bias=bias)

    def out_norm_heads(self, po, st, onz):
        b, S = self.b, self.S
        o = self.osb4
        b.cp('act', o, po)
        b.tt('pool', self.sq4, o, o, ALU.mult)
        b.red(self.ss4, self.h4(self.sq4))
        self.rms_rstd(self.rs4, self.ss4, 128)
        b.tt('dve', self.h4(o), self.h4(o), self.rs4.bc(2, [128, 4, 128]), ALU.mult)
        b.tt('dve', self.ybr[st], o, onz, ALU.mult)

    def z_block(self, wb, c0, onorm_b, per_head=True):
        b = self.b
        for st in range(4):
            pz = self.nps()
            self.proj_tok(pz, st, wb, c0, 512)
            b.act(self.zz[st], pz, AF.Silu)
            if onorm_b is not None:
                if per_head:
                    b.tt('pool', self.h4(self.zz[st]), self.h4(self.zz[st]), onorm_b.bc(1, [128, 4, 128]), ALU.mult)
                else:
                    b.tt('pool', self.zz[st], self.zz[st], onorm_b, ALU.mult)


class GDN(Mixer):
    def __init__(self, ctx):
        super().__init__(ctx)
        S = self.S
        self.cc = 0
        scr = self.scr
        A4 = lambda i: scr('A', i).re("p (h i) -> p h i", h=4)
        B4 = lambda i: scr('B', i).re("p (h i) -> p h i", h=4)
        self.fm = self.FM[:, 0:12, :]
        self.sqa, self.sqb = B4(9), B4(10)
        self.halo = [S.sb("gdn_h%d" % c, [128, 3], BF16) for c in range(12)]
        self.convw = S.sb("gdn_cw", [128, 12, 4], F32)
        self.nega = S.sb("gdn_nega", [128, 4], F32)
        self.dtb = S.sb("gdn_dtb", [128, 4], F32)
        self.onorm = S.sb("gdn_on", [128, 128], F32)
        self.Sf = S.sb("gdn_Sf", [128, 4, 128], F32)
        self.Sb = S.sb("gdn_Sb", [128, 4, 128], BF16)
        f = lambda n, k: S.sb("gdn_" + n, [128, k], F32)
        self.lnss, self.lnr, self.ba, self.e1, self.nlb = f("lnss", 8), f("lnr", 8), f("ba", 8), f("e1", 4), f("nlb", 4)
        self.apb, self.e2, self.sp, self.g, self.gcl = f("apb", 4), f("e2", 4), f("sp", 4), f("g", 4), f("gcl", 8)
        self.C3, self.X4, self.EX = f("C3", 12), f("X4", 16), f("EX", 16)
        self.D3 = [A4(2), A4(3), A4(4)]
        self.BJ, self.BA = A4(5), A4(6)
        self.E1, self.E2, self.E3 = A4(7), A4(8), A4(9)
        self.EB = B4(0)
        self.P = [A4(10), A4(11)]
        self.PT = [A4(12), A4(13)]
        self.AT = [A4(14), A4(15)]
        self.ATb, self.At, self.Rk, self.Kd = B4(1), B4(2), B4(3), B4(4)
        self.Vb, self.nW, self.Vn, self.qg = B4(5), B4(6), B4(7), B4(8)
        self.osb4 = scr('A', 0)
        self.sq4 = scr('A', 1)
        self.ss4, self.rs4 = f("ss4", 4), f("rs4", 4)
        self.ea = f("ea", 4)

    def layer_setup(self, l):
        b, dr, dt = self.b, self.dr, self.dt
        b.dma('pool', self.convw, dt(dr['conv_aT'][l]))
        self.bcast_load(self.ea, dr['a_log_a'][l:l + 1, :], 4)
        b.act(self.nega, self.ea, AF.Exp)
        b.ts('dve', self.nega, self.nega, -1.0, ALU.mult)
        self.bcast_load(self.dtb, dr['dt_bias_a'][l:l + 1, :], 4)
        self.bcast_load(self.onorm, dr['onorm_a'][l:l + 1, :], 128)
        b.memset('dve', self.Sf, 0.0)
        b.memset('dve', self.Sb, 0.0)
        for c in range(12):
            b.memset('pool', self.halo[c], 0.0)

    def macro(self, l, m, w_in_l):
        b, nps, h4 = self.b, self.nps, self.h4
        fm = self.fm
        for blk in range(3):
            wb = self.wload(w_in_l, blk * 512, 512)
            for cc in range(4):
                c = blk * 4 + cc
                p = nps()
                self.proj_feat(p, wb, cc * 128, 128)
                self.conv_chunk(p, self.convw, c, self.halo[c], fm[:, c, :])
        wb3 = self.wload(w_in_l, OFF['gdn_beta'], 520)
        self.z_block(wb3, 8, self.onorm)
        for st in range(4):
            self.sub(st, wb3)
            self.emit_yT(st)

    def sub(self, st, wb3):
        b, nps, h4 = self.b, self.nps, self.h4
        fm = self.fm
        tk = slice(st * 128, (st + 1) * 128)
        bc4 = lambda t: t.bc(2, [128, 4, 128])
        b.tt('pool', self.sqa, fm[:, 4:8, tk], fm[:, 4:8, tk], ALU.mult)
        b.tt('pool', self.sqb, fm[:, 0:4, tk], fm[:, 0:4, tk], ALU.mult)
        pq = nps()
        for c in range(8):
            sq_c = self.sqa[:, c, :] if c < 4 else self.sqb[:, c - 4, :]
            b.mm(pq[:, c:c + 1], sq_c, self.ones_b[:, 0:1])
        self.proj_tok(pq, st, wb3, 0, 8, o0=8)
        b.act(self.lnss, pq[:, 0:8], AF.Ln, bias=NORM_EPS)
        b.ts('dve', self.lnr, self.lnss, -0.5, ALU.mult)
        b.cp('dve', self.ba, pq[:, 8:16])
        b.act(self.e1, self.ba[:, 0:4], AF.Exp, scale=-1.0)
        b.act(self.nlb, self.e1, AF.Ln, bias=1.0)
        b.tt('dve', self.apb, self.ba[:, 4:8], self.dtb, ALU.add)
        b.act(self.e2, self.apb, AF.Exp)
        b.act(self.sp, self.e2, AF.Ln, bias=1.0)
        b.tt('dve', self.g, self.sp, self.nega, ALU.mult)
        pg = nps()
        b.mm(pg[:, 0:4], self.U_f, self.g)
        b.mm(pg[:, 4:8], self.ones_f, self.g)
        b.cp('dve', self.gcl, pg[:, 0:8])
        gc, gl = self.gcl[:, 0:4], self.gcl[:, 4:8]
        lnrk, lnrq = self.lnr[:, 0:4], self.lnr[:, 4:8]
        cA, cB, cJ = self.C3[:, 0:4], self.C3[:, 4:8], self.C3[:, 8:12]
        b.tt('dve', cJ, lnrk, gc, ALU.subtract)
        b.tt('dve', cA, gc, self.nlb, ALU.subtract)
        b.tt('dve', cA, cA, lnrk, ALU.add)
        b.stt(cB, gc, float(np.log(128.0 ** -0.5)), lnrq, ALU.add, ALU.add)
        X4 = self.X4
        b.cp('pool', X4[:, 0:4], cA)
        b.tt('pool', X4[:, 4:8], cJ, gl, ALU.add)
        b.ts('pool', X4[:, 8:12], self.nlb, -1.0, ALU.mult)
        b.cp('pool', X4[:, 12:16], gl)
        b.act(self.EX, X4, AF.Exp)
        sRk, sKd, sVb, dec = self.EX[:, 0:4], self.EX[:, 4:8], self.EX[:, 8:12], self.EX[:, 12:16]
        for v3 in range(3):
            b.tt('dve', self.D3[v3], self.ident_f.bc(1, [128, 4, 128]), bc4(self.C3[:, v3 * 4:(v3 + 1) * 4]), ALU.mult)
        b.cp('pool', self.BJ, bc4(cJ))
        b.cp('pool', self.BA, bc4(cA))
        f4 = lambda t: t.re("p h i -> p (h i)")
        pX1, pX2, pX3, pXB = nps(), nps(), nps(), nps()
        b.mm(pX1, self.ones_f, f4(self.D3[0]), start=True, stop=False)
        b.mm(pX1, self.ident_f, f4(self.BJ), start=False, stop=True)
        b.mm(pX2, self.ones_f, f4(self.D3[1]), start=True, stop=False)
        b.mm(pX2, self.ident_f, f4(self.BJ), start=False, stop=True)
        b.mm(pX3, self.ones_f, f4(self.D3[2]), start=True, stop=False)
        b.mm(pX3, self.ident_f, f4(self.BA), start=False, stop=True)
        b.mm(pXB, self.ones_f, f4(self.D3[1]))
        b.act(f4(self.E1), pX1, AF.Exp)
        b.act(f4(self.E2), pX2, AF.Exp)
        b.act(f4(self.E3), pX3, AF.Exp)
        b.act(f4(self.EB), pXB, AF.Exp)
        b.asel(self.E1, self.E1, [[0, 4], [1, 128]], ALU.is_ge, 0.0, -1, -1)
        b.asel(self.E2, self.E2, [[0, 4], [1, 128]], ALU.is_ge, 0.0, 0, -1)
        b.asel(self.E3, self.E3, [[0, 4], [-1, 128]], ALU.is_ge, 0.0, -1, 1)
        pG, pKQ = nps(), nps()
        for h in range(4):
            b.mm(pG[:, h * 128:(h + 1) * 128], fm[:, 4 + h, tk], fm[:, 4 + h, tk])
        for h in range(4):
            b.mm(pKQ[:, h * 128:(h + 1) * 128], fm[:, 4 + h, tk], fm[:, h, tk])
        P, PT, AT = self.P, self.PT, self.AT
        b.stt(f4(PT[0]), f4(self.E1), -1.0, pG, ALU.mult, ALU.mult)
        b.stt(f4(P[0]), f4(self.E3), -1.0, pG, ALU.mult, ALU.mult)
        b.tt('dve', f4(self.At), f4(self.E2), pKQ, ALU.mult)
        b.tt('pool', AT[0], PT[0], self.ident_f.bc(1, [128, 4, 128]), ALU.add)
        cur = 0
        for lev in range(1, 7):
            nxt = 1 - cur
            pP = nps()
            for h in range(4):
                b.mm(pP[:, h * 128:(h + 1) * 128], PT[cur][:, h, :], P[cur][:, h, :])
            if lev < 6:
                pPT = nps()
                for h in range(4):
                    b.mm(pPT[:, h * 128:(h + 1) * 128], P[cur][:, h, :], PT[cur][:, h, :])
            b.cp('act', f4(P[nxt]), pP)
            if lev < 6:
                b.cp('dve', f4(PT[nxt]), pPT)
            pA = nps()
            for h in range(4):
                b.mm(pA[:, h * 128:(h + 1) * 128], P[nxt][:, h, :], AT[cur][:, h, :])
            b.tt('dve', f4(AT[nxt]), f4(AT[cur]), pA, ALU.add)
            cur = nxt
        b.cp('act', self.ATb, AT[cur])
        PTb = self.PTb
        for h in range(4):
            b.tr(PTb[:, h * 128:(h + 1) * 128], fm[:, 4 + h, tk], self.ident_b)
            b.tr(PTb[:, (4 + h) * 128:(5 + h) * 128], fm[:, 8 + h, tk], self.ident_b)
        pk = PTb[:, 0:512].re("p (h d) -> p h d", h=4)
        pv = PTb[:, 512:1024].re("p (h d) -> p h d", h=4)
        b.tt('dve', self.Rk, pk, bc4(sRk), ALU.mult)
        b.tt('dve', self.Kd, pk, bc4(sKd), ALU.mult)
        b.tt('dve', self.Vb, pv, bc4(sVb), ALU.mult)
        pW = nps()
        for h in range(4):
            b.mm(pW[:, h * 128:(h + 1) * 128], self.Rk[:, h, :], self.ATb[:, h, :])
        b.act(f4(self.nW), pW, AF.Copy, scale=-1.0)
        pV = nps()
        for h in range(4):
            b.mm(pV[:, h * 128:(h + 1) * 128], self.ATb[:, h, :], self.Vb[:, h, :], start=(h == 0), stop=False)
            b.mm(pV[:, h * 128:(h + 1) * 128], self.nW[:, h, :], self.Sb[:, h, :], start=False, stop=True)
        b.cp('act', f4(self.Vn), pV)
        b.tt('dve', self.qg, fm[:, 0:4, tk], self.EB, ALU.mult)
        pO = nps()
        for h in range(4):
            b.mm(pO[:, h * 128:(h + 1) * 128], self.qg[:, h, :], self.Sb[:, h, :], start=(h == 0), stop=False)
            b.mm(pO[:, h * 128:(h + 1) * 128], self.At[:, h, :], self.Vn[:, h, :], start=False, stop=True)
        pS = nps()
        for h in range(4):
            b.mm(pS[:, h * 128:(h + 1) * 128], self.Kd[:, h, :], self.Vn[:, h, :])
        b.tt('dve', self.Sf, self.Sf, bc4(dec), ALU.mult)
        b.tt('dve', f4(self.Sf), f4(self.Sf), pS, ALU.add)
        b.cp('act', self.Sb, self.Sf)
        self.out_norm_heads(pO, st, self.zz[st])


class GLA(Mixer):
    def __init__(self, ctx):
        super().__init__(ctx)
        S = self.S
        scr = self.scr
        A2 = lambda i, o: scr('A', i)[:, o * 256:(o + 1) * 256].re("p (h i) -> p h i", h=2)
        B4 = lambda i: scr('B', i).re("p (h i) -> p h i", h=4)
        B2 = lambda i, o: scr('B', i)[:, o * 256:(o + 1) * 256].re("p (h i) -> p h i", h=2)
        self.fm = self.FM[:, 0:4, :]
        self.vt = [scr('B', 4 + i) for i in range(4)]
        self.gklo = scr('A', 10)
        self.wgk = S.sb("gla_wgk", [128, 256], F32)
        self.nbg = S.sb("gla_nbg", [128, 2], F32)
        self.onorm = S.sb("gla_on", [128, 128], F32)
        self.e = scr('A', 2)
        self.sp = [scr('A', 6), scr('A', 7)]
        self.nb = [scr('A', 8), scr('A', 9)]
        self.negc = S.sb("gla_negc", [128, 2, 2], F32)
        self.Eq, self.Ek = A2(3, 0), A2(3, 1)
        self.Eg, self.Ed = A2(4, 0), A2(4, 1)
        self.qt, self.kd = B2(0, 0), B2(0, 1)
        self.kt = B4(1)
        self.qg = B4(2)
        self.kdt = scr('B', 3)[:, 0:256]
        self.At = B4(8)
        self.Sf = S.sb("gla_Sf", [128, 2, 128], F32)
        self.Sb = S.sb("gla_Sb", [128, 2, 128], BF16)
        self.osb4 = scr('A', 0)
        self.sq4 = scr('A', 1)
        self.ss4 = S.sb("gla_ss4", [128, 4], F32)
        self.rs4 = S.sb("gla_rs4", [128, 4], F32)
        self.rm = S.sb("gla_rm", [128, 4], F32)
        self.Sd = scr('A', 5)[:, 0:128]

    def layer_setup(self, l):
        b, dr, dt = self.b, self.dr, self.dt
        b.memset('dve', self.wgk, 0.0)
        b.dma('pool', self.wgk[0:16, :], dt(dr['w_gk'][l]))
        b.dma('pool', self.nbg, dt(dr['b_gkT'][l]))
        b.ts('dve', self.nbg, self.nbg, -1.0, ALU.mult)
        self.bcast_load(self.onorm, dr['onorm_b'][l:l + 1, :], 128)
        b.memset('dve', self.Sf, 0.0)
        b.memset('dve', self.Sb, 0.0)
        if l == 0:
            b.memset('dve', self.rm, 0.0)
            b.memset('dve', self.rm[0:64, 0:1], 1.0)
            b.memset('dve', self.rm[64:128, 1:2], 1.0)
            b.memset('dve', self.rm[0:64, 2:3], 0.125)
            b.memset('dve', self.rm[64:128, 3:4], 0.125)

    def macro(self, l, m, w_in_l):
        b, nps = self.b, self.nps
        wb = self.wload(w_in_l, OFF['gla_q'], 512)
        for c in range(4):
            p = nps()
            self.proj_feat(p, wb, c * 128, 128)
            b.cp('act', self.fm[:, c, :], p)
        wb = self.wload(w_in_l, OFF['gla_v'], 512)
        for st in range(4):
            p = nps()
            self.proj_tok(p, st, wb, 0, 512)
            b.cp('act', self.vt[st], p)
        wb = self.wload(w_in_l, OFF['gla_gk'], 528)
        p = nps()
        self.proj_feat(p, wb, 0, 128)
        b.memset('pool', self.gklo, 0.0)
        b.cp('dve', self.gklo[0:16, :], p[0:16, :])
        for c in range(2):
            p = nps()
            b.mm(p, self.wgk[:, c * 128:(c + 1) * 128], self.gklo)
            b.act(self.e, p, AF.Exp, scale=-1.0, bias=self.nbg[:, c:c + 1])
            b.act(self.sp[c], self.e, AF.Ln, bias=1.0)
            b.ts('dve', self.sp[c], self.sp[c], 1.0 / 16.0, ALU.mult)
            for st in range(4):
                tk = slice(st * 128, (st + 1) * 128)
                b.scan(self.nb[c][:, tk], self.ones_f, self.sp[c][:, tk], 0.0, ALU.mult, ALU.add)
        self.z_block(wb, 16, self.onorm)
        for st in range(4):
            self.sub(st)
            self.emit_yT(st)

    def sub(self, st):
        b, nps = self.b, self.nps
        tk = slice(st * 128, (st + 1) * 128)
        fm = self.fm
        for c in range(2):
            nbs = self.nb[c][:, tk]
            ref = self.nb[c][:, st * 128 + 64:st * 128 + 65]
            last = self.nb[c][:, st * 128 + 127:st * 128 + 128]
            b.ts('dve', self.negc[:, c, 0:1], ref, -1.0, ALU.mult)
            b.ts('dve', self.negc[:, c, 1:2], last, -1.0, ALU.mult)
            b.act(self.Eq[:, c, :], nbs, AF.Exp, scale=-1.0, bias=ref)
            b.act(self.Ek[:, c, :], nbs, AF.Exp, bias=self.negc[:, c, 0:1])
            b.act(self.Eg[:, c, :], nbs, AF.Exp, scale=-1.0)
            b.act(self.Ed[:, c, :], nbs, AF.Exp, bias=self.negc[:, c, 1:2])
        b.stt(self.qt, self.Eq, 0.125, fm[:, 0:2, tk], ALU.mult, ALU.mult)
        b.tt('pool', self.kd, self.Ed, fm[:, 2:4, tk], ALU.mult)
        for h in range(4):
            c, r = h // 2, h % 2
            b.stt(self.kt[:, h, :], self.Ek[:, c, :], self.rm[:, r:r + 1], fm[:, 2 + c, tk], ALU.mult, ALU.mult)
            b.stt(self.qg[:, h, :], self.Eg[:, c, :], self.rm[:, 2 + r:3 + r], fm[:, c, tk], ALU.mult, ALU.mult)
        pA = nps()
        for h in range(4):
            c = h // 2
            b.mm(pA[:, h * 128:(h + 1) * 128], self.kt[:, h, :], self.qt[:, c, :])
        b.tt('dve', self.At, self.h4(pA), self.U_b.bc(1, [128, 4, 128]), ALU.mult)
        for c in range(2):
            b.tr(self.PTb[:, c * 128:(c + 1) * 128], self.kd[:, c, :], self.ident_b)
        b.cp('act', self.kdt, self.PTb[:, 0:256])
        pO = nps()
        for h in range(4):
            c = h // 2
            hs = slice(h * 128, (h + 1) * 128)
            b.mm(pO[:, hs], self.At[:, h, :], self.vt[st][:, hs], start=(h == 0), stop=False)
            b.mm(pO[:, hs], self.qg[:, h, :], self.Sb[:, c, :], start=False, stop=True)
        pS = nps()
        for h in range(4):
            c = h // 2
            b.mm(pS[:, h * 128:(h + 1) * 128], self.kdt[:, c * 128:(c + 1) * 128], self.vt[st][:, h * 128:(h + 1) * 128])
        for c in range(2):
            b.ts('dve', self.Sd, self.Sf[:, c, :], self.Eg[:, c, 127:128], ALU.mult)
            b.stt(self.Sd, pS[:, (2 * c) * 128:(2 * c + 1) * 128], self.rm[:, 0:1], self.Sd, ALU.mult, ALU.add)
            b.stt(self.Sf[:, c, :], pS[:, (2 * c + 1) * 128:(2 * c + 2) * 128], self.rm[:, 1:2], self.Sd, ALU.mult, ALU.add)
        b.cp('act', self.Sb, self.Sf)
        self.out_norm_heads(pO, st, self.zz[st])


class SSD(Mixer):
    def __init__(self, ctx):
        super().__init__(ctx)
        S = self.S
        self.cc = 0
        scr = self.scr
        A4 = lambda i: scr('A', i).re("p (h i) -> p h i", h=4)
        B4 = lambda i: scr('B', i).re("p (h i) -> p h i", h=4)
        self.fm = self.FM[:, 0:8, :]
        self.halo = [S.sb("ssd_h%d" % c, [128, 3], BF16) for c in range(8)]
        self.convw = S.sb("ssd_cw", [128, 8, 4], F32)
        self.convb = S.sb("ssd_cb", [128, 8], F32)
        f = lambda n, k: S.sb("ssd_" + n, [128, k], F32)
        self.nega, self.dtb, self.dsk, self.ea = f("nega", 8), f("dtb", 8), f("dsk", 8), f("ea", 8)
        self.onc = S.sb("ssd_onc", [128, 512], F32)
        self.dtr, self.apb, self.e, self.dtv, self.da, self.acl = f("dtr", 8), f("apb", 8), f("e", 8), f("dtv", 8), f("da", 8), f("acl", 16)
        self.X, self.EX, self.sdtd = f("X", 24), f("EX", 24), f("sdtd", 8)
        self.D8 = [A4(2), A4(3)]
        self.Bn = [A4(4), A4(5)]
        self.E = [A4(6), A4(7)]
        self.Mt = [B4(0), B4(1)]
        self.xdt = scr('B', 2)
        self.xdtd = scr('B', 3)
        self.xsk = scr('A', 8)
        self.Btok = scr('B', 4)[:, 0:256]
        self.yo = scr('A', 9)
        self.Hf = S.sb("ssd_Hf", [128, 512], F32)
        self.Hb = S.sb("ssd_Hb", [128, 512], BF16)
        self.ssq = f("ssq", 1)
        self.rstd = f("rstd", 1)
        self.junk = scr('B', 5)

    def layer_setup(self, l):
        b, dr, dt = self.b, self.dr, self.dt
        b.dma('pool', self.convw, dt(dr['conv_cT'][l]))
        b.dma('pool', self.convb, dt(dr['conv_bias_cT'][l]))
        self.bcast_load(self.ea, dr['a_log_c'][l:l + 1, :], 8)
        b.act(self.nega, self.ea, AF.Exp)
        b.ts('dve', self.nega, self.nega, -1.0, ALU.mult)
        self.bcast_load(self.dtb, dr['dt_bias_c'][l:l + 1, :], 8)
        self.bcast_load(self.dsk, dr['d_skip_c'][l:l + 1, :], 8)
        self.bcast_load(self.onc, dr['onorm_c'][l:l + 1, :], 512)
        b.memset('dve', self.Hf, 0.0)
        b.memset('dve', self.Hb, 0.0)
        for c in range(8):
            b.memset('pool', self.halo[c], 0.0)

    def macro(self, l, m, w_in_l):
        b, nps = self.b, self.nps
        for blk in range(2):
            wb = self.wload(w_in_l, OFF['ssd_x'] + blk * 512, 512)
            for cc in range(4):
                c = blk * 4 + cc
                p = nps()
                self.proj_feat(p, wb, cc * 128, 128)
                self.conv_chunk(p, self.convw, c, self.halo[c], self.fm[:, c, :], bias=self.convb[:, c:c + 1])
        wb3 = self.wload(w_in_l, OFF['ssd_dt'], 520)
        self.z_block(wb3, 8, None)
        for st in range(4):
            self.sub(st, wb3)
            self.emit_yT(st)

    def sub(self, st, wb3):
        b, nps = self.b, self.nps
        fm = self.fm
        tk = slice(st * 128, (st + 1) * 128)
        h8 = lambda t, k=64: t.re("p (h i) -> p h i", h=8)
        bc8 = lambda t, k: t.bc(2, [128, 8, k])
        pq = nps()
        self.proj_tok(pq, st, wb3, 0, 8)
        b.cp('dve', self.dtr, pq[:, 0:8])
        b.tt('dve', self.apb, self.dtr, self.dtb, ALU.add)
        b.act(self.e, self.apb, AF.Exp)
        b.act(self.dtv, self.e, AF.Ln, bias=1.0)
        b.tt('dve', self.da, self.dtv, self.nega, ALU.mult)
        pg = nps()
        b.mm(pg[:, 0:8], self.U_f, self.da)
        b.mm(pg[:, 8:16], self.ones_f, self.da)
        b.cp('dve', self.acl, pg[:, 0:16])
        acs, alast = self.acl[:, 0:8], self.acl[:, 8:16]
        b.cp('pool', self.X[:, 0:8], acs)
        b.tt('pool', self.X[:, 8:16], alast, acs, ALU.subtract)
        b.cp('pool', self.X[:, 16:24], alast)
        b.act(self.EX, self.X, AF.Exp)
        eacs, edst, dec = self.EX[:, 0:8], self.EX[:, 8:16], self.EX[:, 16:24]
        b.tt('dve', self.sdtd, self.dtv, edst, ALU.mult)
        bc4 = lambda t: t.bc(2, [128, 4, 128])
        f4 = lambda t: t.re("p h i -> p (h i)")
        pCB = nps()
        for g in range(2):
            b.mm(pCB[:, g * 128:(g + 1) * 128], fm[:, 4 + g, tk], fm[:, 6 + g, tk])
        for g in range(2):
            hs = slice(g * 4, g * 4 + 4)
            b.tt('dve', self.D8[g], self.ident_f.bc(1, [128, 4, 128]), bc4(acs[:, hs]), ALU.mult)
            b.ts('pool', self.Bn[g], bc4(acs[:, hs]), -1.0, ALU.mult)
            pX = nps()
            b.mm(pX, self.ones_f, f4(self.D8[g]), start=True, stop=False)
            b.mm(pX, self.ident_f, f4(self.Bn[g]), start=False, stop=True)
            b.act(f4(self.E[g]), pX, AF.Exp)
            b.asel(self.E[g], self.E[g], [[0, 4], [1, 128]], ALU.is_ge, 0.0, 0, -1)
            b.tt('dve', self.Mt[g], self.E[g], pCB[:, g * 128:(g + 1) * 128].bc(1, [128, 4, 128]), ALU.mult)
        PTb = self.PTb
        for c in range(4):
            b.tr(PTb[:, c * 128:(c + 1) * 128], fm[:, c, tk], self.ident_b)
        for g in range(2):
            b.tr(PTb[:, 512 + g * 128:512 + (g + 1) * 128], fm[:, 4 + g, tk], self.ident_b)
        xtok = h8(PTb[:, 0:512])
        b.tt('dve', h8(self.xdt), xtok, bc8(self.dtv, 64), ALU.mult)
        b.tt('dve', h8(self.xdtd), xtok, bc8(self.sdtd, 64), ALU.mult)
        b.tt('dve', h8(self.xsk), xtok, bc8(self.dsk, 64), ALU.mult)
        b.cp('act', self.Btok, PTb[:, 512:768])
        pY = nps()
        for h in range(8):
            b.mm(pY[:, h * 64:(h + 1) * 64], self.Mt[h // 4][:, h % 4, :], self.xdt[:, h * 64:(h + 1) * 64])
        pF = nps()
        for g in range(2):
            b.mm(pF[:, g * 256:(g + 1) * 256], fm[:, 6 + g, tk], self.Hb[:, g * 256:(g + 1) * 256])
        b.tt('dve', h8(self.yo), h8(pF), bc8(eacs, 64), ALU.mult)
        b.tt('pool', self.yo, self.yo, self.xsk, ALU.add)
        b.tt('dve', self.yo, self.yo, pY, ALU.add)
        pH = nps()
        for g in range(2):
            b.mm(pH[:, g * 256:(g + 1) * 256], self.Btok[:, g * 128:(g + 1) * 128], self.xdtd[:, g * 256:(g + 1) * 256])
        b.tt('dve', h8(self.Hf), h8(self.Hf), bc8(dec, 64), ALU.mult)
        b.tt('dve', self.Hf, self.Hf, pH, ALU.add)
        b.cp('act', self.Hb, self.Hf)
        b.tt('dve', self.yo, self.yo, self.zz[st], ALU.mult)
        b.act(self.junk, self.yo, AF.Square, accum=self.ssq)
        self.rms_rstd(self.rstd, self.ssq, 512)
        b.stt(self.ybr[st], self.yo, self.rstd[:, 0:1], self.onc, ALU.mult, ALU.mult)


class NSA(Mixer):
    def __init__(self, ctx):
        super().__init__(ctx)
        S, scr, S_len, NT = self.S, self.scr, self.S_len, self.NT
        self.kS = [S.sb("nsa_kS%d" % g, [128, S_len], BF16) for g in range(2)]
        self.kW = [S.sb("nsa_kW%d" % g, [128, 8 * 128], BF16) for g in range(2)]
        self.vS = [S.sb("nsa_vS%d" % g, [128, NT, 66], BF16) for g in range(2)]
        self.vW = [S.sb("nsa_vW%d" % g, [128, 8, 66], BF16) for g in range(2)]
        self.kC = [S.sb("nsa_kC%d" % g, [128, 256], BF16) for g in range(2)]
        self.vcT = [S.sb("nsa_vcT%d" % g, [128, 256], BF16) for g in range(2)]
        self.vC = [S.sb("nsa_vC%d" % g, [128, 2, 130], BF16) for g in range(2)]
        self.EK = S.sb("nsa_EK", [128, S_len], BF16)
        self.EKc = S.sb("nsa_EKc", [128, 256], BF16)
        self.qm = self.FM[:, 0:8, :]
        self.qA = self.FM[:, 8:16, :]
        self.qS = S.sb("nsa_qS", [128, 4, 128], BF16)
        self.rawc = [S.sb("nsa_rawc%d" % g, [128, 528], BF16) for g in range(2)]
        self.W1 = S.sb("nsa_W1", [128, 32, 128], BF16)
        self.W2k = S.sb("nsa_W2k", [128, 128], BF16)
        self.W2v = S.sb("nsa_W2v", [128, 128], BF16)
        self.posT = S.sb("nsa_posT", [128, 32], BF16)
        self.cpos = S.sb("nsa_cpos", [128, 1], F32)
        self.wsel = S.sb("nsa_wsel", [128, 8, 128], BF16)
        self.gsig = [S.sb("nsa_gs%d" % i, [128, 24], F32) for i in range(4)]
        self.rmq = S.sb("nsa_rmq", [128, 2], F32)
        self.hid = S.sb("nsa_hid", [128, 32], BF16)
        f = lambda n, k: S.sb("nsa_" + n, [128, k], F32)
        self.dall = S.sb("nsa_dall", [128, 3, 4], F32)
        self.rall = S.sb("nsa_rall", [128, 3, 4], F32)
        self.coef = S.sb("nsa_coef", [128, 3, 4], F32)
        self.imp, self.imp2, self.m8a, self.m8b, self.thr, self.selb = f("imp", 64), f("imp2", 64), f("m8a", 8), f("m8b", 8), f("thr", 1), f("selb", 64)
        self.selbb = S.sb("nsa_selbb", [128, 128], BF16)
        self.cur, self.val, self.fz = f("cur", 1), f("val", 64), f("fz", 64)
        self.Pb = [scr('B', i).re("p (h i) -> p h i", h=4) for i in range(3)]
        self.pi = 0
        self.on = scr('A', 2).re("p (h d) -> p h d", h=8)
        self.tmp4 = scr('A', 3)[:, 0:256].re("p (h d) -> p h d", h=4)
        self.tmp5 = scr('A', 4)[:, 0:256].re("p (h d) -> p h d", h=4)

    def layer_setup(self, l):
        b, dr, dt, nps = self.b, self.dr, self.dt, self.nps
        if NSTAGE < 0:
            return
        if l == 0:
            for g in range(2):
                for t in (self.kS[g], self.kW[g], self.vS[g], self.vW[g], self.kC[g], self.vcT[g], self.vC[g]):
                    b.memset('pool', t, 0.0)
                b.memset('pool', self.vS[g][:, :, 64:65], 1.0)
                b.memset('pool', self.vW[g][:, :, 64:65], 1.0)
                b.memset('pool', self.vC[g][:, :, 64:65], 1.0)
                b.memset('pool', self.vC[g][0:1, 0, 64:65], 0.0)
                for kt in range(2):
                    b.dma('pool', self.vC[g][:, kt, 65:129], dt(dr['overlap'][kt * 128:(kt + 1) * 128, :]))
            b.memset('pool', self.EK, 0.0)
            for c0 in range(0, self.S_len, 1024):
                c1 = min(c0 + 1024, self.S_len)
                b.dma('pool', self.EK[0:64, c0:c1], dt(dr['esel'][:, c0:c1]))
                b.dma('pool', self.EK[64:68, c0:c1], dt(dr['kaug'][:, c0:c1]))
            b.memset('pool', self.EKc, 0.0)
            b.dma('pool', self.EKc[64:68, :], dt(dr['kaugc']))
            b.memset('pool', self.qS, 0.0)
            b.memset('pool', self.selbb, 0.0)
            b.memset('pool', self.rmq, 0.0)
            b.memset('pool', self.rmq[0:64, 0:1], 0.125)
            b.memset('pool', self.rmq[64:128, 1:2], 0.125)
        b.memset('pool', self.W1, 0.0)
        b.dma('pool', self.W1[0:64, :, 0:64], dt(dr['w_ck1'][l].rearrange("(p d) o -> d p o", d=64)))
        b.dma('pool', self.W1[64:128, :, 64:128], dt(dr['w_cv1'][l].rearrange("(p d) o -> d p o", d=64)))
        b.memset('pool', self.W2k, 0.0)
        b.dma('pool', self.W2k[0:64, 0:64], dt(dr['w_ck2'][l]))
        b.dma('pool', self.W2k[0:64, 64:128], dt(dr['w_ck2'][l]))
        b.memset('pool', self.W2v, 0.0)
        b.dma('pool', self.W2v[64:128, 0:64], dt(dr['w_cv2'][l]))
        b.dma('pool', self.posT[0:64, :], dt(dr['cmp_pos_kT'][l]))
        b.dma('pool', self.posT[64:128, :], dt(dr['cmp_pos_vT'][l]))
        pc = nps()
        for p in range(32):
            b.mm(pc[:, 0:1], self.W1[:, p, :], self.posT[:, p:p + 1], start=p == 0, stop=p == 31)
        b.cp('dve', self.cpos, pc[:, 0:1])
        for g in range(2):
            b.memset('pool', self.rawc[g], 0.0)

    def macro(self, l, m, w_in_l):
        b, nps = self.b, self.nps
        if NSTAGE < 1:
            for st in range(4):
                self.emit_yT(st)
            return
        n0 = 4 * m
        wsel = self.wsel
        wb = self.wload(w_in_l, OFF['nsa_q'], 512)
        for c in range(4):
            p = nps()
            self.proj_feat(p, wb, c * 128, 128)
            b.ts('dve', self.qm[:, 2 * c, :], p, self.rmq[:, 0:1], ALU.mult)
            b.act(self.qm[:, 2 * c + 1, :], p, AF.Copy, scale=self.rmq[:, 1:2])
        def bail():
            for st in range(4):
                self.emit_yT(st)
        if NSTAGE < 1.15:
            return bail()
        b.memset('pool', self.qA, 0.0)
        b.dma('pool', self.qA[64:68, :, :], self.dt(self.dr['qaug'][:, :, m * MT:(m + 1) * MT]))
        if NSTAGE < 1.25:
            return bail()
        wb = self.wload(w_in_l, OFF['nsa_kc'], 512)
        for g in range(2):
            b.cp('dve', wsel[:, :, 0:64], wb[:, :, g * 64:(g + 1) * 64])
            b.cp('dve', wsel[:, :, 64:128], wb[:, :, 128 + g * 64:128 + (g + 1) * 64])
            p = nps()
            self.proj_feat(p, wsel, 0, 128)
            b.cp('dve', self.rawc[g][:, 0:16], self.rawc[g][:, 512:528])
            b.cp('act', self.rawc[g][:, 16:528], p)
        if NSTAGE < 1.27:
            return bail()
        for g in range(2):
            b.cp('dve', wsel[:, :, 0:64], wb[:, :, 256 + g * 64:256 + (g + 1) * 64])
            b.cp('dve', wsel[:, :, 64:128], wb[:, :, 256 + g * 64:256 + (g + 1) * 64])
            p = nps()
            self.proj_feat(p, wsel, 0, 128)
            b.cp('act', self.kS[g][:, m * MT:(m + 1) * MT], p)
        if NSTAGE < 1.29:
            return bail()
        for st in range(4):
            p = nps()
            self.proj_tok(p, st, wb, 384, 128)
            for g in range(2 if NSTAGE >= 1.2915 else 0):
                b.cp('dve', self.vS[g][:, n0 + st, 0:64], p[:, g * 64:(g + 1) * 64])
        if NSTAGE < 1.35:
            return bail()
        wb = self.wload(w_in_l, OFF['nsa_kw'], 280)
        for g in range(2):
            b.cp('dve', wsel[:, :, 0:64], wb[:, :, g * 64:(g + 1) * 64])
            b.cp('dve', wsel[:, :, 64:128], wb[:, :, g * 64:(g + 1) * 64])
            p = nps()
            self.proj_feat(p, wsel, 0, 128)
            s0 = (n0 % 8) * 128
            b.cp('act', self.kW[g][:, s0:s0 + 512], p)
        for st in range(4):
            p = nps()
            self.proj_tok(p, st, wb, 128, 152)
            for g in range(2):
                b.cp('dve', self.vW[g][:, (n0 + st) % 8, 0:64], p[:, g * 64:(g + 1) * 64])
            b.act(self.gsig[st], p[:, 128:152], AF.Sigmoid)
        if NSTAGE < 1.45:
            return bail()
        wb = self.wload(w_in_l, OFF['nsa_z'], 512)
        self.z_block(wb, 0, None)
        for g in range(2 if NSTAGE >= 2 else 0):
            pC = nps()
            for p_ in range(32):
                b.mm(pC[:, 0:32], self.W1[:, p_, :], self.rawc[g][:, p_:p_ + 497:16], start=p_ == 0, stop=p_ == 31)
            b.act(self.hid, pC[:, 0:32], AF.Silu, bias=self.cpos[:, 0:1])
            pK = nps()
            b.mm(pK[:, 0:32], self.W2k, self.hid)
            b.mm(pK[:, 32:64], self.W2v, self.hid)
            c0 = 32 * m
            cnt = 32
            b.cp('dve', self.kC[g][:, c0:c0 + cnt], pK[:, 0:32])
            b.cp('dve', self.vcT[g][:, c0:c0 + cnt], pK[:, 32:64])
            if m == 0:
                b.memset('pool', self.kC[g][:, 0:1], 0.0)
                b.memset('pool', self.vcT[g][:, 0:1], 0.0)
            for kt in sorted({c0 // 128, (c0 + cnt - 1) // 128}):
                b.tr(self.PTb[:, 0:128], self.vcT[g][:, kt * 128:(kt + 1) * 128], self.ident_b)
                b.cp('dve', self.vC[g][:, kt, 0:64], self.PTb[:, 0:64])
        for st in range(4):
            if NSTAGE >= 3:
                self.sub(m, st)
            self.emit_yT(st)

    def pbuf(self):
        self.pi += 1
        return self.Pb[self.pi % 3]

    def sub(self, m, st):
        b, nps, PS = self.b, self.nps, self.PS
        n = 4 * m + st
        tk = slice(st * 128, (st + 1) * 128)
        f4 = lambda t: t.re("p h i -> p (h i)")
        pOc, pOs, pOw = [PS[0], PS[1]], PS[2], PS[3]
        on = self.on
        causal = lambda t: b.asel(t, t, [[0, 4], [1, 128]], ALU.is_ge, 0.0, 0, -1)
        for g in range(2):
            qrhs = self.qm[:, 4 * g:4 * g + 4, tk]
            arhs = self.qA[:, 4 * g:4 * g + 4, tk]
            kts = [kt for kt in (0, 1) if n >= 16 * kt]
            for ki, kt in enumerate(kts):
                ks_ = slice(kt * 128, (kt + 1) * 128)
                pS = nps(4)
                b.mm(pS, self.kC[g][:, ks_], qrhs, start=True, stop=False)
                b.mm(pS, self.EKc[:, ks_], arhs, start=False, stop=True)
                Pt = self.pbuf()
                b.act(f4(Pt), pS, AF.Exp)
                if n < 16 * kt + 16:
                    b.asel(Pt, Pt, [[0, 4], [1, 128]], ALU.is_ge, 0.0, 128 * n - 2048 * kt - 15, -16)
                for hh in range(4):
                    col = (hh % 2) * 129
                    b.mm(pOc[hh // 2][:, col:col + 129], Pt[:, hh, :], self.vC[g][:, kt, 0:129],
                         start=(ki == 0 and hh % 2 == 0), stop=(ki == len(kts) - 1))
            for bnk in range(2):
                v = pOc[bnk][:, 0:258].re("p (h c) -> p h c", h=2)
                b.cp('dve', self.dall[:, 0, 2 * bnk:2 * bnk + 2], v[:, :, 64])
            rcc = self.rall[:, 0, :]
            b.ts('dve', rcc, self.dall[:, 0, :], 1e-30, ALU.max)
            b.recip(rcc, rcc)
            imp = self.imp
            b.ts('dve', imp, pOc[0][:, 65:129], rcc[:, 0:1], ALU.mult)
            b.stt(imp, pOc[0][:, 194:258], rcc[:, 1:2], imp, ALU.mult, ALU.add)
            b.stt(imp, pOc[1][:, 65:129], rcc[:, 2:3], imp, ALU.mult, ALU.add)
            b.stt(imp, pOc[1][:, 194:258], rcc[:, 3:4], imp, ALU.mult, ALU.add)
            cur, val, fz = self.cur, self.val, self.fz
            b.ts('dve', cur, self.half01, float(2 * n), ALU.add)
            b.ts('dve', val, self.jidx, cur[:, 0:1], ALU.is_le)
            b.tt('dve', imp, imp, val, ALU.mult)
            b.ts('dve', val, val, -1.0, ALU.add)
            b.tt('dve', imp, imp, val, ALU.add)
            b.ts('dve', fz, self.jidx, cur[:, 0:1], ALU.is_equal)
            b.ts('dve', val, self.jidx, 1.0, ALU.add, cur[:, 0:1], ALU.is_equal)
            b.tt('dve', fz, fz, val, ALU.add)
            b.tt('dve', fz, fz, self.e0, ALU.add)
            b.stt(imp, fz, 1.0e4, imp, ALU.mult, ALU.max)
            b.max8(self.m8a, imp)
            b.mrep(self.imp2, self.m8a, imp, -2.0)
            b.max8(self.m8b, self.imp2)
            b.ts('dve', self.thr, self.m8b[:, 7:8], 0.0, ALU.max)
            b.ts('dve', self.selb, imp, self.thr[:, 0:1], ALU.is_ge, 30000.0, ALU.mult)
            b.ts('dve', self.selbb[:, 0:64], self.selb, -30000.0, ALU.add)
            def scores(kind, kt):
                ks_ = slice(kt * 128, (kt + 1) * 128)
                pS = nps(4)
                if kind == 's':
                    b.mm(pS, self.kS[g][:, ks_], qrhs, start=True, stop=False)
                    b.mm(pS, self.EK[:, ks_], f4(self.qS), start=False, stop=True)
                else:
                    sl = kt % 8
                    b.mm(pS, self.kW[g][:, sl * 128:(sl + 1) * 128], qrhs, start=True, stop=False)
                    b.mm(pS, self.EK[:, ks_], arhs, start=False, stop=True)
                Pt = self.pbuf()
                b.act(f4(Pt), pS, AF.Exp)
                if kt == n:
                    causal(Pt)
                if kind == 'w' and kt == n - 4:
                    b.asel(Pt, Pt, [[0, 4], [-1, 128]], ALU.is_ge, 0.0, -1, 1)
                return Pt

            def pv(kind, kt, Pt, first, last):
                for hh in range(4):
                    if kind == 's':
                        b.mm(pOs[:, hh * 65:(hh + 1) * 65], Pt[:, hh, :], self.vS[g][:, kt, 0:65],
                             start=(first and hh == 0), stop=last)
                    else:
                        b.mm(pOw[:, hh * 65:(hh + 1) * 65], Pt[:, hh, :], self.vW[g][:, kt % 8, 0:65],
                             start=(first and hh == 0), stop=last)

            wk0 = max(0, n - 4)

            def run_items(items):
                pend = []
                for it in items:
                    Pt = scores(it[0], it[1])
                    pend.append((it, Pt))
                    if len(pend) > 2:
                        i0, P0 = pend.pop(0)
                        pv(i0[0], i0[1], P0, i0[2], i0[3])
                for i0, P0 in pend:
                    pv(i0[0], i0[1], P0, i0[2], i0[3])

            run_items([('w', kt, kt == wk0, kt == n) for kt in range(wk0, n + 1)])
            b.tr(self.PTb[:, 0:128], self.selbb, self.ident_b)
            b.cp('dve', self.qS[0:64], self.PTb[0:64, 0:128].bc(1, [64, 4, 128]))
            b.cp('act', self.qS[64:68], self.qA[64:68, 4 * g:4 * g + 4, tk])
            run_items([('s', kt, kt == 0, kt == n) for kt in range(n + 1)])
            vs_ = pOs[:, 0:260].re("p (h c) -> p h c", h=4)
            vw_ = pOw[:, 0:260].re("p (h c) -> p h c", h=4)
            b.cp('dve', self.dall[:, 1, :], vs_[:, :, 64])
            b.cp('dve', self.dall[:, 2, :], vw_[:, :, 64])
            b.ts('dve', self.rall[:, 1:3, :], self.dall[:, 1:3, :], 1e-30, ALU.max)
            b.recip(self.rall[:, 1:3, :], self.rall[:, 1:3, :])
            gv = self.gsig[st][:, 12 * g:12 * g + 12].re("p (h b) -> p b h", b=3)
            b.tt('dve', self.coef, self.rall, gv, ALU.mult)
            for bnk in range(2):
                v = pOc[bnk][:, 0:258].re("p (h c) -> p h c", h=2)
                b.tt('dve', on[:, 4 * g + 2 * bnk:4 * g + 2 * bnk + 2, :], v[:, :, 0:64],
                     self.coef[:, 0, 2 * bnk:2 * bnk + 2].bc(2, [128, 2, 64]), ALU.mult)
            b.tt('dve', self.tmp4, vs_[:, :, 0:64], self.coef[:, 1, :].bc(2, [128, 4, 64]), ALU.mult)
            b.tt('pool', on[:, 4 * g:4 * g + 4, :], on[:, 4 * g:4 * g + 4, :], self.tmp4, ALU.add)
            b.tt('dve', self.tmp5, vw_[:, :, 0:64], self.coef[:, 2, :].bc(2, [128, 4, 64]), ALU.mult)
            b.tt('pool', on[:, 4 * g:4 * g + 4, :], on[:, 4 * g:4 * g + 4, :], self.tmp5, ALU.add)
        b.tt('dve', self.ybr[st], on.re("p h d -> p (h d)"), self.zz[st], ALU.mult)


def kernel(**inputs):
    depth, S_len, n_cores = 4, 4096, 8
    x = np.asarray(inputs['x'], dtype=np.float32)
    nc, _ = build(S_len, depth)
    pin = prep_inputs(inputs, depth)
    hc = host_constants(S_len)
    in_maps = []
    for i in range(n_cores):
        d = dict(pin)
        d.update(hc)
        d['x'] = np.ascontiguousarray(x[i])
        in_maps.append(d)
    res = run_bass_kernel_spmd(nc, in_maps, core_ids=list(range(n_cores)))
    return np.stack([np.asarray(r['y'], dtype=np.float32) for r in res.results], axis=0)
```
